# Optimizing a Trainium2 kernel written in Bass

```python
import jax, jax.numpy as jnp
from jax import lax
import numpy as np

D_MODEL = 1024
BATCH = 8
SEQ = 4096
DEPTH = 1

CHUNK = 64
N_MEM = 256
ROPE_THETA = 10000.0
EPS = 1e-6
ATTN_HEADS = 8
HEAD_DIM = 64
ATTN_WIDTH = ATTN_HEADS * HEAD_DIM
IDX_HEADS = 8
IDX_DIM = HEAD_DIM
MAX_TOPK = 256
Q_BLOCK = CHUNK
CONV_CH = D_MODEL // 2
CONV_WIDTH = 31
N_BRANCHES = 2
IN_SPLITS = (ATTN_WIDTH, ATTN_WIDTH, ATTN_WIDTH, IDX_HEADS * IDX_DIM, IDX_DIM, IDX_HEADS,
             2 * CONV_CH, N_BRANCHES * D_MODEL)
IN_WIDTH = sum(IN_SPLITS)
X_HEADS = 4
X_HEAD_DIM = D_MODEL // X_HEADS
N_GROUPS = 4
EXPERTS_PER_GROUP = 8
TOP_K_EXPERTS = 2
EXPERT_FF = D_MODEL // 4

kernel_name = "hybrid_dsa_conformer_hmoe_block"


def rmsnorm(x, g):
    xf = x.astype(jnp.float32)
    y = xf * lax.rsqrt(jnp.mean(xf * xf, axis=-1, keepdims=True) + EPS)
    return (y * g.astype(jnp.float32)).astype(x.dtype)


def layernorm(x, g, b):
    xf = x.astype(jnp.float32)
    mu = jnp.mean(xf, axis=-1, keepdims=True)
    var = jnp.mean(jnp.square(xf - mu), axis=-1, keepdims=True)
    y = (xf - mu) * lax.rsqrt(var + EPS)
    return (y * g.astype(jnp.float32) + b.astype(jnp.float32)).astype(x.dtype)


def rope_tables(positions, dim):
    inv_freq = ROPE_THETA ** (-jnp.arange(0, dim, 2, dtype=jnp.float32) / dim)
    ang = positions.astype(jnp.float32)[..., None] * inv_freq
    return jnp.cos(ang)[:, :, None, :], jnp.sin(ang)[:, :, None, :]


def apply_rope(x, cos, sin):
    xf = x.astype(jnp.float32)
    half = xf.shape[-1] // 2
    x1, x2 = xf[..., :half], xf[..., half:]
    return jnp.concatenate([x1 * cos - x2 * sin, x2 * cos + x1 * sin], axis=-1).astype(x.dtype)


def _split_offsets():
    offs, acc = [], 0
    for w in IN_SPLITS[:-1]:
        acc += w
        offs.append(acc)
    return offs


def dsa_attention(q, k, v, q_idx, k_idx, w_idx):
    B, S = q.shape[0], q.shape[1]
    top_k = min(MAX_TOPK, S // 4)
    n_blk = S // Q_BLOCK
    key_pos = jnp.arange(S, dtype=jnp.int32)
    scale_attn = HEAD_DIM ** -0.5
    scale_idx = (IDX_DIM ** -0.5) * (IDX_HEADS ** -0.5)
    gather = jax.vmap(lambda src, idx: src[idx])

    def to_blocks(a):
        return a.reshape(B, n_blk, Q_BLOCK, *a.shape[2:]).swapaxes(0, 1)

    def block(args):
        qb, qib, wb, start = args
        qpos = start + jnp.arange(Q_BLOCK, dtype=jnp.int32)
        limit = (qpos // CHUNK + 1) * CHUNK
        admissible = key_pos[None, :] < limit[:, None]
        rel = jax.nn.relu(jnp.einsum('bqhd,bsd->bqhs', qib, k_idx).astype(jnp.float32))
        score = jnp.einsum('bqhs,bqh->bqs', rel, wb.astype(jnp.float32)) * scale_idx
        score = jnp.where(admissible[None], score, -jnp.inf)
        _, sel = lax.top_k(score, top_k)
        valid = sel < limit[None, :, None]
        k_sel = gather(k, sel)
        v_sel = gather(v, sel)
        logits = jnp.einsum('bqhd,bqkhd->bqhk', qb, k_sel).astype(jnp.float32) * scale_attn
        logits = jnp.where(valid[:, :, None, :], logits, -jnp.inf)
        p = jax.nn.softmax(logits, axis=-1).astype(v.dtype)
        return jnp.einsum('bqhk,bqkhd->bqhd', p, v_sel)

    starts = jnp.arange(n_blk, dtype=jnp.int32) * Q_BLOCK
    out = lax.map(block, (to_blocks(q), to_blocks(q_idx), to_blocks(w_idx), starts))
    return out.swapaxes(0, 1).reshape(B, S, ATTN_WIDTH)


def conformer_conv(a, b, conv_w, conv_b, ln_g, ln_b, w_pw):
    u = a * jax.nn.sigmoid(b)
    u = lax.conv_general_dilated(u, conv_w[:, None, :], window_strides=(1,),
                                 padding=[(CONV_WIDTH - 1, 0)],
                                 dimension_numbers=('NWC', 'WIO', 'NWC'),
                                 feature_group_count=CONV_CH) + conv_b
    u = jax.nn.silu(layernorm(u, ln_g, ln_b))
    return u @ w_pw


def mixing_sublayer(x, cos, sin, norm_g, w_in, w_o_attn, conv_w, conv_b, conv_ln_g, conv_ln_b,
                    w_conv_out, w_out):
    B, S, _ = x.shape
    h = rmsnorm(x, norm_g)
    q, k, v, qi, ki, wi, glu, gates = jnp.split(h @ w_in, _split_offsets(), axis=-1)
    q = apply_rope(q.reshape(B, S, ATTN_HEADS, HEAD_DIM), cos, sin)
    k = apply_rope(k.reshape(B, S, ATTN_HEADS, HEAD_DIM), cos, sin)
    v = v.reshape(B, S, ATTN_HEADS, HEAD_DIM)
    qi = apply_rope(qi.reshape(B, S, IDX_HEADS, IDX_DIM), cos, sin)
    ki = apply_rope(ki[:, :, None, :], cos, sin)[:, :, 0]
    y_attn = dsa_attention(q, k, v, qi, ki, wi) @ w_o_attn
    y_conv = conformer_conv(glu[..., :CONV_CH], glu[..., CONV_CH:], conv_w, conv_b,
                            conv_ln_g, conv_ln_b, w_conv_out)
    g = jax.nn.sigmoid(gates).reshape(B, S, N_BRANCHES, D_MODEL)
    return x + (g[:, :, 0] * y_attn + g[:, :, 1] * y_conv) @ w_out


def memory_cross_attention(x, mem, norm_g, norm_mem_g, w_q, w_kv, w_o):
    B, S, _ = x.shape
    M = mem.shape[1]
    q = (rmsnorm(x, norm_g) @ w_q).reshape(B, S, X_HEADS, X_HEAD_DIM)
    kv = (rmsnorm(mem, norm_mem_g) @ w_kv).reshape(B, M, 2, X_HEADS, X_HEAD_DIM)
    k, v = kv[:, :, 0], kv[:, :, 1]
    logits = jnp.einsum('bshd,bmhd->bhsm', q, k).astype(jnp.float32) * (X_HEAD_DIM ** -0.5)
    p = jax.nn.softmax(logits, axis=-1).astype(v.dtype)
    o = jnp.einsum('bhsm,bmhd->bshd', p, v).reshape(B, S, D_MODEL)
    return x + o @ w_o


def hier_moe(h, w_rg, b_rg, w_re, b_re, w_gate, w_up, w_down):
    B, S, D = h.shape
    t = h.reshape(-1, D)
    group_logits = (t @ w_rg + b_rg).astype(jnp.float32)
    g_sel = jnp.argmax(group_logits, axis=-1)
    g_gate = jnp.take_along_axis(jax.nn.softmax(group_logits, axis=-1), g_sel[:, None], axis=-1)
    exp_logits = (jnp.einsum('td,gde->tge', t, w_re) + b_re).astype(jnp.float32)
    exp_logits = jnp.take_along_axis(exp_logits, g_sel[:, None, None], axis=1)[:, 0]
    top_val, top_idx = lax.top_k(exp_logits, TOP_K_EXPERTS)
    top_w = jax.nn.softmax(top_val, axis=-1) * g_gate
    w_exp = jnp.einsum('tk,tke->te', top_w,
                       jax.nn.one_hot(top_idx, EXPERTS_PER_GROUP, dtype=jnp.float32))
    combine = (jax.nn.one_hot(g_sel, N_GROUPS, dtype=jnp.float32)[:, :, None]
               * w_exp[:, None, :]).astype(h.dtype)
    y = jnp.zeros_like(t)
    for g in range(N_GROUPS):
        act = (jax.nn.silu(jnp.einsum('td,edf->tef', t, w_gate[g]))
               * jnp.einsum('td,edf->tef', t, w_up[g]))
        y = y + jnp.einsum('tef,efd->td', act * combine[:, g, :, None], w_down[g])
    return y.reshape(B, S, D)


def setup_inputs(seed: int = 0) -> dict:
    key = jax.random.key(seed)
    ks = iter(jax.random.split(key, 40))
    L, D, G, E, F = DEPTH, D_MODEL, N_GROUPS, EXPERTS_PER_GROUP, EXPERT_FF

    def nrm(shape, scale):
        return jax.random.normal(next(ks), shape, jnp.float32) * scale

    def gain(shape):
        return 1.0 + nrm(shape, 0.01)

    x = nrm((BATCH, SEQ, D), 1.0)
    mem = nrm((BATCH, N_MEM, D), 1.0)
    offset = jax.random.randint(next(ks), (BATCH, 1), 0, 64, dtype=jnp.int32) * CHUNK
    positions = (offset + jnp.arange(SEQ, dtype=jnp.int32)[None, :]).astype(jnp.int32)
    return {
        "x": x,
        "mem": mem,
        "positions": positions,
        "norm_mix_g": gain((L, D)),
        "w_in": nrm((L, D, IN_WIDTH), D ** -0.5),
        "w_o_attn": nrm((L, ATTN_WIDTH, D), ATTN_WIDTH ** -0.5),
        "conv_w": nrm((L, CONV_WIDTH, CONV_CH), CONV_WIDTH ** -0.5),
        "conv_b": nrm((L, CONV_CH), 0.01),
        "conv_ln_g": gain((L, CONV_CH)),
        "conv_ln_b": nrm((L, CONV_CH), 0.01),
        "w_conv_out": nrm((L, CONV_CH, D), CONV_CH ** -0.5),
        "w_out": nrm((L, D, D), D ** -0.5),
        "norm_x_g": gain((L, D)),
        "norm_mem_g": gain((L, D)),
        "w_q_x": nrm((L, D, D), D ** -0.5),
        "w_kv_x": nrm((L, D, 2 * D), D ** -0.5),
        "w_o_x": nrm((L, D, D), D ** -0.5),
        "norm_moe_g": gain((L, D)),
        "w_router_group": nrm((L, D, G), D ** -0.5),
        "b_router_group": nrm((L, G), 0.01),
        "w_router_expert": nrm((L, G, D, E), D ** -0.5),
        "b_router_expert": nrm((L, G, E), 0.01),
        "w_exp_gate": nrm((L, G, E, D, F), D ** -0.5),
        "w_exp_up": nrm((L, G, E, D, F), D ** -0.5),
        "w_exp_down": nrm((L, G, E, F, D), F ** -0.5),
        "norm_final_g": gain((D,)),
    }


def reference(x, mem, positions, norm_mix_g, w_in, w_o_attn, conv_w, conv_b, conv_ln_g, conv_ln_b,
              w_conv_out, w_out, norm_x_g, norm_mem_g, w_q_x, w_kv_x, w_o_x, norm_moe_g,
              w_router_group, b_router_group, w_router_expert, b_router_expert,
              w_exp_gate, w_exp_up, w_exp_down, norm_final_g):
    cos, sin = rope_tables(positions, HEAD_DIM)
    for l in range(DEPTH):
        x = mixing_sublayer(x, cos, sin, norm_mix_g[l], w_in[l], w_o_attn[l], conv_w[l], conv_b[l],
                            conv_ln_g[l], conv_ln_b[l], w_conv_out[l], w_out[l])
        x = memory_cross_attention(x, mem, norm_x_g[l], norm_mem_g[l], w_q_x[l], w_kv_x[l], w_o_x[l])
        x = x + hier_moe(rmsnorm(x, norm_moe_g[l]), w_router_group[l], b_router_group[l],
                         w_router_expert[l], b_router_expert[l],
                         w_exp_gate[l], w_exp_up[l], w_exp_down[l])
    return rmsnorm(x, norm_final_g)
```

```python
import bisect
from contextlib import ExitStack

import numpy as np
import concourse.bass as bass
import concourse.mybir as mybir
from concourse.bass_utils import run_bass_kernel_spmd

F32 = mybir.dt.float32
BF16 = mybir.dt.bfloat16
I32 = mybir.dt.int32
AF = mybir.ActivationFunctionType
ALU = mybir.AluOpType
AX = mybir.AxisListType

S = 4096
D = 1024
NT = S // 128
NSB = S // 512
EPS = 1e-6
NEG_BIG = -30000.0
BIS_ITERS = 18
A_OFF = 2120
B_OFF = 2632
G_OFF = 3144
NA_COLS = 3144


class SemBox:
    __slots__ = ("name", "sem", "count")

    def __init__(self, name):
        self.name = name
        self.sem = None
        self.count = 0


class Buf:
    __slots__ = ("name", "last_w", "readers", "box")

    def __init__(self, name, box=None):
        self.name = name
        self.last_w = None
        self.readers = []
        self.box = box if box is not None else SemBox(name)


class Ctx:
    COMPUTE = ("pe", "act", "dve", "pool")

    def __init__(self, nc, es):
        self.nc = nc
        self.es = es
        self.eng = {"pe": nc.tensor, "act": nc.scalar, "dve": nc.vector, "pool": nc.gpsimd, "sp": nc.sync}
        self.sem = {e: es.enter_context(nc.semaphore("s_" + e)) for e in self.COMPUTE}
        self.mile = {e: 0 for e in self.COMPUTE}
        self.nissued = {e: 0 for e in self.eng}
        self.sigpts = {e: ([], []) for e in self.COMPUTE}
        self.last_ins = {e: None for e in self.eng}
        self.last_sig = {e: True for e in self.eng}
        self.waited = {}
        self.dma_sems = []
        self.all_bufs = []

    def buf(self, name):
        b = Buf(name)
        self.all_bufs.append(b)
        return b

    def bufs(self, name, n, share=False):
        if not share:
            return [self.buf("%s%d" % (name, i)) for i in range(n)]
        box = SemBox(name)
        out = []
        for i in range(n):
            b = Buf("%s%d" % (name, i), box)
            self.all_bufs.append(b)
            out.append(b)
        return out

    def _resolve(self, tok):
        if tok[0] == "d":
            return tok[1], tok[2]
        _, e, idx = tok
        idxs, miles = self.sigpts[e]
        k = bisect.bisect_left(idxs, idx)
        if k < len(idxs):
            return self.sem[e], miles[k]
        assert not self.last_sig[e]
        self.last_ins[e].then_inc(self.sem[e], 1)
        self.mile[e] += 1
        idxs.append(self.nissued[e] - 1)
        miles.append(self.mile[e])
        self.last_sig[e] = True
        return self.sem[e], self.mile[e]

    def _wait(self, engname, toks):
        need = {}
        for tok in toks:
            if tok is None:
                continue
            if tok[0] == "c" and tok[1] == engname and engname == "pe":
                continue
            sem, val = self._resolve(tok)
            key = id(sem)
            if key not in need or need[key][1] < val:
                need[key] = (sem, val)
        for key, (sem, val) in need.items():
            wk = (engname, key)
            if self.waited.get(wk, 0) >= val:
                continue
            self.eng[engname].wait_ge(sem, val)
            self.waited[wk] = val

    def _deps(self, engname, reads, writes, waw=True):
        toks = []
        for b in reads:
            toks.append(b.last_w)
        for b in writes:
            if waw:
                if not (b.last_w is not None and b.last_w[0] == "c" and b.last_w[1] == engname):
                    toks.append(b.last_w)
            for r in b.readers:
                if r[0] == "c" and r[1] == engname and engname == "pe":
                    continue
                toks.append(r)
        return toks

    def op(self, engname, method, reads=(), writes=(), sig=None, **kw):
        assert engname in self.COMPUTE
        if sig is None:
            sig = engname != "pe"
        self._wait(engname, self._deps(engname, reads, writes))
        ins = getattr(self.eng[engname], method)(**kw)
        idx = self.nissued[engname]
        self.nissued[engname] += 1
        self.last_ins[engname] = ins
        self.last_sig[engname] = False
        if sig:
            ins.then_inc(self.sem[engname], 1)
            self.mile[engname] += 1
            self.sigpts[engname][0].append(idx)
            self.sigpts[engname][1].append(self.mile[engname])
            self.last_sig[engname] = True
        tok = ("c", engname, idx)
        for b in reads:
            b.readers.append(tok)
        for b in writes:
            b.last_w = tok
            b.readers = []
        return ins

    def dma(self, q, out, in_, reads, writes, waw=False, sem_from=None, **kw):
        assert len(writes) == 1
        wb = (sem_from if sem_from is not None else writes[0]).box
        if wb.sem is None:
            wb.sem = self.es.enter_context(self.nc.semaphore("d_" + wb.name))
            self.dma_sems.append(wb)
        self._wait(q, self._deps(q, reads, writes, waw=waw))
        ins = self.eng[q].dma_start(out=out, in_=in_, **kw)
        ins.then_inc(wb.sem, 16)
        wb.count += 16
        self.nissued[q] += 1
        if q in self.COMPUTE:
            self.last_ins[q] = ins
            self.last_sig[q] = True
        tok = ("d", wb.sem, wb.count)
        for b in reads:
            b.readers.append(tok)
        writes[0].last_w = tok
        writes[0].readers = []
        return ins

    def barrier(self):
        toks = []
        for e in self.COMPUTE:
            if self.nissued[e] > 0 and self.last_ins[e] is not None:
                if not self.last_sig[e]:
                    toks.append(("c", e, self.nissued[e] - 1))
                else:
                    idxs, miles = self.sigpts[e]
                    if idxs:
                        toks.append(("c", e, idxs[-1]))
        for b in self.dma_sems:
            toks.append(("d", b.sem, b.count))
        for e in list(self.COMPUTE) + ["sp"]:
            self._wait(e, toks)

    def final_wait(self, q, bufs):
        self._wait(q, [("d", b.box.sem, b.box.count) for b in bufs if b.box.sem is not None])


def bc(ap, shape):
    return ap.broadcast_to(list(shape))


def build(stop_after="all", dbg=(), a2_tiles=None):
    nc = bass.Bass("TRN2", target_bir_lowering=False)

    def din(name, shape, dt=F32):
        return nc.dram_tensor(name, list(shape), dt, kind="ExternalInput").ap()

    x_d = din("x", [S, D])
    mem_d = din("mem", [256, D])
    pos_d = din("pos", [128, NT], I32)
    w_in_d = din("w_in", [D, 5192])
    w_o_attn_d = din("w_o_attn", [512, D])
    convw_d = din("convw", [128, 4, 31])
    convb_d = din("convb", [128, 4])
    lng_d = din("lng", [128, 4])
    lnb_d = din("lnb", [128, 4])
    w_conv_out_d = din("w_conv_out", [512, D])
    w_out_d = din("w_out", [D, D])
    w_q_x_d = din("w_q_x", [D, D])
    w_kv_x_d = din("w_kv_x", [D, 2 * D])
    w_o_x_d = din("w_o_x", [D, D])
    g_mix_d = din("g_mix", [128, 8])
    g_x_d = din("g_x", [128, 8])
    g_mem_d = din("g_mem", [128, 8])
    g_moe_d = din("g_moe", [128, 8])
    w_router_d = din("w_router", [D, 36])
    b_router_d = din("b_router", [1, 36])
    w_eg_d = din("w_eg", [32, D, 256])
    w_eu_d = din("w_eu", [32, D, 256])
    w_ed_d = din("w_ed", [32, 256, D])
    g_final_d = din("g_final", [1, D])
    ident_d = din("ident", [128, 128])
    invf_d = din("invf", [128, 32])
    pow2_d = din("pow2", [128, BIS_ITERS + 2])

    out_d = nc.dram_tensor("out", [S, D], F32, kind="ExternalOutput").ap()

    def dscr(name, shape, dt):
        return nc.dram_tensor(name, list(shape), dt, kind="Internal").ap()

    qT_scr = dscr("qT_scr", [NT, 128, 512], BF16)
    qiT_scr = dscr("qiT_scr", [NT, 128, 512], BF16)
    z_scr = dscr("z_scr", [NSB, 128, 2048], BF16)
    ao_scr = dscr("ao_scr", [NT, 128, 512], BF16)
    x2_scr = dscr("x2_scr", [S, D], F32)

    dbg_out = {}
    for name, shape, dt in dbg:
        dbg_out[name] = nc.dram_tensor("dbg_" + name, list(shape), dt, kind="ExternalOutput").ap()

    with ExitStack() as es:
        C = Ctx(nc, es)
        out_b = C.buf("out")

        P0 = ExitStack()
        es.enter_context(P0)

        def sb(stack, name, shape, dt):
            return stack.enter_context(nc.sbuf_tensor("sb_" + name, list(shape), dt))

        def ps(stack, name, shape, dt):
            return stack.enter_context(nc.psum_tensor("ps_" + name, list(shape), dt))

        identf = sb(P0, "identf", [128, 128], F32)
        identb = sb(P0, "identb", [128, 128], BF16)
        onesf = sb(P0, "onesf", [128, 128], F32)
        onesb = sb(P0, "onesb", [128, 128], BF16)
        b_const = C.buf("const")
        C.dma("sp", identf[:], ident_d[:, :], [], [b_const])
        C.op("dve", "tensor_copy", [b_const], [b_const], out=identb[:], in_=identf[:])
        C.op("dve", "memset", [], [b_const], ap=onesf[:], constant=1.0 / 512.0)
        C.op("dve", "memset", [], [b_const], ap=onesb[:], constant=1.0)

        PA = ExitStack()
        es.enter_context(PA)
        kT = sb(PA, "kT", [128, 4, S], BF16)
        v_sb = sb(PA, "v_sb", [128, NT, 8, 65], BF16)
        kiT2 = sb(PA, "kiT2", [128, S], BF16)
        wi_sb = sb(PA, "wi_sb", [128, NT, 8], F32)
        kT_b = C.bufs("kT", NT)
        v_b = C.bufs("v", NT)
        kiT_b = C.bufs("kiT", NT)
        wi_b = C.bufs("wi", NT)
        for i in range(NT):
            C.op("pool", "memset", [], [v_b[i]], ap=v_sb[:, i, :, 64:65], constant=1.0)

        with ExitStack() as A1:
            w_sb = sb(A1, "w_inA", [128, 8, NA_COLS], BF16)
            w_b = [C.buf("w_inA")] * 8
            for kc in range(8):
                for (c0, c1) in ((0, 1024), (1024, 2048), (2048, NA_COLS)):
                    C.dma("pool", w_sb[:, kc, c0:c1], w_in_d[kc * 128:(kc + 1) * 128, c0:c1], [], [w_b[kc]])
            gfm = sb(A1, "gfm", [128, 8], F32)
            cwT = sb(A1, "cwT", [128, 4, 31], F32)
            cb4 = sb(A1, "cb4", [128, 4], F32)
            lng4 = sb(A1, "lng4", [128, 4], F32)
            lnb4 = sb(A1, "lnb4", [128, 4], F32)
            invf = sb(A1, "invf", [128, 32], F32)
            posi = sb(A1, "posi", [128, NT], I32)
            posf = sb(A1, "posf", [128, NT], F32)
            b_small = C.buf("smallA")
            C.dma("sp", gfm[:], g_mix_d[:, :], [], [b_small])
            C.dma("sp", cwT[:], convw_d[:, :, :], [], [b_small])
            C.dma("sp", cb4[:], convb_d[:, :], [], [b_small])
            C.dma("sp", lng4[:], lng_d[:, :], [], [b_small])
            C.dma("sp", lnb4[:], lnb_d[:, :], [], [b_small])
            C.dma("sp", invf[:], invf_d[:, :], [], [b_small])
            C.dma("sp", posi[:], pos_d[:, :], [], [b_small])
            cosT = sb(A1, "cosT", [128, NT, 32], F32)
            sinT = sb(A1, "sinT", [128, NT, 32], F32)
            with ExitStack() as T0:
                ua = sb(T0, "ua", [128, NT, 32], F32)
                ub = sb(T0, "ub", [128, NT, 32], F32)
                uci = sb(T0, "uci", [128, NT, 32], I32)
                b_rope = C.buf("ropetab")
                b_ua = C.buf("ua")
                b_ub = C.buf("ub")
                b_uc = C.buf("uc")
                C.op("dve", "tensor_copy", [b_small], [b_ua], out=posf[:], in_=posi[:])
                C.op("dve", "tensor_tensor", [b_ua, b_small], [b_ub], out=ua[:],
                     in0=bc(posf[:, :].unsqueeze(2), [128, NT, 32]), in1=bc(invf[:, :].unsqueeze(1), [128, NT, 32]), op=ALU.mult)
                for (tab, shift) in ((sinT, 0.0), (cosT, 0.25)):
                    C.op("dve", "tensor_scalar", [b_ub], [b_ua], out=ub[:], in0=ua[:], scalar1=shift, scalar2=None, op0=ALU.add)
                    C.op("dve", "tensor_copy", [b_ua], [b_uc], out=uci[:], in_=ub[:])
                    C.op("dve", "tensor_copy", [b_uc], [b_rope], out=tab[:], in_=uci[:])
                    C.op("dve", "tensor_tensor", [b_ua, b_rope], [b_ua], out=ub[:], in0=ub[:], in1=tab[:], op=ALU.subtract)
                    C.op("dve", "tensor_scalar", [b_ua], [b_rope], out=tab[:], in0=ub[:], scalar1=0.5, scalar2=None, op0=ALU.is_gt)
                    C.op("dve", "tensor_tensor", [b_ua, b_rope], [b_ua], out=ub[:], in0=ub[:], in1=tab[:], op=ALU.subtract)
                    C.op("dve", "tensor_scalar", [b_ua], [b_rope], out=tab[:], in0=ub[:], scalar1=-0.5, scalar2=None, op0=ALU.is_lt)
                    C.op("dve", "tensor_tensor", [b_ua, b_rope], [b_ua], out=ub[:], in0=ub[:], in1=tab[:], op=ALU.add)
                    C.op("act", "activation", [b_ua], [b_rope], out=tab[:], in_=ub[:], func=AF.Sin, scale=2.0 * np.pi)

                C.barrier()

            xt = [sb(A1, "xt%d" % j, [128, D], F32) for j in range(2)]
            xt_b = C.bufs("xt", 2)
            hn = [sb(A1, "hn%d" % j, [128, D], BF16) for j in range(2)]
            hn_b = C.bufs("hn", 2)
            st = sb(A1, "stat", [128, 8], F32)
            st_b = C.buf("stat")
            hT = sb(A1, "hT", [128, 8, 512], BF16)
            hT_b = C.bufs("hT", 4)
            rt = [sb(A1, "rt%d" % j, [128, 8, 32], F32) for j in range(4)]
            rt_b = C.bufs("rt", 4)
            rq = [sb(A1, "rq%d" % j, [128, 8, 64], BF16) for j in range(2)]
            rq_b = C.bufs("rq", 2)
            rqk = sb(A1, "rqk", [128, 2, 64], BF16)
            rqk_b = C.buf("rqk")
            qst = [sb(A1, "qst%d" % j, [128, 4, 128], BF16) for j in range(2)]
            qst_b = C.bufs("qst", 2)
            uT = [sb(A1, "uT%d" % j, [128, 4, 542], BF16) for j in range(2)]
            uT_b = [C.bufs("uT%d_" % j, 4) for j in range(2)]
            uTpad_b = C.bufs("uTpad", 2)
            sg = sb(A1, "sg", [128, 512], F32)
            sg_b = C.buf("sg")
            Dw = sb(A1, "Dw", [128, 31, 128], BF16)
            Dw_b = C.buf("Dw")
            co = sb(A1, "co", [128, 4, 512], F32)
            co_b = C.bufs("co", 4)
            sq = [sb(A1, "sq%d" % j, [128, 512], F32) for j in range(2)]
            sq_b = C.bufs("sq", 2)
            mean_sb = sb(A1, "mean_sb", [128, 512], F32)
            m2_sb = sb(A1, "m2_sb", [128, 512], F32)
            rstd_sb = sb(A1, "rstd_sb", [128, 512], F32)
            mean_b = C.buf("mean")
            m2_b = C.buf("m2")
            rstd_b = C.buf("rstdc")
            dtmp = [sb(A1, "dtmp%d" % j, [128, 512], F32) for j in range(2)]
            dtmp_b = C.bufs("dtmp", 2)
            zT = sb(A1, "zT", [128, 4, 512], BF16)
            zT_b = C.buf("zT")
            pA = ps(A1, "pA", [128, 512], F32)
            pB = ps(A1, "pB", [128, 512], F32)
            pC = ps(A1, "pC", [128, 512], F32)
            pM = ps(A1, "pM", [128, 512], F32)
            pE = ps(A1, "pE", [128, 512], F32)
            pT0 = ps(A1, "pT0", [128, 512], F32)
            pT1 = ps(A1, "pT1", [128, 512], F32)
            pTP = ps(A1, "pTP", [128, 1024], BF16)
            pA_b, pB_b, pC_b, pM_b, pE_b, pTP_b = (C.buf(n) for n in ("pA", "pB", "pC", "pM", "pE", "pTP"))
            pT = [pT0, pT1]
            pT_b = C.bufs("pT", 2)
            q_scr_b = C.bufs("qscr", NT, share=True)
            qi_scr_b = C.bufs("qiscr", NT, share=True)
            z_scr_b = C.bufs("zscr", NSB, share=True)

            C.op("pool", "memset", [], [uTpad_b[0]], ap=uT[0][:, :, 0:30], constant=0.0)
            tmc = [0]

            def rope_block(psv, nh, i, dst, dst_bufs, dup=False):
                cos_b = bc(cosT[:, i, :].unsqueeze(1), [128, nh, 32])
                sin_b = bc(sinT[:, i, :].unsqueeze(1), [128, nh, 32])
                x1 = psv[:, :, 0:32]
                x2 = psv[:, :, 32:64]
                pb = psv_buf[0]
                C.op("dve", "tensor_tensor", [pb, b_rope], [rt_b[0]], out=rt[0][:, 0:nh, :], in0=x1, in1=cos_b, op=ALU.mult)
                C.op("dve", "tensor_tensor", [pb, b_rope], [rt_b[1]], out=rt[1][:, 0:nh, :], in0=x2, in1=sin_b, op=ALU.mult)
                C.op("dve", "tensor_tensor", [pb, b_rope], [rt_b[2]], out=rt[2][:, 0:nh, :], in0=x2, in1=cos_b, op=ALU.mult)
                C.op("dve", "tensor_tensor", [pb, b_rope], [rt_b[3]], out=rt[3][:, 0:nh, :], in0=x1, in1=sin_b, op=ALU.mult)
                C.op("pool", "tensor_tensor", [rt_b[0], rt_b[1]], dst_bufs, out=dst[:, :, 0:32], in0=rt[0][:, 0:nh, :], in1=rt[1][:, 0:nh, :], op=ALU.subtract)
                C.op("pool", "tensor_tensor", [rt_b[2], rt_b[3]], dst_bufs, out=dst[:, :, 32:64], in0=rt[2][:, 0:nh, :], in1=rt[3][:, 0:nh, :], op=ALU.add)

            psv_buf = [None]

            def chain(i):
                j = i % 2
                C.dma("sp", xt[j][:], x_d[i * 128:(i + 1) * 128, :], [], [xt_b[j]])
                C.op("act", "activation", [xt_b[j]], [hn_b[j], st_b], out=hn[j][:], in_=xt[j][:], func=AF.Square, accum_out=st[:, 0:1])
                C.op("act", "activation", [st_b], [st_b], out=st[:, 1:2], in_=st[:, 0:1], func=AF.Sqrt, scale=1.0 / D, bias=EPS)
                C.op("dve", "reciprocal", [st_b], [st_b], out=st[:, 2:3], in_=st[:, 1:2])
                C.op("act", "activation", [xt_b[j], st_b], [hn_b[j]], out=hn[j][:], in_=xt[j][:], func=AF.Copy, scale=st[:, 2:3])

            def trans(i):
                j = i % 2
                t = i % 4
                for c in range(8):
                    C.op("pe", "transpose", [hn_b[j], b_const], [pTP_b], sig=(c == 7), out=pTP[:, c * 128:(c + 1) * 128],
                         in_=hn[j][:, c * 128:(c + 1) * 128], identity=identb[:])
                C.op("dve", "tensor_tensor", [pTP_b, b_small], [hT_b[t]], out=hT[:, :, t * 128:(t + 1) * 128],
                     in0=pTP[:, :].rearrange("p (c n) -> p c n", c=8), in1=bc(gfm[:, :].unsqueeze(2), [128, 8, 128]), op=ALU.mult)

            pending = []

            def flush():
                while pending:
                    pending.pop(0)()

            chain(0)
            for sbi in range(NSB):
                cur = sbi % 2
                nxt = 1 - cur
                for t in range(4):
                    i = sbi * 4 + t
                    trans(i)
                    if i + 1 < NT:
                        chain(i + 1)
                    for (name, c0, ncols) in (("q", 0, 512), ("k", 512, 512), ("v", 1024, 512), ("qi", 1536, 512), ("kw", 2048, 72)):
                        bk = tmc[0] % 2
                        tmc[0] += 1
                        for kc in range(8):
                            C.op("pe", "matmul", [hT_b[t], w_b[kc]], [pT_b[bk]], sig=(kc == 7), out=pT[bk][:, 0:512],
                                 lhsT=hT[:, kc, t * 128:(t + 1) * 128], rhs=w_sb[:, kc, c0:c0 + 512], start=(kc == 0), stop=(kc == 7))
                        flush()
                        psv_buf[0] = pT_b[bk]
                        if name == "v":
                            C.op("act", "activation", [pT_b[bk]], [v_b[i]], out=v_sb[:, i, :, 0:64],
                                 in_=pT[bk][:, :].rearrange("p (h d) -> p h d", h=8), func=AF.Copy)
                        elif name == "kw":
                            C.op("act", "activation", [pT_b[bk]], [wi_b[i]], out=wi_sb[:, i, :], in_=pT[bk][:, 64:72], func=AF.Copy)
                            psv = pT[bk][:, 0:64].rearrange("p (h d) -> p h d", h=1)
                            rope_block(psv, 1, i, rqk[:, 0:1, :], [rqk_b])
                            C.op("pool", "tensor_copy", [rqk_b], [rqk_b], out=rqk[:, 1:2, :], in_=rqk[:, 0:1, :])

                            def fin_kw(i=i):
                                C.op("pe", "transpose", [rqk_b, b_const], [pTP_b], sig=True, out=pTP[:, 0:128],
                                     in_=rqk[:, :, :].rearrange("p a d -> p (a d)"), identity=identb[:])
                                C.op("act", "activation", [pTP_b], [kiT_b[i]], out=kiT2[:, i * 128:(i + 1) * 128], in_=pTP[:, 0:128], func=AF.Copy)
                            pending.append(fin_kw)
                        else:
                            rj = tmc[0] % 2
                            psv = pT[bk][:, :].rearrange("p (h d) -> p h d", h=8)
                            rope_block(psv, 8, i, rq[rj][:, :, :], [rq_b[rj]])

                            def fin_rope(i=i, rj=rj, name=name, sj=tmc[0] % 2):
                                for c in range(4):
                                    C.op("pe", "transpose", [rq_b[rj], b_const], [pTP_b], sig=(c == 3), out=pTP[:, c * 128:(c + 1) * 128],
                                         in_=rq[rj][:, 2 * c:2 * c + 2, :].rearrange("p a d -> p (a d)"), identity=identb[:])
                                src = pTP[:, 0:512].rearrange("p (c n) -> p c n", c=4)
                                if name == "k":
                                    C.op("act", "activation", [pTP_b], [kT_b[i]], out=kT[:, :, i * 128:(i + 1) * 128], in_=src, func=AF.Copy)
                                else:
                                    C.op("act", "activation", [pTP_b], [qst_b[sj]], out=qst[sj][:, :, :], in_=src, func=AF.Copy)
                                    if name == "q":
                                        C.dma("sp", qT_scr[i].rearrange("p (c n) -> p c n", c=4), qst[sj][:, :, :], [qst_b[sj]], [q_scr_b[i]], sem_from=qst_b[sj])
                                    else:
                                        C.dma("sp", qiT_scr[i].rearrange("p (c n) -> p c n", c=4), qst[sj][:, :, :], [qst_b[sj]], [qi_scr_b[i]], sem_from=qst_b[sj])
                            pending.append(fin_rope)
                for cc in range(4):
                    for jj in range(31):
                        C.op("dve", "tensor_scalar", [b_const, b_small], [Dw_b], out=Dw[:, jj, :], in0=identb[:],
                             scalar1=cwT[:, cc, jj:jj + 1], scalar2=None, op0=ALU.mult)
                    for kc in range(8):
                        C.op("pe", "matmul", hT_b + [w_b[kc]], [pA_b], sig=(kc == 7), out=pA[:, :],
                             lhsT=w_sb[:, kc, A_OFF + cc * 128:A_OFF + (cc + 1) * 128], rhs=hT[:, kc, :], start=(kc == 0), stop=(kc == 7))
                    flush()
                    for kc in range(8):
                        C.op("pe", "matmul", hT_b + [w_b[kc]], [pB_b], sig=(kc == 7), out=pB[:, :],
                             lhsT=w_sb[:, kc, B_OFF + cc * 128:B_OFF + (cc + 1) * 128], rhs=hT[:, kc, :], start=(kc == 0), stop=(kc == 7))
                    C.op("act", "activation", [pB_b], [sg_b], out=sg[:], in_=pB[:, :], func=AF.Sigmoid)
                    C.op("dve", "tensor_tensor", [pA_b, sg_b], [uT_b[cur][cc]], out=uT[cur][:, cc, 30:542], in0=pA[:, :], in1=sg[:], op=ALU.mult)
                    for jj in range(31):
                        C.op("pe", "matmul", [Dw_b, uT_b[cur][cc], uTpad_b[cur]], [pC_b], sig=(jj == 30), out=pC[:, :],
                             lhsT=Dw[:, jj, :], rhs=uT[cur][:, cc, jj:jj + 512], start=(jj == 0), stop=(jj == 30))
                    C.op("act", "activation", [pC_b, b_small], [co_b[cc]], out=co[:, cc, :], in_=pC[:, :], func=AF.Identity, bias=cb4[:, cc:cc + 1])
                    C.op("act", "activation", [co_b[cc]], [sq_b[cc % 2]], out=sq[cc % 2][:], in_=co[:, cc, :], func=AF.Square)
                    C.op("pe", "matmul", [b_const, co_b[cc]], [pM_b], sig=(cc == 3), out=pM[:, :], lhsT=onesf[:], rhs=co[:, cc, :],
                         start=(cc == 0), stop=(cc == 3))
                    C.op("pe", "matmul", [b_const, sq_b[cc % 2]], [pE_b], sig=(cc == 3), out=pE[:, :], lhsT=onesf[:], rhs=sq[cc % 2][:],
                         start=(cc == 0), stop=(cc == 3))
                if sbi + 1 < NSB:
                    C.op("pool", "tensor_copy", uT_b[cur], [uTpad_b[nxt]], out=uT[nxt][:, :, 0:30], in_=uT[cur][:, :, 512:542])
                C.op("act", "activation", [pM_b], [mean_b], out=mean_sb[:], in_=pM[:, :], func=AF.Copy)
                C.op("pool", "tensor_tensor", [mean_b], [m2_b], out=m2_sb[:], in0=mean_sb[:], in1=mean_sb[:], op=ALU.mult)
                C.op("dve", "tensor_tensor", [pE_b, m2_b], [m2_b], out=m2_sb[:], in0=pE[:, :], in1=m2_sb[:], op=ALU.subtract)
                C.op("act", "activation", [m2_b], [m2_b], out=m2_sb[:], in_=m2_sb[:], func=AF.Sqrt, bias=EPS, scale=1.0)
                C.op("dve", "reciprocal", [m2_b], [rstd_b], out=rstd_sb[:], in_=m2_sb[:])
                for cc in range(4):
                    dj = cc % 2
                    C.op("pool", "tensor_tensor", [co_b[cc], mean_b], [dtmp_b[dj]], out=dtmp[dj][:], in0=co[:, cc, :], in1=mean_sb[:], op=ALU.subtract)
                    C.op("pool", "tensor_tensor", [dtmp_b[dj], rstd_b], [dtmp_b[dj]], out=dtmp[dj][:], in0=dtmp[dj][:], in1=rstd_sb[:], op=ALU.mult)
                    C.op("act", "activation", [dtmp_b[dj], b_small], [zT_b], out=zT[:, cc, :], in_=dtmp[dj][:], func=AF.Silu,
                         scale=lng4[:, cc:cc + 1], bias=lnb4[:, cc:cc + 1])
                C.dma("sp", z_scr[sbi].rearrange("p (c n) -> p c n", c=4), zT[:, :, :], [zT_b], [z_scr_b[sbi]], sem_from=zT_b)
            C.barrier()

        if stop_after == "A1":
            if "kT" in dbg_out:
                db = C.buf("dbg")
                C.dma("sp", dbg_out["kT"].rearrange("p (c n) -> p c n", c=4), kT[:, :, :], kT_b, [db])
                C.dma("sp", dbg_out["kiT2"][:, :], kiT2[:, :], kiT_b, [db])
                C.dma("sp", dbg_out["v"].rearrange("p (t h d) -> p t h d", t=NT, h=8), v_sb[:, :, :, :], v_b, [db])
                C.dma("sp", dbg_out["wi"].rearrange("p (t h) -> p t h", t=NT), wi_sb[:, :, :], wi_b, [db])
                C.dma("sp", dbg_out["qT"].rearrange("(t p) n -> p t n", p=128), qT_scr.rearrange("t p n -> p t n"), q_scr_b, [db])
                C.dma("sp", dbg_out["qiT"].rearrange("(t p) n -> p t n", p=128), qiT_scr.rearrange("t p n -> p t n"), qi_scr_b, [db])
                C.dma("sp", dbg_out["z"].rearrange("(t p) n -> p t n", p=128), z_scr.rearrange("t p n -> p t n"), z_scr_b, [db])
                C.final_wait("sp", [db])
            C.barrier()
            C.final_wait("sp", q_scr_b + qi_scr_b + z_scr_b)
            return nc

        with ExitStack() as A2:
            qTz = [[sb(A2, "qTz%d_%d" % (par, j), [128, 4, 128], BF16) for j in range(2)] for par in range(2)]
            qiTz = [[sb(A2, "qiTz%d_%d" % (par, j), [128, 4, 128], BF16) for j in range(2)] for par in range(2)]
            qT_tb = C.bufs("qT_t", 2)
            qiT_tb = C.bufs("qiT_t", 2)
            Dg = sb(A2, "Dg", [128, 8, 128], BF16)
            Dg_b = C.buf("Dg")
            R_sb = [sb(A2, "R_sb%d" % j, [128, 512], BF16) for j in range(2)]
            R_b = C.bufs("R_sb", 2)
            sc = [sb(A2, "sc%d" % j, [128, S], F32) for j in range(2)]
            sc_b = C.bufs("sc", 2)
            NM = [sb(A2, "NM%d" % j, [128, S], BF16) for j in range(2)]
            NM_b = C.bufs("NM", 2)
            bs = sb(A2, "bs", [128, 8], F32)
            bs_b = C.buf("bs")
            wk = sb(A2, "wk", [128, BIS_ITERS + 2], F32)
            pow2 = sb(A2, "pow2", [128, BIS_ITERS + 2], F32)
            thrc = sb(A2, "thrc", [128, 1], F32)
            b_c2 = C.buf("constA2")
            C.dma("sp", pow2[:], pow2_d[:, :], [], [b_c2])
            C.op("pool", "memset", [], [b_c2], ap=thrc[:], constant=-1e29)
            for par in range(2):
                for j in range(2):
                    C.op("dve", "memset", [], [qT_tb[j]], ap=qTz[par][j][:, :, :], constant=0.0)
                    C.op("dve", "memset", [], [qiT_tb[j]], ap=qiTz[par][j][:, :, :], constant=0.0)
            PT = [sb(A2, "PT%d" % j, [128, 512], BF16) for j in range(2)]
            PT_b = C.bufs("PT", 2)
            rden = sb(A2, "rden", [128, 8], F32)
            rden_b = C.buf("rden")
            ao = sb(A2, "ao", [128, 8, 64], BF16)
            ao_b = C.buf("ao")
            aoT_st = [sb(A2, "aoT_st%d" % j, [128, 4, 128], BF16) for j in range(2)]
            aoT_b = C.bufs("aoT_st", 2)
            pR = [ps(A2, "pR%d" % j, [128, 512], F32) for j in range(2)]
            pR_b = C.bufs("pR", 2)
            pSC = ps(A2, "pSC", [128, 512], F32)
            pSC_b = C.buf("pSC")
            pST = [ps(A2, "pST%d" % j, [128, 512], F32) for j in range(2)]
            pST_b = C.bufs("pST", 2)
            pPV = [ps(A2, "pPV%d" % j, [128, 512], F32) for j in range(2)]
            pPV_b = C.bufs("pPV", 2)
            pTP2 = ps(A2, "pTP2", [128, 1024], BF16)
            pTP2_b = C.buf("pTP2")
            ao_scr_b = C.bufs("aoscr", NT, share=True)

            C.barrier()
            tiles = list(range(NT)) if a2_tiles is None else list(a2_tiles)
            def index_phase(n_i, i):
                s2 = n_i % 2
                NK = 128 * (i + 1)
                nkc = i + 1
                nkb = (NK + 511) // 512
                for par in range(2):
                    pr = slice(par * 64, (par + 1) * 64)
                    C.dma("sp", qTz[par][s2][pr, :, :], qT_scr[i][pr, :].rearrange("p (c n) -> p c n", c=4), [q_scr_b[i]], [qT_tb[s2]])
                    C.dma("sp", qiTz[par][s2][pr, :, :], qiT_scr[i][pr, :].rearrange("p (c n) -> p c n", c=4), [qi_scr_b[i]], [qiT_tb[s2]])
                for h in range(8):
                    C.op("act", "activation", [b_const, wi_b[i]], [Dg_b], out=Dg[:, h, :], in_=identb[:], func=AF.Copy,
                         scale=wi_sb[:, i, h:h + 1])
                for kb in range(nkb):
                    k0 = kb * 512
                    W = min(512, NK - k0)
                    kbufs = kiT_b[k0 // 128:(k0 + W) // 128]

                    def r_mm(h):
                        C.op("pe", "matmul", [qiT_tb[s2]] + kbufs, [pR_b[h % 2]], sig=True, out=pR[h % 2][:, 0:W],
                             lhsT=qiTz[h % 2][s2][:, h // 2, :], rhs=kiT2[:, k0:k0 + W], start=True, stop=True)
                        C.op("act", "activation", [pR_b[h % 2]], [R_b[h % 2]], out=R_sb[h % 2][:, 0:W], in_=pR[h % 2][:, 0:W], func=AF.Relu)

                    def s_mm(h):
                        C.op("pe", "matmul", [Dg_b, R_b[h % 2]], [pSC_b], sig=(h == 7), out=pSC[:, 0:W], lhsT=Dg[:, h, :],
                             rhs=R_sb[h % 2][:, 0:W], start=(h == 0), stop=(h == 7))

                    r_mm(0)
                    for h in range(8):
                        if h + 1 < 8:
                            r_mm(h + 1)
                        s_mm(h)
                    C.op("act", "activation", [pSC_b], [sc_b[s2]], out=sc[s2][:, k0:k0 + W], in_=pSC[:, 0:W], func=AF.Copy)
                C.op("pool", "memset", [], [sc_b[s2]], ap=sc[s2][0:64, NK - 64:NK], constant=-1e30)

            def thresh_phase(n_i, i):
                s2 = n_i % 2
                NK = 128 * (i + 1)
                nkc = i + 1
                nkb = (NK + 511) // 512
                if i >= 2:
                    C.op("dve", "tensor_reduce", [sc_b[s2]], [bs_b], out=bs[:, 0:1], in_=sc[s2][:, 0:NK], axis=AX.X, op=ALU.max)
                    C.op("dve", "tensor_reduce", [sc_b[s2]], [bs_b], out=bs[:, 1:2], in_=sc[s2][:, 0:320], axis=AX.X, op=ALU.min)
                    C.op("dve", "tensor_tensor", [bs_b], [bs_b], out=bs[:, 2:3], in0=bs[:, 0:1], in1=bs[:, 1:2], op=ALU.subtract)
                    C.op("dve", "tensor_scalar", [bs_b, b_c2], [bs_b], out=wk[:], in0=pow2[:], scalar1=bs[:, 2:3], scalar2=None, op0=ALU.mult)
                    C.op("dve", "tensor_tensor", [bs_b], [bs_b], out=bs[:, 3:4], in0=bs[:, 1:2], in1=wk[:, 1:2], op=ALU.add)
                    for k in range(BIS_ITERS):
                        C.op("dve", "tensor_scalar", [sc_b[s2], bs_b], [NM_b[s2], bs_b], out=NM[s2][:, 0:NK], in0=sc[s2][:, 0:NK],
                             scalar1=bs[:, 3:4], scalar2=None, op0=ALU.is_ge, op1=ALU.add, accum_out=bs[:, 4:5])
                        C.op("dve", "tensor_scalar", [bs_b], [bs_b], out=bs[:, 5:6], in0=bs[:, 4:5], scalar1=255.5, scalar2=0.5,
                             op0=ALU.is_ge, op1=ALU.subtract)
                        if k < BIS_ITERS - 1:
                            C.op("dve", "scalar_tensor_tensor", [bs_b], [bs_b], out=bs[:, 3:4], in0=bs[:, 5:6], scalar=wk[:, k + 1:k + 2],
                                 in1=bs[:, 3:4], op0=ALU.mult, op1=ALU.add)
                    C.op("dve", "tensor_scalar", [bs_b], [bs_b], out=bs[:, 5:6], in0=bs[:, 5:6], scalar1=-0.5, scalar2=None, op0=ALU.add)
                    C.op("dve", "scalar_tensor_tensor", [bs_b], [bs_b], out=bs[:, 6:7], in0=bs[:, 5:6], scalar=wk[:, BIS_ITERS:BIS_ITERS + 1],
                         in1=bs[:, 3:4], op0=ALU.mult, op1=ALU.add)
                    thr_ap = bs[:, 6:7]
                    thr_bufs = [bs_b]
                else:
                    thr_ap = thrc[:, 0:1]
                    thr_bufs = [b_c2]
                C.op("dve", "tensor_scalar", [sc_b[s2]] + thr_bufs, [NM_b[s2]], out=NM[s2][:, 0:NK], in0=sc[s2][:, 0:NK],
                     scalar1=thr_ap, scalar2=NEG_BIG, op0=ALU.is_lt, op1=ALU.mult)

            def attn_phase(n_i, i):
                s2 = n_i % 2
                NK = 128 * (i + 1)
                nkc = i + 1
                nkb = (NK + 511) // 512
                items = [(h, kb) for h in range(8) for kb in range(nkb)]

                def st_block(n):
                    h, kb = items[n]
                    sl = n % 2
                    pb = (h % 2) * 64
                    c = h // 2
                    kcs = list(range(kb * 4, min(nkc, kb * 4 + 4)))
                    for kcl, kc in enumerate(kcs):
                        C.op("pe", "matmul", [kT_b[kc], qT_tb[s2]], [pST_b[sl]], out=pST[sl][:, kcl * 128:(kcl + 1) * 128],
                             lhsT=kT[:, c, kc * 128:(kc + 1) * 128], rhs=qTz[h % 2][s2][:, c, :], start=True, stop=False)
                        C.op("pe", "matmul", [NM_b[s2], b_const], [pST_b[sl]], sig=(kcl == len(kcs) - 1),
                             out=pST[sl][:, kcl * 128:(kcl + 1) * 128],
                             lhsT=NM[s2][:, kc * 128:(kc + 1) * 128], rhs=identb[:], start=False, stop=True)
                    C.op("act", "activation", [pST_b[sl]], [PT_b[sl]], out=PT[sl][:, 0:len(kcs) * 128], in_=pST[sl][:, 0:len(kcs) * 128],
                         func=AF.Exp, scale=0.125)

                def pv_block(n):
                    h, kb = items[n]
                    sl = n % 2
                    kcs = list(range(kb * 4, min(nkc, kb * 4 + 4)))
                    for kcl, kc in enumerate(kcs):
                        C.op("pe", "matmul", [PT_b[sl], v_b[kc]], [pPV_b[h // 4]], out=pPV[h // 4][:, (h % 4) * 65:(h % 4) * 65 + 65],
                             lhsT=PT[sl][:, kcl * 128:(kcl + 1) * 128], rhs=v_sb[:, kc, h, :], start=(kc == 0), stop=(kc == nkc - 1))

                st_block(0)
                for n in range(len(items)):
                    if n + 1 < len(items):
                        st_block(n + 1)
                    pv_block(n)
                fs = len(items) % 2
                C.op("pe", "matmul", [b_const], [pST_b[fs]], sig=True, out=pST[fs][:, 0:128], lhsT=identb[:], rhs=identb[:], start=True, stop=True)

            def attn_tail(n_i, i):
                s2 = n_i % 2
                for hh in range(2):
                    pvv = pPV[hh][:, 0:260].rearrange("p (h d) -> p h d", h=4)
                    C.op("dve", "reciprocal", [pPV_b[hh]], [rden_b], out=rden[:, hh * 4:hh * 4 + 4].unsqueeze(2), in_=pvv[:, :, 64:65])
                    C.op("dve", "tensor_tensor", [pPV_b[hh], rden_b], [ao_b], out=ao[:, hh * 4:hh * 4 + 4, :], in0=pvv[:, :, 0:64],
                         in1=bc(rden[:, hh * 4:hh * 4 + 4].unsqueeze(2), [128, 4, 64]), op=ALU.mult)
                for c in range(4):
                    C.op("pe", "transpose", [ao_b, b_const], [pTP2_b], sig=(c == 3), out=pTP2[:, c * 128:(c + 1) * 128],
                         in_=ao[:, 2 * c:2 * c + 2, :].rearrange("p a d -> p (a d)"), identity=identb[:])
                C.op("act", "activation", [pTP2_b], [aoT_b[s2]], out=aoT_st[s2][:, :, :], in_=pTP2[:, 0:512].rearrange("p (c n) -> p c n", c=4), func=AF.Copy)
                C.dma("sp", ao_scr[i].rearrange("p (c n) -> p c n", c=4), aoT_st[s2][:, :, :], [aoT_b[s2]], [ao_scr_b[i]], sem_from=aoT_b[s2])

            nt_ = len(tiles)
            index_phase(0, tiles[0])
            thresh_phase(0, tiles[0])
            if nt_ > 1:
                index_phase(1, tiles[1])
            for n_i in range(nt_):
                attn_phase(n_i, tiles[n_i])
                if n_i + 1 < nt_:
                    thresh_phase(n_i + 1, tiles[n_i + 1])
                if n_i + 2 < nt_:
                    index_phase(n_i + 2, tiles[n_i + 2])
                attn_tail(n_i, tiles[n_i])
            s2 = (nt_ - 1) % 2
            if "nm" in dbg_out:
                dbn = C.buf("dbgnm")
                C.dma("sp", dbg_out["nm"][:, :], NM[s2][:, :], [NM_b[s2]], [dbn])
                C.dma("sp", dbg_out["sc"][:, :], sc[s2][:, :], [sc_b[s2]], [dbn])
                C.final_wait("sp", [dbn])
            C.barrier()

        if stop_after == "A2":
            db = C.buf("dbg2")
            C.dma("sp", dbg_out["ao"].rearrange("(t p) n -> p t n", p=128), ao_scr.rearrange("t p n -> p t n"), ao_scr_b, [db])
            C.final_wait("sp", [db])
            return nc

        PA.close()

        xn_scr = dscr("xn_scr", [NT, 128, 1024], BF16)
        xn_scr_b = C.bufs("xnscr", NT, share=True)
        x2_scr_b = C.bufs("x2scr", NT, share=True)
        PBC = ExitStack()
        es.enter_context(PBC)
        comb = sb(PBC, "comb", [128, NT, 32], F32)
        comb_b = C.bufs("comb", NT)
        lgall = sb(PBC, "lgall", [128, NT, 36], F32)
        lgall_b = C.buf("lgall")

        def load_w(stack, name, src, rows, c0, c1, bufname):
            nkc = rows // 128
            t = sb(stack, name, [128, nkc, c1 - c0], BF16)
            b = C.buf(bufname)
            for kc in range(nkc):
                C.dma("pool", t[:, kc, :], src[kc * 128:(kc + 1) * 128, c0:c1], [], [b])
            return t, b

        def rms_rstd(x_ap, x_bufs, junk_ap, junk_bufs, st, st_b, scale_n=D):
            C.op("act", "activation", x_bufs, junk_bufs + [st_b], out=junk_ap, in_=x_ap, func=AF.Square, accum_out=st[:, 0:1])
            C.op("act", "activation", [st_b], [st_b], out=st[:, 1:2], in_=st[:, 0:1], func=AF.Sqrt, scale=1.0 / scale_n, bias=EPS)
            C.op("dve", "reciprocal", [st_b], [st_b], out=st[:, 2:3], in_=st[:, 1:2])

        with ExitStack() as B:
            wg_sb, wg_b = load_w(B, "w_gates", w_in_d, D, G_OFF, G_OFF + 2048, "w_gates")
            woa_sb, woa_b = load_w(B, "w_oa", w_o_attn_d, 512, 0, D, "w_oa")
            wco_sb, wco_b = load_w(B, "w_co", w_conv_out_d, 512, 0, D, "w_co")
            wout_sb, wout_b = load_w(B, "w_outb", w_out_d, D, 0, D, "w_outb")
            wqx_sb, wqx_b = load_w(B, "w_qxb", w_q_x_d, D, 0, D, "w_qxb")
            wox_sb, wox_b = load_w(B, "w_oxb", w_o_x_d, D, 0, D, "w_oxb")
            kmT = sb(B, "kmT", [128, 8, 256], BF16)
            vm = sb(B, "vm", [128, 2, D], BF16)
            kmT_b = C.buf("kmT")
            vm_b = C.buf("vm")
            gfm = sb(B, "gfmB", [128, 8], F32)
            gxm = sb(B, "gxm", [128, 8], F32)
            gmm = sb(B, "gmm", [128, 8], F32)
            gmo = sb(B, "gmo", [128, 8], F32)
            wr_sb = sb(B, "wr_sb", [128, 8, 36], F32)
            br_sb = sb(B, "br_sb", [128, 36], F32)
            b_smB = C.buf("smallB")
            C.dma("sp", gfm[:], g_mix_d[:, :], [], [b_smB])
            C.dma("sp", gxm[:], g_x_d[:, :], [], [b_smB])
            C.dma("sp", gmm[:], g_mem_d[:, :], [], [b_smB])
            C.dma("sp", gmo[:], g_moe_d[:, :], [], [b_smB])
            C.dma("sp", wr_sb[:, :, :], w_router_d.rearrange("(c p) n -> p c n", p=128), [], [b_smB])
            C.dma("sp", br_sb[:], bc(b_router_d[0:1, :], [128, 36]), [], [b_smB])
            stB = sb(B, "statB", [128, 8], F32)
            stB_b = C.buf("statB")
            pb = [ps(B, "pb%d" % j, [128, 512], F32) for j in range(7)]
            pb_b = C.bufs("pb", 7)
            pTPb = ps(B, "pTPb", [128, 1024], BF16)
            pTPb_b = C.buf("pTPb")

            sqj_ref = []

            def norm_chain(x_ap, x_bufs, hn_ap, hn_bufs):
                if sqj_ref:
                    rms_rstd(x_ap, x_bufs, sqj_ref[0][:, :], [sqj_ref[1]], stB, stB_b)
                else:
                    rms_rstd(x_ap, x_bufs, hn_ap, hn_bufs, stB, stB_b)
                C.op("act", "activation", x_bufs + [stB_b], hn_bufs, out=hn_ap, in_=x_ap, func=AF.Copy, scale=stB[:, 2:3])

            def norm_trans(hn_ap, hn_bufs, g_sb, dstT, dst_bufs, col0):
                for c in range(8):
                    C.op("pe", "transpose", hn_bufs + [b_const], [pTPb_b], sig=(c == 7), out=pTPb[:, c * 128:(c + 1) * 128],
                         in_=hn_ap[:, c * 128:(c + 1) * 128], identity=identb[:])
                C.op("dve", "tensor_tensor", [pTPb_b, b_smB], dst_bufs, out=dstT[:, :, col0:col0 + 128],
                     in0=pTPb[:, :].rearrange("p (c n) -> p c n", c=8), in1=bc(g_sb[:, :].unsqueeze(2), [128, 8, 128]), op=ALU.mult)

            def norm_T(x_ap, x_bufs, hn_ap, hn_bufs, g_sb, dstT, dst_bufs, col0):
                norm_chain(x_ap, x_bufs, hn_ap, hn_bufs)
                norm_trans(hn_ap, hn_bufs, g_sb, dstT, dst_bufs, col0)

            with ExitStack() as BK:
                wkv_sb, wkv_b = load_w(BK, "w_kvb", w_kv_x_d, D, 0, 2 * D, "w_kvb")
                memt = sb(BK, "memt", [128, D], F32)
                memt_b = C.buf("memt")
                memh = sb(BK, "memh", [128, D], BF16)
                memh_b = C.buf("memh")
                memnT = sb(BK, "memnT", [128, 8, 256], BF16)
                memnT_b = C.buf("memnT")
                for mt in range(2):
                    C.dma("sp", memt[:], mem_d[mt * 128:(mt + 1) * 128, :], [], [memt_b])
                    norm_T(memt[:, :], [memt_b], memh[:, :], [memh_b], gmm, memnT, [memnT_b], mt * 128)
                for jx in range(8):
                    bk = jx % 2
                    for kc in range(8):
                        C.op("pe", "matmul", [wkv_b, memnT_b], [pb_b[bk]], sig=(kc == 7), out=pb[bk][:, 0:256],
                             lhsT=wkv_sb[:, kc, jx * 128:(jx + 1) * 128], rhs=memnT[:, kc, :], start=(kc == 0), stop=(kc == 7))
                    C.op("act", "activation", [pb_b[bk]], [kmT_b], out=kmT[:, jx, :], in_=pb[bk][:, 0:256], func=AF.Copy)
                for mc in range(2):
                    for cb in range(2):
                        bk = 2 + (mc * 2 + cb) % 2
                        for kc in range(8):
                            C.op("pe", "matmul", [wkv_b, memnT_b], [pb_b[bk]], sig=(kc == 7), out=pb[bk][:, :],
                                 lhsT=memnT[:, kc, mc * 128:(mc + 1) * 128], rhs=wkv_sb[:, kc, D + cb * 512:D + (cb + 1) * 512],
                                 start=(kc == 0), stop=(kc == 7))
                        C.op("act", "activation", [pb_b[bk]], [vm_b], out=vm[:, mc, cb * 512:(cb + 1) * 512], in_=pb[bk][:, :], func=AF.Copy)
                C.barrier()

            xt4 = sb(B, "xt4", [128, 4, D], F32)
            xt4_b = C.bufs("xt4", 4)
            hnB = [sb(B, "hnB%d" % j, [128, D], BF16) for j in range(4)]
            hnB_b = C.bufs("hnB", 4)
            sqj = sb(B, "sqj", [128, D], BF16)
            sqj_b = C.buf("sqj")
            sqj_ref.extend([sqj, sqj_b])
            bufA = sb(B, "bufA", [128, 8, 512], BF16)
            bufA_b = C.bufs("bufA", 4)
            bufB = sb(B, "bufB", [128, 8, 512], BF16)
            bufB_b = C.bufs("bufB", 8)
            bufC = sb(B, "bufC", [128, 8, 512], BF16)
            bufC_b = C.bufs("bufC", 8)
            aoTb = sb(B, "aoTb", [128, 4, 512], BF16)
            aoTb_b = C.buf("aoTb")
            zTb = sb(B, "zTb", [128, 4, 512], BF16)
            zTb_b = C.buf("zTb")
            sgA = sb(B, "sgA", [128, 512], F32)
            sgB = sb(B, "sgB", [128, 512], F32)
            sgA_b = C.buf("sgA")
            sgB_b = C.buf("sgB")
            PT2 = sb(B, "PT2", [128, 2, 512], BF16)
            PT2_b = C.bufs("PT2", 2)
            rden2 = sb(B, "rden2", [128, 512], F32)
            rden2_b = C.buf("rden2")
            xn32s = [sb(B, "xn32_%d" % j, [128, D], F32) for j in range(2)]
            xn32s_b = C.bufs("xn32", 2)
            xnT32 = sb(B, "xnT32", [128, 8, 128], F32)
            xnT32_b = C.buf("xnT32")
            xnTb = [sb(B, "xnTb%d" % j, [128, 8, 128], BF16) for j in range(2)]
            xnTb_b = C.bufs("xnTb", 2)
            rt_ = sb(B, "rtr", [128, 128], F32)
            rt_b2 = C.buf("rtr")

            def load_x(blk, t):
                i = blk * 4 + t
                C.dma("sp", xt4[:, t, :], x_d[i * 128:(i + 1) * 128, :], [], [xt4_b[t]])

            def load_aoz(blk):
                for t in range(4):
                    i = blk * 4 + t
                    C.dma("sp", aoTb[:, :, t * 128:(t + 1) * 128], ao_scr[i].rearrange("p (c n) -> p c n", c=4), [ao_scr_b[i]], [aoTb_b])
                C.dma("sp", zTb[:, :, :], z_scr[blk].rearrange("p (c n) -> p c n", c=4), [z_scr_b[blk]], [zTb_b])

            def pipelined_norms(g_sb, ready=None):
                def ch(t):
                    norm_chain(xt4[:, t, :], [xt4_b[t]], hnB[t][:, :], [hnB_b[t]])

                def tr(t):
                    norm_trans(hnB[t][:, :], [hnB_b[t]], g_sb, bufA, [bufA_b[t]], t * 128)
                return ch, tr

            for t in range(4):
                load_x(0, t)
            load_aoz(0)
            for blk in range(NSB):
                ch, tr = pipelined_norms(gfm)
                for t in range(4):
                    ch(t)
                for t in range(4):
                    tr(t)
                for fo in range(8):
                    fs_ = slice(fo * 128, (fo + 1) * 128)
                    b0, b1, b2 = (0, 1, 2) if fo % 2 == 0 else (4, 5, 6)
                    for c in range(4):
                        C.op("pe", "matmul", [woa_b, aoTb_b], [pb_b[b0]], sig=(c == 3), out=pb[b0][:, :], lhsT=woa_sb[:, c, fs_], rhs=aoTb[:, c, :],
                             start=(c == 0), stop=(c == 3))
                    for c in range(4):
                        C.op("pe", "matmul", [wco_b, zTb_b], [pb_b[b1]], sig=(c == 3), out=pb[b1][:, :], lhsT=wco_sb[:, c, fs_], rhs=zTb[:, c, :],
                             start=(c == 0), stop=(c == 3))
                    for kc in range(8):
                        C.op("pe", "matmul", [wg_b] + bufA_b, [pb_b[b2]], sig=(kc == 7), out=pb[b2][:, :], lhsT=wg_sb[:, kc, fs_], rhs=bufA[:, kc, :],
                             start=(kc == 0), stop=(kc == 7))
                    for kc in range(8):
                        C.op("pe", "matmul", [wg_b] + bufA_b, [pb_b[3]], sig=(kc == 7), out=pb[3][:, :],
                             lhsT=wg_sb[:, kc, D + fo * 128:D + (fo + 1) * 128], rhs=bufA[:, kc, :], start=(kc == 0), stop=(kc == 7))
                    C.op("act", "activation", [pb_b[b2]], [sgA_b], out=sgA[:], in_=pb[b2][:, :], func=AF.Sigmoid)
                    C.op("act", "activation", [pb_b[3]], [sgB_b], out=sgB[:], in_=pb[3][:, :], func=AF.Sigmoid)
                    C.op("dve", "tensor_tensor", [pb_b[b0], sgA_b], [sgA_b], out=sgA[:], in0=pb[b0][:, :], in1=sgA[:], op=ALU.mult)
                    C.op("dve", "tensor_tensor", [pb_b[b1], sgB_b], [sgB_b], out=sgB[:], in0=pb[b1][:, :], in1=sgB[:], op=ALU.mult)
                    C.op("pool", "tensor_tensor", [sgA_b, sgB_b], [bufB_b[fo]], out=bufB[:, fo, :], in0=sgA[:], in1=sgB[:], op=ALU.add)
                if blk + 1 < NSB:
                    load_aoz(blk + 1)
                ch, tr = pipelined_norms(gxm)
                for t in range(4):
                    if t >= 2:
                        tr(t - 2)
                    for cb in range(2):
                        bk = 4 + (t * 2 + cb) % 2
                        for kc in range(8):
                            C.op("pe", "matmul", [wout_b, bufB_b[kc]], [pb_b[bk]], sig=(kc == 7), out=pb[bk][:, :],
                                 lhsT=bufB[:, kc, t * 128:(t + 1) * 128], rhs=wout_sb[:, kc, cb * 512:(cb + 1) * 512], start=(kc == 0), stop=(kc == 7))
                        C.op("dve", "tensor_tensor", [pb_b[bk], xt4_b[t]], [xt4_b[t]], out=xt4[:, t, cb * 512:(cb + 1) * 512], in0=pb[bk][:, :],
                             in1=xt4[:, t, cb * 512:(cb + 1) * 512], op=ALU.add)
                    ch(t)
                tr(2)
                tr(3)
                for fo in range(8):
                    bk = 4 + fo % 2
                    for kc in range(8):
                        C.op("pe", "matmul", [wqx_b] + bufA_b, [pb_b[bk]], sig=(kc == 7), out=pb[bk][:, :],
                             lhsT=wqx_sb[:, kc, fo * 128:(fo + 1) * 128], rhs=bufA[:, kc, :], start=(kc == 0), stop=(kc == 7))
                    C.op("act", "activation", [pb_b[bk]], [bufB_b[fo]], out=bufB[:, fo, :], in_=pb[bk][:, :], func=AF.Copy)
                for h in range(4):
                    for mc in range(2):
                        for dc in range(2):
                            C.op("pe", "matmul", [kmT_b, bufB_b[h * 2 + dc]], [pb_b[mc]], sig=(dc == 1), out=pb[mc][:, :],
                                 lhsT=kmT[:, h * 2 + dc, mc * 128:(mc + 1) * 128], rhs=bufB[:, h * 2 + dc, :], start=(dc == 0), stop=(dc == 1))
                        C.op("act", "activation", [pb_b[mc]], [PT2_b[mc]], out=PT2[:, mc, :], in_=pb[mc][:, :], func=AF.Exp, scale=1.0 / 16.0)
                    for mc in range(2):
                        C.op("pe", "matmul", [b_const, PT2_b[mc]], [pb_b[2]], sig=(mc == 1), out=pb[2][:, :], lhsT=onesb[:], rhs=PT2[:, mc, :],
                             start=(mc == 0), stop=(mc == 1))
                    C.op("dve", "reciprocal", [pb_b[2]], [rden2_b], out=rden2[:], in_=pb[2][:, :])
                    for dvc in range(2):
                        bk = 3 if dvc == 0 else 6
                        for mc in range(2):
                            C.op("pe", "matmul", [vm_b, PT2_b[mc]], [pb_b[bk]], sig=(mc == 1), out=pb[bk][:, :],
                                 lhsT=vm[:, mc, h * 256 + dvc * 128:h * 256 + (dvc + 1) * 128], rhs=PT2[:, mc, :], start=(mc == 0), stop=(mc == 1))
                        C.op("dve", "tensor_tensor", [pb_b[bk], rden2_b], [bufC_b[h * 2 + dvc]], out=bufC[:, h * 2 + dvc, :], in0=pb[bk][:, :],
                             in1=rden2[:], op=ALU.mult)
                def wox(t):
                    i = blk * 4 + t
                    for cb in range(2):
                        bk = 4 + (t * 2 + cb) % 2
                        for kc in range(8):
                            C.op("pe", "matmul", [wox_b, bufC_b[kc]], [pb_b[bk]], sig=(kc == 7), out=pb[bk][:, :],
                                 lhsT=bufC[:, kc, t * 128:(t + 1) * 128], rhs=wox_sb[:, kc, cb * 512:(cb + 1) * 512], start=(kc == 0), stop=(kc == 7))
                        C.op("dve", "tensor_tensor", [pb_b[bk], xt4_b[t]], [xt4_b[t]], out=xt4[:, t, cb * 512:(cb + 1) * 512], in0=pb[bk][:, :],
                             in1=xt4[:, t, cb * 512:(cb + 1) * 512], op=ALU.add)
                    C.dma("sp", x2_scr[i * 128:(i + 1) * 128, :], xt4[:, t, :], [xt4_b[t]], [x2_scr_b[i]], sem_from=xt4_b[t])
                    xn32, xn32_b = xn32s[t % 2], xn32s_b[t % 2]
                    rms_rstd(xt4[:, t, :], [xt4_b[t]], sqj[:, :], [sqj_b], stB, stB_b)
                    C.op("act", "activation", [xt4_b[t], stB_b], [xn32_b], out=xn32[:, :], in_=xt4[:, t, :], func=AF.Copy, scale=stB[:, 2:3])

                def b6(t):
                    i = blk * 4 + t
                    xn32, xn32_b = xn32s[t % 2], xn32s_b[t % 2]
                    for c in range(8):
                        C.op("pe", "transpose", [xn32_b, b_const], [pb_b[c // 4]], sig=(c % 4 == 3), out=pb[c // 4][:, (c % 4) * 128:(c % 4 + 1) * 128],
                             in_=xn32[:, c * 128:(c + 1) * 128], identity=identf[:])
                    for hh in range(2):
                        C.op("dve", "tensor_tensor", [pb_b[hh], b_smB], [xnT32_b], out=xnT32[:, hh * 4:hh * 4 + 4, :],
                             in0=pb[hh][:, :].rearrange("p (c n) -> p c n", c=4), in1=bc(gmo[:, hh * 4:hh * 4 + 4].unsqueeze(2), [128, 4, 128]), op=ALU.mult)
                    xj = i % 2
                    C.op("pool", "tensor_copy", [xnT32_b], [xnTb_b[xj]], out=xnTb[xj][:, :, :], in_=xnT32[:, :, :])
                    C.dma("sp", xn_scr[i].rearrange("p (c n) -> p c n", c=8), xnTb[xj][:, :, :], [xnTb_b[xj]], [xn_scr_b[i]], sem_from=xnTb_b[xj])
                    for kc in range(8):
                        C.op("pe", "matmul", [xnT32_b, b_smB], [pb_b[2]], out=pb[2][:, 0:36], lhsT=xnT32[:, kc, :], rhs=wr_sb[:, kc, :],
                             start=(kc == 0), stop=(kc == 7))
                    C.op("pe", "matmul", [b_const], [pb_b[3]], sig=True, out=pb[3][:, 0:128], lhsT=identb[:], rhs=identb[:], start=True, stop=True)
                    C.op("dve", "tensor_tensor", [pb_b[2], b_smB], [lgall_b], out=lgall[:, i, :], in0=pb[2][:, 0:36], in1=br_sb[:, :], op=ALU.add)
                wox(0)
                wox(1)
                b6(0)
                wox(2)
                b6(1)
                wox(3)
                b6(2)
                if blk + 1 < NSB:
                    for t in range(3):
                        load_x(blk + 1, t)
                b6(3)
                if blk + 1 < NSB:
                    load_x(blk + 1, 3)
            C.barrier()

        with ExitStack() as BR:
            T = NT
            gmax = sb(BR, "r_gmax", [128, T], F32)
            oneh = sb(BR, "r_oneh", [128, T, 4], F32)
            dgl = sb(BR, "r_dgl", [128, T, 4], F32)
            sume = sb(BR, "r_sume", [128, T], F32)
            ggate = sb(BR, "r_ggate", [128, T], F32)
            tmpe = sb(BR, "r_tmpe", [128, T, 32], F32)
            elsel = sb(BR, "r_elsel", [128, T, 8], F32)
            m8 = sb(BR, "r_m8", [128, T, 8], F32)
            sc1 = sb(BR, "r_sc1", [128, 4, T], F32)
            eqa = sb(BR, "r_eqa", [128, T, 8], F32)
            eqb = sb(BR, "r_eqb", [128, T, 8], F32)
            rb_ = C.buf("routing")
            rr, ww = [lgall_b, rb_], [rb_]
            gl = lgall[:, :, 0:4]
            el4 = lgall[:, :, 4:36].rearrange("p t (g e) -> p t g e", g=4)
            C.op("dve", "tensor_reduce", rr, ww, out=gmax[:, :], in_=gl, axis=AX.X, op=ALU.max)
            C.op("dve", "tensor_tensor", rr, ww, out=oneh[:, :, :], in0=gl, in1=bc(gmax[:, :].unsqueeze(2), [128, T, 4]), op=ALU.is_equal)
            C.op("dve", "tensor_tensor", rr, ww, out=dgl[:, :, :], in0=gl, in1=bc(gmax[:, :].unsqueeze(2), [128, T, 4]), op=ALU.subtract)
            C.op("act", "activation", rr, ww, out=dgl[:, :, :], in_=dgl[:, :, :], func=AF.Exp)
            C.op("dve", "tensor_reduce", rr, ww, out=sume[:, :], in_=dgl[:, :, :], axis=AX.X, op=ALU.add)
            C.op("dve", "reciprocal", rr, ww, out=ggate[:, :], in_=sume[:, :])
            C.op("dve", "tensor_tensor", rr, ww, out=tmpe[:, :, :].rearrange("p t (g e) -> p t g e", g=4), in0=el4,
                 in1=bc(oneh[:, :, :].unsqueeze(3), [128, T, 4, 8]), op=ALU.mult)
            C.op("dve", "tensor_reduce", rr, ww, out=elsel[:, :, :], in_=tmpe[:, :, :].rearrange("p t (g e) -> p t e g", g=4), axis=AX.X, op=ALU.add)
            for t in range(T):
                C.op("dve", "max", rr, ww, out=m8[:, t, :], in_=elsel[:, t, :])
            m1 = m8[:, :, 0]
            m2 = m8[:, :, 1]
            e2, s1, w1, w2 = sc1[:, 0, :], sc1[:, 1, :], sc1[:, 2, :], sc1[:, 3, :]
            C.op("dve", "tensor_tensor", rr, ww, out=e2, in0=m2, in1=m1, op=ALU.subtract)
            C.op("act", "activation", rr, ww, out=e2, in_=e2, func=AF.Exp)
            C.op("dve", "tensor_scalar", rr, ww, out=s1, in0=e2, scalar1=1.0, scalar2=None, op0=ALU.add)
            C.op("dve", "reciprocal", rr, ww, out=s1, in_=s1)
            C.op("dve", "tensor_tensor", rr, ww, out=w1, in0=s1, in1=ggate[:, :], op=ALU.mult)
            C.op("dve", "tensor_tensor", rr, ww, out=w2, in0=w1, in1=e2, op=ALU.mult)
            C.op("dve", "tensor_tensor", rr, ww, out=eqa[:, :, :], in0=elsel[:, :, :], in1=bc(m1.unsqueeze(2), [128, T, 8]), op=ALU.is_equal)
            C.op("dve", "tensor_tensor", rr, ww, out=eqa[:, :, :], in0=eqa[:, :, :], in1=bc(w1.unsqueeze(2), [128, T, 8]), op=ALU.mult)
            C.op("dve", "tensor_tensor", rr, ww, out=eqb[:, :, :], in0=elsel[:, :, :], in1=bc(m2.unsqueeze(2), [128, T, 8]), op=ALU.is_equal)
            C.op("dve", "tensor_tensor", rr, ww, out=eqb[:, :, :], in0=eqb[:, :, :], in1=bc(w2.unsqueeze(2), [128, T, 8]), op=ALU.mult)
            C.op("dve", "tensor_tensor", rr, ww, out=eqa[:, :, :], in0=eqa[:, :, :], in1=eqb[:, :, :], op=ALU.add)
            C.op("dve", "tensor_tensor", rr, comb_b, out=comb[:, :, :].rearrange("p t (g e) -> p t g e", g=4),
                 in0=bc(oneh[:, :, :].unsqueeze(3), [128, T, 4, 8]), in1=bc(eqa[:, :, :].unsqueeze(2), [128, T, 4, 8]), op=ALU.mult)
            C.barrier()

        if stop_after == "B":
            db = C.buf("dbgB")
            C.dma("sp", dbg_out["x2"][:, :], x2_scr[:, :], x2_scr_b, [db])
            C.dma("sp", dbg_out["comb"].rearrange("p (t e) -> p t e", t=NT), comb[:, :, :], comb_b, [db])
            C.dma("sp", dbg_out["xn"].rearrange("(t p) n -> p t n", p=128), xn_scr.rearrange("t p n -> p t n"), xn_scr_b, [db])
            C.final_wait("sp", [db])
            return nc

        with ExitStack() as CC:
            xnT = sb(CC, "xnT", [128, 8, S], BF16)
            xnT_blk = C.bufs("xnT", NSB)
            xnT_b = [xnT_blk[i // 4] for i in range(NT)]
            for i in range(NT):
                C.dma("sp", xnT[:, :, i * 128:(i + 1) * 128], xn_scr[i].rearrange("p (c n) -> p c n", c=8), [xn_scr_b[i]], [xnT_b[i]])
            ysb = sb(CC, "ysb", [128, 16, D], F32)
            ysb_b = C.bufs("ysb", 16)
            NSLOT = 3
            wgs = [sb(CC, "wgs%d" % j, [128, 8, 256], BF16) for j in range(NSLOT)]
            wus = [sb(CC, "wus%d" % j, [128, 8, 256], BF16) for j in range(NSLOT)]
            wds = [sb(CC, "wds%d" % j, [128, 2, D], BF16) for j in range(NSLOT)]
            wslot_b = C.bufs("wslot", NSLOT)
            actT = [sb(CC, "actT%d" % j, [128, 2, 512], BF16) for j in range(2)]
            actT_b = [C.bufs("actT%d_" % j, 2) for j in range(2)]
            ssb = [sb(CC, "ssb%d" % j, [128, 512], BF16) for j in range(2)]
            ssb_b = C.bufs("ssb", 2)
            x2t = [sb(CC, "x2t%d" % j, [128, D], F32) for j in range(2)]
            x2t_b = C.bufs("x2t", 2)
            ot = [sb(CC, "ot%d" % j, [128, D], F32) for j in range(2)]
            ot_b = C.bufs("ot", 2)
            gfin = sb(CC, "gfin", [128, D], F32)
            gfin_b = C.buf("gfin")
            C.dma("sp", gfin[:], bc(g_final_d[0:1, :], [128, D]), [], [gfin_b])
            stC = sb(CC, "statC", [128, 8], F32)
            stC_b = C.buf("statC")
            pg = [ps(CC, "pg%d" % j, [128, 512], F32) for j in range(2)]
            pu = [ps(CC, "pu%d" % j, [128, 512], F32) for j in range(2)]
            py = [ps(CC, "py%d" % j, [128, 1024], F32) for j in range(2)]
            pg_b = C.bufs("pg", 2)
            pu_b = C.bufs("pu", 2)
            py_b = C.bufs("py", 2)

            for hf in range(2):
                units = [(e, tb) for e in range(32) for tb in range(4)]

                def load_expert(e):
                    sl = e % NSLOT
                    C.dma("pool", wgs[sl][:, :, :], w_eg_d[e].rearrange("(c p) f -> p c f", p=128), [], [wslot_b[sl]])
                    C.dma("pool", wus[sl][:, :, :], w_eu_d[e].rearrange("(c p) f -> p c f", p=128), [], [wslot_b[sl]])
                    C.dma("pool", wds[sl][:, :, :], w_ed_d[e].rearrange("(c p) n -> p c n", p=128), [], [wslot_b[sl]])

                gu_cnt = [0]

                def gu(n):
                    e, tb = units[n]
                    sl = e % NSLOT
                    a = n % 2
                    tok0 = (hf * 16 + tb * 4) * 128
                    xb = xnT_b[hf * 16 + tb * 4:hf * 16 + tb * 4 + 4]
                    for fc in range(2):
                        k2 = gu_cnt[0] % 2
                        gu_cnt[0] += 1
                        for kc in range(8):
                            C.op("pe", "matmul", [wslot_b[sl]] + xb, [pg_b[k2]], sig=(kc == 7), out=pg[k2][:, :],
                                 lhsT=wgs[sl][:, kc, fc * 128:(fc + 1) * 128], rhs=xnT[:, kc, tok0:tok0 + 512], start=(kc == 0), stop=(kc == 7))
                        for kc in range(8):
                            C.op("pe", "matmul", [wslot_b[sl]] + xb, [pu_b[k2]], sig=(kc == 7), out=pu[k2][:, :],
                                 lhsT=wus[sl][:, kc, fc * 128:(fc + 1) * 128], rhs=xnT[:, kc, tok0:tok0 + 512], start=(kc == 0), stop=(kc == 7))
                        C.op("act", "activation", [pg_b[k2]], [ssb_b[k2]], out=ssb[k2][:], in_=pg[k2][:, :], func=AF.Silu)
                        C.op("dve", "tensor_tensor", [pu_b[k2], ssb_b[k2]], [actT_b[a][fc]], out=actT[a][:, fc, :], in0=pu[k2][:, :], in1=ssb[k2][:], op=ALU.mult)

                def down(n):
                    e, tb = units[n]
                    sl = e % NSLOT
                    a = n % 2
                    for t in range(4):
                        yt = tb * 4 + t
                        i = hf * 16 + yt
                        k2 = (n * 4 + t) % 2
                        for cb in range(2):
                            for fc in range(2):
                                C.op("pe", "matmul", [wslot_b[sl], actT_b[a][fc]], [py_b[k2]], sig=(cb == 1 and fc == 1),
                                     out=py[k2][:, cb * 512:(cb + 1) * 512], lhsT=actT[a][:, fc, t * 128:(t + 1) * 128],
                                     rhs=wds[sl][:, fc, cb * 512:(cb + 1) * 512], start=(fc == 0), stop=(fc == 1))
                        if e == 0:
                            j = yt % 2
                            C.dma("sp", x2t[j][:], x2_scr[i * 128:(i + 1) * 128, :], [x2_scr_b[i]], [x2t_b[j]])
                            C.op("dve", "scalar_tensor_tensor", [py_b[k2], comb_b[i], x2t_b[j]], [ysb_b[yt]], out=ysb[:, yt, :], in0=py[k2][:, :],
                                 scalar=comb[:, i, e:e + 1], in1=x2t[j][:], op0=ALU.mult, op1=ALU.add)
                        else:
                            C.op("dve", "scalar_tensor_tensor", [py_b[k2], comb_b[i], ysb_b[yt]], [ysb_b[yt]], out=ysb[:, yt, :], in0=py[k2][:, :],
                                 scalar=comb[:, i, e:e + 1], in1=ysb[:, yt, :], op0=ALU.mult, op1=ALU.add)

                def tail(yt):
                    i = hf * 16 + yt
                    j = yt % 2
                    rms_rstd(ysb[:, yt, :], [ysb_b[yt]], ot[j][:, :], [ot_b[j]], stC, stC_b)
                    C.op("dve", "scalar_tensor_tensor", [ysb_b[yt], stC_b, gfin_b], [ot_b[j]], out=ot[j][:], in0=ysb[:, yt, :], scalar=stC[:, 2:3],
                         in1=gfin[:], op0=ALU.mult, op1=ALU.mult)
                    C.dma("sp", out_d[i * 128:(i + 1) * 128, :], ot[j][:], [ot_b[j]], [out_b])

                load_expert(0)
                load_expert(1)
                gu(0)
                for n in range(len(units)):
                    e, tb = units[n]
                    if tb == 0 and e + 2 < 32:
                        load_expert(e + 2)
                    if n + 1 < len(units):
                        gu(n + 1)
                    down(n)
                    if e == 31:
                        for t in range(4):
                            tail(tb * 4 + t)
            C.barrier()
        C.final_wait("sp", [out_b])
    return nc


def make_in_maps(inputs):
    f32 = np.float32
    x = np.asarray(inputs["x"], f32)
    mem = np.asarray(inputs["mem"], f32)
    pos = np.asarray(inputs["positions"]).astype(np.int32)
    B = x.shape[0]

    def fm(v, n):
        return np.ascontiguousarray(np.asarray(v, f32).reshape(n, 128).T)

    w_re = np.asarray(inputs["w_router_expert"], f32)[0]
    w_router = np.concatenate([np.asarray(inputs["w_router_group"], f32)[0],
                               np.ascontiguousarray(w_re.transpose(1, 0, 2)).reshape(D, 32)], axis=1)
    b_router = np.concatenate([np.asarray(inputs["b_router_group"], f32)[0].reshape(-1),
                               np.asarray(inputs["b_router_expert"], f32)[0].reshape(-1)])[None, :]
    convw = np.asarray(inputs["conv_w"], f32)[0]
    convwT = np.ascontiguousarray(convw.T.reshape(4, 128, 31).transpose(1, 0, 2))
    inv_freq = (10000.0 ** (-np.arange(0, 64, 2, dtype=np.float64) / 64)).astype(np.float32)
    invf = np.tile((inv_freq.astype(np.float64) / (2 * np.pi)).astype(f32)[None, :], (128, 1))
    shared = {
        "w_in": np.ascontiguousarray(np.asarray(inputs["w_in"], f32)[0]),
        "w_o_attn": np.ascontiguousarray(np.asarray(inputs["w_o_attn"], f32)[0]),
        "convw": convwT,
        "convb": fm(np.asarray(inputs["conv_b"])[0], 4),
        "lng": fm(np.asarray(inputs["conv_ln_g"])[0], 4),
        "lnb": fm(np.asarray(inputs["conv_ln_b"])[0], 4),
        "w_conv_out": np.ascontiguousarray(np.asarray(inputs["w_conv_out"], f32)[0]),
        "w_out": np.ascontiguousarray(np.asarray(inputs["w_out"], f32)[0]),
        "w_q_x": np.ascontiguousarray(np.asarray(inputs["w_q_x"], f32)[0]),
        "w_kv_x": np.ascontiguousarray(np.asarray(inputs["w_kv_x"], f32)[0]),
        "w_o_x": np.ascontiguousarray(np.asarray(inputs["w_o_x"], f32)[0]),
        "g_mix": fm(np.asarray(inputs["norm_mix_g"])[0], 8),
        "g_x": fm(np.asarray(inputs["norm_x_g"])[0], 8),
        "g_mem": fm(np.asarray(inputs["norm_mem_g"])[0], 8),
        "g_moe": fm(np.asarray(inputs["norm_moe_g"])[0], 8),
        "w_router": np.ascontiguousarray(w_router),
        "b_router": np.ascontiguousarray(b_router.astype(f32)),
        "w_eg": np.ascontiguousarray(np.asarray(inputs["w_exp_gate"], f32)[0].reshape(32, D, 256)),
        "w_eu": np.ascontiguousarray(np.asarray(inputs["w_exp_up"], f32)[0].reshape(32, D, 256)),
        "w_ed": np.ascontiguousarray(np.asarray(inputs["w_exp_down"], f32)[0].reshape(32, 256, D)),
        "g_final": np.ascontiguousarray(np.asarray(inputs["norm_final_g"], f32).reshape(1, D)),
        "ident": np.eye(128, dtype=f32),
        "invf": invf,
        "pow2": np.tile((2.0 ** -np.arange(BIS_ITERS + 2)).astype(f32)[None, :], (128, 1)),
    }
    maps = []
    for b in range(B):
        m = dict(shared)
        m["x"] = np.ascontiguousarray(x[b])
        m["mem"] = np.ascontiguousarray(mem[b])
        m["pos"] = np.ascontiguousarray(pos[b].reshape(NT, 128).T)
        maps.append(m)
    return maps


def kernel(**inputs):
    maps = make_in_maps(inputs)
    nc = build()
    res = run_bass_kernel_spmd(nc, maps, core_ids=list(range(len(maps))))
    return np.stack([np.asarray(r["out"], np.float32) for r in res.results], axis=0)
```

```python
import bisect
from contextlib import ExitStack

import numpy as np
import concourse.bass as bass
import concourse.mybir as mybir
from concourse.bass_utils import run_bass_kernel_spmd

F32 = mybir.dt.float32
BF16 = mybir.dt.bfloat16
I32 = mybir.dt.int32
AF = mybir.ActivationFunctionType
ALU = mybir.AluOpType
AX = mybir.AxisListType

S = 4096
D = 1024
NT = S // 128
NSB = S // 512
EPS = 1e-6
NEG_BIG = -30000.0
BIS_ITERS = 18
A_OFF = 2120
B_OFF = 2632
G_OFF = 3144
NA_COLS = 3144


class SemBox:
    __slots__ = ("name", "sem", "count")

    def __init__(self, name):
        self.name = name
        self.sem = None
        self.count = 0


class Buf:
    __slots__ = ("name", "last_w", "readers", "box")

    def __init__(self, name, box=None):
        self.name = name
        self.last_w = None
        self.readers = []
        self.box = box if box is not None else SemBox(name)


class Ctx:
    COMPUTE = ("pe", "act", "dve", "pool")

    def __init__(self, nc, es):
        self.nc = nc
        self.es = es
        self.eng = {"pe": nc.tensor, "act": nc.scalar, "dve": nc.vector, "pool": nc.gpsimd, "sp": nc.sync}
        self.sem = {e: es.enter_context(nc.semaphore("s_" + e)) for e in self.COMPUTE}
        self.mile = {e: 0 for e in self.COMPUTE}
        self.nissued = {e: 0 for e in self.eng}
        self.sigpts = {e: ([], []) for e in self.COMPUTE}
        self.last_ins = {e: None for e in self.eng}
        self.last_sig = {e: True for e in self.eng}
        self.waited = {}
        self.dma_sems = []
        self.all_bufs = []

    def buf(self, name):
        b = Buf(name)
        self.all_bufs.append(b)
        return b

    def bufs(self, name, n, share=False):
        if not share:
            return [self.buf("%s%d" % (name, i)) for i in range(n)]
        box = SemBox(name)
        out = []
        for i in range(n):
            b = Buf("%s%d" % (name, i), box)
            self.all_bufs.append(b)
            out.append(b)
        return out

    def _resolve(self, tok):
        if tok[0] == "d":
            return tok[1], tok[2]
        _, e, idx = tok
        idxs, miles = self.sigpts[e]
        k = bisect.bisect_left(idxs, idx)
        if k < len(idxs):
            return self.sem[e], miles[k]
        assert not self.last_sig[e]
        self.last_ins[e].then_inc(self.sem[e], 1)
        self.mile[e] += 1
        idxs.append(self.nissued[e] - 1)
        miles.append(self.mile[e])
        self.last_sig[e] = True
        return self.sem[e], self.mile[e]

    def _wait(self, engname, toks):
        need = {}
        for tok in toks:
            if tok is None:
                continue
            if tok[0] == "c" and tok[1] == engname and engname == "pe":
                continue
            sem, val = self._resolve(tok)
            key = id(sem)
            if key not in need or need[key][1] < val:
                need[key] = (sem, val)
        for key, (sem, val) in need.items():
            wk = (engname, key)
            if self.waited.get(wk, 0) >= val:
                continue
            self.eng[engname].wait_ge(sem, val)
            self.waited[wk] = val

    def _deps(self, engname, reads, writes, waw=True):
        toks = []
        for b in reads:
            toks.append(b.last_w)
        for b in writes:
            if waw:
                if not (b.last_w is not None and b.last_w[0] == "c" and b.last_w[1] == engname):
                    toks.append(b.last_w)
            for r in b.readers:
                if r[0] == "c" and r[1] == engname and engname == "pe":
                    continue
                toks.append(r)
        return toks

    def op(self, engname, method, reads=(), writes=(), sig=None, **kw):
        assert engname in self.COMPUTE
        if sig is None:
            sig = engname != "pe"
        self._wait(engname, self._deps(engname, reads, writes))
        ins = getattr(self.eng[engname], method)(**kw)
        idx = self.nissued[engname]
        self.nissued[engname] += 1
        self.last_ins[engname] = ins
        self.last_sig[engname] = False
        if sig:
            ins.then_inc(self.sem[engname], 1)
            self.mile[engname] += 1
            self.sigpts[engname][0].append(idx)
            self.sigpts[engname][1].append(self.mile[engname])
            self.last_sig[engname] = True
        tok = ("c", engname, idx)
        for b in reads:
            b.readers.append(tok)
        for b in writes:
            b.last_w = tok
            b.readers = []
        return ins

    def dma(self, q, out, in_, reads, writes, waw=False, sem_from=None, **kw):
        assert len(writes) == 1
        wb = (sem_from if sem_from is not None else writes[0]).box
        if wb.sem is None:
            wb.sem = self.es.enter_context(self.nc.semaphore("d_" + wb.name))
            self.dma_sems.append(wb)
        self._wait(q, self._deps(q, reads, writes, waw=waw))
        ins = self.eng[q].dma_start(out=out, in_=in_, **kw)
        ins.then_inc(wb.sem, 16)
        wb.count += 16
        self.nissued[q] += 1
        if q in self.COMPUTE:
            self.last_ins[q] = ins
            self.last_sig[q] = True
        tok = ("d", wb.sem, wb.count)
        for b in reads:
            b.readers.append(tok)
        writes[0].last_w = tok
        writes[0].readers = []
        return ins

    def barrier(self):
        toks = []
        for e in self.COMPUTE:
            if self.nissued[e] > 0 and self.last_ins[e] is not None:
                if not self.last_sig[e]:
                    toks.append(("c", e, self.nissued[e] - 1))
                else:
                    idxs, miles = self.sigpts[e]
                    if idxs:
                        toks.append(("c", e, idxs[-1]))
        for b in self.dma_sems:
            toks.append(("d", b.sem, b.count))
        for e in list(self.COMPUTE) + ["sp"]:
            self._wait(e, toks)

    def final_wait(self, q, bufs):
        self._wait(q, [("d", b.box.sem, b.box.count) for b in bufs if b.box.sem is not None])


def bc(ap, shape):
    return ap.broadcast_to(list(shape))


def build(stop_after="all", dbg=(), a2_tiles=None):
    nc = bass.Bass("TRN2", target_bir_lowering=False)

    def din(name, shape, dt=F32):
        return nc.dram_tensor(name, list(shape), dt, kind="ExternalInput").ap()

    x_d = din("x", [S, D])
    mem_d = din("mem", [256, D])
    pos_d = din("pos", [128, NT], I32)
    w_in_d = din("w_in", [D, 5192])
    w_o_attn_d = din("w_o_attn", [512, D])
    convw_d = din("convw", [128, 4, 31])
    convb_d = din("convb", [128, 4])
    lng_d = din("lng", [128, 4])
    lnb_d = din("lnb", [128, 4])
    w_conv_out_d = din("w_conv_out", [512, D])
    w_out_d = din("w_out", [D, D])
    w_q_x_d = din("w_q_x", [D, D])
    w_kv_x_d = din("w_kv_x", [D, 2 * D])
    w_o_x_d = din("w_o_x", [D, D])
    g_mix_d = din("g_mix", [128, 8])
    g_x_d = din("g_x", [128, 8])
    g_mem_d = din("g_mem", [128, 8])
    g_moe_d = din("g_moe", [128, 8])
    w_router_d = din("w_router", [D, 36])
    b_router_d = din("b_router", [1, 36])
    w_eg_d = din("w_eg", [32, D, 256])
    w_eu_d = din("w_eu", [32, D, 256])
    w_ed_d = din("w_ed", [32, 256, D])
    g_final_d = din("g_final", [1, D])
    ident_d = din("ident", [128, 128])
    invf_d = din("invf", [128, 32])
    pow2_d = din("pow2", [128, BIS_ITERS + 2])

    out_d = nc.dram_tensor("out", [S, D], F32, kind="ExternalOutput").ap()

    def dscr(name, shape, dt):
        return nc.dram_tensor(name, list(shape), dt, kind="Internal").ap()

    qT_scr = dscr("qT_scr", [NT, 128, 512], BF16)
    qiT_scr = dscr("qiT_scr", [NT, 128, 512], BF16)
    z_scr = dscr("z_scr", [NSB, 128, 2048], BF16)
    ao_scr = dscr("ao_scr", [NT, 128, 512], BF16)
    x2_scr = dscr("x2_scr", [S, D], F32)

    dbg_out = {}
    for name, shape, dt in dbg:
        dbg_out[name] = nc.dram_tensor("dbg_" + name, list(shape), dt, kind="ExternalOutput").ap()

    with ExitStack() as es:
        C = Ctx(nc, es)
        out_b = C.buf("out")

        P0 = ExitStack()
        es.enter_context(P0)

        def sb(stack, name, shape, dt):
            return stack.enter_context(nc.sbuf_tensor("sb_" + name, list(shape), dt))

        def ps(stack, name, shape, dt):
            return stack.enter_context(nc.psum_tensor("ps_" + name, list(shape), dt))

        identf = sb(P0, "identf", [128, 128], F32)
        identb = sb(P0, "identb", [128, 128], BF16)
        onesf = sb(P0, "onesf", [128, 128], F32)
        onesb = sb(P0, "onesb", [128, 128], BF16)
        b_const = C.buf("const")
        C.dma("sp", identf[:], ident_d[:, :], [], [b_const])
        C.op("dve", "tensor_copy", [b_const], [b_const], out=identb[:], in_=identf[:])
        C.op("dve", "memset", [], [b_const], ap=onesf[:], constant=1.0 / 512.0)
        C.op("dve", "memset", [], [b_const], ap=onesb[:], constant=1.0)

        PA = ExitStack()
        es.enter_context(PA)
        kT = sb(PA, "kT", [128, 4, S], BF16)
        v_sb = sb(PA, "v_sb", [128, NT, 8, 65], BF16)
        kiT2 = sb(PA, "kiT2", [128, S], BF16)
        wi_sb = sb(PA, "wi_sb", [128, NT, 8], F32)
        kT_b = C.bufs("kT", NT)
        v_b = C.bufs("v", NT)
        kiT_b = C.bufs("kiT", NT)
        wi_b = C.bufs("wi", NT)
        for i in range(NT):
            C.op("pool", "memset", [], [v_b[i]], ap=v_sb[:, i, :, 64:65], constant=1.0)

        with ExitStack() as A1:
            w_sb = sb(A1, "w_inA", [128, 8, NA_COLS], BF16)
            w_b = [C.buf("w_inA")] * 8
            for kc in range(8):
                for (c0, c1) in ((0, 1024), (1024, 2048), (2048, NA_COLS)):
                    C.dma("pool", w_sb[:, kc, c0:c1], w_in_d[kc * 128:(kc + 1) * 128, c0:c1], [], [w_b[kc]])
            gfm = sb(A1, "gfm", [128, 8], F32)
            cwT = sb(A1, "cwT", [128, 4, 31], F32)
            cb4 = sb(A1, "cb4", [128, 4], F32)
            lng4 = sb(A1, "lng4", [128, 4], F32)
            lnb4 = sb(A1, "lnb4", [128, 4], F32)
            invf = sb(A1, "invf", [128, 32], F32)
            posi = sb(A1, "posi", [128, NT], I32)
            posf = sb(A1, "posf", [128, NT], F32)
            b_small = C.buf("smallA")
            C.dma("sp", gfm[:], g_mix_d[:, :], [], [b_small])
            C.dma("sp", cwT[:], convw_d[:, :, :], [], [b_small])
            C.dma("sp", cb4[:], convb_d[:, :], [], [b_small])
            C.dma("sp", lng4[:], lng_d[:, :], [], [b_small])
            C.dma("sp", lnb4[:], lnb_d[:, :], [], [b_small])
            C.dma("sp", invf[:], invf_d[:, :], [], [b_small])
            C.dma("sp", posi[:], pos_d[:, :], [], [b_small])
            cosT = sb(A1, "cosT", [128, NT, 32], F32)
            sinT = sb(A1, "sinT", [128, NT, 32], F32)
            with ExitStack() as T0:
                ua = sb(T0, "ua", [128, NT, 32], F32)
                ub = sb(T0, "ub", [128, NT, 32], F32)
                uci = sb(T0, "uci", [128, NT, 32], I32)
                b_rope = C.buf("ropetab")
                b_ua = C.buf("ua")
                b_ub = C.buf("ub")
                b_uc = C.buf("uc")
                C.op("dve", "tensor_copy", [b_small], [b_ua], out=posf[:], in_=posi[:])
                C.op("dve", "tensor_tensor", [b_ua, b_small], [b_ub], out=ua[:],
                     in0=bc(posf[:, :].unsqueeze(2), [128, NT, 32]), in1=bc(invf[:, :].unsqueeze(1), [128, NT, 32]), op=ALU.mult)
                for (tab, shift) in ((sinT, 0.0), (cosT, 0.25)):
                    C.op("dve", "tensor_scalar", [b_ub], [b_ua], out=ub[:], in0=ua[:], scalar1=shift, scalar2=None, op0=ALU.add)
                    C.op("dve", "tensor_copy", [b_ua], [b_uc], out=uci[:], in_=ub[:])
                    C.op("dve", "tensor_copy", [b_uc], [b_rope], out=tab[:], in_=uci[:])
                    C.op("dve", "tensor_tensor", [b_ua, b_rope], [b_ua], out=ub[:], in0=ub[:], in1=tab[:], op=ALU.subtract)
                    C.op("dve", "tensor_scalar", [b_ua], [b_rope], out=tab[:], in0=ub[:], scalar1=0.5, scalar2=None, op0=ALU.is_gt)
                    C.op("dve", "tensor_tensor", [b_ua, b_rope], [b_ua], out=ub[:], in0=ub[:], in1=tab[:], op=ALU.subtract)
                    C.op("dve", "tensor_scalar", [b_ua], [b_rope], out=tab[:], in0=ub[:], scalar1=-0.5, scalar2=None, op0=ALU.is_lt)
                    C.op("dve", "tensor_tensor", [b_ua, b_rope], [b_ua], out=ub[:], in0=ub[:], in1=tab[:], op=ALU.add)
                    C.op("act", "activation", [b_ua], [b_rope], out=tab[:], in_=ub[:], func=AF.Sin, scale=2.0 * np.pi)

                C.barrier()

            xt = [sb(A1, "xt%d" % j, [128, D], F32) for j in range(2)]
            xt_b = C.bufs("xt", 2)
            hn = [sb(A1, "hn%d" % j, [128, D], BF16) for j in range(2)]
            hn_b = C.bufs("hn", 2)
            st = sb(A1, "stat", [128, 8], F32)
            st_b = C.buf("stat")
            hT = sb(A1, "hT", [128, 8, 512], BF16)
            hT_b = C.bufs("hT", 4)
            rt = [sb(A1, "rt%d" % j, [128, 8, 32], F32) for j in range(4)]
            rt_b = C.bufs("rt", 4)
            rq = [sb(A1, "rq%d" % j, [128, 8, 64], BF16) for j in range(2)]
            rq_b = C.bufs("rq", 2)
            rqk = sb(A1, "rqk", [128, 2, 64], BF16)
            rqk_b = C.buf("rqk")
            qst = [sb(A1, "qst%d" % j, [128, 4, 128], BF16) for j in range(2)]
            qst_b = C.bufs("qst", 2)
            uT = [sb(A1, "uT%d" % j, [128, 4, 542], BF16) for j in range(2)]
            uT_b = [C.bufs("uT%d_" % j, 4) for j in range(2)]
            uTpad_b = C.bufs("uTpad", 2)
            sg = sb(A1, "sg", [128, 512], F32)
            sg_b = C.buf("sg")
            Dw = sb(A1, "Dw", [128, 31, 128], BF16)
            Dw_b = C.buf("Dw")
            co = sb(A1, "co", [128, 4, 512], F32)
            co_b = C.bufs("co", 4)
            sq = [sb(A1, "sq%d" % j, [128, 512], F32) for j in range(2)]
            sq_b = C.bufs("sq", 2)
            mean_sb = sb(A1, "mean_sb", [128, 512], F32)
            m2_sb = sb(A1, "m2_sb", [128, 512], F32)
            rstd_sb = sb(A1, "rstd_sb", [128, 512], F32)
            mean_b = C.buf("mean")
            m2_b = C.buf("m2")
            rstd_b = C.buf("rstdc")
            dtmp = [sb(A1, "dtmp%d" % j, [128, 512], F32) for j in range(2)]
            dtmp_b = C.bufs("dtmp", 2)
            zT = sb(A1, "zT", [128, 4, 512], BF16)
            zT_b = C.buf("zT")
            pA = ps(A1, "pA", [128, 512], F32)
            pB = ps(A1, "pB", [128, 512], F32)
            pC = ps(A1, "pC", [128, 512], F32)
            pM = ps(A1, "pM", [128, 512], F32)
            pE = ps(A1, "pE", [128, 512], F32)
            pT0 = ps(A1, "pT0", [128, 512], F32)
            pT1 = ps(A1, "pT1", [128, 512], F32)
            pTP = ps(A1, "pTP", [128, 1024], BF16)
            pA_b, pB_b, pC_b, pM_b, pE_b, pTP_b = (C.buf(n) for n in ("pA", "pB", "pC", "pM", "pE", "pTP"))
            pT = [pT0, pT1]
            pT_b = C.bufs("pT", 2)
            q_scr_b = C.bufs("qscr", NT, share=True)
            qi_scr_b = C.bufs("qiscr", NT, share=True)
            z_scr_b = C.bufs("zscr", NSB, share=True)

            C.op("pool", "memset", [], [uTpad_b[0]], ap=uT[0][:, :, 0:30], constant=0.0)
            tmc = [0]

            def rope_block(psv, nh, i, dst, dst_bufs, dup=False):
                cos_b = bc(cosT[:, i, :].unsqueeze(1), [128, nh, 32])
                sin_b = bc(sinT[:, i, :].unsqueeze(1), [128, nh, 32])
                x1 = psv[:, :, 0:32]
                x2 = psv[:, :, 32:64]
                pb = psv_buf[0]
                C.op("dve", "tensor_tensor", [pb, b_rope], [rt_b[0]], out=rt[0][:, 0:nh, :], in0=x1, in1=cos_b, op=ALU.mult)
                C.op("dve", "tensor_tensor", [pb, b_rope], [rt_b[1]], out=rt[1][:, 0:nh, :], in0=x2, in1=sin_b, op=ALU.mult)
                C.op("dve", "tensor_tensor", [pb, b_rope], [rt_b[2]], out=rt[2][:, 0:nh, :], in0=x2, in1=cos_b, op=ALU.mult)
                C.op("dve", "tensor_tensor", [pb, b_rope], [rt_b[3]], out=rt[3][:, 0:nh, :], in0=x1, in1=sin_b, op=ALU.mult)
                C.op("pool", "tensor_tensor", [rt_b[0], rt_b[1]], dst_bufs, out=dst[:, :, 0:32], in0=rt[0][:, 0:nh, :], in1=rt[1][:, 0:nh, :], op=ALU.subtract)
                C.op("pool", "tensor_tensor", [rt_b[2], rt_b[3]], dst_bufs, out=dst[:, :, 32:64], in0=rt[2][:, 0:nh, :], in1=rt[3][:, 0:nh, :], op=ALU.add)

            psv_buf = [None]

            def chain(i):
                j = i % 2
                C.dma("sp", xt[j][:], x_d[i * 128:(i + 1) * 128, :], [], [xt_b[j]])
                C.op("act", "activation", [xt_b[j]], [hn_b[j], st_b], out=hn[j][:], in_=xt[j][:], func=AF.Square, accum_out=st[:, 0:1])
                C.op("act", "activation", [st_b], [st_b], out=st[:, 1:2], in_=st[:, 0:1], func=AF.Sqrt, scale=1.0 / D, bias=EPS)
                C.op("dve", "reciprocal", [st_b], [st_b], out=st[:, 2:3], in_=st[:, 1:2])
                C.op("act", "activation", [xt_b[j], st_b], [hn_b[j]], out=hn[j][:], in_=xt[j][:], func=AF.Copy, scale=st[:, 2:3])

            def trans(i):
                j = i % 2
                t = i % 4
                for c in range(8):
                    C.op("pe", "transpose", [hn_b[j], b_const], [pTP_b], sig=(c == 7), out=pTP[:, c * 128:(c + 1) * 128],
                         in_=hn[j][:, c * 128:(c + 1) * 128], identity=identb[:])
                C.op("dve", "tensor_tensor", [pTP_b, b_small], [hT_b[t]], out=hT[:, :, t * 128:(t + 1) * 128],
                     in0=pTP[:, :].rearrange("p (c n) -> p c n", c=8), in1=bc(gfm[:, :].unsqueeze(2), [128, 8, 128]), op=ALU.mult)

            pending = []

            def flush():
                while pending:
                    pending.pop(0)()

            chain(0)
            for sbi in range(NSB):
                cur = sbi % 2
                nxt = 1 - cur
                for t in range(4):
                    i = sbi * 4 + t
                    trans(i)
                    if i + 1 < NT:
                        chain(i + 1)
                    for (name, c0, ncols) in (("q", 0, 512), ("k", 512, 512), ("v", 1024, 512), ("qi", 1536, 512), ("kw", 2048, 72)):
                        bk = tmc[0] % 2
                        tmc[0] += 1
                        for kc in range(8):
                            C.op("pe", "matmul", [hT_b[t], w_b[kc]], [pT_b[bk]], sig=(kc == 7), out=pT[bk][:, 0:512],
                                 lhsT=hT[:, kc, t * 128:(t + 1) * 128], rhs=w_sb[:, kc, c0:c0 + 512], start=(kc == 0), stop=(kc == 7))
                        flush()
                        psv_buf[0] = pT_b[bk]
                        if name == "v":
                            C.op("act", "activation", [pT_b[bk]], [v_b[i]], out=v_sb[:, i, :, 0:64],
                                 in_=pT[bk][:, :].rearrange("p (h d) -> p h d", h=8), func=AF.Copy)
                        elif name == "kw":
                            C.op("act", "activation", [pT_b[bk]], [wi_b[i]], out=wi_sb[:, i, :], in_=pT[bk][:, 64:72], func=AF.Copy)
                            psv = pT[bk][:, 0:64].rearrange("p (h d) -> p h d", h=1)
                            rope_block(psv, 1, i, rqk[:, 0:1, :], [rqk_b])
                            C.op("pool", "tensor_copy", [rqk_b], [rqk_b], out=rqk[:, 1:2, :], in_=rqk[:, 0:1, :])

                            def fin_kw(i=i):
                                C.op("pe", "transpose", [rqk_b, b_const], [pTP_b], sig=True, out=pTP[:, 0:128],
                                     in_=rqk[:, :, :].rearrange("p a d -> p (a d)"), identity=identb[:])
                                C.op("act", "activation", [pTP_b], [kiT_b[i]], out=kiT2[:, i * 128:(i + 1) * 128], in_=pTP[:, 0:128], func=AF.Copy)
                            pending.append(fin_kw)
                        else:
                            rj = tmc[0] % 2
                            psv = pT[bk][:, :].rearrange("p (h d) -> p h d", h=8)
                            rope_block(psv, 8, i, rq[rj][:, :, :], [rq_b[rj]])

                            def fin_rope(i=i, rj=rj, name=name, sj=tmc[0] % 2):
                                for c in range(4):
                                    C.op("pe", "transpose", [rq_b[rj], b_const], [pTP_b], sig=(c == 3), out=pTP[:, c * 128:(c + 1) * 128],
                                         in_=rq[rj][:, 2 * c:2 * c + 2, :].rearrange("p a d -> p (a d)"), identity=identb[:])
                                src = pTP[:, 0:512].rearrange("p (c n) -> p c n", c=4)
                                if name == "k":
                                    C.op("act", "activation", [pTP_b], [kT_b[i]], out=kT[:, :, i * 128:(i + 1) * 128], in_=src, func=AF.Copy)
                                else:
                                    C.op("act", "activation", [pTP_b], [qst_b[sj]], out=qst[sj][:, :, :], in_=src, func=AF.Copy)
                                    if name == "q":
                                        C.dma("sp", qT_scr[i].rearrange("p (c n) -> p c n", c=4), qst[sj][:, :, :], [qst_b[sj]], [q_scr_b[i]], sem_from=qst_b[sj])
                                    else:
                                        C.dma("sp", qiT_scr[i].rearrange("p (c n) -> p c n", c=4), qst[sj][:, :, :], [qst_b[sj]], [qi_scr_b[i]], sem_from=qst_b[sj])
                            pending.append(fin_rope)
                for cc in range(4):
                    for jj in range(31):
                        C.op("dve", "tensor_scalar", [b_const, b_small], [Dw_b], out=Dw[:, jj, :], in0=identb[:],
                             scalar1=cwT[:, cc, jj:jj + 1], scalar2=None, op0=ALU.mult)
                    for kc in range(8):
                        C.op("pe", "matmul", hT_b + [w_b[kc]], [pA_b], sig=(kc == 7), out=pA[:, :],
                             lhsT=w_sb[:, kc, A_OFF + cc * 128:A_OFF + (cc + 1) * 128], rhs=hT[:, kc, :], start=(kc == 0), stop=(kc == 7))
                    flush()
                    for kc in range(8):
                        C.op("pe", "matmul", hT_b + [w_b[kc]], [pB_b], sig=(kc == 7), out=pB[:, :],
                             lhsT=w_sb[:, kc, B_OFF + cc * 128:B_OFF + (cc + 1) * 128], rhs=hT[:, kc, :], start=(kc == 0), stop=(kc == 7))
                    C.op("act", "activation", [pB_b], [sg_b], out=sg[:], in_=pB[:, :], func=AF.Sigmoid)
                    C.op("dve", "tensor_tensor", [pA_b, sg_b], [uT_b[cur][cc]], out=uT[cur][:, cc, 30:542], in0=pA[:, :], in1=sg[:], op=ALU.mult)
                    for jj in range(31):
                        C.op("pe", "matmul", [Dw_b, uT_b[cur][cc], uTpad_b[cur]], [pC_b], sig=(jj == 30), out=pC[:, :],
                             lhsT=Dw[:, jj, :], rhs=uT[cur][:, cc, jj:jj + 512], start=(jj == 0), stop=(jj == 30))
                    C.op("act", "activation", [pC_b, b_small], [co_b[cc]], out=co[:, cc, :], in_=pC[:, :], func=AF.Identity, bias=cb4[:, cc:cc + 1])
                    C.op("act", "activation", [co_b[cc]], [sq_b[cc % 2]], out=sq[cc % 2][:], in_=co[:, cc, :], func=AF.Square)
                    C.op("pe", "matmul", [b_const, co_b[cc]], [pM_b], sig=(cc == 3), out=pM[:, :], lhsT=onesf[:], rhs=co[:, cc, :],
                         start=(cc == 0), stop=(cc == 3))
                    C.op("pe", "matmul", [b_const, sq_b[cc % 2]], [pE_b], sig=(cc == 3), out=pE[:, :], lhsT=onesf[:], rhs=sq[cc % 2][:],
                         start=(cc == 0), stop=(cc == 3))
                if sbi + 1 < NSB:
                    C.op("pool", "tensor_copy", uT_b[cur], [uTpad_b[nxt]], out=uT[nxt][:, :, 0:30], in_=uT[cur][:, :, 512:542])
                C.op("act", "activation", [pM_b], [mean_b], out=mean_sb[:], in_=pM[:, :], func=AF.Copy)
                C.op("pool", "tensor_tensor", [mean_b], [m2_b], out=m2_sb[:], in0=mean_sb[:], in1=mean_sb[:], op=ALU.mult)
                C.op("dve", "tensor_tensor", [pE_b, m2_b], [m2_b], out=m2_sb[:], in0=pE[:, :], in1=m2_sb[:], op=ALU.subtract)
                C.op("act", "activation", [m2_b], [m2_b], out=m2_sb[:], in_=m2_sb[:], func=AF.Sqrt, bias=EPS, scale=1.0)
                C.op("dve", "reciprocal", [m2_b], [rstd_b], out=rstd_sb[:], in_=m2_sb[:])
                for cc in range(4):
                    dj = cc % 2
                    C.op("pool", "tensor_tensor", [co_b[cc], mean_b], [dtmp_b[dj]], out=dtmp[dj][:], in0=co[:, cc, :], in1=mean_sb[:], op=ALU.subtract)
                    C.op("pool", "tensor_tensor", [dtmp_b[dj], rstd_b], [dtmp_b[dj]], out=dtmp[dj][:], in0=dtmp[dj][:], in1=rstd_sb[:], op=ALU.mult)
                    C.op("act", "activation", [dtmp_b[dj], b_small], [zT_b], out=zT[:, cc, :], in_=dtmp[dj][:], func=AF.Silu,
                         scale=lng4[:, cc:cc + 1], bias=lnb4[:, cc:cc + 1])
                C.dma("sp", z_scr[sbi].rearrange("p (c n) -> p c n", c=4), zT[:, :, :], [zT_b], [z_scr_b[sbi]], sem_from=zT_b)
            C.barrier()

        if stop_after == "A1":
            if "kT" in dbg_out:
                db = C.buf("dbg")
                C.dma("sp", dbg_out["kT"].rearrange("p (c n) -> p c n", c=4), kT[:, :, :], kT_b, [db])
                C.dma("sp", dbg_out["kiT2"][:, :], kiT2[:, :], kiT_b, [db])
                C.dma("sp", dbg_out["v"].rearrange("p (t h d) -> p t h d", t=NT, h=8), v_sb[:, :, :, :], v_b, [db])
                C.dma("sp", dbg_out["wi"].rearrange("p (t h) -> p t h", t=NT), wi_sb[:, :, :], wi_b, [db])
                C.dma("sp", dbg_out["qT"].rearrange("(t p) n -> p t n", p=128), qT_scr.rearrange("t p n -> p t n"), q_scr_b, [db])
                C.dma("sp", dbg_out["qiT"].rearrange("(t p) n -> p t n", p=128), qiT_scr.rearrange("t p n -> p t n"), qi_scr_b, [db])
                C.dma("sp", dbg_out["z"].rearrange("(t p) n -> p t n", p=128), z_scr.rearrange("t p n -> p t n"), z_scr_b, [db])
                C.final_wait("sp", [db])
            C.barrier()
            C.final_wait("sp", q_scr_b + qi_scr_b + z_scr_b)
            return nc

        with ExitStack() as A2:
            qTz = [[sb(A2, "qTz%d_%d" % (par, j), [128, 4, 128], BF16) for j in range(2)] for par in range(2)]
            qiTz = [[sb(A2, "qiTz%d_%d" % (par, j), [128, 4, 128], BF16) for j in range(2)] for par in range(2)]
            qT_tb = C.bufs("qT_t", 2)
            qiT_tb = C.bufs("qiT_t", 2)
            Dg = sb(A2, "Dg", [128, 8, 128], BF16)
            Dg_b = C.buf("Dg")
            R_sb = [sb(A2, "R_sb%d" % j, [128, 512], BF16) for j in range(2)]
            R_b = C.bufs("R_sb", 2)
            sc = [sb(A2, "sc%d" % j, [128, S], F32) for j in range(2)]
            sc_b = C.bufs("sc", 2)
            NM = [sb(A2, "NM%d" % j, [128, S], BF16) for j in range(2)]
            NM_b = C.bufs("NM", 2)
            bs = sb(A2, "bs", [128, 8], F32)
            bs_b = C.buf("bs")
            wk = sb(A2, "wk", [128, BIS_ITERS + 2], F32)
            pow2 = sb(A2, "pow2", [128, BIS_ITERS + 2], F32)
            thrc = sb(A2, "thrc", [128, 1], F32)
            b_c2 = C.buf("constA2")
            C.dma("sp", pow2[:], pow2_d[:, :], [], [b_c2])
            C.op("pool", "memset", [], [b_c2], ap=thrc[:], constant=-1e29)
            for par in range(2):
                for j in range(2):
                    C.op("dve", "memset", [], [qT_tb[j]], ap=qTz[par][j][:, :, :], constant=0.0)
                    C.op("dve", "memset", [], [qiT_tb[j]], ap=qiTz[par][j][:, :, :], constant=0.0)
            PT = [sb(A2, "PT%d" % j, [128, 512], BF16) for j in range(2)]
            PT_b = C.bufs("PT", 2)
            rden = sb(A2, "rden", [128, 8], F32)
            rden_b = C.buf("rden")
            ao = sb(A2, "ao", [128, 8, 64], BF16)
            ao_b = C.buf("ao")
            aoT_st = [sb(A2, "aoT_st%d" % j, [128, 4, 128], BF16) for j in range(2)]
            aoT_b = C.bufs("aoT_st", 2)
            pR = [ps(A2, "pR%d" % j, [128, 512], F32) for j in range(2)]
            pR_b = C.bufs("pR", 2)
            pSC = ps(A2, "pSC", [128, 512], F32)
            pSC_b = C.buf("pSC")
            pST = [ps(A2, "pST%d" % j, [128, 512], F32) for j in range(2)]
            pST_b = C.bufs("pST", 2)
            pPV = [ps(A2, "pPV%d" % j, [128, 512], F32) for j in range(2)]
            pPV_b = C.bufs("pPV", 2)
            pTP2 = ps(A2, "pTP2", [128, 1024], BF16)
            pTP2_b = C.buf("pTP2")
            ao_scr_b = C.bufs("aoscr", NT, share=True)

            C.barrier()
            tiles = list(range(NT)) if a2_tiles is None else list(a2_tiles)
            def index_phase(n_i, i):
                s2 = n_i % 2
                NK = 128 * (i + 1)
                nkc = i + 1
                nkb = (NK + 511) // 512
                for par in range(2):
                    pr = slice(par * 64, (par + 1) * 64)
                    C.dma("sp", qTz[par][s2][pr, :, :], qT_scr[i][pr, :].rearrange("p (c n) -> p c n", c=4), [q_scr_b[i]], [qT_tb[s2]])
                    C.dma("sp", qiTz[par][s2][pr, :, :], qiT_scr[i][pr, :].rearrange("p (c n) -> p c n", c=4), [qi_scr_b[i]], [qiT_tb[s2]])
                for h in range(8):
                    C.op("act", "activation", [b_const, wi_b[i]], [Dg_b], out=Dg[:, h, :], in_=identb[:], func=AF.Copy,
                         scale=wi_sb[:, i, h:h + 1])
                for kb in range(nkb):
                    k0 = kb * 512
                    W = min(512, NK - k0)
                    kbufs = kiT_b[k0 // 128:(k0 + W) // 128]

                    def r_mm(h):
                        C.op("pe", "matmul", [qiT_tb[s2]] + kbufs, [pR_b[h % 2]], sig=True, out=pR[h % 2][:, 0:W],
                             lhsT=qiTz[h % 2][s2][:, h // 2, :], rhs=kiT2[:, k0:k0 + W], start=True, stop=True)
                        C.op("act", "activation", [pR_b[h % 2]], [R_b[h % 2]], out=R_sb[h % 2][:, 0:W], in_=pR[h % 2][:, 0:W], func=AF.Relu)

                    def s_mm(h):
                        C.op("pe", "matmul", [Dg_b, R_b[h % 2]], [pSC_b], sig=(h == 7), out=pSC[:, 0:W], lhsT=Dg[:, h, :],
                             rhs=R_sb[h % 2][:, 0:W], start=(h == 0), stop=(h == 7))

                    r_mm(0)
                    for h in range(8):
                        if h + 1 < 8:
                            r_mm(h + 1)
                        s_mm(h)
                    C.op("act", "activation", [pSC_b], [sc_b[s2]], out=sc[s2][:, k0:k0 + W], in_=pSC[:, 0:W], func=AF.Copy)
                C.op("pool", "memset", [], [sc_b[s2]], ap=sc[s2][0:64, NK - 64:NK], constant=-1e30)

            def thresh_phase(n_i, i):
                s2 = n_i % 2
                NK = 128 * (i + 1)
                nkc = i + 1
                nkb = (NK + 511) // 512
                if i >= 2:
                    C.op("dve", "tensor_reduce", [sc_b[s2]], [bs_b], out=bs[:, 0:1], in_=sc[s2][:, 0:NK], axis=AX.X, op=ALU.max)
                    C.op("dve", "tensor_reduce", [sc_b[s2]], [bs_b], out=bs[:, 1:2], in_=sc[s2][:, 0:320], axis=AX.X, op=ALU.min)
                    C.op("dve", "tensor_tensor", [bs_b], [bs_b], out=bs[:, 2:3], in0=bs[:, 0:1], in1=bs[:, 1:2], op=ALU.subtract)
                    C.op("dve", "tensor_scalar", [bs_b, b_c2], [bs_b], out=wk[:], in0=pow2[:], scalar1=bs[:, 2:3], scalar2=None, op0=ALU.mult)
                    C.op("dve", "tensor_tensor", [bs_b], [bs_b], out=bs[:, 3:4], in0=bs[:, 1:2], in1=wk[:, 1:2], op=ALU.add)
                    for k in range(BIS_ITERS):
                        C.op("dve", "tensor_scalar", [sc_b[s2], bs_b], [NM_b[s2], bs_b], out=NM[s2][:, 0:NK], in0=sc[s2][:, 0:NK],
                             scalar1=bs[:, 3:4], scalar2=None, op0=ALU.is_ge, op1=ALU.add, accum_out=bs[:, 4:5])
                        C.op("dve", "tensor_scalar", [bs_b], [bs_b], out=bs[:, 5:6], in0=bs[:, 4:5], scalar1=255.5, scalar2=0.5,
                             op0=ALU.is_ge, op1=ALU.subtract)
                        if k < BIS_ITERS - 1:
                            C.op("dve", "scalar_tensor_tensor", [bs_b], [bs_b], out=bs[:, 3:4], in0=bs[:, 5:6], scalar=wk[:, k + 1:k + 2],
                                 in1=bs[:, 3:4], op0=ALU.mult, op1=ALU.add)
                    C.op("dve", "tensor_scalar", [bs_b], [bs_b], out=bs[:, 5:6], in0=bs[:, 5:6], scalar1=-0.5, scalar2=None, op0=ALU.add)
                    C.op("dve", "scalar_tensor_tensor", [bs_b], [bs_b], out=bs[:, 6:7], in0=bs[:, 5:6], scalar=wk[:, BIS_ITERS:BIS_ITERS + 1],
                         in1=bs[:, 3:4], op0=ALU.mult, op1=ALU.add)
                    thr_ap = bs[:, 6:7]
                    thr_bufs = [bs_b]
                else:
                    thr_ap = thrc[:, 0:1]
                    thr_bufs = [b_c2]
                C.op("dve", "tensor_scalar", [sc_b[s2]] + thr_bufs, [NM_b[s2]], out=NM[s2][:, 0:NK], in0=sc[s2][:, 0:NK],
                     scalar1=thr_ap, scalar2=NEG_BIG, op0=ALU.is_lt, op1=ALU.mult)

            def attn_phase(n_i, i):
                s2 = n_i % 2
                NK = 128 * (i + 1)
                nkc = i + 1
                nkb = (NK + 511) // 512
                items = [(h, kb) for h in range(8) for kb in range(nkb)]

                def st_block(n):
                    h, kb = items[n]
                    sl = n % 2
                    pb = (h % 2) * 64
                    c = h // 2
                    kcs = list(range(kb * 4, min(nkc, kb * 4 + 4)))
                    for kcl, kc in enumerate(kcs):
                        C.op("pe", "matmul", [kT_b[kc], qT_tb[s2]], [pST_b[sl]], out=pST[sl][:, kcl * 128:(kcl + 1) * 128],
                             lhsT=kT[:, c, kc * 128:(kc + 1) * 128], rhs=qTz[h % 2][s2][:, c, :], start=True, stop=False)
                        C.op("pe", "matmul", [NM_b[s2], b_const], [pST_b[sl]], sig=(kcl == len(kcs) - 1),
                             out=pST[sl][:, kcl * 128:(kcl + 1) * 128],
                             lhsT=NM[s2][:, kc * 128:(kc + 1) * 128], rhs=identb[:], start=False, stop=True)
                    C.op("act", "activation", [pST_b[sl]], [PT_b[sl]], out=PT[sl][:, 0:len(kcs) * 128], in_=pST[sl][:, 0:len(kcs) * 128],
                         func=AF.Exp, scale=0.125)

                def pv_block(n):
                    h, kb = items[n]
                    sl = n % 2
                    kcs = list(range(kb * 4, min(nkc, kb * 4 + 4)))
                    for kcl, kc in enumerate(kcs):
                        C.op("pe", "matmul", [PT_b[sl], v_b[kc]], [pPV_b[h // 4]], out=pPV[h // 4][:, (h % 4) * 65:(h % 4) * 65 + 65],
                             lhsT=PT[sl][:, kcl * 128:(kcl + 1) * 128], rhs=v_sb[:, kc, h, :], start=(kc == 0), stop=(kc == nkc - 1))

                st_block(0)
                for n in range(len(items)):
                    if n + 1 < len(items):
                        st_block(n + 1)
                    pv_block(n)
                fs = len(items) % 2
                C.op("pe", "matmul", [b_const], [pST_b[fs]], sig=True, out=pST[fs][:, 0:128], lhsT=identb[:], rhs=identb[:], start=True, stop=True)

            def attn_tail(n_i, i):
                s2 = n_i % 2
                for hh in range(2):
                    pvv = pPV[hh][:, 0:260].rearrange("p (h d) -> p h d", h=4)
                    C.op("dve", "reciprocal", [pPV_b[hh]], [rden_b], out=rden[:, hh * 4:hh * 4 + 4].unsqueeze(2), in_=pvv[:, :, 64:65])
                    C.op("dve", "tensor_tensor", [pPV_b[hh], rden_b], [ao_b], out=ao[:, hh * 4:hh * 4 + 4, :], in0=pvv[:, :, 0:64],
                         in1=bc(rden[:, hh * 4:hh * 4 + 4].unsqueeze(2), [128, 4, 64]), op=ALU.mult)
                for c in range(4):
                    C.op("pe", "transpose", [ao_b, b_const], [pTP2_b], sig=(c == 3), out=pTP2[:, c * 128:(c + 1) * 128],
                         in_=ao[:, 2 * c:2 * c + 2, :].rearrange("p a d -> p (a d)"), identity=identb[:])
                C.op("act", "activation", [pTP2_b], [aoT_b[s2]], out=aoT_st[s2][:, :, :], in_=pTP2[:, 0:512].rearrange("p (c n) -> p c n", c=4), func=AF.Copy)
                C.dma("sp", ao_scr[i].rearrange("p (c n) -> p c n", c=4), aoT_st[s2][:, :, :], [aoT_b[s2]], [ao_scr_b[i]], sem_from=aoT_b[s2])

            nt_ = len(tiles)
            index_phase(0, tiles[0])
            thresh_phase(0, tiles[0])
            if nt_ > 1:
                index_phase(1, tiles[1])
            for n_i in range(nt_):
                attn_phase(n_i, tiles[n_i])
                if n_i + 1 < nt_:
                    thresh_phase(n_i + 1, tiles[n_i + 1])
                if n_i + 2 < nt_:
                    index_phase(n_i + 2, tiles[n_i + 2])
                attn_tail(n_i, tiles[n_i])
            s2 = (nt_ - 1) % 2
            if "nm" in dbg_out:
                dbn = C.buf("dbgnm")
                C.dma("sp", dbg_out["nm"][:, :], NM[s2][:, :], [NM_b[s2]], [dbn])
                C.dma("sp", dbg_out["sc"][:, :], sc[s2][:, :], [sc_b[s2]], [dbn])
                C.final_wait("sp", [dbn])
            C.barrier()

        if stop_after == "A2":
            db = C.buf("dbg2")
            C.dma("sp", dbg_out["ao"].rearrange("(t p) n -> p t n", p=128), ao_scr.rearrange("t p n -> p t n"), ao_scr_b, [db])
            C.final_wait("sp", [db])
            return nc

        PA.close()

        xn_scr = dscr("xn_scr", [NT, 128, 1024], BF16)
        xn_scr_b = C.bufs("xnscr", NT, share=True)
        x2_scr_b = C.bufs("x2scr", NT, share=True)
        PBC = ExitStack()
        es.enter_context(PBC)
        comb = sb(PBC, "comb", [128, NT, 32], F32)
        comb_b = C.bufs("comb", NT)
        lgall = sb(PBC, "lgall", [128, NT, 36], F32)
        lgall_b = C.buf("lgall")

        def load_w(stack, name, src, rows, c0, c1, bufname, defer=False):
            nkc = rows // 128
            t = sb(stack, name, [128, nkc, c1 - c0], BF16)
            b = C.buf(bufname)

            def issue():
                for kc in range(nkc):
                    C.dma("pool", t[:, kc, :], src[kc * 128:(kc + 1) * 128, c0:c1], [], [b])
            if defer:
                return t, b, issue
            issue()
            return t, b

        def rms_rstd(x_ap, x_bufs, junk_ap, junk_bufs, st, st_b, scale_n=D):
            C.op("act", "activation", x_bufs, junk_bufs + [st_b], out=junk_ap, in_=x_ap, func=AF.Square, accum_out=st[:, 0:1])
            C.op("act", "activation", [st_b], [st_b], out=st[:, 1:2], in_=st[:, 0:1], func=AF.Sqrt, scale=1.0 / scale_n, bias=EPS)
            C.op("dve", "reciprocal", [st_b], [st_b], out=st[:, 2:3], in_=st[:, 1:2])

        with ExitStack() as B:
            wg_sb, wg_b, ld_wg = load_w(B, "w_gates", w_in_d, D, G_OFF, G_OFF + 2048, "w_gates", defer=True)
            woa_sb, woa_b, ld_woa = load_w(B, "w_oa", w_o_attn_d, 512, 0, D, "w_oa", defer=True)
            wco_sb, wco_b, ld_wco = load_w(B, "w_co", w_conv_out_d, 512, 0, D, "w_co", defer=True)
            wout_sb, wout_b, ld_wout = load_w(B, "w_outb", w_out_d, D, 0, D, "w_outb", defer=True)
            wqx_sb, wqx_b, ld_wqx = load_w(B, "w_qxb", w_q_x_d, D, 0, D, "w_qxb", defer=True)
            wox_sb, wox_b, ld_wox = load_w(B, "w_oxb", w_o_x_d, D, 0, D, "w_oxb", defer=True)
            kmT = sb(B, "kmT", [128, 8, 256], BF16)
            vm = sb(B, "vm", [128, 2, D], BF16)
            kmT_b = C.buf("kmT")
            vm_b = C.buf("vm")
            gfm = sb(B, "gfmB", [128, 8], F32)
            gxm = sb(B, "gxm", [128, 8], F32)
            gmm = sb(B, "gmm", [128, 8], F32)
            gmo = sb(B, "gmo", [128, 8], F32)
            wr_sb = sb(B, "wr_sb", [128, 8, 36], F32)
            br_sb = sb(B, "br_sb", [128, 36], F32)
            b_smB = C.buf("smallB")
            C.dma("sp", gfm[:], g_mix_d[:, :], [], [b_smB])
            C.dma("sp", gxm[:], g_x_d[:, :], [], [b_smB])
            C.dma("sp", gmm[:], g_mem_d[:, :], [], [b_smB])
            C.dma("sp", gmo[:], g_moe_d[:, :], [], [b_smB])
            C.dma("sp", wr_sb[:, :, :], w_router_d.rearrange("(c p) n -> p c n", p=128), [], [b_smB])
            C.dma("sp", br_sb[:], bc(b_router_d[0:1, :], [128, 36]), [], [b_smB])
            stB = sb(B, "statB", [128, 8], F32)
            stB_b = C.buf("statB")
            pb = [ps(B, "pb%d" % j, [128, 512], F32) for j in range(7)]
            pb_b = C.bufs("pb", 7)
            pTPb = ps(B, "pTPb", [128, 1024], BF16)
            pTPb_b = C.buf("pTPb")

            sqj_ref = []

            def norm_chain(x_ap, x_bufs, hn_ap, hn_bufs):
                if sqj_ref:
                    rms_rstd(x_ap, x_bufs, sqj_ref[0][:, :], [sqj_ref[1]], stB, stB_b)
                else:
                    rms_rstd(x_ap, x_bufs, hn_ap, hn_bufs, stB, stB_b)
                C.op("act", "activation", x_bufs + [stB_b], hn_bufs, out=hn_ap, in_=x_ap, func=AF.Copy, scale=stB[:, 2:3])

            def norm_trans(hn_ap, hn_bufs, g_sb, dstT, dst_bufs, col0):
                for c in range(8):
                    C.op("pe", "transpose", hn_bufs + [b_const], [pTPb_b], sig=(c == 7), out=pTPb[:, c * 128:(c + 1) * 128],
                         in_=hn_ap[:, c * 128:(c + 1) * 128], identity=identb[:])
                C.op("dve", "tensor_tensor", [pTPb_b, b_smB], dst_bufs, out=dstT[:, :, col0:col0 + 128],
                     in0=pTPb[:, :].rearrange("p (c n) -> p c n", c=8), in1=bc(g_sb[:, :].unsqueeze(2), [128, 8, 128]), op=ALU.mult)

            def norm_T(x_ap, x_bufs, hn_ap, hn_bufs, g_sb, dstT, dst_bufs, col0):
                norm_chain(x_ap, x_bufs, hn_ap, hn_bufs)
                norm_trans(hn_ap, hn_bufs, g_sb, dstT, dst_bufs, col0)

            with ExitStack() as BK:
                wkv_sb, wkv_b = load_w(BK, "w_kvb", w_kv_x_d, D, 0, 2 * D, "w_kvb")
                for ld in (ld_wg, ld_woa, ld_wco, ld_wout, ld_wqx, ld_wox):
                    ld()
                memt2 = [sb(BK, "memt%d" % j, [128, D], F32) for j in range(2)]
                memt2_b = C.bufs("memt", 2)
                memh = sb(BK, "memh", [128, D], BF16)
                memh_b = C.buf("memh")
                memnT = sb(BK, "memnT", [128, 8, 256], BF16)
                memnT_b = C.buf("memnT")
                for mt in range(2):
                    C.dma("sp", memt2[mt][:], mem_d[mt * 128:(mt + 1) * 128, :], [], [memt2_b[mt]])
                for mt in range(2):
                    norm_T(memt2[mt][:, :], [memt2_b[mt]], memh[:, :], [memh_b], gmm, memnT, [memnT_b], mt * 128)
                for jx in range(8):
                    bk = jx % 2
                    for kc in range(8):
                        C.op("pe", "matmul", [wkv_b, memnT_b], [pb_b[bk]], sig=(kc == 7), out=pb[bk][:, 0:256],
                             lhsT=wkv_sb[:, kc, jx * 128:(jx + 1) * 128], rhs=memnT[:, kc, :], start=(kc == 0), stop=(kc == 7))
                    C.op("act", "activation", [pb_b[bk]], [kmT_b], out=kmT[:, jx, :], in_=pb[bk][:, 0:256], func=AF.Copy)
                for mc in range(2):
                    for cb in range(2):
                        bk = 2 + (mc * 2 + cb) % 2
                        for kc in range(8):
                            C.op("pe", "matmul", [wkv_b, memnT_b], [pb_b[bk]], sig=(kc == 7), out=pb[bk][:, :],
                                 lhsT=memnT[:, kc, mc * 128:(mc + 1) * 128], rhs=wkv_sb[:, kc, D + cb * 512:D + (cb + 1) * 512],
                                 start=(kc == 0), stop=(kc == 7))
                        C.op("act", "activation", [pb_b[bk]], [vm_b], out=vm[:, mc, cb * 512:(cb + 1) * 512], in_=pb[bk][:, :], func=AF.Copy)
                C.barrier()

            xt4 = sb(B, "xt4", [128, 4, D], F32)
            xt4_b = C.bufs("xt4", 4)
            hnB = [sb(B, "hnB%d" % j, [128, D], BF16) for j in range(4)]
            hnB_b = C.bufs("hnB", 4)
            sqj = sb(B, "sqj", [128, D], BF16)
            sqj_b = C.buf("sqj")
            sqj_ref.extend([sqj, sqj_b])
            bufA = sb(B, "bufA", [128, 8, 512], BF16)
            bufA_b = C.bufs("bufA", 4)
            bufB = sb(B, "bufB", [128, 8, 512], BF16)
            bufB_b = C.bufs("bufB", 8)
            bufC = sb(B, "bufC", [128, 8, 512], BF16)
            bufC_b = C.bufs("bufC", 8)
            aoTb = sb(B, "aoTb", [128, 4, 512], BF16)
            aoTb_b = C.buf("aoTb")
            zTb = sb(B, "zTb", [128, 4, 512], BF16)
            zTb_b = C.buf("zTb")
            sgA = sb(B, "sgA", [128, 512], F32)
            sgB = sb(B, "sgB", [128, 512], F32)
            sgA_b = C.buf("sgA")
            sgB_b = C.buf("sgB")
            PT2 = sb(B, "PT2", [128, 2, 512], BF16)
            PT2_b = C.bufs("PT2", 2)
            rden2 = sb(B, "rden2", [128, 512], F32)
            rden2_b = C.buf("rden2")
            xn32s = [sb(B, "xn32_%d" % j, [128, D], F32) for j in range(2)]
            xn32s_b = C.bufs("xn32", 2)
            xnT32 = sb(B, "xnT32", [128, 8, 128], F32)
            xnT32_b = C.buf("xnT32")
            xnTb = [sb(B, "xnTb%d" % j, [128, 8, 128], BF16) for j in range(2)]
            xnTb_b = C.bufs("xnTb", 2)
            rt_ = sb(B, "rtr", [128, 128], F32)
            rt_b2 = C.buf("rtr")

            def load_x(blk, t):
                i = blk * 4 + t
                C.dma("sp", xt4[:, t, :], x_d[i * 128:(i + 1) * 128, :], [], [xt4_b[t]])

            def load_aoz(blk):
                for t in range(4):
                    i = blk * 4 + t
                    C.dma("sp", aoTb[:, :, t * 128:(t + 1) * 128], ao_scr[i].rearrange("p (c n) -> p c n", c=4), [ao_scr_b[i]], [aoTb_b])
                C.dma("sp", zTb[:, :, :], z_scr[blk].rearrange("p (c n) -> p c n", c=4), [z_scr_b[blk]], [zTb_b])

            def pipelined_norms(g_sb, ready=None):
                def ch(t):
                    norm_chain(xt4[:, t, :], [xt4_b[t]], hnB[t][:, :], [hnB_b[t]])

                def tr(t):
                    norm_trans(hnB[t][:, :], [hnB_b[t]], g_sb, bufA, [bufA_b[t]], t * 128)
                return ch, tr

            for t in range(4):
                load_x(0, t)
            load_aoz(0)
            for blk in range(NSB):
                ch, tr = pipelined_norms(gfm)
                for t in range(4):
                    ch(t)
                for t in range(4):
                    tr(t)
                for fo in range(8):
                    fs_ = slice(fo * 128, (fo + 1) * 128)
                    b0, b1, b2 = (0, 1, 2) if fo % 2 == 0 else (4, 5, 6)
                    for c in range(4):
                        C.op("pe", "matmul", [woa_b, aoTb_b], [pb_b[b0]], sig=(c == 3), out=pb[b0][:, :], lhsT=woa_sb[:, c, fs_], rhs=aoTb[:, c, :],
                             start=(c == 0), stop=(c == 3))
                    for c in range(4):
                        C.op("pe", "matmul", [wco_b, zTb_b], [pb_b[b1]], sig=(c == 3), out=pb[b1][:, :], lhsT=wco_sb[:, c, fs_], rhs=zTb[:, c, :],
                             start=(c == 0), stop=(c == 3))
                    for kc in range(8):
                        C.op("pe", "matmul", [wg_b] + bufA_b, [pb_b[b2]], sig=(kc == 7), out=pb[b2][:, :], lhsT=wg_sb[:, kc, fs_], rhs=bufA[:, kc, :],
                             start=(kc == 0), stop=(kc == 7))
                    for kc in range(8):
                        C.op("pe", "matmul", [wg_b] + bufA_b, [pb_b[3]], sig=(kc == 7), out=pb[3][:, :],
                             lhsT=wg_sb[:, kc, D + fo * 128:D + (fo + 1) * 128], rhs=bufA[:, kc, :], start=(kc == 0), stop=(kc == 7))
                    C.op("act", "activation", [pb_b[b2]], [sgA_b], out=sgA[:], in_=pb[b2][:, :], func=AF.Sigmoid)
                    C.op("act", "activation", [pb_b[3]], [sgB_b], out=sgB[:], in_=pb[3][:, :], func=AF.Sigmoid)
                    C.op("dve", "tensor_tensor", [pb_b[b0], sgA_b], [sgA_b], out=sgA[:], in0=pb[b0][:, :], in1=sgA[:], op=ALU.mult)
                    C.op("dve", "tensor_tensor", [pb_b[b1], sgB_b], [sgB_b], out=sgB[:], in0=pb[b1][:, :], in1=sgB[:], op=ALU.mult)
                    C.op("pool", "tensor_tensor", [sgA_b, sgB_b], [bufB_b[fo]], out=bufB[:, fo, :], in0=sgA[:], in1=sgB[:], op=ALU.add)
                if blk + 1 < NSB:
                    load_aoz(blk + 1)
                ch, tr = pipelined_norms(gxm)
                for t in range(4):
                    if t >= 2:
                        tr(t - 2)
                    for cb in range(2):
                        bk = 4 + (t * 2 + cb) % 2
                        for kc in range(8):
                            C.op("pe", "matmul", [wout_b, bufB_b[kc]], [pb_b[bk]], sig=(kc == 7), out=pb[bk][:, :],
                                 lhsT=bufB[:, kc, t * 128:(t + 1) * 128], rhs=wout_sb[:, kc, cb * 512:(cb + 1) * 512], start=(kc == 0), stop=(kc == 7))
                        C.op("dve", "tensor_tensor", [pb_b[bk], xt4_b[t]], [xt4_b[t]], out=xt4[:, t, cb * 512:(cb + 1) * 512], in0=pb[bk][:, :],
                             in1=xt4[:, t, cb * 512:(cb + 1) * 512], op=ALU.add)
                    ch(t)
                tr(2)
                tr(3)
                for fo in range(8):
                    bk = 4 + fo % 2
                    for kc in range(8):
                        C.op("pe", "matmul", [wqx_b] + bufA_b, [pb_b[bk]], sig=(kc == 7), out=pb[bk][:, :],
                             lhsT=wqx_sb[:, kc, fo * 128:(fo + 1) * 128], rhs=bufA[:, kc, :], start=(kc == 0), stop=(kc == 7))
                    C.op("act", "activation", [pb_b[bk]], [bufB_b[fo]], out=bufB[:, fo, :], in_=pb[bk][:, :], func=AF.Copy)
                for h in range(4):
                    for mc in range(2):
                        for dc in range(2):
                            C.op("pe", "matmul", [kmT_b, bufB_b[h * 2 + dc]], [pb_b[mc]], sig=(dc == 1), out=pb[mc][:, :],
                                 lhsT=kmT[:, h * 2 + dc, mc * 128:(mc + 1) * 128], rhs=bufB[:, h * 2 + dc, :], start=(dc == 0), stop=(dc == 1))
                        C.op("act", "activation", [pb_b[mc]], [PT2_b[mc]], out=PT2[:, mc, :], in_=pb[mc][:, :], func=AF.Exp, scale=1.0 / 16.0)
                    for mc in range(2):
                        C.op("pe", "matmul", [b_const, PT2_b[mc]], [pb_b[2]], sig=(mc == 1), out=pb[2][:, :], lhsT=onesb[:], rhs=PT2[:, mc, :],
                             start=(mc == 0), stop=(mc == 1))
                    C.op("dve", "reciprocal", [pb_b[2]], [rden2_b], out=rden2[:], in_=pb[2][:, :])
                    for dvc in range(2):
                        bk = 3 if dvc == 0 else 6
                        for mc in range(2):
                            C.op("pe", "matmul", [vm_b, PT2_b[mc]], [pb_b[bk]], sig=(mc == 1), out=pb[bk][:, :],
                                 lhsT=vm[:, mc, h * 256 + dvc * 128:h * 256 + (dvc + 1) * 128], rhs=PT2[:, mc, :], start=(mc == 0), stop=(mc == 1))
                        C.op("dve", "tensor_tensor", [pb_b[bk], rden2_b], [bufC_b[h * 2 + dvc]], out=bufC[:, h * 2 + dvc, :], in0=pb[bk][:, :],
                             in1=rden2[:], op=ALU.mult)
                def wox(t):
                    i = blk * 4 + t
                    for cb in range(2):
                        bk = 4 + (t * 2 + cb) % 2
                        for kc in range(8):
                            C.op("pe", "matmul", [wox_b, bufC_b[kc]], [pb_b[bk]], sig=(kc == 7), out=pb[bk][:, :],
                                 lhsT=bufC[:, kc, t * 128:(t + 1) * 128], rhs=wox_sb[:, kc, cb * 512:(cb + 1) * 512], start=(kc == 0), stop=(kc == 7))
                        C.op("dve", "tensor_tensor", [pb_b[bk], xt4_b[t]], [xt4_b[t]], out=xt4[:, t, cb * 512:(cb + 1) * 512], in0=pb[bk][:, :],
                             in1=xt4[:, t, cb * 512:(cb + 1) * 512], op=ALU.add)
                    C.dma("sp", x2_scr[i * 128:(i + 1) * 128, :], xt4[:, t, :], [xt4_b[t]], [x2_scr_b[i]], sem_from=xt4_b[t])
                    xn32, xn32_b = xn32s[t % 2], xn32s_b[t % 2]
                    rms_rstd(xt4[:, t, :], [xt4_b[t]], sqj[:, :], [sqj_b], stB, stB_b)
                    C.op("act", "activation", [xt4_b[t], stB_b], [xn32_b], out=xn32[:, :], in_=xt4[:, t, :], func=AF.Copy, scale=stB[:, 2:3])

                def b6(t):
                    i = blk * 4 + t
                    xn32, xn32_b = xn32s[t % 2], xn32s_b[t % 2]
                    for c in range(8):
                        C.op("pe", "transpose", [xn32_b, b_const], [pb_b[c // 4]], sig=(c % 4 == 3), out=pb[c // 4][:, (c % 4) * 128:(c % 4 + 1) * 128],
                             in_=xn32[:, c * 128:(c + 1) * 128], identity=identf[:])
                    for hh in range(2):
                        C.op("dve", "tensor_tensor", [pb_b[hh], b_smB], [xnT32_b], out=xnT32[:, hh * 4:hh * 4 + 4, :],
                             in0=pb[hh][:, :].rearrange("p (c n) -> p c n", c=4), in1=bc(gmo[:, hh * 4:hh * 4 + 4].unsqueeze(2), [128, 4, 128]), op=ALU.mult)
                    xj = i % 2
                    C.op("pool", "tensor_copy", [xnT32_b], [xnTb_b[xj]], out=xnTb[xj][:, :, :], in_=xnT32[:, :, :])
                    C.dma("sp", xn_scr[i].rearrange("p (c n) -> p c n", c=8), xnTb[xj][:, :, :], [xnTb_b[xj]], [xn_scr_b[i]], sem_from=xnTb_b[xj])
                    for kc in range(8):
                        C.op("pe", "matmul", [xnT32_b, b_smB], [pb_b[2]], out=pb[2][:, 0:36], lhsT=xnT32[:, kc, :], rhs=wr_sb[:, kc, :],
                             start=(kc == 0), stop=(kc == 7))
                    C.op("pe", "matmul", [b_const], [pb_b[3]], sig=True, out=pb[3][:, 0:128], lhsT=identb[:], rhs=identb[:], start=True, stop=True)
                    C.op("dve", "tensor_tensor", [pb_b[2], b_smB], [lgall_b], out=lgall[:, i, :], in0=pb[2][:, 0:36], in1=br_sb[:, :], op=ALU.add)
                wox(0)
                wox(1)
                b6(0)
                wox(2)
                b6(1)
                wox(3)
                b6(2)
                if blk + 1 < NSB:
                    for t in range(3):
                        load_x(blk + 1, t)
                b6(3)
                if blk + 1 < NSB:
                    load_x(blk + 1, 3)
            C.barrier()

        if stop_after == "B":
            db = C.buf("dbgB")
            C.dma("sp", dbg_out["x2"][:, :], x2_scr[:, :], x2_scr_b, [db])
            C.dma("sp", dbg_out["comb"].rearrange("p (t e) -> p t e", t=NT), comb[:, :, :], comb_b, [db])
            C.dma("sp", dbg_out["xn"].rearrange("(t p) n -> p t n", p=128), xn_scr.rearrange("t p n -> p t n"), xn_scr_b, [db])
            C.final_wait("sp", [db])
            return nc

        with ExitStack() as CC:
            xnT = sb(CC, "xnT", [128, 8, S], BF16)
            xnT_blk = C.bufs("xnT", NSB)
            xnT_b = [xnT_blk[i // 4] for i in range(NT)]
            for i in range(NT):
                C.dma("sp", xnT[:, :, i * 128:(i + 1) * 128], xn_scr[i].rearrange("p (c n) -> p c n", c=8), [xn_scr_b[i]], [xnT_b[i]])
            ysb = sb(CC, "ysb", [128, 16, D], F32)
            ysb_b = C.bufs("ysb", 16)
            NSLOT = 3
            wgs = [sb(CC, "wgs%d" % j, [128, 8, 256], BF16) for j in range(NSLOT)]
            wus = [sb(CC, "wus%d" % j, [128, 8, 256], BF16) for j in range(NSLOT)]
            wds = [sb(CC, "wds%d" % j, [128, 2, D], BF16) for j in range(NSLOT)]
            wslot_b = C.bufs("wslot", NSLOT)
            actT = [sb(CC, "actT%d" % j, [128, 2, 512], BF16) for j in range(2)]
            actT_b = [C.bufs("actT%d_" % j, 2) for j in range(2)]
            ssb = [sb(CC, "ssb%d" % j, [128, 512], BF16) for j in range(2)]
            ssb_b = C.bufs("ssb", 2)
            x2t = [sb(CC, "x2t%d" % j, [128, D], F32) for j in range(2)]
            x2t_b = C.bufs("x2t", 2)
            ot = [sb(CC, "ot%d" % j, [128, D], F32) for j in range(2)]
            ot_b = C.bufs("ot", 2)
            gfin = sb(CC, "gfin", [128, D], F32)
            gfin_b = C.buf("gfin")
            C.dma("sp", gfin[:], bc(g_final_d[0:1, :], [128, D]), [], [gfin_b])
            stC = sb(CC, "statC", [128, 8], F32)
            stC_b = C.buf("statC")
            pg = [ps(CC, "pg%d" % j, [128, 512], F32) for j in range(2)]
            pu = [ps(CC, "pu%d" % j, [128, 512], F32) for j in range(2)]
            py = [ps(CC, "py%d" % j, [128, 1024], F32) for j in range(2)]
            pg_b = C.bufs("pg", 2)
            pu_b = C.bufs("pu", 2)
            py_b = C.bufs("py", 2)

            with ExitStack() as BR:
                T = NT
                o1 = ot[1]
                gmax = o1[:, 0:32]
                oneh = o1[:, 32:160].rearrange("p (t g) -> p t g", g=4)
                dgl = o1[:, 160:288].rearrange("p (t g) -> p t g", g=4)
                sume = o1[:, 288:320]
                ggate = o1[:, 320:352]
                elsel = o1[:, 352:608].rearrange("p (t e) -> p t e", e=8)
                m8 = o1[:, 608:864].rearrange("p (t e) -> p t e", e=8)
                sc1 = o1[:, 864:992].rearrange("p (k t) -> p k t", k=4)
                tmpe = ot[0][:, :].rearrange("p (t f) -> p t f", f=32)
                eqa = x2t[0][:, 0:256].rearrange("p (t e) -> p t e", e=8)
                eqb = x2t[0][:, 256:512].rearrange("p (t e) -> p t e", e=8)
                rb_ = C.buf("routing")
                rr, ww = [lgall_b, rb_], [rb_, ot_b[0], ot_b[1], x2t_b[0]]
                gl = lgall[:, :, 0:4]
                el4 = lgall[:, :, 4:36].rearrange("p t (g e) -> p t g e", g=4)
                C.op("dve", "tensor_reduce", rr, ww, out=gmax[:, :], in_=gl, axis=AX.X, op=ALU.max)
                C.op("dve", "tensor_tensor", rr, ww, out=oneh[:, :, :], in0=gl, in1=bc(gmax[:, :].unsqueeze(2), [128, T, 4]), op=ALU.is_equal)
                C.op("dve", "tensor_tensor", rr, ww, out=dgl[:, :, :], in0=gl, in1=bc(gmax[:, :].unsqueeze(2), [128, T, 4]), op=ALU.subtract)
                C.op("act", "activation", rr, ww, out=dgl[:, :, :], in_=dgl[:, :, :], func=AF.Exp)
                C.op("dve", "tensor_reduce", rr, ww, out=sume[:, :], in_=dgl[:, :, :], axis=AX.X, op=ALU.add)
                C.op("dve", "reciprocal", rr, ww, out=ggate[:, :], in_=sume[:, :])
                C.op("dve", "tensor_tensor", rr, ww, out=tmpe[:, :, :].rearrange("p t (g e) -> p t g e", g=4), in0=el4,
                     in1=bc(oneh[:, :, :].unsqueeze(3), [128, T, 4, 8]), op=ALU.mult)
                C.op("dve", "tensor_reduce", rr, ww, out=elsel[:, :, :], in_=tmpe[:, :, :].rearrange("p t (g e) -> p t e g", g=4), axis=AX.X, op=ALU.add)
                for t in range(T):
                    C.op("dve", "max", rr, ww, out=m8[:, t, :], in_=elsel[:, t, :])
                m1 = m8[:, :, 0]
                m2 = m8[:, :, 1]
                e2, s1, w1, w2 = sc1[:, 0, :], sc1[:, 1, :], sc1[:, 2, :], sc1[:, 3, :]
                C.op("dve", "tensor_tensor", rr, ww, out=e2, in0=m2, in1=m1, op=ALU.subtract)
                C.op("act", "activation", rr, ww, out=e2, in_=e2, func=AF.Exp)
                C.op("dve", "tensor_scalar", rr, ww, out=s1, in0=e2, scalar1=1.0, scalar2=None, op0=ALU.add)
                C.op("dve", "reciprocal", rr, ww, out=s1, in_=s1)
                C.op("dve", "tensor_tensor", rr, ww, out=w1, in0=s1, in1=ggate[:, :], op=ALU.mult)
                C.op("dve", "tensor_tensor", rr, ww, out=w2, in0=w1, in1=e2, op=ALU.mult)
                C.op("dve", "tensor_tensor", rr, ww, out=eqa[:, :, :], in0=elsel[:, :, :], in1=bc(m1.unsqueeze(2), [128, T, 8]), op=ALU.is_equal)
                C.op("dve", "tensor_tensor", rr, ww, out=eqa[:, :, :], in0=eqa[:, :, :], in1=bc(w1.unsqueeze(2), [128, T, 8]), op=ALU.mult)
                C.op("dve", "tensor_tensor", rr, ww, out=eqb[:, :, :], in0=elsel[:, :, :], in1=bc(m2.unsqueeze(2), [128, T, 8]), op=ALU.is_equal)
                C.op("dve", "tensor_tensor", rr, ww, out=eqb[:, :, :], in0=eqb[:, :, :], in1=bc(w2.unsqueeze(2), [128, T, 8]), op=ALU.mult)
                C.op("dve", "tensor_tensor", rr, ww, out=eqa[:, :, :], in0=eqa[:, :, :], in1=eqb[:, :, :], op=ALU.add)
                C.op("dve", "tensor_tensor", rr, comb_b, out=comb[:, :, :].rearrange("p t (g e) -> p t g e", g=4),
                     in0=bc(oneh[:, :, :].unsqueeze(3), [128, T, 4, 8]), in1=bc(eqa[:, :, :].unsqueeze(2), [128, T, 4, 8]), op=ALU.mult)


            for hf in range(2):
                units = [(e, tb) for e in range(32) for tb in range(4)]

                def load_expert(e):
                    sl = e % NSLOT
                    C.dma("pool", wgs[sl][:, :, :], w_eg_d[e].rearrange("(c p) f -> p c f", p=128), [], [wslot_b[sl]])
                    C.dma("pool", wus[sl][:, :, :], w_eu_d[e].rearrange("(c p) f -> p c f", p=128), [], [wslot_b[sl]])
                    C.dma("pool", wds[sl][:, :, :], w_ed_d[e].rearrange("(c p) n -> p c n", p=128), [], [wslot_b[sl]])

                gu_cnt = [0]

                def gu(n):
                    e, tb = units[n]
                    sl = e % NSLOT
                    a = n % 2
                    tok0 = (hf * 16 + tb * 4) * 128
                    xb = xnT_b[hf * 16 + tb * 4:hf * 16 + tb * 4 + 4]
                    for fc in range(2):
                        k2 = gu_cnt[0] % 2
                        gu_cnt[0] += 1
                        for kc in range(8):
                            C.op("pe", "matmul", [wslot_b[sl]] + xb, [pg_b[k2]], sig=(kc == 7), out=pg[k2][:, :],
                                 lhsT=wgs[sl][:, kc, fc * 128:(fc + 1) * 128], rhs=xnT[:, kc, tok0:tok0 + 512], start=(kc == 0), stop=(kc == 7))
                        for kc in range(8):
                            C.op("pe", "matmul", [wslot_b[sl]] + xb, [pu_b[k2]], sig=(kc == 7), out=pu[k2][:, :],
                                 lhsT=wus[sl][:, kc, fc * 128:(fc + 1) * 128], rhs=xnT[:, kc, tok0:tok0 + 512], start=(kc == 0), stop=(kc == 7))
                        C.op("act", "activation", [pg_b[k2]], [ssb_b[k2]], out=ssb[k2][:], in_=pg[k2][:, :], func=AF.Silu)
                        C.op("dve", "tensor_tensor", [pu_b[k2], ssb_b[k2]], [actT_b[a][fc]], out=actT[a][:, fc, :], in0=pu[k2][:, :], in1=ssb[k2][:], op=ALU.mult)

                def down(n):
                    e, tb = units[n]
                    sl = e % NSLOT
                    a = n % 2
                    for t in range(4):
                        yt = tb * 4 + t
                        i = hf * 16 + yt
                        k2 = (n * 4 + t) % 2
                        for cb in range(2):
                            for fc in range(2):
                                C.op("pe", "matmul", [wslot_b[sl], actT_b[a][fc]], [py_b[k2]], sig=(cb == 1 and fc == 1),
                                     out=py[k2][:, cb * 512:(cb + 1) * 512], lhsT=actT[a][:, fc, t * 128:(t + 1) * 128],
                                     rhs=wds[sl][:, fc, cb * 512:(cb + 1) * 512], start=(fc == 0), stop=(fc == 1))
                        if e == 0:
                            j = yt % 2
                            C.dma("sp", x2t[j][:], x2_scr[i * 128:(i + 1) * 128, :], [x2_scr_b[i]], [x2t_b[j]])
                            C.op("dve", "scalar_tensor_tensor", [py_b[k2], comb_b[i], x2t_b[j]], [ysb_b[yt]], out=ysb[:, yt, :], in0=py[k2][:, :],
                                 scalar=comb[:, i, e:e + 1], in1=x2t[j][:], op0=ALU.mult, op1=ALU.add)
                        else:
                            C.op("dve", "scalar_tensor_tensor", [py_b[k2], comb_b[i], ysb_b[yt]], [ysb_b[yt]], out=ysb[:, yt, :], in0=py[k2][:, :],
                                 scalar=comb[:, i, e:e + 1], in1=ysb[:, yt, :], op0=ALU.mult, op1=ALU.add)

                def tail(yt):
                    i = hf * 16 + yt
                    j = yt % 2
                    rms_rstd(ysb[:, yt, :], [ysb_b[yt]], ot[j][:, :], [ot_b[j]], stC, stC_b)
                    C.op("dve", "scalar_tensor_tensor", [ysb_b[yt], stC_b, gfin_b], [ot_b[j]], out=ot[j][:], in0=ysb[:, yt, :], scalar=stC[:, 2:3],
                         in1=gfin[:], op0=ALU.mult, op1=ALU.mult)
                    C.dma("sp", out_d[i * 128:(i + 1) * 128, :], ot[j][:], [ot_b[j]], [out_b])

                load_expert(0)
                load_expert(1)
                gu(0)
                for n in range(len(units)):
                    e, tb = units[n]
                    if tb == 0 and e + 2 < 32:
                        load_expert(e + 2)
                    if n + 1 < len(units):
                        gu(n + 1)
                    down(n)
                    if e == 31:
                        for t in range(4):
                            tail(tb * 4 + t)
            C.barrier()
        C.final_wait("sp", [out_b])
    return nc


def make_in_maps(inputs):
    f32 = np.float32
    x = np.asarray(inputs["x"], f32)
    mem = np.asarray(inputs["mem"], f32)
    pos = np.asarray(inputs["positions"]).astype(np.int32)
    B = x.shape[0]

    def fm(v, n):
        return np.ascontiguousarray(np.asarray(v, f32).reshape(n, 128).T)

    w_re = np.asarray(inputs["w_router_expert"], f32)[0]
    w_router = np.concatenate([np.asarray(inputs["w_router_group"], f32)[0],
                               np.ascontiguousarray(w_re.transpose(1, 0, 2)).reshape(D, 32)], axis=1)
    b_router = np.concatenate([np.asarray(inputs["b_router_group"], f32)[0].reshape(-1),
                               np.asarray(inputs["b_router_expert"], f32)[0].reshape(-1)])[None, :]
    convw = np.asarray(inputs["conv_w"], f32)[0]
    convwT = np.ascontiguousarray(convw.T.reshape(4, 128, 31).transpose(1, 0, 2))
    inv_freq = (10000.0 ** (-np.arange(0, 64, 2, dtype=np.float64) / 64)).astype(np.float32)
    invf = np.tile((inv_freq.astype(np.float64) / (2 * np.pi)).astype(f32)[None, :], (128, 1))
    shared = {
        "w_in": np.ascontiguousarray(np.asarray(inputs["w_in"], f32)[0]),
        "w_o_attn": np.ascontiguousarray(np.asarray(inputs["w_o_attn"], f32)[0]),
        "convw": convwT,
        "convb": fm(np.asarray(inputs["conv_b"])[0], 4),
        "lng": fm(np.asarray(inputs["conv_ln_g"])[0], 4),
        "lnb": fm(np.asarray(inputs["conv_ln_b"])[0], 4),
        "w_conv_out": np.ascontiguousarray(np.asarray(inputs["w_conv_out"], f32)[0]),
        "w_out": np.ascontiguousarray(np.asarray(inputs["w_out"], f32)[0]),
        "w_q_x": np.ascontiguousarray(np.asarray(inputs["w_q_x"], f32)[0]),
        "w_kv_x": np.ascontiguousarray(np.asarray(inputs["w_kv_x"], f32)[0]),
        "w_o_x": np.ascontiguousarray(np.asarray(inputs["w_o_x"], f32)[0]),
        "g_mix": fm(np.asarray(inputs["norm_mix_g"])[0], 8),
        "g_x": fm(np.asarray(inputs["norm_x_g"])[0], 8),
        "g_mem": fm(np.asarray(inputs["norm_mem_g"])[0], 8),
        "g_moe": fm(np.asarray(inputs["norm_moe_g"])[0], 8),
        "w_router": np.ascontiguousarray(w_router),
        "b_router": np.ascontiguousarray(b_router.astype(f32)),
        "w_eg": np.ascontiguousarray(np.asarray(inputs["w_exp_gate"], f32)[0].reshape(32, D, 256)),
        "w_eu": np.ascontiguousarray(np.asarray(inputs["w_exp_up"], f32)[0].reshape(32, D, 256)),
        "w_ed": np.ascontiguousarray(np.asarray(inputs["w_exp_down"], f32)[0].reshape(32, 256, D)),
        "g_final": np.ascontiguousarray(np.asarray(inputs["norm_final_g"], f32).reshape(1, D)),
        "ident": np.eye(128, dtype=f32),
        "invf": invf,
        "pow2": np.tile((2.0 ** -np.arange(BIS_ITERS + 2)).astype(f32)[None, :], (128, 1)),
    }
    maps = []
    for b in range(B):
        m = dict(shared)
        m["x"] = np.ascontiguousarray(x[b])
        m["mem"] = np.ascontiguousarray(mem[b])
        m["pos"] = np.ascontiguousarray(pos[b].reshape(NT, 128).T)
        maps.append(m)
    return maps


def kernel(**inputs):
    maps = make_in_maps(inputs)
    nc = build()
    res = run_bass_kernel_spmd(nc, maps, core_ids=list(range(len(maps))))
    return np.stack([np.asarray(r["out"], np.float32) for r in res.results], axis=0)
```

```python
import bisect
from contextlib import ExitStack

import numpy as np
import concourse.bass as bass
import concourse.mybir as mybir
from concourse.bass_utils import run_bass_kernel_spmd

F32 = mybir.dt.float32
BF16 = mybir.dt.bfloat16
I32 = mybir.dt.int32
AF = mybir.ActivationFunctionType
ALU = mybir.AluOpType
AX = mybir.AxisListType

S = 4096
D = 1024
NT = S // 128
NSB = S // 512
EPS = 1e-6
NEG_BIG = -30000.0
BIS_ITERS = 18
A_OFF = 2120
B_OFF = 2632
G_OFF = 3144
NA_COLS = 3144


class SemBox:
    __slots__ = ("name", "sem", "count")

    def __init__(self, name):
        self.name = name
        self.sem = None
        self.count = 0


class Buf:
    __slots__ = ("name", "last_w", "readers", "box")

    def __init__(self, name, box=None):
        self.name = name
        self.last_w = None
        self.readers = []
        self.box = box if box is not None else SemBox(name)


class Ctx:
    COMPUTE = ("pe", "act", "dve", "pool")

    def __init__(self, nc, es):
        self.nc = nc
        self.es = es
        self.eng = {"pe": nc.tensor, "act": nc.scalar, "dve": nc.vector, "pool": nc.gpsimd, "sp": nc.sync}
        self.sem = {e: es.enter_context(nc.semaphore("s_" + e)) for e in self.COMPUTE}
        self.mile = {e: 0 for e in self.COMPUTE}
        self.nissued = {e: 0 for e in self.eng}
        self.sigpts = {e: ([], []) for e in self.COMPUTE}
        self.last_ins = {e: None for e in self.eng}
        self.last_sig = {e: True for e in self.eng}
        self.waited = {}
        self.dma_sems = []
        self.all_bufs = []

    def buf(self, name):
        b = Buf(name)
        self.all_bufs.append(b)
        return b

    def bufs(self, name, n, share=False):
        if not share:
            return [self.buf("%s%d" % (name, i)) for i in range(n)]
        box = SemBox(name)
        out = []
        for i in range(n):
            b = Buf("%s%d" % (name, i), box)
            self.all_bufs.append(b)
            out.append(b)
        return out

    def _resolve(self, tok):
        if tok[0] == "d":
            return tok[1], tok[2]
        _, e, idx = tok
        idxs, miles = self.sigpts[e]
        k = bisect.bisect_left(idxs, idx)
        if k < len(idxs):
            return self.sem[e], miles[k]
        assert not self.last_sig[e]
        self.last_ins[e].then_inc(self.sem[e], 1)
        self.mile[e] += 1
        idxs.append(self.nissued[e] - 1)
        miles.append(self.mile[e])
        self.last_sig[e] = True
        return self.sem[e], self.mile[e]

    def _wait(self, engname, toks):
        need = {}
        for tok in toks:
            if tok is None:
                continue
            if tok[0] == "c" and tok[1] == engname and engname == "pe":
                continue
            sem, val = self._resolve(tok)
            key = id(sem)
            if key not in need or need[key][1] < val:
                need[key] = (sem, val)
        for key, (sem, val) in need.items():
            wk = (engname, key)
            if self.waited.get(wk, 0) >= val:
                continue
            self.eng[engname].wait_ge(sem, val)
            self.waited[wk] = val

    def _deps(self, engname, reads, writes, waw=True):
        toks = []
        for b in reads:
            toks.append(b.last_w)
        for b in writes:
            if waw:
                if not (b.last_w is not None and b.last_w[0] == "c" and b.last_w[1] == engname):
                    toks.append(b.last_w)
            for r in b.readers:
                if r[0] == "c" and r[1] == engname and engname == "pe":
                    continue
                toks.append(r)
        return toks

    def op(self, engname, method, reads=(), writes=(), sig=None, **kw):
        assert engname in self.COMPUTE
        if sig is None:
            sig = engname != "pe"
        self._wait(engname, self._deps(engname, reads, writes))
        ins = getattr(self.eng[engname], method)(**kw)
        idx = self.nissued[engname]
        self.nissued[engname] += 1
        self.last_ins[engname] = ins
        self.last_sig[engname] = False
        if sig:
            ins.then_inc(self.sem[engname], 1)
            self.mile[engname] += 1
            self.sigpts[engname][0].append(idx)
            self.sigpts[engname][1].append(self.mile[engname])
            self.last_sig[engname] = True
        tok = ("c", engname, idx)
        for b in reads:
            b.readers.append(tok)
        for b in writes:
            b.last_w = tok
            b.readers = []
        return ins

    def dma(self, q, out, in_, reads, writes, waw=False, sem_from=None, **kw):
        assert len(writes) == 1
        wb = (sem_from if sem_from is not None else writes[0]).box
        if wb.sem is None:
            wb.sem = self.es.enter_context(self.nc.semaphore("d_" + wb.name))
            self.dma_sems.append(wb)
        self._wait(q, self._deps(q, reads, writes, waw=waw))
        ins = self.eng[q].dma_start(out=out, in_=in_, **kw)
        ins.then_inc(wb.sem, 16)
        wb.count += 16
        self.nissued[q] += 1
        if q in self.COMPUTE:
            self.last_ins[q] = ins
            self.last_sig[q] = True
        tok = ("d", wb.sem, wb.count)
        for b in reads:
            b.readers.append(tok)
        writes[0].last_w = tok
        writes[0].readers = []
        return ins

    def barrier(self):
        toks = []
        for e in self.COMPUTE:
            if self.nissued[e] > 0 and self.last_ins[e] is not None:
                if not self.last_sig[e]:
                    toks.append(("c", e, self.nissued[e] - 1))
                else:
                    idxs, miles = self.sigpts[e]
                    if idxs:
                        toks.append(("c", e, idxs[-1]))
        for b in self.dma_sems:
            toks.append(("d", b.sem, b.count))
        for e in list(self.COMPUTE) + ["sp"]:
            self._wait(e, toks)

    def final_wait(self, q, bufs):
        self._wait(q, [("d", b.box.sem, b.box.count) for b in bufs if b.box.sem is not None])


def bc(ap, shape):
    return ap.broadcast_to(list(shape))


def build(stop_after="all", dbg=(), a2_tiles=None):
    nc = bass.Bass("TRN2", target_bir_lowering=False)

    def din(name, shape, dt=F32):
        return nc.dram_tensor(name, list(shape), dt, kind="ExternalInput").ap()

    x_d = din("x", [S, D])
    mem_d = din("mem", [256, D])
    pos_d = din("pos", [128, NT], I32)
    w_in_d = din("w_in", [D, 5192])
    w_o_attn_d = din("w_o_attn", [512, D])
    convw_d = din("convw", [128, 4, 31])
    convb_d = din("convb", [128, 4])
    lng_d = din("lng", [128, 4])
    lnb_d = din("lnb", [128, 4])
    w_conv_out_d = din("w_conv_out", [512, D])
    w_out_d = din("w_out", [D, D])
    w_q_x_d = din("w_q_x", [D, D])
    w_kv_x_d = din("w_kv_x", [D, 2 * D])
    w_o_x_d = din("w_o_x", [D, D])
    g_mix_d = din("g_mix", [128, 8])
    g_x_d = din("g_x", [128, 8])
    g_mem_d = din("g_mem", [128, 8])
    g_moe_d = din("g_moe", [128, 8])
    w_router_d = din("w_router", [D, 36])
    b_router_d = din("b_router", [1, 36])
    w_eg_d = din("w_eg", [32, D, 256])
    w_eu_d = din("w_eu", [32, D, 256])
    w_ed_d = din("w_ed", [32, 256, D])
    g_final_d = din("g_final", [1, D])
    ident_d = din("ident", [128, 128])
    invf_d = din("invf", [128, 32])
    pow2_d = din("pow2", [128, BIS_ITERS + 2])

    out_d = nc.dram_tensor("out", [S, D], F32, kind="ExternalOutput").ap()

    def dscr(name, shape, dt):
        return nc.dram_tensor(name, list(shape), dt, kind="Internal").ap()

    qT_scr = dscr("qT_scr", [NT, 128, 512], BF16)
    qiT_scr = dscr("qiT_scr", [NT, 128, 512], BF16)
    z_scr = dscr("z_scr", [NSB, 128, 2048], BF16)
    ao_scr = dscr("ao_scr", [NT, 128, 512], BF16)
    x2_scr = dscr("x2_scr", [S, D], F32)

    dbg_out = {}
    for name, shape, dt in dbg:
        dbg_out[name] = nc.dram_tensor("dbg_" + name, list(shape), dt, kind="ExternalOutput").ap()

    with ExitStack() as es:
        C = Ctx(nc, es)
        out_b = C.buf("out")

        P0 = ExitStack()
        es.enter_context(P0)

        def sb(stack, name, shape, dt):
            return stack.enter_context(nc.sbuf_tensor("sb_" + name, list(shape), dt))

        def ps(stack, name, shape, dt):
            return stack.enter_context(nc.psum_tensor("ps_" + name, list(shape), dt))

        identf = sb(P0, "identf", [128, 128], F32)
        identb = sb(P0, "identb", [128, 128], BF16)
        onesf = sb(P0, "onesf", [128, 128], F32)
        onesb = sb(P0, "onesb", [128, 128], BF16)
        b_const = C.buf("const")
        C.dma("sp", identf[:], ident_d[:, :], [], [b_const])
        C.op("dve", "tensor_copy", [b_const], [b_const], out=identb[:], in_=identf[:])
        C.op("dve", "memset", [], [b_const], ap=onesf[:], constant=1.0 / 512.0)
        C.op("dve", "memset", [], [b_const], ap=onesb[:], constant=1.0)

        PA = ExitStack()
        es.enter_context(PA)
        kT = sb(PA, "kT", [128, 4, S], BF16)
        v_sb = sb(PA, "v_sb", [128, NT, 8, 65], BF16)
        kiT2 = sb(PA, "kiT2", [128, S], BF16)
        wi_sb = sb(PA, "wi_sb", [128, NT, 8], F32)
        kT_b = C.bufs("kT", NT)
        v_b = C.bufs("v", NT)
        kiT_b = C.bufs("kiT", NT)
        wi_b = C.bufs("wi", NT)
        for i in range(NT):
            C.op("pool", "memset", [], [v_b[i]], ap=v_sb[:, i, :, 64:65], constant=1.0)

        with ExitStack() as A1:
            w_sb = sb(A1, "w_inA", [128, 8, NA_COLS], BF16)
            w_b = [C.buf("w_inA")] * 8
            for kc in range(8):
                for (c0, c1) in ((0, 1024), (1024, 2048), (2048, NA_COLS)):
                    C.dma("pool", w_sb[:, kc, c0:c1], w_in_d[kc * 128:(kc + 1) * 128, c0:c1], [], [w_b[kc]])
            gfm = sb(A1, "gfm", [128, 8], F32)
            cwT = sb(A1, "cwT", [128, 4, 31], F32)
            cb4 = sb(A1, "cb4", [128, 4], F32)
            lng4 = sb(A1, "lng4", [128, 4], F32)
            lnb4 = sb(A1, "lnb4", [128, 4], F32)
            invf = sb(A1, "invf", [128, 32], F32)
            posi = sb(A1, "posi", [128, NT], I32)
            posf = sb(A1, "posf", [128, NT], F32)
            b_small = C.buf("smallA")
            C.dma("sp", gfm[:], g_mix_d[:, :], [], [b_small])
            C.dma("sp", cwT[:], convw_d[:, :, :], [], [b_small])
            C.dma("sp", cb4[:], convb_d[:, :], [], [b_small])
            C.dma("sp", lng4[:], lng_d[:, :], [], [b_small])
            C.dma("sp", lnb4[:], lnb_d[:, :], [], [b_small])
            C.dma("sp", invf[:], invf_d[:, :], [], [b_small])
            C.dma("sp", posi[:], pos_d[:, :], [], [b_small])
            cosT = sb(A1, "cosT", [128, NT, 32], F32)
            sinT = sb(A1, "sinT", [128, NT, 32], F32)
            with ExitStack() as T0:
                ua = sb(T0, "ua", [128, NT, 32], F32)
                ub = sb(T0, "ub", [128, NT, 32], F32)
                uci = sb(T0, "uci", [128, NT, 32], I32)
                b_rope = C.buf("ropetab")
                b_ua = C.buf("ua")
                b_ub = C.buf("ub")
                b_uc = C.buf("uc")
                C.op("dve", "tensor_copy", [b_small], [b_ua], out=posf[:], in_=posi[:])
                C.op("dve", "tensor_tensor", [b_ua, b_small], [b_ub], out=ua[:],
                     in0=bc(posf[:, :].unsqueeze(2), [128, NT, 32]), in1=bc(invf[:, :].unsqueeze(1), [128, NT, 32]), op=ALU.mult)
                for (tab, shift) in ((sinT, 0.0), (cosT, 0.25)):
                    C.op("dve", "tensor_scalar", [b_ub], [b_ua], out=ub[:], in0=ua[:], scalar1=shift, scalar2=None, op0=ALU.add)
                    C.op("dve", "tensor_copy", [b_ua], [b_uc], out=uci[:], in_=ub[:])
                    C.op("dve", "tensor_copy", [b_uc], [b_rope], out=tab[:], in_=uci[:])
                    C.op("dve", "tensor_tensor", [b_ua, b_rope], [b_ua], out=ub[:], in0=ub[:], in1=tab[:], op=ALU.subtract)
                    C.op("dve", "tensor_scalar", [b_ua], [b_rope], out=tab[:], in0=ub[:], scalar1=0.5, scalar2=None, op0=ALU.is_gt)
                    C.op("dve", "tensor_tensor", [b_ua, b_rope], [b_ua], out=ub[:], in0=ub[:], in1=tab[:], op=ALU.subtract)
                    C.op("dve", "tensor_scalar", [b_ua], [b_rope], out=tab[:], in0=ub[:], scalar1=-0.5, scalar2=None, op0=ALU.is_lt)
                    C.op("dve", "tensor_tensor", [b_ua, b_rope], [b_ua], out=ub[:], in0=ub[:], in1=tab[:], op=ALU.add)
                    C.op("act", "activation", [b_ua], [b_rope], out=tab[:], in_=ub[:], func=AF.Sin, scale=2.0 * np.pi)

                C.barrier()

            xt = [sb(A1, "xt%d" % j, [128, D], F32) for j in range(2)]
            xt_b = C.bufs("xt", 2)
            hn = [sb(A1, "hn%d" % j, [128, D], BF16) for j in range(2)]
            hn_b = C.bufs("hn", 2)
            st = sb(A1, "stat", [128, 8], F32)
            st_b = C.buf("stat")
            hT = sb(A1, "hT", [128, 8, 512], BF16)
            hT_b = C.bufs("hT", 4)
            rt = [sb(A1, "rt%d" % j, [128, 8, 32], F32) for j in range(4)]
            rt_b = C.bufs("rt", 4)
            rq = [sb(A1, "rq%d" % j, [128, 8, 64], BF16) for j in range(2)]
            rq_b = C.bufs("rq", 2)
            rqk = sb(A1, "rqk", [128, 2, 64], BF16)
            rqk_b = C.buf("rqk")
            qst = [sb(A1, "qst%d" % j, [128, 4, 128], BF16) for j in range(2)]
            qst_b = C.bufs("qst", 2)
            uT = [sb(A1, "uT%d" % j, [128, 4, 542], BF16) for j in range(2)]
            uT_b = [C.bufs("uT%d_" % j, 4) for j in range(2)]
            uTpad_b = C.bufs("uTpad", 2)
            sg = sb(A1, "sg", [128, 512], F32)
            sg_b = C.buf("sg")
            Dw = sb(A1, "Dw", [128, 31, 128], BF16)
            Dw_b = C.buf("Dw")
            co = sb(A1, "co", [128, 4, 512], F32)
            co_b = C.bufs("co", 4)
            sq = [sb(A1, "sq%d" % j, [128, 512], F32) for j in range(2)]
            sq_b = C.bufs("sq", 2)
            mean_sb = sb(A1, "mean_sb", [128, 512], F32)
            m2_sb = sb(A1, "m2_sb", [128, 512], F32)
            rstd_sb = sb(A1, "rstd_sb", [128, 512], F32)
            mean_b = C.buf("mean")
            m2_b = C.buf("m2")
            rstd_b = C.buf("rstdc")
            dtmp = [sb(A1, "dtmp%d" % j, [128, 512], F32) for j in range(2)]
            dtmp_b = C.bufs("dtmp", 2)
            zT = sb(A1, "zT", [128, 4, 512], BF16)
            zT_b = C.buf("zT")
            pA = ps(A1, "pA", [128, 512], F32)
            pB = ps(A1, "pB", [128, 512], F32)
            pC = ps(A1, "pC", [128, 512], F32)
            pM = ps(A1, "pM", [128, 512], F32)
            pE = ps(A1, "pE", [128, 512], F32)
            pT0 = ps(A1, "pT0", [128, 512], F32)
            pT1 = ps(A1, "pT1", [128, 512], F32)
            pTP = ps(A1, "pTP", [128, 1024], BF16)
            pA_b, pB_b, pC_b, pM_b, pE_b, pTP_b = (C.buf(n) for n in ("pA", "pB", "pC", "pM", "pE", "pTP"))
            pT = [pT0, pT1]
            pT_b = C.bufs("pT", 2)
            q_scr_b = C.bufs("qscr", NT, share=True)
            qi_scr_b = C.bufs("qiscr", NT, share=True)
            z_scr_b = C.bufs("zscr", NSB, share=True)

            C.op("pool", "memset", [], [uTpad_b[0]], ap=uT[0][:, :, 0:30], constant=0.0)
            tmc = [0]

            def rope_block(psv, nh, i, dst, dst_bufs, dup=False):
                cos_b = bc(cosT[:, i, :].unsqueeze(1), [128, nh, 32])
                sin_b = bc(sinT[:, i, :].unsqueeze(1), [128, nh, 32])
                x1 = psv[:, :, 0:32]
                x2 = psv[:, :, 32:64]
                pb = psv_buf[0]
                C.op("dve", "tensor_tensor", [pb, b_rope], [rt_b[0]], out=rt[0][:, 0:nh, :], in0=x1, in1=cos_b, op=ALU.mult)
                C.op("dve", "tensor_tensor", [pb, b_rope], [rt_b[1]], out=rt[1][:, 0:nh, :], in0=x2, in1=sin_b, op=ALU.mult)
                C.op("dve", "tensor_tensor", [pb, b_rope], [rt_b[2]], out=rt[2][:, 0:nh, :], in0=x2, in1=cos_b, op=ALU.mult)
                C.op("dve", "tensor_tensor", [pb, b_rope], [rt_b[3]], out=rt[3][:, 0:nh, :], in0=x1, in1=sin_b, op=ALU.mult)
                C.op("pool", "tensor_tensor", [rt_b[0], rt_b[1]], dst_bufs, out=dst[:, :, 0:32], in0=rt[0][:, 0:nh, :], in1=rt[1][:, 0:nh, :], op=ALU.subtract)
                C.op("pool", "tensor_tensor", [rt_b[2], rt_b[3]], dst_bufs, out=dst[:, :, 32:64], in0=rt[2][:, 0:nh, :], in1=rt[3][:, 0:nh, :], op=ALU.add)

            psv_buf = [None]

            def chain(i):
                j = i % 2
                C.dma("sp", xt[j][:], x_d[i * 128:(i + 1) * 128, :], [], [xt_b[j]])
                C.op("act", "activation", [xt_b[j]], [hn_b[j], st_b], out=hn[j][:], in_=xt[j][:], func=AF.Square, accum_out=st[:, 0:1])
                C.op("act", "activation", [st_b], [st_b], out=st[:, 1:2], in_=st[:, 0:1], func=AF.Sqrt, scale=1.0 / D, bias=EPS)
                C.op("dve", "reciprocal", [st_b], [st_b], out=st[:, 2:3], in_=st[:, 1:2])
                C.op("act", "activation", [xt_b[j], st_b], [hn_b[j]], out=hn[j][:], in_=xt[j][:], func=AF.Copy, scale=st[:, 2:3])

            def trans(i):
                j = i % 2
                t = i % 4
                for c in range(8):
                    C.op("pe", "transpose", [hn_b[j], b_const], [pTP_b], sig=(c == 7), out=pTP[:, c * 128:(c + 1) * 128],
                         in_=hn[j][:, c * 128:(c + 1) * 128], identity=identb[:])
                C.op("dve", "tensor_tensor", [pTP_b, b_small], [hT_b[t]], out=hT[:, :, t * 128:(t + 1) * 128],
                     in0=pTP[:, :].rearrange("p (c n) -> p c n", c=8), in1=bc(gfm[:, :].unsqueeze(2), [128, 8, 128]), op=ALU.mult)

            pending = []

            def flush():
                while pending:
                    pending.pop(0)()

            chain(0)
            for sbi in range(NSB):
                cur = sbi % 2
                nxt = 1 - cur
                for t in range(4):
                    i = sbi * 4 + t
                    trans(i)
                    if i + 1 < NT:
                        chain(i + 1)
                    for (name, c0, ncols) in (("q", 0, 512), ("k", 512, 512), ("v", 1024, 512), ("qi", 1536, 512), ("kw", 2048, 72)):
                        bk = tmc[0] % 2
                        tmc[0] += 1
                        for kc in range(8):
                            C.op("pe", "matmul", [hT_b[t], w_b[kc]], [pT_b[bk]], sig=(kc == 7), out=pT[bk][:, 0:512],
                                 lhsT=hT[:, kc, t * 128:(t + 1) * 128], rhs=w_sb[:, kc, c0:c0 + 512], start=(kc == 0), stop=(kc == 7))
                        flush()
                        psv_buf[0] = pT_b[bk]
                        if name == "v":
                            C.op("act", "activation", [pT_b[bk]], [v_b[i]], out=v_sb[:, i, :, 0:64],
                                 in_=pT[bk][:, :].rearrange("p (h d) -> p h d", h=8), func=AF.Copy)
                        elif name == "kw":
                            C.op("act", "activation", [pT_b[bk]], [wi_b[i]], out=wi_sb[:, i, :], in_=pT[bk][:, 64:72], func=AF.Copy)
                            psv = pT[bk][:, 0:64].rearrange("p (h d) -> p h d", h=1)
                            rope_block(psv, 1, i, rqk[:, 0:1, :], [rqk_b])
                            C.op("pool", "tensor_copy", [rqk_b], [rqk_b], out=rqk[:, 1:2, :], in_=rqk[:, 0:1, :])

                            def fin_kw(i=i):
                                C.op("pe", "transpose", [rqk_b, b_const], [pTP_b], sig=True, out=pTP[:, 0:128],
                                     in_=rqk[:, :, :].rearrange("p a d -> p (a d)"), identity=identb[:])
                                C.op("act", "activation", [pTP_b], [kiT_b[i]], out=kiT2[:, i * 128:(i + 1) * 128], in_=pTP[:, 0:128], func=AF.Copy)
                            pending.append(fin_kw)
                        else:
                            rj = tmc[0] % 2
                            psv = pT[bk][:, :].rearrange("p (h d) -> p h d", h=8)
                            rope_block(psv, 8, i, rq[rj][:, :, :], [rq_b[rj]])

                            def fin_rope(i=i, rj=rj, name=name, sj=tmc[0] % 2):
                                for c in range(4):
                                    C.op("pe", "transpose", [rq_b[rj], b_const], [pTP_b], sig=(c == 3), out=pTP[:, c * 128:(c + 1) * 128],
                                         in_=rq[rj][:, 2 * c:2 * c + 2, :].rearrange("p a d -> p (a d)"), identity=identb[:])
                                src = pTP[:, 0:512].rearrange("p (c n) -> p c n", c=4)
                                if name == "k":
                                    C.op("act", "activation", [pTP_b], [kT_b[i]], out=kT[:, :, i * 128:(i + 1) * 128], in_=src, func=AF.Copy)
                                else:
                                    C.op("act", "activation", [pTP_b], [qst_b[sj]], out=qst[sj][:, :, :], in_=src, func=AF.Copy)
                                    if name == "q":
                                        C.dma("sp", qT_scr[i].rearrange("p (c n) -> p c n", c=4), qst[sj][:, :, :], [qst_b[sj]], [q_scr_b[i]], sem_from=qst_b[sj])
                                    else:
                                        C.dma("sp", qiT_scr[i].rearrange("p (c n) -> p c n", c=4), qst[sj][:, :, :], [qst_b[sj]], [qi_scr_b[i]], sem_from=qst_b[sj])
                            pending.append(fin_rope)
                for cc in range(4):
                    for jj in range(31):
                        C.op("dve", "tensor_scalar", [b_const, b_small], [Dw_b], out=Dw[:, jj, :], in0=identb[:],
                             scalar1=cwT[:, cc, jj:jj + 1], scalar2=None, op0=ALU.mult)
                    for kc in range(8):
                        C.op("pe", "matmul", hT_b + [w_b[kc]], [pA_b], sig=(kc == 7), out=pA[:, :],
                             lhsT=w_sb[:, kc, A_OFF + cc * 128:A_OFF + (cc + 1) * 128], rhs=hT[:, kc, :], start=(kc == 0), stop=(kc == 7))
                    flush()
                    for kc in range(8):
                        C.op("pe", "matmul", hT_b + [w_b[kc]], [pB_b], sig=(kc == 7), out=pB[:, :],
                             lhsT=w_sb[:, kc, B_OFF + cc * 128:B_OFF + (cc + 1) * 128], rhs=hT[:, kc, :], start=(kc == 0), stop=(kc == 7))
                    C.op("act", "activation", [pB_b], [sg_b], out=sg[:], in_=pB[:, :], func=AF.Sigmoid)
                    C.op("dve", "tensor_tensor", [pA_b, sg_b], [uT_b[cur][cc]], out=uT[cur][:, cc, 30:542], in0=pA[:, :], in1=sg[:], op=ALU.mult)
                    for jj in range(31):
                        C.op("pe", "matmul", [Dw_b, uT_b[cur][cc], uTpad_b[cur]], [pC_b], sig=(jj == 30), out=pC[:, :],
                             lhsT=Dw[:, jj, :], rhs=uT[cur][:, cc, jj:jj + 512], start=(jj == 0), stop=(jj == 30))
                    C.op("act", "activation", [pC_b, b_small], [co_b[cc]], out=co[:, cc, :], in_=pC[:, :], func=AF.Identity, bias=cb4[:, cc:cc + 1])
                    C.op("act", "activation", [co_b[cc]], [sq_b[cc % 2]], out=sq[cc % 2][:], in_=co[:, cc, :], func=AF.Square)
                    C.op("pe", "matmul", [b_const, co_b[cc]], [pM_b], sig=(cc == 3), out=pM[:, :], lhsT=onesf[:], rhs=co[:, cc, :],
                         start=(cc == 0), stop=(cc == 3))
                    C.op("pe", "matmul", [b_const, sq_b[cc % 2]], [pE_b], sig=(cc == 3), out=pE[:, :], lhsT=onesf[:], rhs=sq[cc % 2][:],
                         start=(cc == 0), stop=(cc == 3))
                if sbi + 1 < NSB:
                    C.op("pool", "tensor_copy", uT_b[cur], [uTpad_b[nxt]], out=uT[nxt][:, :, 0:30], in_=uT[cur][:, :, 512:542])
                C.op("act", "activation", [pM_b], [mean_b], out=mean_sb[:], in_=pM[:, :], func=AF.Copy)
                C.op("pool", "tensor_tensor", [mean_b], [m2_b], out=m2_sb[:], in0=mean_sb[:], in1=mean_sb[:], op=ALU.mult)
                C.op("dve", "tensor_tensor", [pE_b, m2_b], [m2_b], out=m2_sb[:], in0=pE[:, :], in1=m2_sb[:], op=ALU.subtract)
                C.op("act", "activation", [m2_b], [m2_b], out=m2_sb[:], in_=m2_sb[:], func=AF.Sqrt, bias=EPS, scale=1.0)
                C.op("dve", "reciprocal", [m2_b], [rstd_b], out=rstd_sb[:], in_=m2_sb[:])
                for cc in range(4):
                    dj = cc % 2
                    C.op("pool", "tensor_tensor", [co_b[cc], mean_b], [dtmp_b[dj]], out=dtmp[dj][:], in0=co[:, cc, :], in1=mean_sb[:], op=ALU.subtract)
                    C.op("pool", "tensor_tensor", [dtmp_b[dj], rstd_b], [dtmp_b[dj]], out=dtmp[dj][:], in0=dtmp[dj][:], in1=rstd_sb[:], op=ALU.mult)
                    C.op("act", "activation", [dtmp_b[dj], b_small], [zT_b], out=zT[:, cc, :], in_=dtmp[dj][:], func=AF.Silu,
                         scale=lng4[:, cc:cc + 1], bias=lnb4[:, cc:cc + 1])
                C.dma("sp", z_scr[sbi].rearrange("p (c n) -> p c n", c=4), zT[:, :, :], [zT_b], [z_scr_b[sbi]], sem_from=zT_b)
            C.barrier()

        if stop_after == "A1":
            if "kT" in dbg_out:
                db = C.buf("dbg")
                C.dma("sp", dbg_out["kT"].rearrange("p (c n) -> p c n", c=4), kT[:, :, :], kT_b, [db])
                C.dma("sp", dbg_out["kiT2"][:, :], kiT2[:, :], kiT_b, [db])
                C.dma("sp", dbg_out["v"].rearrange("p (t h d) -> p t h d", t=NT, h=8), v_sb[:, :, :, :], v_b, [db])
                C.dma("sp", dbg_out["wi"].rearrange("p (t h) -> p t h", t=NT), wi_sb[:, :, :], wi_b, [db])
                C.dma("sp", dbg_out["qT"].rearrange("(t p) n -> p t n", p=128), qT_scr.rearrange("t p n -> p t n"), q_scr_b, [db])
                C.dma("sp", dbg_out["qiT"].rearrange("(t p) n -> p t n", p=128), qiT_scr.rearrange("t p n -> p t n"), qi_scr_b, [db])
                C.dma("sp", dbg_out["z"].rearrange("(t p) n -> p t n", p=128), z_scr.rearrange("t p n -> p t n"), z_scr_b, [db])
                C.final_wait("sp", [db])
            C.barrier()
            C.final_wait("sp", q_scr_b + qi_scr_b + z_scr_b)
            return nc

        with ExitStack() as A2:
            qTz = [[sb(A2, "qTz%d_%d" % (par, j), [128, 4, 128], BF16) for j in range(2)] for par in range(2)]
            qiTz = [[sb(A2, "qiTz%d_%d" % (par, j), [128, 4, 128], BF16) for j in range(2)] for par in range(2)]
            qT_tb = C.bufs("qT_t", 2)
            qiT_tb = C.bufs("qiT_t", 2)
            Dg = sb(A2, "Dg", [128, 8, 128], BF16)
            Dg_b = C.buf("Dg")
            R_sb = [sb(A2, "R_sb%d" % j, [128, 512], BF16) for j in range(2)]
            R_b = C.bufs("R_sb", 2)
            sc = [sb(A2, "sc%d" % j, [128, S], F32) for j in range(2)]
            sc_b = C.bufs("sc", 2)
            NM = [sb(A2, "NM%d" % j, [128, S], BF16) for j in range(2)]
            NM_b = C.bufs("NM", 2)
            bs = sb(A2, "bs", [128, 8], F32)
            bs_b = C.buf("bs")
            wk = sb(A2, "wk", [128, BIS_ITERS + 2], F32)
            pow2 = sb(A2, "pow2", [128, BIS_ITERS + 2], F32)
            thrc = sb(A2, "thrc", [128, 1], F32)
            b_c2 = C.buf("constA2")
            C.dma("sp", pow2[:], pow2_d[:, :], [], [b_c2])
            C.op("pool", "memset", [], [b_c2], ap=thrc[:], constant=-1e29)
            for par in range(2):
                for j in range(2):
                    C.op("dve", "memset", [], [qT_tb[j]], ap=qTz[par][j][:, :, :], constant=0.0)
                    C.op("dve", "memset", [], [qiT_tb[j]], ap=qiTz[par][j][:, :, :], constant=0.0)
            PT = [sb(A2, "PT%d" % j, [128, 512], BF16) for j in range(2)]
            PT_b = C.bufs("PT", 2)
            rden = sb(A2, "rden", [128, 8], F32)
            rden_b = C.buf("rden")
            ao = sb(A2, "ao", [128, 8, 64], BF16)
            ao_b = C.buf("ao")
            aoT_st = [sb(A2, "aoT_st%d" % j, [128, 4, 128], BF16) for j in range(2)]
            aoT_b = C.bufs("aoT_st", 2)
            pR = [ps(A2, "pR%d" % j, [128, 512], F32) for j in range(2)]
            pR_b = C.bufs("pR", 2)
            pSC = ps(A2, "pSC", [128, 512], F32)
            pSC_b = C.buf("pSC")
            pST = [ps(A2, "pST%d" % j, [128, 512], F32) for j in range(2)]
            pST_b = C.bufs("pST", 2)
            pPV = [ps(A2, "pPV%d" % j, [128, 512], F32) for j in range(2)]
            pPV_b = C.bufs("pPV", 2)
            pTP2 = ps(A2, "pTP2", [128, 1024], BF16)
            pTP2_b = C.buf("pTP2")
            ao_scr_b = C.bufs("aoscr", NT, share=True)

            C.barrier()
            tiles = list(range(NT))[::-1] if a2_tiles is None else list(a2_tiles)
            def index_phase(n_i, i):
                s2 = n_i % 2
                NK = 128 * (i + 1)
                nkc = i + 1
                nkb = (NK + 511) // 512
                for par in range(2):
                    pr = slice(par * 64, (par + 1) * 64)
                    C.dma("sp", qTz[par][s2][pr, :, :], qT_scr[i][pr, :].rearrange("p (c n) -> p c n", c=4), [q_scr_b[i]], [qT_tb[s2]])
                    C.dma("sp", qiTz[par][s2][pr, :, :], qiT_scr[i][pr, :].rearrange("p (c n) -> p c n", c=4), [qi_scr_b[i]], [qiT_tb[s2]])
                for h in range(8):
                    C.op("act", "activation", [b_const, wi_b[i]], [Dg_b], out=Dg[:, h, :], in_=identb[:], func=AF.Copy,
                         scale=wi_sb[:, i, h:h + 1])
                for kb in range(nkb):
                    k0 = kb * 512
                    W = min(512, NK - k0)
                    kbufs = kiT_b[k0 // 128:(k0 + W) // 128]

                    def r_mm(h):
                        C.op("pe", "matmul", [qiT_tb[s2]] + kbufs, [pR_b[h % 2]], sig=True, out=pR[h % 2][:, 0:W],
                             lhsT=qiTz[h % 2][s2][:, h // 2, :], rhs=kiT2[:, k0:k0 + W], start=True, stop=True)
                        C.op("act", "activation", [pR_b[h % 2]], [R_b[h % 2]], out=R_sb[h % 2][:, 0:W], in_=pR[h % 2][:, 0:W], func=AF.Relu)

                    def s_mm(h):
                        C.op("pe", "matmul", [Dg_b, R_b[h % 2]], [pSC_b], sig=(h == 7), out=pSC[:, 0:W], lhsT=Dg[:, h, :],
                             rhs=R_sb[h % 2][:, 0:W], start=(h == 0), stop=(h == 7))

                    r_mm(0)
                    for h in range(8):
                        if h + 1 < 8:
                            r_mm(h + 1)
                        s_mm(h)
                    C.op("act", "activation", [pSC_b], [sc_b[s2]], out=sc[s2][:, k0:k0 + W], in_=pSC[:, 0:W], func=AF.Copy)
                C.op("pool", "memset", [], [sc_b[s2]], ap=sc[s2][0:64, NK - 64:NK], constant=-1e30)

            def thresh_phase(n_i, i):
                s2 = n_i % 2
                NK = 128 * (i + 1)
                nkc = i + 1
                nkb = (NK + 511) // 512
                if i >= 2:
                    C.op("dve", "tensor_reduce", [sc_b[s2]], [bs_b], out=bs[:, 0:1], in_=sc[s2][:, 0:NK], axis=AX.X, op=ALU.max)
                    C.op("dve", "tensor_reduce", [sc_b[s2]], [bs_b], out=bs[:, 1:2], in_=sc[s2][:, 0:320], axis=AX.X, op=ALU.min)
                    C.op("dve", "tensor_tensor", [bs_b], [bs_b], out=bs[:, 2:3], in0=bs[:, 0:1], in1=bs[:, 1:2], op=ALU.subtract)
                    C.op("dve", "tensor_scalar", [bs_b, b_c2], [bs_b], out=wk[:], in0=pow2[:], scalar1=bs[:, 2:3], scalar2=None, op0=ALU.mult)
                    C.op("dve", "tensor_tensor", [bs_b], [bs_b], out=bs[:, 3:4], in0=bs[:, 1:2], in1=wk[:, 1:2], op=ALU.add)
                    for k in range(BIS_ITERS):
                        C.op("dve", "tensor_scalar", [sc_b[s2], bs_b], [NM_b[s2], bs_b], out=NM[s2][:, 0:NK], in0=sc[s2][:, 0:NK],
                             scalar1=bs[:, 3:4], scalar2=None, op0=ALU.is_ge, op1=ALU.add, accum_out=bs[:, 4:5])
                        C.op("dve", "tensor_scalar", [bs_b], [bs_b], out=bs[:, 5:6], in0=bs[:, 4:5], scalar1=255.5, scalar2=0.5,
                             op0=ALU.is_ge, op1=ALU.subtract)
                        if k < BIS_ITERS - 1:
                            C.op("dve", "scalar_tensor_tensor", [bs_b], [bs_b], out=bs[:, 3:4], in0=bs[:, 5:6], scalar=wk[:, k + 1:k + 2],
                                 in1=bs[:, 3:4], op0=ALU.mult, op1=ALU.add)
                    C.op("dve", "tensor_scalar", [bs_b], [bs_b], out=bs[:, 5:6], in0=bs[:, 5:6], scalar1=-0.5, scalar2=None, op0=ALU.add)
                    C.op("dve", "scalar_tensor_tensor", [bs_b], [bs_b], out=bs[:, 6:7], in0=bs[:, 5:6], scalar=wk[:, BIS_ITERS:BIS_ITERS + 1],
                         in1=bs[:, 3:4], op0=ALU.mult, op1=ALU.add)
                    thr_ap = bs[:, 6:7]
                    thr_bufs = [bs_b]
                else:
                    thr_ap = thrc[:, 0:1]
                    thr_bufs = [b_c2]
                C.op("dve", "tensor_scalar", [sc_b[s2]] + thr_bufs, [NM_b[s2]], out=NM[s2][:, 0:NK], in0=sc[s2][:, 0:NK],
                     scalar1=thr_ap, scalar2=NEG_BIG, op0=ALU.is_lt, op1=ALU.mult)

            def attn_phase(n_i, i):
                s2 = n_i % 2
                NK = 128 * (i + 1)
                nkc = i + 1
                nkb = (NK + 511) // 512
                items = [(h, kb) for h in range(8) for kb in range(nkb)]

                def st_block(n):
                    h, kb = items[n]
                    sl = n % 2
                    pb = (h % 2) * 64
                    c = h // 2
                    kcs = list(range(kb * 4, min(nkc, kb * 4 + 4)))
                    for kcl, kc in enumerate(kcs):
                        C.op("pe", "matmul", [kT_b[kc], qT_tb[s2]], [pST_b[sl]], out=pST[sl][:, kcl * 128:(kcl + 1) * 128],
                             lhsT=kT[:, c, kc * 128:(kc + 1) * 128], rhs=qTz[h % 2][s2][:, c, :], start=True, stop=False)
                        C.op("pe", "matmul", [NM_b[s2], b_const], [pST_b[sl]], sig=(kcl == len(kcs) - 1),
                             out=pST[sl][:, kcl * 128:(kcl + 1) * 128],
                             lhsT=NM[s2][:, kc * 128:(kc + 1) * 128], rhs=identb[:], start=False, stop=True)
                    C.op("act", "activation", [pST_b[sl]], [PT_b[sl]], out=PT[sl][:, 0:len(kcs) * 128], in_=pST[sl][:, 0:len(kcs) * 128],
                         func=AF.Exp, scale=0.125)

                def pv_block(n):
                    h, kb = items[n]
                    sl = n % 2
                    kcs = list(range(kb * 4, min(nkc, kb * 4 + 4)))
                    for kcl, kc in enumerate(kcs):
                        C.op("pe", "matmul", [PT_b[sl], v_b[kc]], [pPV_b[h // 4]], out=pPV[h // 4][:, (h % 4) * 65:(h % 4) * 65 + 65],
                             lhsT=PT[sl][:, kcl * 128:(kcl + 1) * 128], rhs=v_sb[:, kc, h, :], start=(kc == 0), stop=(kc == nkc - 1))

                st_block(0)
                for n in range(len(items)):
                    if n + 1 < len(items):
                        st_block(n + 1)
                    pv_block(n)
                fs = len(items) % 2
                C.op("pe", "matmul", [b_const], [pST_b[fs]], sig=True, out=pST[fs][:, 0:128], lhsT=identb[:], rhs=identb[:], start=True, stop=True)

            def attn_tail(n_i, i):
                s2 = n_i % 2
                for hh in range(2):
                    pvv = pPV[hh][:, 0:260].rearrange("p (h d) -> p h d", h=4)
                    C.op("dve", "reciprocal", [pPV_b[hh]], [rden_b], out=rden[:, hh * 4:hh * 4 + 4].unsqueeze(2), in_=pvv[:, :, 64:65])
                    C.op("dve", "tensor_tensor", [pPV_b[hh], rden_b], [ao_b], out=ao[:, hh * 4:hh * 4 + 4, :], in0=pvv[:, :, 0:64],
                         in1=bc(rden[:, hh * 4:hh * 4 + 4].unsqueeze(2), [128, 4, 64]), op=ALU.mult)
                for c in range(4):
                    C.op("pe", "transpose", [ao_b, b_const], [pTP2_b], sig=(c == 3), out=pTP2[:, c * 128:(c + 1) * 128],
                         in_=ao[:, 2 * c:2 * c + 2, :].rearrange("p a d -> p (a d)"), identity=identb[:])
                C.op("act", "activation", [pTP2_b], [aoT_b[s2]], out=aoT_st[s2][:, :, :], in_=pTP2[:, 0:512].rearrange("p (c n) -> p c n", c=4), func=AF.Copy)
                C.dma("sp", ao_scr[i].rearrange("p (c n) -> p c n", c=4), aoT_st[s2][:, :, :], [aoT_b[s2]], [ao_scr_b[i]], sem_from=aoT_b[s2])

            nt_ = len(tiles)
            index_phase(0, tiles[0])
            thresh_phase(0, tiles[0])
            if nt_ > 1:
                index_phase(1, tiles[1])
            for n_i in range(nt_):
                attn_phase(n_i, tiles[n_i])
                if n_i + 1 < nt_:
                    thresh_phase(n_i + 1, tiles[n_i + 1])
                if n_i + 2 < nt_:
                    index_phase(n_i + 2, tiles[n_i + 2])
                attn_tail(n_i, tiles[n_i])
            s2 = (nt_ - 1) % 2
            if "nm" in dbg_out:
                dbn = C.buf("dbgnm")
                C.dma("sp", dbg_out["nm"][:, :], NM[s2][:, :], [NM_b[s2]], [dbn])
                C.dma("sp", dbg_out["sc"][:, :], sc[s2][:, :], [sc_b[s2]], [dbn])
                C.final_wait("sp", [dbn])
            C.barrier()

        if stop_after == "A2":
            db = C.buf("dbg2")
            C.dma("sp", dbg_out["ao"].rearrange("(t p) n -> p t n", p=128), ao_scr.rearrange("t p n -> p t n"), ao_scr_b, [db])
            C.final_wait("sp", [db])
            return nc

        PA.close()

        xn_scr = dscr("xn_scr", [NT, 128, 1024], BF16)
        xn_scr_b = C.bufs("xnscr", NT, share=True)
        x2_scr_b = C.bufs("x2scr", NT, share=True)
        PBC = ExitStack()
        es.enter_context(PBC)
        comb = sb(PBC, "comb", [128, NT, 32], F32)
        comb_b = C.bufs("comb", NT)
        lgall = sb(PBC, "lgall", [128, NT, 36], F32)
        lgall_b = C.buf("lgall")

        def load_w(stack, name, src, rows, c0, c1, bufname, defer=False):
            nkc = rows // 128
            t = sb(stack, name, [128, nkc, c1 - c0], BF16)
            b = C.buf(bufname)

            def issue():
                for kc in range(nkc):
                    C.dma("pool", t[:, kc, :], src[kc * 128:(kc + 1) * 128, c0:c1], [], [b])
            if defer:
                return t, b, issue
            issue()
            return t, b

        def rms_rstd(x_ap, x_bufs, junk_ap, junk_bufs, st, st_b, scale_n=D):
            C.op("act", "activation", x_bufs, junk_bufs + [st_b], out=junk_ap, in_=x_ap, func=AF.Square, accum_out=st[:, 0:1])
            C.op("act", "activation", [st_b], [st_b], out=st[:, 1:2], in_=st[:, 0:1], func=AF.Sqrt, scale=1.0 / scale_n, bias=EPS)
            C.op("dve", "reciprocal", [st_b], [st_b], out=st[:, 2:3], in_=st[:, 1:2])

        with ExitStack() as B:
            wg_sb, wg_b, ld_wg = load_w(B, "w_gates", w_in_d, D, G_OFF, G_OFF + 2048, "w_gates", defer=True)
            woa_sb, woa_b, ld_woa = load_w(B, "w_oa", w_o_attn_d, 512, 0, D, "w_oa", defer=True)
            wco_sb, wco_b, ld_wco = load_w(B, "w_co", w_conv_out_d, 512, 0, D, "w_co", defer=True)
            wout_sb, wout_b, ld_wout = load_w(B, "w_outb", w_out_d, D, 0, D, "w_outb", defer=True)
            wqx_sb, wqx_b, ld_wqx = load_w(B, "w_qxb", w_q_x_d, D, 0, D, "w_qxb", defer=True)
            wox_sb, wox_b, ld_wox = load_w(B, "w_oxb", w_o_x_d, D, 0, D, "w_oxb", defer=True)
            kmT = sb(B, "kmT", [128, 8, 256], BF16)
            vm = sb(B, "vm", [128, 2, D], BF16)
            kmT_b = C.buf("kmT")
            vm_b = C.buf("vm")
            gfm = sb(B, "gfmB", [128, 8], F32)
            gxm = sb(B, "gxm", [128, 8], F32)
            gmm = sb(B, "gmm", [128, 8], F32)
            gmo = sb(B, "gmo", [128, 8], F32)
            wr_sb = sb(B, "wr_sb", [128, 8, 36], F32)
            br_sb = sb(B, "br_sb", [128, 36], F32)
            b_smB = C.buf("smallB")
            C.dma("sp", gfm[:], g_mix_d[:, :], [], [b_smB])
            C.dma("sp", gxm[:], g_x_d[:, :], [], [b_smB])
            C.dma("sp", gmm[:], g_mem_d[:, :], [], [b_smB])
            C.dma("sp", gmo[:], g_moe_d[:, :], [], [b_smB])
            C.dma("sp", wr_sb[:, :, :], w_router_d.rearrange("(c p) n -> p c n", p=128), [], [b_smB])
            C.dma("sp", br_sb[:], bc(b_router_d[0:1, :], [128, 36]), [], [b_smB])
            stB = sb(B, "statB", [128, 8], F32)
            stB_b = C.buf("statB")
            pb = [ps(B, "pb%d" % j, [128, 512], F32) for j in range(7)]
            pb_b = C.bufs("pb", 7)
            pTPb = ps(B, "pTPb", [128, 1024], BF16)
            pTPb_b = C.buf("pTPb")

            sqj_ref = []

            def norm_chain(x_ap, x_bufs, hn_ap, hn_bufs):
                if sqj_ref:
                    rms_rstd(x_ap, x_bufs, sqj_ref[0][:, :], [sqj_ref[1]], stB, stB_b)
                else:
                    rms_rstd(x_ap, x_bufs, hn_ap, hn_bufs, stB, stB_b)
                C.op("act", "activation", x_bufs + [stB_b], hn_bufs, out=hn_ap, in_=x_ap, func=AF.Copy, scale=stB[:, 2:3])

            def norm_trans(hn_ap, hn_bufs, g_sb, dstT, dst_bufs, col0):
                for c in range(8):
                    C.op("pe", "transpose", hn_bufs + [b_const], [pTPb_b], sig=(c == 7), out=pTPb[:, c * 128:(c + 1) * 128],
                         in_=hn_ap[:, c * 128:(c + 1) * 128], identity=identb[:])
                C.op("dve", "tensor_tensor", [pTPb_b, b_smB], dst_bufs, out=dstT[:, :, col0:col0 + 128],
                     in0=pTPb[:, :].rearrange("p (c n) -> p c n", c=8), in1=bc(g_sb[:, :].unsqueeze(2), [128, 8, 128]), op=ALU.mult)

            def norm_T(x_ap, x_bufs, hn_ap, hn_bufs, g_sb, dstT, dst_bufs, col0):
                norm_chain(x_ap, x_bufs, hn_ap, hn_bufs)
                norm_trans(hn_ap, hn_bufs, g_sb, dstT, dst_bufs, col0)

            with ExitStack() as BK:
                wkv_sb, wkv_b = load_w(BK, "w_kvb", w_kv_x_d, D, 0, 2 * D, "w_kvb")
                for ld in (ld_wg, ld_woa, ld_wco, ld_wout, ld_wqx, ld_wox):
                    ld()
                memt2 = [sb(BK, "memt%d" % j, [128, D], F32) for j in range(2)]
                memt2_b = C.bufs("memt", 2)
                memh = sb(BK, "memh", [128, D], BF16)
                memh_b = C.buf("memh")
                memnT = sb(BK, "memnT", [128, 8, 256], BF16)
                memnT_b = C.buf("memnT")
                for mt in range(2):
                    C.dma("sp", memt2[mt][:], mem_d[mt * 128:(mt + 1) * 128, :], [], [memt2_b[mt]])
                for mt in range(2):
                    norm_T(memt2[mt][:, :], [memt2_b[mt]], memh[:, :], [memh_b], gmm, memnT, [memnT_b], mt * 128)
                for jx in range(8):
                    bk = jx % 2
                    for kc in range(8):
                        C.op("pe", "matmul", [wkv_b, memnT_b], [pb_b[bk]], sig=(kc == 7), out=pb[bk][:, 0:256],
                             lhsT=wkv_sb[:, kc, jx * 128:(jx + 1) * 128], rhs=memnT[:, kc, :], start=(kc == 0), stop=(kc == 7))
                    C.op("act", "activation", [pb_b[bk]], [kmT_b], out=kmT[:, jx, :], in_=pb[bk][:, 0:256], func=AF.Copy)
                for mc in range(2):
                    for cb in range(2):
                        bk = 2 + (mc * 2 + cb) % 2
                        for kc in range(8):
                            C.op("pe", "matmul", [wkv_b, memnT_b], [pb_b[bk]], sig=(kc == 7), out=pb[bk][:, :],
                                 lhsT=memnT[:, kc, mc * 128:(mc + 1) * 128], rhs=wkv_sb[:, kc, D + cb * 512:D + (cb + 1) * 512],
                                 start=(kc == 0), stop=(kc == 7))
                        C.op("act", "activation", [pb_b[bk]], [vm_b], out=vm[:, mc, cb * 512:(cb + 1) * 512], in_=pb[bk][:, :], func=AF.Copy)
                C.barrier()

            xt4 = sb(B, "xt4", [128, 4, D], F32)
            xt4_b = C.bufs("xt4", 4)
            hnB = [sb(B, "hnB%d" % j, [128, D], BF16) for j in range(4)]
            hnB_b = C.bufs("hnB", 4)
            sqj = sb(B, "sqj", [128, D], BF16)
            sqj_b = C.buf("sqj")
            sqj_ref.extend([sqj, sqj_b])
            bufA = sb(B, "bufA", [128, 8, 512], BF16)
            bufA_b = C.bufs("bufA", 4)
            bufB = sb(B, "bufB", [128, 8, 512], BF16)
            bufB_b = C.bufs("bufB", 8)
            bufC = sb(B, "bufC", [128, 8, 512], BF16)
            bufC_b = C.bufs("bufC", 8)
            aoTb = sb(B, "aoTb", [128, 4, 512], BF16)
            aoTb_b = C.buf("aoTb")
            zTb = sb(B, "zTb", [128, 4, 512], BF16)
            zTb_b = C.buf("zTb")
            sgA = sb(B, "sgA", [128, 512], F32)
            sgB = sb(B, "sgB", [128, 512], F32)
            sgA_b = C.buf("sgA")
            sgB_b = C.buf("sgB")
            PT2 = sb(B, "PT2", [128, 2, 512], BF16)
            PT2_b = C.bufs("PT2", 2)
            rden2 = sb(B, "rden2", [128, 512], F32)
            rden2_b = C.buf("rden2")
            xn32s = [sb(B, "xn32_%d" % j, [128, D], F32) for j in range(2)]
            xn32s_b = C.bufs("xn32", 2)
            xnT32 = sb(B, "xnT32", [128, 8, 128], F32)
            xnT32_b = C.buf("xnT32")
            xnTb = [sb(B, "xnTb%d" % j, [128, 8, 128], BF16) for j in range(2)]
            xnTb_b = C.bufs("xnTb", 2)
            rt_ = sb(B, "rtr", [128, 128], F32)
            rt_b2 = C.buf("rtr")

            def load_x(blk, t):
                i = blk * 4 + t
                C.dma("sp", xt4[:, t, :], x_d[i * 128:(i + 1) * 128, :], [], [xt4_b[t]])

            def load_aoz(blk):
                for t in range(4):
                    i = blk * 4 + t
                    C.dma("sp", aoTb[:, :, t * 128:(t + 1) * 128], ao_scr[i].rearrange("p (c n) -> p c n", c=4), [ao_scr_b[i]], [aoTb_b])
                C.dma("sp", zTb[:, :, :], z_scr[blk].rearrange("p (c n) -> p c n", c=4), [z_scr_b[blk]], [zTb_b])

            def pipelined_norms(g_sb, ready=None):
                def ch(t):
                    norm_chain(xt4[:, t, :], [xt4_b[t]], hnB[t][:, :], [hnB_b[t]])

                def tr(t):
                    norm_trans(hnB[t][:, :], [hnB_b[t]], g_sb, bufA, [bufA_b[t]], t * 128)
                return ch, tr

            for t in range(4):
                load_x(0, t)
            load_aoz(0)
            for blk in range(NSB):
                ch, tr = pipelined_norms(gfm)
                for t in range(4):
                    ch(t)
                for t in range(4):
                    tr(t)
                for fo in range(8):
                    fs_ = slice(fo * 128, (fo + 1) * 128)
                    b0, b1, b2 = (0, 1, 2) if fo % 2 == 0 else (4, 5, 6)
                    for c in range(4):
                        C.op("pe", "matmul", [woa_b, aoTb_b], [pb_b[b0]], sig=(c == 3), out=pb[b0][:, :], lhsT=woa_sb[:, c, fs_], rhs=aoTb[:, c, :],
                             start=(c == 0), stop=(c == 3))
                    for c in range(4):
                        C.op("pe", "matmul", [wco_b, zTb_b], [pb_b[b1]], sig=(c == 3), out=pb[b1][:, :], lhsT=wco_sb[:, c, fs_], rhs=zTb[:, c, :],
                             start=(c == 0), stop=(c == 3))
                    for kc in range(8):
                        C.op("pe", "matmul", [wg_b] + bufA_b, [pb_b[b2]], sig=(kc == 7), out=pb[b2][:, :], lhsT=wg_sb[:, kc, fs_], rhs=bufA[:, kc, :],
                             start=(kc == 0), stop=(kc == 7))
                    for kc in range(8):
                        C.op("pe", "matmul", [wg_b] + bufA_b, [pb_b[3]], sig=(kc == 7), out=pb[3][:, :],
                             lhsT=wg_sb[:, kc, D + fo * 128:D + (fo + 1) * 128], rhs=bufA[:, kc, :], start=(kc == 0), stop=(kc == 7))
                    C.op("act", "activation", [pb_b[b2]], [sgA_b], out=sgA[:], in_=pb[b2][:, :], func=AF.Sigmoid)
                    C.op("act", "activation", [pb_b[3]], [sgB_b], out=sgB[:], in_=pb[3][:, :], func=AF.Sigmoid)
                    C.op("dve", "tensor_tensor", [pb_b[b0], sgA_b], [sgA_b], out=sgA[:], in0=pb[b0][:, :], in1=sgA[:], op=ALU.mult)
                    C.op("dve", "tensor_tensor", [pb_b[b1], sgB_b], [sgB_b], out=sgB[:], in0=pb[b1][:, :], in1=sgB[:], op=ALU.mult)
                    C.op("pool", "tensor_tensor", [sgA_b, sgB_b], [bufB_b[fo]], out=bufB[:, fo, :], in0=sgA[:], in1=sgB[:], op=ALU.add)
                if blk + 1 < NSB:
                    load_aoz(blk + 1)
                ch, tr = pipelined_norms(gxm)
                for t in range(4):
                    if t >= 2:
                        tr(t - 2)
                    for cb in range(2):
                        bk = 4 + (t * 2 + cb) % 2
                        for kc in range(8):
                            C.op("pe", "matmul", [wout_b, bufB_b[kc]], [pb_b[bk]], sig=(kc == 7), out=pb[bk][:, :],
                                 lhsT=bufB[:, kc, t * 128:(t + 1) * 128], rhs=wout_sb[:, kc, cb * 512:(cb + 1) * 512], start=(kc == 0), stop=(kc == 7))
                        C.op("dve", "tensor_tensor", [pb_b[bk], xt4_b[t]], [xt4_b[t]], out=xt4[:, t, cb * 512:(cb + 1) * 512], in0=pb[bk][:, :],
                             in1=xt4[:, t, cb * 512:(cb + 1) * 512], op=ALU.add)
                    ch(t)
                tr(2)
                tr(3)
                for fo in range(8):
                    bk = 4 + fo % 2
                    for kc in range(8):
                        C.op("pe", "matmul", [wqx_b] + bufA_b, [pb_b[bk]], sig=(kc == 7), out=pb[bk][:, :],
                             lhsT=wqx_sb[:, kc, fo * 128:(fo + 1) * 128], rhs=bufA[:, kc, :], start=(kc == 0), stop=(kc == 7))
                    C.op("act", "activation", [pb_b[bk]], [bufB_b[fo]], out=bufB[:, fo, :], in_=pb[bk][:, :], func=AF.Copy)
                for h in range(4):
                    for mc in range(2):
                        for dc in range(2):
                            C.op("pe", "matmul", [kmT_b, bufB_b[h * 2 + dc]], [pb_b[mc]], sig=(dc == 1), out=pb[mc][:, :],
                                 lhsT=kmT[:, h * 2 + dc, mc * 128:(mc + 1) * 128], rhs=bufB[:, h * 2 + dc, :], start=(dc == 0), stop=(dc == 1))
                        C.op("act", "activation", [pb_b[mc]], [PT2_b[mc]], out=PT2[:, mc, :], in_=pb[mc][:, :], func=AF.Exp, scale=1.0 / 16.0)
                    for mc in range(2):
                        C.op("pe", "matmul", [b_const, PT2_b[mc]], [pb_b[2]], sig=(mc == 1), out=pb[2][:, :], lhsT=onesb[:], rhs=PT2[:, mc, :],
                             start=(mc == 0), stop=(mc == 1))
                    C.op("dve", "reciprocal", [pb_b[2]], [rden2_b], out=rden2[:], in_=pb[2][:, :])
                    for dvc in range(2):
                        bk = 3 if dvc == 0 else 6
                        for mc in range(2):
                            C.op("pe", "matmul", [vm_b, PT2_b[mc]], [pb_b[bk]], sig=(mc == 1), out=pb[bk][:, :],
                                 lhsT=vm[:, mc, h * 256 + dvc * 128:h * 256 + (dvc + 1) * 128], rhs=PT2[:, mc, :], start=(mc == 0), stop=(mc == 1))
                        C.op("dve", "tensor_tensor", [pb_b[bk], rden2_b], [bufC_b[h * 2 + dvc]], out=bufC[:, h * 2 + dvc, :], in0=pb[bk][:, :],
                             in1=rden2[:], op=ALU.mult)
                def wox(t):
                    i = blk * 4 + t
                    for cb in range(2):
                        bk = 4 + (t * 2 + cb) % 2
                        for kc in range(8):
                            C.op("pe", "matmul", [wox_b, bufC_b[kc]], [pb_b[bk]], sig=(kc == 7), out=pb[bk][:, :],
                                 lhsT=bufC[:, kc, t * 128:(t + 1) * 128], rhs=wox_sb[:, kc, cb * 512:(cb + 1) * 512], start=(kc == 0), stop=(kc == 7))
                        C.op("dve", "tensor_tensor", [pb_b[bk], xt4_b[t]], [xt4_b[t]], out=xt4[:, t, cb * 512:(cb + 1) * 512], in0=pb[bk][:, :],
                             in1=xt4[:, t, cb * 512:(cb + 1) * 512], op=ALU.add)
                    C.dma("sp", x2_scr[i * 128:(i + 1) * 128, :], xt4[:, t, :], [xt4_b[t]], [x2_scr_b[i]], sem_from=xt4_b[t])
                    xn32, xn32_b = xn32s[t % 2], xn32s_b[t % 2]
                    rms_rstd(xt4[:, t, :], [xt4_b[t]], sqj[:, :], [sqj_b], stB, stB_b)
                    C.op("act", "activation", [xt4_b[t], stB_b], [xn32_b], out=xn32[:, :], in_=xt4[:, t, :], func=AF.Copy, scale=stB[:, 2:3])

                def b6(t):
                    i = blk * 4 + t
                    xn32, xn32_b = xn32s[t % 2], xn32s_b[t % 2]
                    for c in range(8):
                        C.op("pe", "transpose", [xn32_b, b_const], [pb_b[c // 4]], sig=(c % 4 == 3), out=pb[c // 4][:, (c % 4) * 128:(c % 4 + 1) * 128],
                             in_=xn32[:, c * 128:(c + 1) * 128], identity=identf[:])
                    for hh in range(2):
                        C.op("dve", "tensor_tensor", [pb_b[hh], b_smB], [xnT32_b], out=xnT32[:, hh * 4:hh * 4 + 4, :],
                             in0=pb[hh][:, :].rearrange("p (c n) -> p c n", c=4), in1=bc(gmo[:, hh * 4:hh * 4 + 4].unsqueeze(2), [128, 4, 128]), op=ALU.mult)
                    xj = i % 2
                    C.op("pool", "tensor_copy", [xnT32_b], [xnTb_b[xj]], out=xnTb[xj][:, :, :], in_=xnT32[:, :, :])
                    C.dma("sp", xn_scr[i].rearrange("p (c n) -> p c n", c=8), xnTb[xj][:, :, :], [xnTb_b[xj]], [xn_scr_b[i]], sem_from=xnTb_b[xj])
                    for kc in range(8):
                        C.op("pe", "matmul", [xnT32_b, b_smB], [pb_b[2]], out=pb[2][:, 0:36], lhsT=xnT32[:, kc, :], rhs=wr_sb[:, kc, :],
                             start=(kc == 0), stop=(kc == 7))
                    C.op("pe", "matmul", [b_const], [pb_b[3]], sig=True, out=pb[3][:, 0:128], lhsT=identb[:], rhs=identb[:], start=True, stop=True)
                    C.op("dve", "tensor_tensor", [pb_b[2], b_smB], [lgall_b], out=lgall[:, i, :], in0=pb[2][:, 0:36], in1=br_sb[:, :], op=ALU.add)
                wox(0)
                wox(1)
                b6(0)
                wox(2)
                b6(1)
                wox(3)
                b6(2)
                if blk + 1 < NSB:
                    for t in range(3):
                        load_x(blk + 1, t)
                b6(3)
                if blk + 1 < NSB:
                    load_x(blk + 1, 3)
            C.barrier()

        if stop_after == "B":
            db = C.buf("dbgB")
            C.dma("sp", dbg_out["x2"][:, :], x2_scr[:, :], x2_scr_b, [db])
            C.dma("sp", dbg_out["comb"].rearrange("p (t e) -> p t e", t=NT), comb[:, :, :], comb_b, [db])
            C.dma("sp", dbg_out["xn"].rearrange("(t p) n -> p t n", p=128), xn_scr.rearrange("t p n -> p t n"), xn_scr_b, [db])
            C.final_wait("sp", [db])
            return nc

        with ExitStack() as CC:
            xnT = sb(CC, "xnT", [128, 8, S], BF16)
            xnT_blk = C.bufs("xnT", NSB)
            xnT_b = [xnT_blk[i // 4] for i in range(NT)]
            for i in range(NT):
                C.dma("sp", xnT[:, :, i * 128:(i + 1) * 128], xn_scr[i].rearrange("p (c n) -> p c n", c=8), [xn_scr_b[i]], [xnT_b[i]])
            ysb = sb(CC, "ysb", [128, 16, D], F32)
            ysb_b = C.bufs("ysb", 16)
            NSLOT = 3
            wgs = [sb(CC, "wgs%d" % j, [128, 8, 256], BF16) for j in range(NSLOT)]
            wus = [sb(CC, "wus%d" % j, [128, 8, 256], BF16) for j in range(NSLOT)]
            wds = [sb(CC, "wds%d" % j, [128, 2, D], BF16) for j in range(NSLOT)]
            wslot_b = C.bufs("wslot", NSLOT)
            actT = [sb(CC, "actT%d" % j, [128, 2, 512], BF16) for j in range(2)]
            actT_b = [C.bufs("actT%d_" % j, 2) for j in range(2)]
            ssb = [sb(CC, "ssb%d" % j, [128, 512], BF16) for j in range(2)]
            ssb_b = C.bufs("ssb", 2)
            x2t = [sb(CC, "x2t%d" % j, [128, D], F32) for j in range(2)]
            x2t_b = C.bufs("x2t", 2)
            ot = [sb(CC, "ot%d" % j, [128, D], F32) for j in range(2)]
            ot_b = C.bufs("ot", 2)
            gfin = sb(CC, "gfin", [128, D], F32)
            gfin_b = C.buf("gfin")
            C.dma("sp", gfin[:], bc(g_final_d[0:1, :], [128, D]), [], [gfin_b])
            stC = sb(CC, "statC", [128, 8], F32)
            stC_b = C.buf("statC")
            pg = [ps(CC, "pg%d" % j, [128, 512], F32) for j in range(2)]
            pu = [ps(CC, "pu%d" % j, [128, 512], F32) for j in range(2)]
            py = [ps(CC, "py%d" % j, [128, 1024], F32) for j in range(2)]
            pg_b = C.bufs("pg", 2)
            pu_b = C.bufs("pu", 2)
            py_b = C.bufs("py", 2)

            with ExitStack() as BR:
                T = NT
                o1 = ot[1]
                gmax = o1[:, 0:32]
                oneh = o1[:, 32:160].rearrange("p (t g) -> p t g", g=4)
                dgl = o1[:, 160:288].rearrange("p (t g) -> p t g", g=4)
                sume = o1[:, 288:320]
                ggate = o1[:, 320:352]
                elsel = o1[:, 352:608].rearrange("p (t e) -> p t e", e=8)
                m8 = o1[:, 608:864].rearrange("p (t e) -> p t e", e=8)
                sc1 = o1[:, 864:992].rearrange("p (k t) -> p k t", k=4)
                tmpe = ot[0][:, :].rearrange("p (t f) -> p t f", f=32)
                eqa = x2t[0][:, 0:256].rearrange("p (t e) -> p t e", e=8)
                eqb = x2t[0][:, 256:512].rearrange("p (t e) -> p t e", e=8)
                rb_ = C.buf("routing")
                rr, ww = [lgall_b, rb_], [rb_, ot_b[0], ot_b[1], x2t_b[0]]
                gl = lgall[:, :, 0:4]
                el4 = lgall[:, :, 4:36].rearrange("p t (g e) -> p t g e", g=4)
                C.op("dve", "tensor_reduce", rr, ww, out=gmax[:, :], in_=gl, axis=AX.X, op=ALU.max)
                C.op("dve", "tensor_tensor", rr, ww, out=oneh[:, :, :], in0=gl, in1=bc(gmax[:, :].unsqueeze(2), [128, T, 4]), op=ALU.is_equal)
                C.op("dve", "tensor_tensor", rr, ww, out=dgl[:, :, :], in0=gl, in1=bc(gmax[:, :].unsqueeze(2), [128, T, 4]), op=ALU.subtract)
                C.op("act", "activation", rr, ww, out=dgl[:, :, :], in_=dgl[:, :, :], func=AF.Exp)
                C.op("dve", "tensor_reduce", rr, ww, out=sume[:, :], in_=dgl[:, :, :], axis=AX.X, op=ALU.add)
                C.op("dve", "reciprocal", rr, ww, out=ggate[:, :], in_=sume[:, :])
                C.op("dve", "tensor_tensor", rr, ww, out=tmpe[:, :, :].rearrange("p t (g e) -> p t g e", g=4), in0=el4,
                     in1=bc(oneh[:, :, :].unsqueeze(3), [128, T, 4, 8]), op=ALU.mult)
                C.op("dve", "tensor_reduce", rr, ww, out=elsel[:, :, :], in_=tmpe[:, :, :].rearrange("p t (g e) -> p t e g", g=4), axis=AX.X, op=ALU.add)
                for t in range(T):
                    C.op("dve", "max", rr, ww, out=m8[:, t, :], in_=elsel[:, t, :])
                m1 = m8[:, :, 0]
                m2 = m8[:, :, 1]
                e2, s1, w1, w2 = sc1[:, 0, :], sc1[:, 1, :], sc1[:, 2, :], sc1[:, 3, :]
                C.op("dve", "tensor_tensor", rr, ww, out=e2, in0=m2, in1=m1, op=ALU.subtract)
                C.op("act", "activation", rr, ww, out=e2, in_=e2, func=AF.Exp)
                C.op("dve", "tensor_scalar", rr, ww, out=s1, in0=e2, scalar1=1.0, scalar2=None, op0=ALU.add)
                C.op("dve", "reciprocal", rr, ww, out=s1, in_=s1)
                C.op("dve", "tensor_tensor", rr, ww, out=w1, in0=s1, in1=ggate[:, :], op=ALU.mult)
                C.op("dve", "tensor_tensor", rr, ww, out=w2, in0=w1, in1=e2, op=ALU.mult)
                C.op("dve", "tensor_tensor", rr, ww, out=eqa[:, :, :], in0=elsel[:, :, :], in1=bc(m1.unsqueeze(2), [128, T, 8]), op=ALU.is_equal)
                C.op("dve", "tensor_tensor", rr, ww, out=eqa[:, :, :], in0=eqa[:, :, :], in1=bc(w1.unsqueeze(2), [128, T, 8]), op=ALU.mult)
                C.op("dve", "tensor_tensor", rr, ww, out=eqb[:, :, :], in0=elsel[:, :, :], in1=bc(m2.unsqueeze(2), [128, T, 8]), op=ALU.is_equal)
                C.op("dve", "tensor_tensor", rr, ww, out=eqb[:, :, :], in0=eqb[:, :, :], in1=bc(w2.unsqueeze(2), [128, T, 8]), op=ALU.mult)
                C.op("dve", "tensor_tensor", rr, ww, out=eqa[:, :, :], in0=eqa[:, :, :], in1=eqb[:, :, :], op=ALU.add)
                C.op("dve", "tensor_tensor", rr, comb_b, out=comb[:, :, :].rearrange("p t (g e) -> p t g e", g=4),
                     in0=bc(oneh[:, :, :].unsqueeze(3), [128, T, 4, 8]), in1=bc(eqa[:, :, :].unsqueeze(2), [128, T, 4, 8]), op=ALU.mult)


            for hf in range(2):
                units = [(e, tb) for e in range(32) for tb in range(4)]

                def load_expert(e):
                    sl = e % NSLOT
                    C.dma("pool", wgs[sl][:, :, :], w_eg_d[e].rearrange("(c p) f -> p c f", p=128), [], [wslot_b[sl]])
                    C.dma("pool", wus[sl][:, :, :], w_eu_d[e].rearrange("(c p) f -> p c f", p=128), [], [wslot_b[sl]])
                    C.dma("pool", wds[sl][:, :, :], w_ed_d[e].rearrange("(c p) n -> p c n", p=128), [], [wslot_b[sl]])

                gu_cnt = [0]

                def gu(n):
                    e, tb = units[n]
                    sl = e % NSLOT
                    a = n % 2
                    tok0 = (hf * 16 + tb * 4) * 128
                    xb = xnT_b[hf * 16 + tb * 4:hf * 16 + tb * 4 + 4]
                    for fc in range(2):
                        k2 = gu_cnt[0] % 2
                        gu_cnt[0] += 1
                        for kc in range(8):
                            C.op("pe", "matmul", [wslot_b[sl]] + xb, [pg_b[k2]], sig=(kc == 7), out=pg[k2][:, :],
                                 lhsT=wgs[sl][:, kc, fc * 128:(fc + 1) * 128], rhs=xnT[:, kc, tok0:tok0 + 512], start=(kc == 0), stop=(kc == 7))
                        for kc in range(8):
                            C.op("pe", "matmul", [wslot_b[sl]] + xb, [pu_b[k2]], sig=(kc == 7), out=pu[k2][:, :],
                                 lhsT=wus[sl][:, kc, fc * 128:(fc + 1) * 128], rhs=xnT[:, kc, tok0:tok0 + 512], start=(kc == 0), stop=(kc == 7))
                        C.op("act", "activation", [pg_b[k2]], [ssb_b[k2]], out=ssb[k2][:], in_=pg[k2][:, :], func=AF.Silu)
                        C.op("dve", "tensor_tensor", [pu_b[k2], ssb_b[k2]], [actT_b[a][fc]], out=actT[a][:, fc, :], in0=pu[k2][:, :], in1=ssb[k2][:], op=ALU.mult)

                def down(n):
                    e, tb = units[n]
                    sl = e % NSLOT
                    a = n % 2
                    for t in range(4):
                        yt = tb * 4 + t
                        i = hf * 16 + yt
                        k2 = (n * 4 + t) % 2
                        for cb in range(2):
                            for fc in range(2):
                                C.op("pe", "matmul", [wslot_b[sl], actT_b[a][fc]], [py_b[k2]], sig=(cb == 1 and fc == 1),
                                     out=py[k2][:, cb * 512:(cb + 1) * 512], lhsT=actT[a][:, fc, t * 128:(t + 1) * 128],
                                     rhs=wds[sl][:, fc, cb * 512:(cb + 1) * 512], start=(fc == 0), stop=(fc == 1))
                        if e == 0:
                            j = yt % 2
                            C.dma("sp", x2t[j][:], x2_scr[i * 128:(i + 1) * 128, :], [x2_scr_b[i]], [x2t_b[j]])
                            C.op("dve", "scalar_tensor_tensor", [py_b[k2], comb_b[i], x2t_b[j]], [ysb_b[yt]], out=ysb[:, yt, :], in0=py[k2][:, :],
                                 scalar=comb[:, i, e:e + 1], in1=x2t[j][:], op0=ALU.mult, op1=ALU.add)
                        else:
                            C.op("dve", "scalar_tensor_tensor", [py_b[k2], comb_b[i], ysb_b[yt]], [ysb_b[yt]], out=ysb[:, yt, :], in0=py[k2][:, :],
                                 scalar=comb[:, i, e:e + 1], in1=ysb[:, yt, :], op0=ALU.mult, op1=ALU.add)

                def tail(yt):
                    i = hf * 16 + yt
                    j = yt % 2
                    rms_rstd(ysb[:, yt, :], [ysb_b[yt]], ot[j][:, :], [ot_b[j]], stC, stC_b)
                    C.op("dve", "scalar_tensor_tensor", [ysb_b[yt], stC_b, gfin_b], [ot_b[j]], out=ot[j][:], in0=ysb[:, yt, :], scalar=stC[:, 2:3],
                         in1=gfin[:], op0=ALU.mult, op1=ALU.mult)
                    C.dma("sp", out_d[i * 128:(i + 1) * 128, :], ot[j][:], [ot_b[j]], [out_b])

                load_expert(0)
                load_expert(1)
                gu(0)
                for n in range(len(units)):
                    e, tb = units[n]
                    if tb == 0 and e + 2 < 32:
                        load_expert(e + 2)
                    if n + 1 < len(units):
                        gu(n + 1)
                    down(n)
                    if e == 31:
                        for t in range(4):
                            tail(tb * 4 + t)
            C.barrier()
        C.final_wait("sp", [out_b])
    return nc


def make_in_maps(inputs):
    f32 = np.float32
    x = np.asarray(inputs["x"], f32)
    mem = np.asarray(inputs["mem"], f32)
    pos = np.asarray(inputs["positions"]).astype(np.int32)
    B = x.shape[0]

    def fm(v, n):
        return np.ascontiguousarray(np.asarray(v, f32).reshape(n, 128).T)

    w_re = np.asarray(inputs["w_router_expert"], f32)[0]
    w_router = np.concatenate([np.asarray(inputs["w_router_group"], f32)[0],
                               np.ascontiguousarray(w_re.transpose(1, 0, 2)).reshape(D, 32)], axis=1)
    b_router = np.concatenate([np.asarray(inputs["b_router_group"], f32)[0].reshape(-1),
                               np.asarray(inputs["b_router_expert"], f32)[0].reshape(-1)])[None, :]
    convw = np.asarray(inputs["conv_w"], f32)[0]
    convwT = np.ascontiguousarray(convw.T.reshape(4, 128, 31).transpose(1, 0, 2))
    inv_freq = (10000.0 ** (-np.arange(0, 64, 2, dtype=np.float64) / 64)).astype(np.float32)
    invf = np.tile((inv_freq.astype(np.float64) / (2 * np.pi)).astype(f32)[None, :], (128, 1))
    shared = {
        "w_in": np.ascontiguousarray(np.asarray(inputs["w_in"], f32)[0]),
        "w_o_attn": np.ascontiguousarray(np.asarray(inputs["w_o_attn"], f32)[0]),
        "convw": convwT,
        "convb": fm(np.asarray(inputs["conv_b"])[0], 4),
        "lng": fm(np.asarray(inputs["conv_ln_g"])[0], 4),
        "lnb": fm(np.asarray(inputs["conv_ln_b"])[0], 4),
        "w_conv_out": np.ascontiguousarray(np.asarray(inputs["w_conv_out"], f32)[0]),
        "w_out": np.ascontiguousarray(np.asarray(inputs["w_out"], f32)[0]),
        "w_q_x": np.ascontiguousarray(np.asarray(inputs["w_q_x"], f32)[0]),
        "w_kv_x": np.ascontiguousarray(np.asarray(inputs["w_kv_x"], f32)[0]),
        "w_o_x": np.ascontiguousarray(np.asarray(inputs["w_o_x"], f32)[0]),
        "g_mix": fm(np.asarray(inputs["norm_mix_g"])[0], 8),
        "g_x": fm(np.asarray(inputs["norm_x_g"])[0], 8),
        "g_mem": fm(np.asarray(inputs["norm_mem_g"])[0], 8),
        "g_moe": fm(np.asarray(inputs["norm_moe_g"])[0], 8),
        "w_router": np.ascontiguousarray(w_router),
        "b_router": np.ascontiguousarray(b_router.astype(f32)),
        "w_eg": np.ascontiguousarray(np.asarray(inputs["w_exp_gate"], f32)[0].reshape(32, D, 256)),
        "w_eu": np.ascontiguousarray(np.asarray(inputs["w_exp_up"], f32)[0].reshape(32, D, 256)),
        "w_ed": np.ascontiguousarray(np.asarray(inputs["w_exp_down"], f32)[0].reshape(32, 256, D)),
        "g_final": np.ascontiguousarray(np.asarray(inputs["norm_final_g"], f32).reshape(1, D)),
        "ident": np.eye(128, dtype=f32),
        "invf": invf,
        "pow2": np.tile((2.0 ** -np.arange(BIS_ITERS + 2)).astype(f32)[None, :], (128, 1)),
    }
    maps = []
    for b in range(B):
        m = dict(shared)
        m["x"] = np.ascontiguousarray(x[b])
        m["mem"] = np.ascontiguousarray(mem[b])
        m["pos"] = np.ascontiguousarray(pos[b].reshape(NT, 128).T)
        maps.append(m)
    return maps


def kernel(**inputs):
    maps = make_in_maps(inputs)
    nc = build()
    res = run_bass_kernel_spmd(nc, maps, core_ids=list(range(len(maps))))
    return np.stack([np.asarray(r["out"], np.float32) for r in res.results], axis=0)
```

```python
import bisect
from contextlib import ExitStack

import numpy as np
import concourse.bass as bass
import concourse.mybir as mybir
from concourse.bass_utils import run_bass_kernel_spmd

F32 = mybir.dt.float32
BF16 = mybir.dt.bfloat16
I32 = mybir.dt.int32
AF = mybir.ActivationFunctionType
ALU = mybir.AluOpType
AX = mybir.AxisListType

S = 4096
D = 1024
NT = S // 128
NSB = S // 512
EPS = 1e-6
NEG_BIG = -30000.0
BIS_ITERS = 18
A_OFF = 2120
B_OFF = 2632
G_OFF = 3144
NA_COLS = 3144


class SemBox:
    __slots__ = ("name", "sem", "count")

    def __init__(self, name):
        self.name = name
        self.sem = None
        self.count = 0


class Buf:
    __slots__ = ("name", "last_w", "readers", "box")

    def __init__(self, name, box=None):
        self.name = name
        self.last_w = None
        self.readers = []
        self.box = box if box is not None else SemBox(name)


class Ctx:
    COMPUTE = ("pe", "act", "dve", "pool")

    def __init__(self, nc, es):
        self.nc = nc
        self.es = es
        self.eng = {"pe": nc.tensor, "act": nc.scalar, "dve": nc.vector, "pool": nc.gpsimd, "sp": nc.sync}
        self.sem = {e: es.enter_context(nc.semaphore("s_" + e)) for e in self.COMPUTE}
        self.mile = {e: 0 for e in self.COMPUTE}
        self.nissued = {e: 0 for e in self.eng}
        self.sigpts = {e: ([], []) for e in self.COMPUTE}
        self.last_ins = {e: None for e in self.eng}
        self.last_sig = {e: True for e in self.eng}
        self.waited = {}
        self.dma_sems = []
        self.all_bufs = []

    def buf(self, name):
        b = Buf(name)
        self.all_bufs.append(b)
        return b

    def bufs(self, name, n, share=False):
        if not share:
            return [self.buf("%s%d" % (name, i)) for i in range(n)]
        box = SemBox(name)
        out = []
        for i in range(n):
            b = Buf("%s%d" % (name, i), box)
            self.all_bufs.append(b)
            out.append(b)
        return out

    def _resolve(self, tok):
        if tok[0] == "d":
            return tok[1], tok[2]
        _, e, idx = tok
        idxs, miles = self.sigpts[e]
        k = bisect.bisect_left(idxs, idx)
        if k < len(idxs):
            return self.sem[e], miles[k]
        assert not self.last_sig[e]
        self.last_ins[e].then_inc(self.sem[e], 1)
        self.mile[e] += 1
        idxs.append(self.nissued[e] - 1)
        miles.append(self.mile[e])
        self.last_sig[e] = True
        return self.sem[e], self.mile[e]

    def _wait(self, engname, toks):
        need = {}
        for tok in toks:
            if tok is None:
                continue
            if tok[0] == "c" and tok[1] == engname and engname == "pe":
                continue
            sem, val = self._resolve(tok)
            key = id(sem)
            if key not in need or need[key][1] < val:
                need[key] = (sem, val)
        for key, (sem, val) in need.items():
            wk = (engname, key)
            if self.waited.get(wk, 0) >= val:
                continue
            self.eng[engname].wait_ge(sem, val)
            self.waited[wk] = val

    def _deps(self, engname, reads, writes, waw=True):
        toks = []
        for b in reads:
            toks.append(b.last_w)
        for b in writes:
            if waw:
                if not (b.last_w is not None and b.last_w[0] == "c" and b.last_w[1] == engname):
                    toks.append(b.last_w)
            for r in b.readers:
                if r[0] == "c" and r[1] == engname and engname == "pe":
                    continue
                toks.append(r)
        return toks

    def op(self, engname, method, reads=(), writes=(), sig=None, **kw):
        assert engname in self.COMPUTE
        if sig is None:
            sig = engname != "pe"
        self._wait(engname, self._deps(engname, reads, writes))
        ins = getattr(self.eng[engname], method)(**kw)
        idx = self.nissued[engname]
        self.nissued[engname] += 1
        self.last_ins[engname] = ins
        self.last_sig[engname] = False
        if sig:
            ins.then_inc(self.sem[engname], 1)
            self.mile[engname] += 1
            self.sigpts[engname][0].append(idx)
            self.sigpts[engname][1].append(self.mile[engname])
            self.last_sig[engname] = True
        tok = ("c", engname, idx)
        for b in reads:
            b.readers.append(tok)
        for b in writes:
            b.last_w = tok
            b.readers = []
        return ins

    def dma(self, q, out, in_, reads, writes, waw=False, sem_from=None, **kw):
        assert len(writes) == 1
        wb = (sem_from if sem_from is not None else writes[0]).box
        if wb.sem is None:
            wb.sem = self.es.enter_context(self.nc.semaphore("d_" + wb.name))
            self.dma_sems.append(wb)
        self._wait(q, self._deps(q, reads, writes, waw=waw))
        ins = self.eng[q].dma_start(out=out, in_=in_, **kw)
        ins.then_inc(wb.sem, 16)
        wb.count += 16
        self.nissued[q] += 1
        if q in self.COMPUTE:
            self.last_ins[q] = ins
            self.last_sig[q] = True
        tok = ("d", wb.sem, wb.count)
        for b in reads:
            b.readers.append(tok)
        writes[0].last_w = tok
        writes[0].readers = []
        return ins

    def barrier(self):
        toks = []
        for e in self.COMPUTE:
            if self.nissued[e] > 0 and self.last_ins[e] is not None:
                if not self.last_sig[e]:
                    toks.append(("c", e, self.nissued[e] - 1))
                else:
                    idxs, miles = self.sigpts[e]
                    if idxs:
                        toks.append(("c", e, idxs[-1]))
        for b in self.dma_sems:
            toks.append(("d", b.sem, b.count))
        for e in list(self.COMPUTE) + ["sp"]:
            self._wait(e, toks)

    def final_wait(self, q, bufs):
        self._wait(q, [("d", b.box.sem, b.box.count) for b in bufs if b.box.sem is not None])


def bc(ap, shape):
    return ap.broadcast_to(list(shape))


def build(stop_after="all", dbg=(), a2_tiles=None):
    nc = bass.Bass("TRN2", target_bir_lowering=False)

    def din(name, shape, dt=F32):
        return nc.dram_tensor(name, list(shape), dt, kind="ExternalInput").ap()

    x_d = din("x", [S, D])
    mem_d = din("mem", [256, D])
    pos_d = din("pos", [128, NT], I32)
    w_in_d = din("w_in", [D, 5192])
    w_o_attn_d = din("w_o_attn", [512, D])
    convw_d = din("convw", [128, 4, 31])
    convb_d = din("convb", [128, 4])
    lng_d = din("lng", [128, 4])
    lnb_d = din("lnb", [128, 4])
    w_conv_out_d = din("w_conv_out", [512, D])
    w_out_d = din("w_out", [D, D])
    w_q_x_d = din("w_q_x", [D, D])
    w_kv_x_d = din("w_kv_x", [D, 2 * D])
    w_o_x_d = din("w_o_x", [D, D])
    g_mix_d = din("g_mix", [128, 8])
    g_x_d = din("g_x", [128, 8])
    g_mem_d = din("g_mem", [128, 8])
    g_moe_d = din("g_moe", [128, 8])
    w_router_d = din("w_router", [D, 36])
    b_router_d = din("b_router", [1, 36])
    w_eg_d = din("w_eg", [32, D, 256])
    w_eu_d = din("w_eu", [32, D, 256])
    w_ed_d = din("w_ed", [32, 256, D])
    g_final_d = din("g_final", [1, D])
    ident_d = din("ident", [128, 128])
    invf_d = din("invf", [128, 32])
    pow2_d = din("pow2", [128, BIS_ITERS + 2])

    out_d = nc.dram_tensor("out", [S, D], F32, kind="ExternalOutput").ap()

    def dscr(name, shape, dt):
        return nc.dram_tensor(name, list(shape), dt, kind="Internal").ap()

    qT_scr = dscr("qT_scr", [NT, 128, 512], BF16)
    qiT_scr = dscr("qiT_scr", [NT, 128, 512], BF16)
    z_scr = dscr("z_scr", [NSB, 128, 2048], BF16)
    ao_scr = dscr("ao_scr", [NT, 128, 512], BF16)
    x2_scr = dscr("x2_scr", [S, D], F32)

    dbg_out = {}
    for name, shape, dt in dbg:
        dbg_out[name] = nc.dram_tensor("dbg_" + name, list(shape), dt, kind="ExternalOutput").ap()

    with ExitStack() as es:
        C = Ctx(nc, es)
        out_b = C.buf("out")

        P0 = ExitStack()
        es.enter_context(P0)

        def sb(stack, name, shape, dt):
            return stack.enter_context(nc.sbuf_tensor("sb_" + name, list(shape), dt))

        def ps(stack, name, shape, dt):
            return stack.enter_context(nc.psum_tensor("ps_" + name, list(shape), dt))

        identf = sb(P0, "identf", [128, 128], F32)
        identb = sb(P0, "identb", [128, 128], BF16)
        onesf = sb(P0, "onesf", [128, 128], F32)
        onesb = sb(P0, "onesb", [128, 128], BF16)
        b_const = C.buf("const")
        C.dma("sp", identf[:], ident_d[:, :], [], [b_const])
        C.op("dve", "tensor_copy", [b_const], [b_const], out=identb[:], in_=identf[:])
        C.op("dve", "memset", [], [b_const], ap=onesf[:], constant=1.0 / 512.0)
        C.op("dve", "memset", [], [b_const], ap=onesb[:], constant=1.0)

        PA = ExitStack()
        es.enter_context(PA)
        kT = sb(PA, "kT", [128, 4, S], BF16)
        v_sb = sb(PA, "v_sb", [128, NT, 8, 65], BF16)
        kiT2 = sb(PA, "kiT2", [128, S], BF16)
        wi_sb = sb(PA, "wi_sb", [128, NT, 8], F32)
        kT_b = C.bufs("kT", NT)
        v_b = C.bufs("v", NT)
        kiT_b = C.bufs("kiT", NT)
        wi_b = C.bufs("wi", NT)
        for i in range(NT):
            C.op("pool", "memset", [], [v_b[i]], sig=(i == NT - 1), ap=v_sb[:, i, :, 64:65], constant=1.0)

        with ExitStack() as A1:
            w_sb = sb(A1, "w_inA", [128, 8, NA_COLS], BF16)
            w_b = [C.buf("w_inA")] * 8
            for kc in range(8):
                for (c0, c1) in ((0, 1024), (1024, 2048), (2048, NA_COLS)):
                    C.dma("pool", w_sb[:, kc, c0:c1], w_in_d[kc * 128:(kc + 1) * 128, c0:c1], [], [w_b[kc]])
            gfm = sb(A1, "gfm", [128, 8], F32)
            cwT = sb(A1, "cwT", [128, 4, 31], F32)
            cb4 = sb(A1, "cb4", [128, 4], F32)
            lng4 = sb(A1, "lng4", [128, 4], F32)
            lnb4 = sb(A1, "lnb4", [128, 4], F32)
            invf = sb(A1, "invf", [128, 32], F32)
            posi = sb(A1, "posi", [128, NT], I32)
            posf = sb(A1, "posf", [128, NT], F32)
            b_small = C.buf("smallA")
            C.dma("sp", gfm[:], g_mix_d[:, :], [], [b_small])
            C.dma("sp", cwT[:], convw_d[:, :, :], [], [b_small])
            C.dma("sp", cb4[:], convb_d[:, :], [], [b_small])
            C.dma("sp", lng4[:], lng_d[:, :], [], [b_small])
            C.dma("sp", lnb4[:], lnb_d[:, :], [], [b_small])
            C.dma("sp", invf[:], invf_d[:, :], [], [b_small])
            C.dma("sp", posi[:], pos_d[:, :], [], [b_small])
            cosT = sb(A1, "cosT", [128, NT, 32], F32)
            sinT = sb(A1, "sinT", [128, NT, 32], F32)
            with ExitStack() as T0:
                ua = sb(T0, "ua", [128, NT, 32], F32)
                ub = sb(T0, "ub", [128, NT, 32], F32)
                uci = sb(T0, "uci", [128, NT, 32], I32)
                b_rope = C.buf("ropetab")
                b_ua = C.buf("ua")
                b_ub = C.buf("ub")
                b_uc = C.buf("uc")
                C.op("dve", "tensor_copy", [b_small], [b_ua], out=posf[:], in_=posi[:])
                C.op("dve", "tensor_tensor", [b_ua, b_small], [b_ub], out=ua[:],
                     in0=bc(posf[:, :].unsqueeze(2), [128, NT, 32]), in1=bc(invf[:, :].unsqueeze(1), [128, NT, 32]), op=ALU.mult)
                for (tab, shift) in ((sinT, 0.0), (cosT, 0.25)):
                    C.op("dve", "tensor_scalar", [b_ub], [b_ua], out=ub[:], in0=ua[:], scalar1=shift, scalar2=None, op0=ALU.add)
                    C.op("dve", "tensor_copy", [b_ua], [b_uc], out=uci[:], in_=ub[:])
                    C.op("dve", "tensor_copy", [b_uc], [b_rope], out=tab[:], in_=uci[:])
                    C.op("dve", "tensor_tensor", [b_ua, b_rope], [b_ua], out=ub[:], in0=ub[:], in1=tab[:], op=ALU.subtract)
                    C.op("dve", "tensor_scalar", [b_ua], [b_rope], out=tab[:], in0=ub[:], scalar1=0.5, scalar2=None, op0=ALU.is_gt)
                    C.op("dve", "tensor_tensor", [b_ua, b_rope], [b_ua], out=ub[:], in0=ub[:], in1=tab[:], op=ALU.subtract)
                    C.op("dve", "tensor_scalar", [b_ua], [b_rope], out=tab[:], in0=ub[:], scalar1=-0.5, scalar2=None, op0=ALU.is_lt)
                    C.op("dve", "tensor_tensor", [b_ua, b_rope], [b_ua], out=ub[:], in0=ub[:], in1=tab[:], op=ALU.add)
                    C.op("act", "activation", [b_ua], [b_rope], out=tab[:], in_=ub[:], func=AF.Sin, scale=2.0 * np.pi)

                C.barrier()

            xt = [sb(A1, "xt%d" % j, [128, D], F32) for j in range(2)]
            xt_b = C.bufs("xt", 2)
            hn = [sb(A1, "hn%d" % j, [128, D], BF16) for j in range(2)]
            hn_b = C.bufs("hn", 2)
            st = sb(A1, "stat", [128, 8], F32)
            st_b = C.buf("stat")
            hT = sb(A1, "hT", [128, 8, 512], BF16)
            hT_b = C.bufs("hT", 4)
            rt = [sb(A1, "rt%d" % j, [128, 8, 32], F32) for j in range(4)]
            rt_b = C.bufs("rt", 4)
            rq = [sb(A1, "rq%d" % j, [128, 8, 64], BF16) for j in range(2)]
            rq_b = C.bufs("rq", 2)
            rqk = sb(A1, "rqk", [128, 2, 64], BF16)
            rqk_b = C.buf("rqk")
            qst = [sb(A1, "qst%d" % j, [128, 4, 128], BF16) for j in range(2)]
            qst_b = C.bufs("qst", 2)
            uT = [sb(A1, "uT%d" % j, [128, 4, 542], BF16) for j in range(2)]
            uT_b = [C.bufs("uT%d_" % j, 4) for j in range(2)]
            uTpad_b = C.bufs("uTpad", 2)
            sg = sb(A1, "sg", [128, 512], F32)
            sg_b = C.buf("sg")
            Dw = sb(A1, "Dw", [128, 31, 128], BF16)
            Dw_b = C.buf("Dw")
            co = sb(A1, "co", [128, 4, 512], F32)
            co_b = C.bufs("co", 4)
            sq = [sb(A1, "sq%d" % j, [128, 512], F32) for j in range(2)]
            sq_b = C.bufs("sq", 2)
            mean_sb = sb(A1, "mean_sb", [128, 512], F32)
            m2_sb = sb(A1, "m2_sb", [128, 512], F32)
            rstd_sb = sb(A1, "rstd_sb", [128, 512], F32)
            mean_b = C.buf("mean")
            m2_b = C.buf("m2")
            rstd_b = C.buf("rstdc")
            dtmp = [sb(A1, "dtmp%d" % j, [128, 512], F32) for j in range(2)]
            dtmp_b = C.bufs("dtmp", 2)
            zT = sb(A1, "zT", [128, 4, 512], BF16)
            zT_b = C.buf("zT")
            pA = ps(A1, "pA", [128, 512], F32)
            pB = ps(A1, "pB", [128, 512], F32)
            pC = ps(A1, "pC", [128, 512], F32)
            pM = ps(A1, "pM", [128, 512], F32)
            pE = ps(A1, "pE", [128, 512], F32)
            pT0 = ps(A1, "pT0", [128, 512], F32)
            pT1 = ps(A1, "pT1", [128, 512], F32)
            pTP = ps(A1, "pTP", [128, 1024], BF16)
            pA_b, pB_b, pC_b, pM_b, pE_b, pTP_b = (C.buf(n) for n in ("pA", "pB", "pC", "pM", "pE", "pTP"))
            pT = [pT0, pT1]
            pT_b = C.bufs("pT", 2)
            q_scr_b = C.bufs("qscr", NT, share=True)
            qi_scr_b = C.bufs("qiscr", NT, share=True)
            z_scr_b = C.bufs("zscr", NSB, share=True)

            C.op("pool", "memset", [], [uTpad_b[0]], ap=uT[0][:, :, 0:30], constant=0.0)
            tmc = [0]

            def rope_block(psv, nh, i, dst, dst_bufs, dup=False):
                cos_b = bc(cosT[:, i, :].unsqueeze(1), [128, nh, 32])
                sin_b = bc(sinT[:, i, :].unsqueeze(1), [128, nh, 32])
                x1 = psv[:, :, 0:32]
                x2 = psv[:, :, 32:64]
                pb = psv_buf[0]
                C.op("dve", "tensor_tensor", [pb, b_rope], [rt_b[0]], out=rt[0][:, 0:nh, :], in0=x1, in1=cos_b, op=ALU.mult)
                C.op("dve", "tensor_tensor", [pb, b_rope], [rt_b[1]], out=rt[1][:, 0:nh, :], in0=x2, in1=sin_b, op=ALU.mult)
                C.op("dve", "tensor_tensor", [pb, b_rope], [rt_b[2]], out=rt[2][:, 0:nh, :], in0=x2, in1=cos_b, op=ALU.mult)
                C.op("dve", "tensor_tensor", [pb, b_rope], [rt_b[3]], out=rt[3][:, 0:nh, :], in0=x1, in1=sin_b, op=ALU.mult)
                C.op("pool", "tensor_tensor", [rt_b[0], rt_b[1]], dst_bufs, out=dst[:, :, 0:32], in0=rt[0][:, 0:nh, :], in1=rt[1][:, 0:nh, :], op=ALU.subtract)
                C.op("pool", "tensor_tensor", [rt_b[2], rt_b[3]], dst_bufs, out=dst[:, :, 32:64], in0=rt[2][:, 0:nh, :], in1=rt[3][:, 0:nh, :], op=ALU.add)

            psv_buf = [None]

            def chain(i):
                j = i % 2
                C.dma("sp", xt[j][:], x_d[i * 128:(i + 1) * 128, :], [], [xt_b[j]])
                C.op("act", "activation", [xt_b[j]], [hn_b[j], st_b], out=hn[j][:], in_=xt[j][:], func=AF.Square, accum_out=st[:, 0:1])
                C.op("act", "activation", [st_b], [st_b], out=st[:, 1:2], in_=st[:, 0:1], func=AF.Sqrt, scale=1.0 / D, bias=EPS)
                C.op("dve", "reciprocal", [st_b], [st_b], out=st[:, 2:3], in_=st[:, 1:2])
                C.op("act", "activation", [xt_b[j], st_b], [hn_b[j]], out=hn[j][:], in_=xt[j][:], func=AF.Copy, scale=st[:, 2:3])

            def trans(i):
                j = i % 2
                t = i % 4
                for c in range(8):
                    C.op("pe", "transpose", [hn_b[j], b_const], [pTP_b], sig=(c == 7), out=pTP[:, c * 128:(c + 1) * 128],
                         in_=hn[j][:, c * 128:(c + 1) * 128], identity=identb[:])
                C.op("dve", "tensor_tensor", [pTP_b, b_small], [hT_b[t]], out=hT[:, :, t * 128:(t + 1) * 128],
                     in0=pTP[:, :].rearrange("p (c n) -> p c n", c=8), in1=bc(gfm[:, :].unsqueeze(2), [128, 8, 128]), op=ALU.mult)

            pending = []

            def flush():
                while pending:
                    pending.pop(0)()

            chain(0)
            for sbi in range(NSB):
                cur = sbi % 2
                nxt = 1 - cur
                for t in range(4):
                    i = sbi * 4 + t
                    trans(i)
                    if i + 1 < NT:
                        chain(i + 1)
                    for (name, c0, ncols) in (("q", 0, 512), ("k", 512, 512), ("v", 1024, 512), ("qi", 1536, 512), ("kw", 2048, 72)):
                        bk = tmc[0] % 2
                        tmc[0] += 1
                        for kc in range(8):
                            C.op("pe", "matmul", [hT_b[t], w_b[kc]], [pT_b[bk]], sig=(kc == 7), out=pT[bk][:, 0:512],
                                 lhsT=hT[:, kc, t * 128:(t + 1) * 128], rhs=w_sb[:, kc, c0:c0 + 512], start=(kc == 0), stop=(kc == 7))
                        flush()
                        psv_buf[0] = pT_b[bk]
                        if name == "v":
                            C.op("act", "activation", [pT_b[bk]], [v_b[i]], out=v_sb[:, i, :, 0:64],
                                 in_=pT[bk][:, :].rearrange("p (h d) -> p h d", h=8), func=AF.Copy)
                        elif name == "kw":
                            C.op("act", "activation", [pT_b[bk]], [wi_b[i]], out=wi_sb[:, i, :], in_=pT[bk][:, 64:72], func=AF.Copy)
                            psv = pT[bk][:, 0:64].rearrange("p (h d) -> p h d", h=1)
                            rope_block(psv, 1, i, rqk[:, 0:1, :], [rqk_b])
                            C.op("pool", "tensor_copy", [rqk_b], [rqk_b], out=rqk[:, 1:2, :], in_=rqk[:, 0:1, :])

                            def fin_kw(i=i):
                                C.op("pe", "transpose", [rqk_b, b_const], [pTP_b], sig=True, out=pTP[:, 0:128],
                                     in_=rqk[:, :, :].rearrange("p a d -> p (a d)"), identity=identb[:])
                                C.op("act", "activation", [pTP_b], [kiT_b[i]], out=kiT2[:, i * 128:(i + 1) * 128], in_=pTP[:, 0:128], func=AF.Copy)
                            pending.append(fin_kw)
                        else:
                            rj = tmc[0] % 2
                            psv = pT[bk][:, :].rearrange("p (h d) -> p h d", h=8)
                            rope_block(psv, 8, i, rq[rj][:, :, :], [rq_b[rj]])

                            def fin_rope(i=i, rj=rj, name=name, sj=tmc[0] % 2):
                                for c in range(4):
                                    C.op("pe", "transpose", [rq_b[rj], b_const], [pTP_b], sig=(c == 3), out=pTP[:, c * 128:(c + 1) * 128],
                                         in_=rq[rj][:, 2 * c:2 * c + 2, :].rearrange("p a d -> p (a d)"), identity=identb[:])
                                src = pTP[:, 0:512].rearrange("p (c n) -> p c n", c=4)
                                if name == "k":
                                    C.op("act", "activation", [pTP_b], [kT_b[i]], out=kT[:, :, i * 128:(i + 1) * 128], in_=src, func=AF.Copy)
                                else:
                                    C.op("act", "activation", [pTP_b], [qst_b[sj]], out=qst[sj][:, :, :], in_=src, func=AF.Copy)
                                    if name == "q":
                                        C.dma("sp", qT_scr[i].rearrange("p (c n) -> p c n", c=4), qst[sj][:, :, :], [qst_b[sj]], [q_scr_b[i]], sem_from=qst_b[sj])
                                    else:
                                        C.dma("sp", qiT_scr[i].rearrange("p (c n) -> p c n", c=4), qst[sj][:, :, :], [qst_b[sj]], [qi_scr_b[i]], sem_from=qst_b[sj])
                            pending.append(fin_rope)
                for cc in range(4):
                    for jj in range(31):
                        C.op("dve", "tensor_scalar", [b_const, b_small], [Dw_b], sig=(jj == 30), out=Dw[:, jj, :], in0=identb[:],
                             scalar1=cwT[:, cc, jj:jj + 1], scalar2=None, op0=ALU.mult)
                    for kc in range(8):
                        C.op("pe", "matmul", hT_b + [w_b[kc]], [pA_b], sig=(kc == 7), out=pA[:, :],
                             lhsT=w_sb[:, kc, A_OFF + cc * 128:A_OFF + (cc + 1) * 128], rhs=hT[:, kc, :], start=(kc == 0), stop=(kc == 7))
                    flush()
                    for kc in range(8):
                        C.op("pe", "matmul", hT_b + [w_b[kc]], [pB_b], sig=(kc == 7), out=pB[:, :],
                             lhsT=w_sb[:, kc, B_OFF + cc * 128:B_OFF + (cc + 1) * 128], rhs=hT[:, kc, :], start=(kc == 0), stop=(kc == 7))
                    C.op("act", "activation", [pB_b], [sg_b], out=sg[:], in_=pB[:, :], func=AF.Sigmoid)
                    C.op("dve", "tensor_tensor", [pA_b, sg_b], [uT_b[cur][cc]], out=uT[cur][:, cc, 30:542], in0=pA[:, :], in1=sg[:], op=ALU.mult)
                    for jj in range(31):
                        C.op("pe", "matmul", [Dw_b, uT_b[cur][cc], uTpad_b[cur]], [pC_b], sig=(jj == 30), out=pC[:, :],
                             lhsT=Dw[:, jj, :], rhs=uT[cur][:, cc, jj:jj + 512], start=(jj == 0), stop=(jj == 30))
                    C.op("act", "activation", [pC_b, b_small], [co_b[cc]], out=co[:, cc, :], in_=pC[:, :], func=AF.Identity, bias=cb4[:, cc:cc + 1])
                    C.op("act", "activation", [co_b[cc]], [sq_b[cc % 2]], out=sq[cc % 2][:], in_=co[:, cc, :], func=AF.Square)
                    C.op("pe", "matmul", [b_const, co_b[cc]], [pM_b], sig=(cc == 3), out=pM[:, :], lhsT=onesf[:], rhs=co[:, cc, :],
                         start=(cc == 0), stop=(cc == 3))
                    C.op("pe", "matmul", [b_const, sq_b[cc % 2]], [pE_b], sig=(cc == 3), out=pE[:, :], lhsT=onesf[:], rhs=sq[cc % 2][:],
                         start=(cc == 0), stop=(cc == 3))
                if sbi + 1 < NSB:
                    C.op("pool", "tensor_copy", uT_b[cur], [uTpad_b[nxt]], out=uT[nxt][:, :, 0:30], in_=uT[cur][:, :, 512:542])
                C.op("act", "activation", [pM_b], [mean_b], out=mean_sb[:], in_=pM[:, :], func=AF.Copy)
                C.op("pool", "tensor_tensor", [mean_b], [m2_b], out=m2_sb[:], in0=mean_sb[:], in1=mean_sb[:], op=ALU.mult)
                C.op("dve", "tensor_tensor", [pE_b, m2_b], [m2_b], out=m2_sb[:], in0=pE[:, :], in1=m2_sb[:], op=ALU.subtract)
                C.op("act", "activation", [m2_b], [m2_b], out=m2_sb[:], in_=m2_sb[:], func=AF.Sqrt, bias=EPS, scale=1.0)
                C.op("dve", "reciprocal", [m2_b], [rstd_b], out=rstd_sb[:], in_=m2_sb[:])
                for cc in range(4):
                    dj = cc % 2
                    C.op("pool", "tensor_tensor", [co_b[cc], mean_b], [dtmp_b[dj]], out=dtmp[dj][:], in0=co[:, cc, :], in1=mean_sb[:], op=ALU.subtract)
                    C.op("pool", "tensor_tensor", [dtmp_b[dj], rstd_b], [dtmp_b[dj]], out=dtmp[dj][:], in0=dtmp[dj][:], in1=rstd_sb[:], op=ALU.mult)
                    C.op("act", "activation", [dtmp_b[dj], b_small], [zT_b], out=zT[:, cc, :], in_=dtmp[dj][:], func=AF.Silu,
                         scale=lng4[:, cc:cc + 1], bias=lnb4[:, cc:cc + 1])
                C.dma("sp", z_scr[sbi].rearrange("p (c n) -> p c n", c=4), zT[:, :, :], [zT_b], [z_scr_b[sbi]], sem_from=zT_b)
            C.barrier()

        if stop_after == "A1":
            if "kT" in dbg_out:
                db = C.buf("dbg")
                C.dma("sp", dbg_out["kT"].rearrange("p (c n) -> p c n", c=4), kT[:, :, :], kT_b, [db])
                C.dma("sp", dbg_out["kiT2"][:, :], kiT2[:, :], kiT_b, [db])
                C.dma("sp", dbg_out["v"].rearrange("p (t h d) -> p t h d", t=NT, h=8), v_sb[:, :, :, :], v_b, [db])
                C.dma("sp", dbg_out["wi"].rearrange("p (t h) -> p t h", t=NT), wi_sb[:, :, :], wi_b, [db])
                C.dma("sp", dbg_out["qT"].rearrange("(t p) n -> p t n", p=128), qT_scr.rearrange("t p n -> p t n"), q_scr_b, [db])
                C.dma("sp", dbg_out["qiT"].rearrange("(t p) n -> p t n", p=128), qiT_scr.rearrange("t p n -> p t n"), qi_scr_b, [db])
                C.dma("sp", dbg_out["z"].rearrange("(t p) n -> p t n", p=128), z_scr.rearrange("t p n -> p t n"), z_scr_b, [db])
                C.final_wait("sp", [db])
            C.barrier()
            C.final_wait("sp", q_scr_b + qi_scr_b + z_scr_b)
            return nc

        with ExitStack() as A2:
            qTz = [[sb(A2, "qTz%d_%d" % (par, j), [128, 4, 128], BF16) for j in range(2)] for par in range(2)]
            qiTz = [[sb(A2, "qiTz%d_%d" % (par, j), [128, 4, 128], BF16) for j in range(2)] for par in range(2)]
            qT_tb = C.bufs("qT_t", 2)
            qiT_tb = C.bufs("qiT_t", 2)
            Dg = sb(A2, "Dg", [128, 8, 128], BF16)
            Dg_b = C.buf("Dg")
            R_sb = [sb(A2, "R_sb%d" % j, [128, 512], BF16) for j in range(2)]
            R_b = C.bufs("R_sb", 2)
            sc = [sb(A2, "sc%d" % j, [128, S], F32) for j in range(2)]
            sc_b = C.bufs("sc", 2)
            NM = [sb(A2, "NM%d" % j, [128, S], BF16) for j in range(2)]
            NM_b = C.bufs("NM", 2)
            bs = sb(A2, "bs", [128, 8], F32)
            bs_b = C.buf("bs")
            wk = sb(A2, "wk", [128, BIS_ITERS + 2], F32)
            pow2 = sb(A2, "pow2", [128, BIS_ITERS + 2], F32)
            thrc = sb(A2, "thrc", [128, 1], F32)
            b_c2 = C.buf("constA2")
            C.dma("sp", pow2[:], pow2_d[:, :], [], [b_c2])
            C.op("pool", "memset", [], [b_c2], ap=thrc[:], constant=-1e29)
            for par in range(2):
                for j in range(2):
                    C.op("dve", "memset", [], [qT_tb[j]], ap=qTz[par][j][:, :, :], constant=0.0)
                    C.op("dve", "memset", [], [qiT_tb[j]], ap=qiTz[par][j][:, :, :], constant=0.0)
            PT = [sb(A2, "PT%d" % j, [128, 512], BF16) for j in range(2)]
            PT_b = C.bufs("PT", 2)
            rden = sb(A2, "rden", [128, 8], F32)
            rden_b = C.buf("rden")
            ao = sb(A2, "ao", [128, 8, 64], BF16)
            ao_b = C.buf("ao")
            aoT_st = [sb(A2, "aoT_st%d" % j, [128, 4, 128], BF16) for j in range(2)]
            aoT_b = C.bufs("aoT_st", 2)
            pR = [ps(A2, "pR%d" % j, [128, 512], F32) for j in range(2)]
            pR_b = C.bufs("pR", 2)
            pSC = ps(A2, "pSC", [128, 512], F32)
            pSC_b = C.buf("pSC")
            pST = [ps(A2, "pST%d" % j, [128, 512], F32) for j in range(2)]
            pST_b = C.bufs("pST", 2)
            pPV = [ps(A2, "pPV%d" % j, [128, 512], F32) for j in range(2)]
            pPV_b = C.bufs("pPV", 2)
            pTP2 = ps(A2, "pTP2", [128, 1024], BF16)
            pTP2_b = C.buf("pTP2")
            ao_scr_b = C.bufs("aoscr", NT, share=True)

            C.barrier()
            tiles = list(range(NT)) if a2_tiles is None else list(a2_tiles)
            def index_phase(n_i, i):
                s2 = n_i % 2
                NK = 128 * (i + 1)
                nkc = i + 1
                nkb = (NK + 511) // 512
                for par in range(2):
                    pr = slice(par * 64, (par + 1) * 64)
                    C.dma("sp", qTz[par][s2][pr, :, :], qT_scr[i][pr, :].rearrange("p (c n) -> p c n", c=4), [q_scr_b[i]], [qT_tb[s2]])
                    C.dma("sp", qiTz[par][s2][pr, :, :], qiT_scr[i][pr, :].rearrange("p (c n) -> p c n", c=4), [qi_scr_b[i]], [qiT_tb[s2]])
                for h in range(8):
                    C.op("act", "activation", [b_const, wi_b[i]], [Dg_b], sig=(h == 7), out=Dg[:, h, :], in_=identb[:], func=AF.Copy,
                         scale=wi_sb[:, i, h:h + 1])
                for kb in range(nkb):
                    k0 = kb * 512
                    W = min(512, NK - k0)
                    kbufs = kiT_b[k0 // 128:(k0 + W) // 128]

                    def r_mm(h):
                        C.op("pe", "matmul", [qiT_tb[s2]] + kbufs, [pR_b[h % 2]], sig=True, out=pR[h % 2][:, 0:W],
                             lhsT=qiTz[h % 2][s2][:, h // 2, :], rhs=kiT2[:, k0:k0 + W], start=True, stop=True)
                        C.op("act", "activation", [pR_b[h % 2]], [R_b[h % 2]], out=R_sb[h % 2][:, 0:W], in_=pR[h % 2][:, 0:W], func=AF.Relu)

                    def s_mm(h):
                        C.op("pe", "matmul", [Dg_b, R_b[h % 2]], [pSC_b], sig=(h == 7), out=pSC[:, 0:W], lhsT=Dg[:, h, :],
                             rhs=R_sb[h % 2][:, 0:W], start=(h == 0), stop=(h == 7))

                    r_mm(0)
                    for h in range(8):
                        if h + 1 < 8:
                            r_mm(h + 1)
                        s_mm(h)
                    C.op("act", "activation", [pSC_b], [sc_b[s2]], out=sc[s2][:, k0:k0 + W], in_=pSC[:, 0:W], func=AF.Copy)
                C.op("pool", "memset", [], [sc_b[s2]], ap=sc[s2][0:64, NK - 64:NK], constant=-1e30)

            def thresh_phase(n_i, i):
                s2 = n_i % 2
                NK = 128 * (i + 1)
                nkc = i + 1
                nkb = (NK + 511) // 512
                if i >= 2:
                    C.op("dve", "tensor_reduce", [sc_b[s2]], [bs_b], out=bs[:, 0:1], in_=sc[s2][:, 0:NK], axis=AX.X, op=ALU.max)
                    C.op("dve", "tensor_reduce", [sc_b[s2]], [bs_b], out=bs[:, 1:2], in_=sc[s2][:, 0:320], axis=AX.X, op=ALU.min)
                    C.op("dve", "tensor_tensor", [bs_b], [bs_b], out=bs[:, 2:3], in0=bs[:, 0:1], in1=bs[:, 1:2], op=ALU.subtract)
                    C.op("dve", "tensor_scalar", [bs_b, b_c2], [bs_b], out=wk[:], in0=pow2[:], scalar1=bs[:, 2:3], scalar2=None, op0=ALU.mult)
                    C.op("dve", "tensor_tensor", [bs_b], [bs_b], out=bs[:, 3:4], in0=bs[:, 1:2], in1=wk[:, 1:2], op=ALU.add)
                    for k in range(BIS_ITERS):
                        C.op("dve", "tensor_scalar", [sc_b[s2], bs_b], [NM_b[s2], bs_b], out=NM[s2][:, 0:NK], in0=sc[s2][:, 0:NK],
                             scalar1=bs[:, 3:4], scalar2=None, op0=ALU.is_ge, op1=ALU.add, accum_out=bs[:, 4:5])
                        C.op("dve", "tensor_scalar", [bs_b], [bs_b], out=bs[:, 5:6], in0=bs[:, 4:5], scalar1=255.5, scalar2=0.5,
                             op0=ALU.is_ge, op1=ALU.subtract)
                        if k < BIS_ITERS - 1:
                            C.op("dve", "scalar_tensor_tensor", [bs_b], [bs_b], out=bs[:, 3:4], in0=bs[:, 5:6], scalar=wk[:, k + 1:k + 2],
                                 in1=bs[:, 3:4], op0=ALU.mult, op1=ALU.add)
                    C.op("dve", "tensor_scalar", [bs_b], [bs_b], out=bs[:, 5:6], in0=bs[:, 5:6], scalar1=-0.5, scalar2=None, op0=ALU.add)
                    C.op("dve", "scalar_tensor_tensor", [bs_b], [bs_b], out=bs[:, 6:7], in0=bs[:, 5:6], scalar=wk[:, BIS_ITERS:BIS_ITERS + 1],
                         in1=bs[:, 3:4], op0=ALU.mult, op1=ALU.add)
                    thr_ap = bs[:, 6:7]
                    thr_bufs = [bs_b]
                else:
                    thr_ap = thrc[:, 0:1]
                    thr_bufs = [b_c2]
                C.op("dve", "tensor_scalar", [sc_b[s2]] + thr_bufs, [NM_b[s2]], out=NM[s2][:, 0:NK], in0=sc[s2][:, 0:NK],
                     scalar1=thr_ap, scalar2=NEG_BIG, op0=ALU.is_lt, op1=ALU.mult)

            def attn_phase(n_i, i):
                s2 = n_i % 2
                NK = 128 * (i + 1)
                nkc = i + 1
                nkb = (NK + 511) // 512
                items = [(h, kb) for h in range(8) for kb in range(nkb)]

                def st_block(n):
                    h, kb = items[n]
                    sl = n % 2
                    pb = (h % 2) * 64
                    c = h // 2
                    kcs = list(range(kb * 4, min(nkc, kb * 4 + 4)))
                    for kcl, kc in enumerate(kcs):
                        C.op("pe", "matmul", [kT_b[kc], qT_tb[s2]], [pST_b[sl]], out=pST[sl][:, kcl * 128:(kcl + 1) * 128],
                             lhsT=kT[:, c, kc * 128:(kc + 1) * 128], rhs=qTz[h % 2][s2][:, c, :], start=True, stop=False)
                        C.op("pe", "matmul", [NM_b[s2], b_const], [pST_b[sl]], sig=(kcl == len(kcs) - 1),
                             out=pST[sl][:, kcl * 128:(kcl + 1) * 128],
                             lhsT=NM[s2][:, kc * 128:(kc + 1) * 128], rhs=identb[:], start=False, stop=True)
                    C.op("act", "activation", [pST_b[sl]], [PT_b[sl]], out=PT[sl][:, 0:len(kcs) * 128], in_=pST[sl][:, 0:len(kcs) * 128],
                         func=AF.Exp, scale=0.125)

                def pv_block(n):
                    h, kb = items[n]
                    sl = n % 2
                    kcs = list(range(kb * 4, min(nkc, kb * 4 + 4)))
                    for kcl, kc in enumerate(kcs):
                        C.op("pe", "matmul", [PT_b[sl], v_b[kc]], [pPV_b[h // 4]], out=pPV[h // 4][:, (h % 4) * 65:(h % 4) * 65 + 65],
                             lhsT=PT[sl][:, kcl * 128:(kcl + 1) * 128], rhs=v_sb[:, kc, h, :], start=(kc == 0), stop=(kc == nkc - 1))

                st_block(0)
                for n in range(len(items)):
                    if n + 1 < len(items):
                        st_block(n + 1)
                    pv_block(n)
                fs = len(items) % 2
                C.op("pe", "matmul", [b_const], [pST_b[fs]], sig=True, out=pST[fs][:, 0:128], lhsT=identb[:], rhs=identb[:], start=True, stop=True)

            def attn_tail(n_i, i):
                s2 = n_i % 2
                for hh in range(2):
                    pvv = pPV[hh][:, 0:260].rearrange("p (h d) -> p h d", h=4)
                    C.op("dve", "reciprocal", [pPV_b[hh]], [rden_b], out=rden[:, hh * 4:hh * 4 + 4].unsqueeze(2), in_=pvv[:, :, 64:65])
                    C.op("dve", "tensor_tensor", [pPV_b[hh], rden_b], [ao_b], out=ao[:, hh * 4:hh * 4 + 4, :], in0=pvv[:, :, 0:64],
                         in1=bc(rden[:, hh * 4:hh * 4 + 4].unsqueeze(2), [128, 4, 64]), op=ALU.mult)
                for c in range(4):
                    C.op("pe", "transpose", [ao_b, b_const], [pTP2_b], sig=(c == 3), out=pTP2[:, c * 128:(c + 1) * 128],
                         in_=ao[:, 2 * c:2 * c + 2, :].rearrange("p a d -> p (a d)"), identity=identb[:])
                C.op("act", "activation", [pTP2_b], [aoT_b[s2]], out=aoT_st[s2][:, :, :], in_=pTP2[:, 0:512].rearrange("p (c n) -> p c n", c=4), func=AF.Copy)
                C.dma("sp", ao_scr[i].rearrange("p (c n) -> p c n", c=4), aoT_st[s2][:, :, :], [aoT_b[s2]], [ao_scr_b[i]], sem_from=aoT_b[s2])

            nt_ = len(tiles)
            index_phase(0, tiles[0])
            thresh_phase(0, tiles[0])
            if nt_ > 1:
                index_phase(1, tiles[1])
            for n_i in range(nt_):
                attn_phase(n_i, tiles[n_i])
                if n_i + 1 < nt_:
                    thresh_phase(n_i + 1, tiles[n_i + 1])
                if n_i + 2 < nt_:
                    index_phase(n_i + 2, tiles[n_i + 2])
                attn_tail(n_i, tiles[n_i])
            s2 = (nt_ - 1) % 2
            if "nm" in dbg_out:
                dbn = C.buf("dbgnm")
                C.dma("sp", dbg_out["nm"][:, :], NM[s2][:, :], [NM_b[s2]], [dbn])
                C.dma("sp", dbg_out["sc"][:, :], sc[s2][:, :], [sc_b[s2]], [dbn])
                C.final_wait("sp", [dbn])
            C.barrier()

        if stop_after == "A2":
            db = C.buf("dbg2")
            C.dma("sp", dbg_out["ao"].rearrange("(t p) n -> p t n", p=128), ao_scr.rearrange("t p n -> p t n"), ao_scr_b, [db])
            C.final_wait("sp", [db])
            return nc

        PA.close()

        xn_scr = dscr("xn_scr", [NT, 128, 1024], BF16)
        xn_scr_b = C.bufs("xnscr", NT, share=True)
        x2_scr_b = C.bufs("x2scr", NT, share=True)
        PBC = ExitStack()
        es.enter_context(PBC)
        comb = sb(PBC, "comb", [128, NT, 32], F32)
        comb_b = C.bufs("comb", NT)
        lgall = sb(PBC, "lgall", [128, NT, 36], F32)
        lgall_b = C.buf("lgall")

        def load_w(stack, name, src, rows, c0, c1, bufname, defer=False):
            nkc = rows // 128
            t = sb(stack, name, [128, nkc, c1 - c0], BF16)
            b = C.buf(bufname)

            def issue():
                for kc in range(nkc):
                    C.dma("pool", t[:, kc, :], src[kc * 128:(kc + 1) * 128, c0:c1], [], [b])
            if defer:
                return t, b, issue
            issue()
            return t, b

        def rms_rstd(x_ap, x_bufs, junk_ap, junk_bufs, st, st_b, scale_n=D):
            C.op("act", "activation", x_bufs, junk_bufs + [st_b], out=junk_ap, in_=x_ap, func=AF.Square, accum_out=st[:, 0:1])
            C.op("act", "activation", [st_b], [st_b], out=st[:, 1:2], in_=st[:, 0:1], func=AF.Sqrt, scale=1.0 / scale_n, bias=EPS)
            C.op("dve", "reciprocal", [st_b], [st_b], out=st[:, 2:3], in_=st[:, 1:2])

        with ExitStack() as B:
            wg_sb, wg_b, ld_wg = load_w(B, "w_gates", w_in_d, D, G_OFF, G_OFF + 2048, "w_gates", defer=True)
            woa_sb, woa_b, ld_woa = load_w(B, "w_oa", w_o_attn_d, 512, 0, D, "w_oa", defer=True)
            wco_sb, wco_b, ld_wco = load_w(B, "w_co", w_conv_out_d, 512, 0, D, "w_co", defer=True)
            wout_sb, wout_b, ld_wout = load_w(B, "w_outb", w_out_d, D, 0, D, "w_outb", defer=True)
            wqx_sb, wqx_b, ld_wqx = load_w(B, "w_qxb", w_q_x_d, D, 0, D, "w_qxb", defer=True)
            wox_sb, wox_b, ld_wox = load_w(B, "w_oxb", w_o_x_d, D, 0, D, "w_oxb", defer=True)
            kmT = sb(B, "kmT", [128, 8, 256], BF16)
            vm = sb(B, "vm", [128, 2, D], BF16)
            kmT_b = C.buf("kmT")
            vm_b = C.buf("vm")
            gfm = sb(B, "gfmB", [128, 8], F32)
            gxm = sb(B, "gxm", [128, 8], F32)
            gmm = sb(B, "gmm", [128, 8], F32)
            gmo = sb(B, "gmo", [128, 8], F32)
            wr_sb = sb(B, "wr_sb", [128, 8, 36], F32)
            br_sb = sb(B, "br_sb", [128, 36], F32)
            b_smB = C.buf("smallB")
            C.dma("sp", gfm[:], g_mix_d[:, :], [], [b_smB])
            C.dma("sp", gxm[:], g_x_d[:, :], [], [b_smB])
            C.dma("sp", gmm[:], g_mem_d[:, :], [], [b_smB])
            C.dma("sp", gmo[:], g_moe_d[:, :], [], [b_smB])
            C.dma("sp", wr_sb[:, :, :], w_router_d.rearrange("(c p) n -> p c n", p=128), [], [b_smB])
            C.dma("sp", br_sb[:], bc(b_router_d[0:1, :], [128, 36]), [], [b_smB])
            stB = sb(B, "statB", [128, 8], F32)
            stB_b = C.buf("statB")
            pb = [ps(B, "pb%d" % j, [128, 512], F32) for j in range(7)]
            pb_b = C.bufs("pb", 7)
            pTPb = ps(B, "pTPb", [128, 1024], BF16)
            pTPb_b = C.buf("pTPb")

            sqj_ref = []

            def norm_chain(x_ap, x_bufs, hn_ap, hn_bufs):
                if sqj_ref:
                    rms_rstd(x_ap, x_bufs, sqj_ref[0][:, :], [sqj_ref[1]], stB, stB_b)
                else:
                    rms_rstd(x_ap, x_bufs, hn_ap, hn_bufs, stB, stB_b)
                C.op("act", "activation", x_bufs + [stB_b], hn_bufs, out=hn_ap, in_=x_ap, func=AF.Copy, scale=stB[:, 2:3])

            def norm_trans(hn_ap, hn_bufs, g_sb, dstT, dst_bufs, col0):
                for c in range(8):
                    C.op("pe", "transpose", hn_bufs + [b_const], [pTPb_b], sig=(c == 7), out=pTPb[:, c * 128:(c + 1) * 128],
                         in_=hn_ap[:, c * 128:(c + 1) * 128], identity=identb[:])
                C.op("dve", "tensor_tensor", [pTPb_b, b_smB], dst_bufs, out=dstT[:, :, col0:col0 + 128],
                     in0=pTPb[:, :].rearrange("p (c n) -> p c n", c=8), in1=bc(g_sb[:, :].unsqueeze(2), [128, 8, 128]), op=ALU.mult)

            def norm_T(x_ap, x_bufs, hn_ap, hn_bufs, g_sb, dstT, dst_bufs, col0):
                norm_chain(x_ap, x_bufs, hn_ap, hn_bufs)
                norm_trans(hn_ap, hn_bufs, g_sb, dstT, dst_bufs, col0)

            with ExitStack() as BK:
                wkv_sb, wkv_b = load_w(BK, "w_kvb", w_kv_x_d, D, 0, 2 * D, "w_kvb")
                for ld in (ld_wg, ld_woa, ld_wco, ld_wout, ld_wqx, ld_wox):
                    ld()
                memt2 = [sb(BK, "memt%d" % j, [128, D], F32) for j in range(2)]
                memt2_b = C.bufs("memt", 2)
                memh = sb(BK, "memh", [128, D], BF16)
                memh_b = C.buf("memh")
                memnT = sb(BK, "memnT", [128, 8, 256], BF16)
                memnT_b = C.buf("memnT")
                for mt in range(2):
                    C.dma("sp", memt2[mt][:], mem_d[mt * 128:(mt + 1) * 128, :], [], [memt2_b[mt]])
                for mt in range(2):
                    norm_T(memt2[mt][:, :], [memt2_b[mt]], memh[:, :], [memh_b], gmm, memnT, [memnT_b], mt * 128)
                for jx in range(8):
                    bk = jx % 2
                    for kc in range(8):
                        C.op("pe", "matmul", [wkv_b, memnT_b], [pb_b[bk]], sig=(kc == 7), out=pb[bk][:, 0:256],
                             lhsT=wkv_sb[:, kc, jx * 128:(jx + 1) * 128], rhs=memnT[:, kc, :], start=(kc == 0), stop=(kc == 7))
                    C.op("act", "activation", [pb_b[bk]], [kmT_b], out=kmT[:, jx, :], in_=pb[bk][:, 0:256], func=AF.Copy)
                for mc in range(2):
                    for cb in range(2):
                        bk = 2 + (mc * 2 + cb) % 2
                        for kc in range(8):
                            C.op("pe", "matmul", [wkv_b, memnT_b], [pb_b[bk]], sig=(kc == 7), out=pb[bk][:, :],
                                 lhsT=memnT[:, kc, mc * 128:(mc + 1) * 128], rhs=wkv_sb[:, kc, D + cb * 512:D + (cb + 1) * 512],
                                 start=(kc == 0), stop=(kc == 7))
                        C.op("act", "activation", [pb_b[bk]], [vm_b], out=vm[:, mc, cb * 512:(cb + 1) * 512], in_=pb[bk][:, :], func=AF.Copy)
                C.barrier()

            xt4 = sb(B, "xt4", [128, 4, D], F32)
            xt4_b = C.bufs("xt4", 4)
            hnB = [sb(B, "hnB%d" % j, [128, D], BF16) for j in range(4)]
            hnB_b = C.bufs("hnB", 4)
            sqj = sb(B, "sqj", [128, D], BF16)
            sqj_b = C.buf("sqj")
            sqj_ref.extend([sqj, sqj_b])
            bufA = sb(B, "bufA", [128, 8, 512], BF16)
            bufA_b = C.bufs("bufA", 4)
            bufB = sb(B, "bufB", [128, 8, 512], BF16)
            bufB_b = C.bufs("bufB", 8)
            bufC = sb(B, "bufC", [128, 8, 512], BF16)
            bufC_b = C.bufs("bufC", 8)
            aoTb = sb(B, "aoTb", [128, 4, 512], BF16)
            aoTb_b = C.buf("aoTb")
            zTb = sb(B, "zTb", [128, 4, 512], BF16)
            zTb_b = C.buf("zTb")
            sgA = sb(B, "sgA", [128, 512], F32)
            sgB = sb(B, "sgB", [128, 512], F32)
            sgA_b = C.buf("sgA")
            sgB_b = C.buf("sgB")
            PT2 = sb(B, "PT2", [128, 2, 512], BF16)
            PT2_b = C.bufs("PT2", 2)
            rden2 = sb(B, "rden2", [128, 512], F32)
            rden2_b = C.buf("rden2")
            xn32s = [sb(B, "xn32_%d" % j, [128, D], F32) for j in range(2)]
            xn32s_b = C.bufs("xn32", 2)
            xnT32 = sb(B, "xnT32", [128, 8, 128], F32)
            xnT32_b = C.buf("xnT32")
            xnTb = [sb(B, "xnTb%d" % j, [128, 8, 128], BF16) for j in range(2)]
            xnTb_b = C.bufs("xnTb", 2)
            rt_ = sb(B, "rtr", [128, 128], F32)
            rt_b2 = C.buf("rtr")

            def load_x(blk, t):
                i = blk * 4 + t
                C.dma("sp", xt4[:, t, :], x_d[i * 128:(i + 1) * 128, :], [], [xt4_b[t]])

            def load_aoz(blk):
                for t in range(4):
                    i = blk * 4 + t
                    C.dma("sp", aoTb[:, :, t * 128:(t + 1) * 128], ao_scr[i].rearrange("p (c n) -> p c n", c=4), [ao_scr_b[i]], [aoTb_b])
                C.dma("sp", zTb[:, :, :], z_scr[blk].rearrange("p (c n) -> p c n", c=4), [z_scr_b[blk]], [zTb_b])

            def pipelined_norms(g_sb, ready=None):
                def ch(t):
                    norm_chain(xt4[:, t, :], [xt4_b[t]], hnB[t][:, :], [hnB_b[t]])

                def tr(t):
                    norm_trans(hnB[t][:, :], [hnB_b[t]], g_sb, bufA, [bufA_b[t]], t * 128)
                return ch, tr

            for t in range(4):
                load_x(0, t)
            load_aoz(0)
            for blk in range(NSB):
                ch, tr = pipelined_norms(gfm)
                for t in range(4):
                    ch(t)
                for t in range(4):
                    tr(t)
                for fo in range(8):
                    fs_ = slice(fo * 128, (fo + 1) * 128)
                    b0, b1, b2 = (0, 1, 2) if fo % 2 == 0 else (4, 5, 6)
                    for c in range(4):
                        C.op("pe", "matmul", [woa_b, aoTb_b], [pb_b[b0]], sig=(c == 3), out=pb[b0][:, :], lhsT=woa_sb[:, c, fs_], rhs=aoTb[:, c, :],
                             start=(c == 0), stop=(c == 3))
                    for c in range(4):
                        C.op("pe", "matmul", [wco_b, zTb_b], [pb_b[b1]], sig=(c == 3), out=pb[b1][:, :], lhsT=wco_sb[:, c, fs_], rhs=zTb[:, c, :],
                             start=(c == 0), stop=(c == 3))
                    for kc in range(8):
                        C.op("pe", "matmul", [wg_b] + bufA_b, [pb_b[b2]], sig=(kc == 7), out=pb[b2][:, :], lhsT=wg_sb[:, kc, fs_], rhs=bufA[:, kc, :],
                             start=(kc == 0), stop=(kc == 7))
                    for kc in range(8):
                        C.op("pe", "matmul", [wg_b] + bufA_b, [pb_b[3]], sig=(kc == 7), out=pb[3][:, :],
                             lhsT=wg_sb[:, kc, D + fo * 128:D + (fo + 1) * 128], rhs=bufA[:, kc, :], start=(kc == 0), stop=(kc == 7))
                    C.op("act", "activation", [pb_b[b2]], [sgA_b], out=sgA[:], in_=pb[b2][:, :], func=AF.Sigmoid)
                    C.op("act", "activation", [pb_b[3]], [sgB_b], out=sgB[:], in_=pb[3][:, :], func=AF.Sigmoid)
                    C.op("dve", "tensor_tensor", [pb_b[b0], sgA_b], [sgA_b], out=sgA[:], in0=pb[b0][:, :], in1=sgA[:], op=ALU.mult)
                    C.op("dve", "tensor_tensor", [pb_b[b1], sgB_b], [sgB_b], out=sgB[:], in0=pb[b1][:, :], in1=sgB[:], op=ALU.mult)
                    C.op("pool", "tensor_tensor", [sgA_b, sgB_b], [bufB_b[fo]], out=bufB[:, fo, :], in0=sgA[:], in1=sgB[:], op=ALU.add)
                if blk + 1 < NSB:
                    load_aoz(blk + 1)
                ch, tr = pipelined_norms(gxm)
                for t in range(4):
                    if t >= 2:
                        tr(t - 2)
                    for cb in range(2):
                        bk = 4 + (t * 2 + cb) % 2
                        for kc in range(8):
                            C.op("pe", "matmul", [wout_b, bufB_b[kc]], [pb_b[bk]], sig=(kc == 7), out=pb[bk][:, :],
                                 lhsT=bufB[:, kc, t * 128:(t + 1) * 128], rhs=wout_sb[:, kc, cb * 512:(cb + 1) * 512], start=(kc == 0), stop=(kc == 7))
                        C.op("dve", "tensor_tensor", [pb_b[bk], xt4_b[t]], [xt4_b[t]], out=xt4[:, t, cb * 512:(cb + 1) * 512], in0=pb[bk][:, :],
                             in1=xt4[:, t, cb * 512:(cb + 1) * 512], op=ALU.add)
                    ch(t)
                tr(2)
                tr(3)
                for fo in range(8):
                    bk = 4 + fo % 2
                    for kc in range(8):
                        C.op("pe", "matmul", [wqx_b] + bufA_b, [pb_b[bk]], sig=(kc == 7), out=pb[bk][:, :],
                             lhsT=wqx_sb[:, kc, fo * 128:(fo + 1) * 128], rhs=bufA[:, kc, :], start=(kc == 0), stop=(kc == 7))
                    C.op("act", "activation", [pb_b[bk]], [bufB_b[fo]], out=bufB[:, fo, :], in_=pb[bk][:, :], func=AF.Copy)
                for h in range(4):
                    for mc in range(2):
                        for dc in range(2):
                            C.op("pe", "matmul", [kmT_b, bufB_b[h * 2 + dc]], [pb_b[mc]], sig=(dc == 1), out=pb[mc][:, :],
                                 lhsT=kmT[:, h * 2 + dc, mc * 128:(mc + 1) * 128], rhs=bufB[:, h * 2 + dc, :], start=(dc == 0), stop=(dc == 1))
                        C.op("act", "activation", [pb_b[mc]], [PT2_b[mc]], out=PT2[:, mc, :], in_=pb[mc][:, :], func=AF.Exp, scale=1.0 / 16.0)
                    for mc in range(2):
                        C.op("pe", "matmul", [b_const, PT2_b[mc]], [pb_b[2]], sig=(mc == 1), out=pb[2][:, :], lhsT=onesb[:], rhs=PT2[:, mc, :],
                             start=(mc == 0), stop=(mc == 1))
                    C.op("dve", "reciprocal", [pb_b[2]], [rden2_b], out=rden2[:], in_=pb[2][:, :])
                    for dvc in range(2):
                        bk = 3 if dvc == 0 else 6
                        for mc in range(2):
                            C.op("pe", "matmul", [vm_b, PT2_b[mc]], [pb_b[bk]], sig=(mc == 1), out=pb[bk][:, :],
                                 lhsT=vm[:, mc, h * 256 + dvc * 128:h * 256 + (dvc + 1) * 128], rhs=PT2[:, mc, :], start=(mc == 0), stop=(mc == 1))
                        C.op("dve", "tensor_tensor", [pb_b[bk], rden2_b], [bufC_b[h * 2 + dvc]], out=bufC[:, h * 2 + dvc, :], in0=pb[bk][:, :],
                             in1=rden2[:], op=ALU.mult)
                def wox(t):
                    i = blk * 4 + t
                    for cb in range(2):
                        bk = 4 + (t * 2 + cb) % 2
                        for kc in range(8):
                            C.op("pe", "matmul", [wox_b, bufC_b[kc]], [pb_b[bk]], sig=(kc == 7), out=pb[bk][:, :],
                                 lhsT=bufC[:, kc, t * 128:(t + 1) * 128], rhs=wox_sb[:, kc, cb * 512:(cb + 1) * 512], start=(kc == 0), stop=(kc == 7))
                        C.op("dve", "tensor_tensor", [pb_b[bk], xt4_b[t]], [xt4_b[t]], out=xt4[:, t, cb * 512:(cb + 1) * 512], in0=pb[bk][:, :],
                             in1=xt4[:, t, cb * 512:(cb + 1) * 512], op=ALU.add)
                    C.dma("sp", x2_scr[i * 128:(i + 1) * 128, :], xt4[:, t, :], [xt4_b[t]], [x2_scr_b[i]], sem_from=xt4_b[t])
                    xn32, xn32_b = xn32s[t % 2], xn32s_b[t % 2]
                    rms_rstd(xt4[:, t, :], [xt4_b[t]], sqj[:, :], [sqj_b], stB, stB_b)
                    C.op("act", "activation", [xt4_b[t], stB_b], [xn32_b], out=xn32[:, :], in_=xt4[:, t, :], func=AF.Copy, scale=stB[:, 2:3])

                def b6(t):
                    i = blk * 4 + t
                    xn32, xn32_b = xn32s[t % 2], xn32s_b[t % 2]
                    for c in range(8):
                        C.op("pe", "transpose", [xn32_b, b_const], [pb_b[c // 4]], sig=(c % 4 == 3), out=pb[c // 4][:, (c % 4) * 128:(c % 4 + 1) * 128],
                             in_=xn32[:, c * 128:(c + 1) * 128], identity=identf[:])
                    for hh in range(2):
                        C.op("dve", "tensor_tensor", [pb_b[hh], b_smB], [xnT32_b], out=xnT32[:, hh * 4:hh * 4 + 4, :],
                             in0=pb[hh][:, :].rearrange("p (c n) -> p c n", c=4), in1=bc(gmo[:, hh * 4:hh * 4 + 4].unsqueeze(2), [128, 4, 128]), op=ALU.mult)
                    xj = i % 2
                    C.op("pool", "tensor_copy", [xnT32_b], [xnTb_b[xj]], out=xnTb[xj][:, :, :], in_=xnT32[:, :, :])
                    C.dma("sp", xn_scr[i].rearrange("p (c n) -> p c n", c=8), xnTb[xj][:, :, :], [xnTb_b[xj]], [xn_scr_b[i]], sem_from=xnTb_b[xj])
                    for kc in range(8):
                        C.op("pe", "matmul", [xnT32_b, b_smB], [pb_b[2]], out=pb[2][:, 0:36], lhsT=xnT32[:, kc, :], rhs=wr_sb[:, kc, :],
                             start=(kc == 0), stop=(kc == 7))
                    C.op("pe", "matmul", [b_const], [pb_b[3]], sig=True, out=pb[3][:, 0:128], lhsT=identb[:], rhs=identb[:], start=True, stop=True)
                    C.op("dve", "tensor_tensor", [pb_b[2], b_smB], [lgall_b], out=lgall[:, i, :], in0=pb[2][:, 0:36], in1=br_sb[:, :], op=ALU.add)
                wox(0)
                wox(1)
                b6(0)
                wox(2)
                b6(1)
                wox(3)
                b6(2)
                if blk + 1 < NSB:
                    for t in range(3):
                        load_x(blk + 1, t)
                b6(3)
                if blk + 1 < NSB:
                    load_x(blk + 1, 3)
            C.barrier()

        if stop_after == "B":
            db = C.buf("dbgB")
            C.dma("sp", dbg_out["x2"][:, :], x2_scr[:, :], x2_scr_b, [db])
            C.dma("sp", dbg_out["comb"].rearrange("p (t e) -> p t e", t=NT), comb[:, :, :], comb_b, [db])
            C.dma("sp", dbg_out["xn"].rearrange("(t p) n -> p t n", p=128), xn_scr.rearrange("t p n -> p t n"), xn_scr_b, [db])
            C.final_wait("sp", [db])
            return nc

        with ExitStack() as CC:
            xnT = sb(CC, "xnT", [128, 8, S], BF16)
            xnT_blk = C.bufs("xnT", NSB)
            xnT_b = [xnT_blk[i // 4] for i in range(NT)]
            for i in range(NT):
                C.dma("sp", xnT[:, :, i * 128:(i + 1) * 128], xn_scr[i].rearrange("p (c n) -> p c n", c=8), [xn_scr_b[i]], [xnT_b[i]])
            ysb = sb(CC, "ysb", [128, 16, D], F32)
            ysb_b = C.bufs("ysb", 16)
            NSLOT = 3
            wgs = [sb(CC, "wgs%d" % j, [128, 8, 256], BF16) for j in range(NSLOT)]
            wus = [sb(CC, "wus%d" % j, [128, 8, 256], BF16) for j in range(NSLOT)]
            wds = [sb(CC, "wds%d" % j, [128, 2, D], BF16) for j in range(NSLOT)]
            wslot_b = C.bufs("wslot", NSLOT)
            actT = [sb(CC, "actT%d" % j, [128, 2, 512], BF16) for j in range(2)]
            actT_b = [C.bufs("actT%d_" % j, 2) for j in range(2)]
            ssb = [sb(CC, "ssb%d" % j, [128, 512], BF16) for j in range(2)]
            ssb_b = C.bufs("ssb", 2)
            x2t = [sb(CC, "x2t%d" % j, [128, D], F32) for j in range(2)]
            x2t_b = C.bufs("x2t", 2)
            ot = [sb(CC, "ot%d" % j, [128, D], F32) for j in range(2)]
            ot_b = C.bufs("ot", 2)
            gfin = sb(CC, "gfin", [128, D], F32)
            gfin_b = C.buf("gfin")
            C.dma("sp", gfin[:], bc(g_final_d[0:1, :], [128, D]), [], [gfin_b])
            stC = sb(CC, "statC", [128, 8], F32)
            stC_b = C.buf("statC")
            pg = [ps(CC, "pg%d" % j, [128, 512], F32) for j in range(2)]
            pu = [ps(CC, "pu%d" % j, [128, 512], F32) for j in range(2)]
            py = [ps(CC, "py%d" % j, [128, 1024], F32) for j in range(2)]
            pg_b = C.bufs("pg", 2)
            pu_b = C.bufs("pu", 2)
            py_b = C.bufs("py", 2)

            with ExitStack() as BR:
                T = NT
                o1 = ot[1]
                gmax = o1[:, 0:32]
                oneh = o1[:, 32:160].rearrange("p (t g) -> p t g", g=4)
                dgl = o1[:, 160:288].rearrange("p (t g) -> p t g", g=4)
                sume = o1[:, 288:320]
                ggate = o1[:, 320:352]
                elsel = o1[:, 352:608].rearrange("p (t e) -> p t e", e=8)
                m8 = o1[:, 608:864].rearrange("p (t e) -> p t e", e=8)
                sc1 = o1[:, 864:992].rearrange("p (k t) -> p k t", k=4)
                tmpe = ot[0][:, :].rearrange("p (t f) -> p t f", f=32)
                eqa = x2t[0][:, 0:256].rearrange("p (t e) -> p t e", e=8)
                eqb = x2t[0][:, 256:512].rearrange("p (t e) -> p t e", e=8)
                rb_ = C.buf("routing")
                rr, ww = [lgall_b, rb_], [rb_, ot_b[0], ot_b[1], x2t_b[0]]
                gl = lgall[:, :, 0:4]
                el4 = lgall[:, :, 4:36].rearrange("p t (g e) -> p t g e", g=4)
                C.op("dve", "tensor_reduce", rr, ww, out=gmax[:, :], in_=gl, axis=AX.X, op=ALU.max)
                C.op("dve", "tensor_tensor", rr, ww, out=oneh[:, :, :], in0=gl, in1=bc(gmax[:, :].unsqueeze(2), [128, T, 4]), op=ALU.is_equal)
                C.op("dve", "tensor_tensor", rr, ww, out=dgl[:, :, :], in0=gl, in1=bc(gmax[:, :].unsqueeze(2), [128, T, 4]), op=ALU.subtract)
                C.op("act", "activation", rr, ww, out=dgl[:, :, :], in_=dgl[:, :, :], func=AF.Exp)
                C.op("dve", "tensor_reduce", rr, ww, out=sume[:, :], in_=dgl[:, :, :], axis=AX.X, op=ALU.add)
                C.op("dve", "reciprocal", rr, ww, out=ggate[:, :], in_=sume[:, :])
                C.op("dve", "tensor_tensor", rr, ww, out=tmpe[:, :, :].rearrange("p t (g e) -> p t g e", g=4), in0=el4,
                     in1=bc(oneh[:, :, :].unsqueeze(3), [128, T, 4, 8]), op=ALU.mult)
                C.op("dve", "tensor_reduce", rr, ww, out=elsel[:, :, :], in_=tmpe[:, :, :].rearrange("p t (g e) -> p t e g", g=4), axis=AX.X, op=ALU.add)
                for t in range(T):
                    C.op("dve", "max", rr, ww, sig=(t == T - 1), out=m8[:, t, :], in_=elsel[:, t, :])
                m1 = m8[:, :, 0]
                m2 = m8[:, :, 1]
                e2, s1, w1, w2 = sc1[:, 0, :], sc1[:, 1, :], sc1[:, 2, :], sc1[:, 3, :]
                C.op("dve", "tensor_tensor", rr, ww, out=e2, in0=m2, in1=m1, op=ALU.subtract)
                C.op("act", "activation", rr, ww, out=e2, in_=e2, func=AF.Exp)
                C.op("dve", "tensor_scalar", rr, ww, out=s1, in0=e2, scalar1=1.0, scalar2=None, op0=ALU.add)
                C.op("dve", "reciprocal", rr, ww, out=s1, in_=s1)
                C.op("dve", "tensor_tensor", rr, ww, out=w1, in0=s1, in1=ggate[:, :], op=ALU.mult)
                C.op("dve", "tensor_tensor", rr, ww, out=w2, in0=w1, in1=e2, op=ALU.mult)
                C.op("dve", "tensor_tensor", rr, ww, out=eqa[:, :, :], in0=elsel[:, :, :], in1=bc(m1.unsqueeze(2), [128, T, 8]), op=ALU.is_equal)
                C.op("dve", "tensor_tensor", rr, ww, out=eqa[:, :, :], in0=eqa[:, :, :], in1=bc(w1.unsqueeze(2), [128, T, 8]), op=ALU.mult)
                C.op("dve", "tensor_tensor", rr, ww, out=eqb[:, :, :], in0=elsel[:, :, :], in1=bc(m2.unsqueeze(2), [128, T, 8]), op=ALU.is_equal)
                C.op("dve", "tensor_tensor", rr, ww, out=eqb[:, :, :], in0=eqb[:, :, :], in1=bc(w2.unsqueeze(2), [128, T, 8]), op=ALU.mult)
                C.op("dve", "tensor_tensor", rr, ww, out=eqa[:, :, :], in0=eqa[:, :, :], in1=eqb[:, :, :], op=ALU.add)
                C.op("dve", "tensor_tensor", rr, comb_b, out=comb[:, :, :].rearrange("p t (g e) -> p t g e", g=4),
                     in0=bc(oneh[:, :, :].unsqueeze(3), [128, T, 4, 8]), in1=bc(eqa[:, :, :].unsqueeze(2), [128, T, 4, 8]), op=ALU.mult)


            for hf in range(2):
                units = [(e, tb) for e in range(32) for tb in range(4)]

                def load_expert(e):
                    sl = e % NSLOT
                    C.dma("pool", wgs[sl][:, :, :], w_eg_d[e].rearrange("(c p) f -> p c f", p=128), [], [wslot_b[sl]])
                    C.dma("pool", wus[sl][:, :, :], w_eu_d[e].rearrange("(c p) f -> p c f", p=128), [], [wslot_b[sl]])
                    C.dma("pool", wds[sl][:, :, :], w_ed_d[e].rearrange("(c p) n -> p c n", p=128), [], [wslot_b[sl]])

                gu_cnt = [0]

                def gu(n):
                    e, tb = units[n]
                    sl = e % NSLOT
                    a = n % 2
                    tok0 = (hf * 16 + tb * 4) * 128
                    xb = xnT_b[hf * 16 + tb * 4:hf * 16 + tb * 4 + 4]
                    for fc in range(2):
                        k2 = gu_cnt[0] % 2
                        gu_cnt[0] += 1
                        for kc in range(8):
                            C.op("pe", "matmul", [wslot_b[sl]] + xb, [pg_b[k2]], sig=(kc == 7), out=pg[k2][:, :],
                                 lhsT=wgs[sl][:, kc, fc * 128:(fc + 1) * 128], rhs=xnT[:, kc, tok0:tok0 + 512], start=(kc == 0), stop=(kc == 7))
                        for kc in range(8):
                            C.op("pe", "matmul", [wslot_b[sl]] + xb, [pu_b[k2]], sig=(kc == 7), out=pu[k2][:, :],
                                 lhsT=wus[sl][:, kc, fc * 128:(fc + 1) * 128], rhs=xnT[:, kc, tok0:tok0 + 512], start=(kc == 0), stop=(kc == 7))
                        C.op("act", "activation", [pg_b[k2]], [ssb_b[k2]], out=ssb[k2][:], in_=pg[k2][:, :], func=AF.Silu)
                        C.op("dve", "tensor_tensor", [pu_b[k2], ssb_b[k2]], [actT_b[a][fc]], out=actT[a][:, fc, :], in0=pu[k2][:, :], in1=ssb[k2][:], op=ALU.mult)

                def down(n):
                    e, tb = units[n]
                    sl = e % NSLOT
                    a = n % 2
                    for t in range(4):
                        yt = tb * 4 + t
                        i = hf * 16 + yt
                        k2 = (n * 4 + t) % 2
                        for cb in range(2):
                            for fc in range(2):
                                C.op("pe", "matmul", [wslot_b[sl], actT_b[a][fc]], [py_b[k2]], sig=(cb == 1 and fc == 1),
                                     out=py[k2][:, cb * 512:(cb + 1) * 512], lhsT=actT[a][:, fc, t * 128:(t + 1) * 128],
                                     rhs=wds[sl][:, fc, cb * 512:(cb + 1) * 512], start=(fc == 0), stop=(fc == 1))
                        if e == 0:
                            j = yt % 2
                            C.dma("sp", x2t[j][:], x2_scr[i * 128:(i + 1) * 128, :], [x2_scr_b[i]], [x2t_b[j]])
                            C.op("dve", "scalar_tensor_tensor", [py_b[k2], comb_b[i], x2t_b[j]], [ysb_b[yt]], out=ysb[:, yt, :], in0=py[k2][:, :],
                                 scalar=comb[:, i, e:e + 1], in1=x2t[j][:], op0=ALU.mult, op1=ALU.add)
                        else:
                            C.op("dve", "scalar_tensor_tensor", [py_b[k2], comb_b[i], ysb_b[yt]], [ysb_b[yt]], out=ysb[:, yt, :], in0=py[k2][:, :],
                                 scalar=comb[:, i, e:e + 1], in1=ysb[:, yt, :], op0=ALU.mult, op1=ALU.add)

                def tail(yt):
                    i = hf * 16 + yt
                    j = yt % 2
                    rms_rstd(ysb[:, yt, :], [ysb_b[yt]], ot[j][:, :], [ot_b[j]], stC, stC_b)
                    C.op("dve", "scalar_tensor_tensor", [ysb_b[yt], stC_b, gfin_b], [ot_b[j]], out=ot[j][:], in0=ysb[:, yt, :], scalar=stC[:, 2:3],
                         in1=gfin[:], op0=ALU.mult, op1=ALU.mult)
                    C.dma("sp", out_d[i * 128:(i + 1) * 128, :], ot[j][:], [ot_b[j]], [out_b])

                load_expert(0)
                load_expert(1)
                gu(0)
                for n in range(len(units)):
                    e, tb = units[n]
                    if tb == 0 and e + 2 < 32:
                        load_expert(e + 2)
                    if n + 1 < len(units):
                        gu(n + 1)
                    down(n)
                    if e == 31:
                        for t in range(4):
                            tail(tb * 4 + t)
            C.barrier()
        C.final_wait("sp", [out_b])
    return nc


def make_in_maps(inputs):
    f32 = np.float32
    x = np.asarray(inputs["x"], f32)
    mem = np.asarray(inputs["mem"], f32)
    pos = np.asarray(inputs["positions"]).astype(np.int32)
    B = x.shape[0]

    def fm(v, n):
        return np.ascontiguousarray(np.asarray(v, f32).reshape(n, 128).T)

    w_re = np.asarray(inputs["w_router_expert"], f32)[0]
    w_router = np.concatenate([np.asarray(inputs["w_router_group"], f32)[0],
                               np.ascontiguousarray(w_re.transpose(1, 0, 2)).reshape(D, 32)], axis=1)
    b_router = np.concatenate([np.asarray(inputs["b_router_group"], f32)[0].reshape(-1),
                               np.asarray(inputs["b_router_expert"], f32)[0].reshape(-1)])[None, :]
    convw = np.asarray(inputs["conv_w"], f32)[0]
    convwT = np.ascontiguousarray(convw.T.reshape(4, 128, 31).transpose(1, 0, 2))
    inv_freq = (10000.0 ** (-np.arange(0, 64, 2, dtype=np.float64) / 64)).astype(np.float32)
    invf = np.tile((inv_freq.astype(np.float64) / (2 * np.pi)).astype(f32)[None, :], (128, 1))
    shared = {
        "w_in": np.ascontiguousarray(np.asarray(inputs["w_in"], f32)[0]),
        "w_o_attn": np.ascontiguousarray(np.asarray(inputs["w_o_attn"], f32)[0]),
        "convw": convwT,
        "convb": fm(np.asarray(inputs["conv_b"])[0], 4),
        "lng": fm(np.asarray(inputs["conv_ln_g"])[0], 4),
        "lnb": fm(np.asarray(inputs["conv_ln_b"])[0], 4),
        "w_conv_out": np.ascontiguousarray(np.asarray(inputs["w_conv_out"], f32)[0]),
        "w_out": np.ascontiguousarray(np.asarray(inputs["w_out"], f32)[0]),
        "w_q_x": np.ascontiguousarray(np.asarray(inputs["w_q_x"], f32)[0]),
        "w_kv_x": np.ascontiguousarray(np.asarray(inputs["w_kv_x"], f32)[0]),
        "w_o_x": np.ascontiguousarray(np.asarray(inputs["w_o_x"], f32)[0]),
        "g_mix": fm(np.asarray(inputs["norm_mix_g"])[0], 8),
        "g_x": fm(np.asarray(inputs["norm_x_g"])[0], 8),
        "g_mem": fm(np.asarray(inputs["norm_mem_g"])[0], 8),
        "g_moe": fm(np.asarray(inputs["norm_moe_g"])[0], 8),
        "w_router": np.ascontiguousarray(w_router),
        "b_router": np.ascontiguousarray(b_router.astype(f32)),
        "w_eg": np.ascontiguousarray(np.asarray(inputs["w_exp_gate"], f32)[0].reshape(32, D, 256)),
        "w_eu": np.ascontiguousarray(np.asarray(inputs["w_exp_up"], f32)[0].reshape(32, D, 256)),
        "w_ed": np.ascontiguousarray(np.asarray(inputs["w_exp_down"], f32)[0].reshape(32, 256, D)),
        "g_final": np.ascontiguousarray(np.asarray(inputs["norm_final_g"], f32).reshape(1, D)),
        "ident": np.eye(128, dtype=f32),
        "invf": invf,
        "pow2": np.tile((2.0 ** -np.arange(BIS_ITERS + 2)).astype(f32)[None, :], (128, 1)),
    }
    maps = []
    for b in range(B):
        m = dict(shared)
        m["x"] = np.ascontiguousarray(x[b])
        m["mem"] = np.ascontiguousarray(mem[b])
        m["pos"] = np.ascontiguousarray(pos[b].reshape(NT, 128).T)
        maps.append(m)
    return maps


def kernel(**inputs):
    maps = make_in_maps(inputs)
    nc = build()
    res = run_bass_kernel_spmd(nc, maps, core_ids=list(range(len(maps))))
    return np.stack([np.asarray(r["out"], np.float32) for r in res.results], axis=0)
```

```python
import bisect
from contextlib import ExitStack

import numpy as np
import concourse.bass as bass
import concourse.mybir as mybir
from concourse.bass_utils import run_bass_kernel_spmd

F32 = mybir.dt.float32
BF16 = mybir.dt.bfloat16
I32 = mybir.dt.int32
AF = mybir.ActivationFunctionType
ALU = mybir.AluOpType
AX = mybir.AxisListType

S = 4096
D = 1024
NT = S // 128
NSB = S // 512
EPS = 1e-6
NEG_BIG = -30000.0
BIS_ITERS = 18
A_OFF = 2120
B_OFF = 2632
G_OFF = 3144
NA_COLS = 3144


class SemBox:
    __slots__ = ("name", "sem", "count")

    def __init__(self, name):
        self.name = name
        self.sem = None
        self.count = 0


class Buf:
    __slots__ = ("name", "last_w", "readers", "box")

    def __init__(self, name, box=None):
        self.name = name
        self.last_w = None
        self.readers = []
        self.box = box if box is not None else SemBox(name)


class Ctx:
    COMPUTE = ("pe", "act", "dve", "pool")

    def __init__(self, nc, es):
        self.nc = nc
        self.es = es
        self.eng = {"pe": nc.tensor, "act": nc.scalar, "dve": nc.vector, "pool": nc.gpsimd, "sp": nc.sync}
        self.sem = {e: es.enter_context(nc.semaphore("s_" + e)) for e in self.COMPUTE}
        self.mile = {e: 0 for e in self.COMPUTE}
        self.nissued = {e: 0 for e in self.eng}
        self.sigpts = {e: ([], []) for e in self.COMPUTE}
        self.last_ins = {e: None for e in self.eng}
        self.last_sig = {e: True for e in self.eng}
        self.waited = {}
        self.dma_sems = []
        self.all_bufs = []

    def buf(self, name):
        b = Buf(name)
        self.all_bufs.append(b)
        return b

    def bufs(self, name, n, share=False):
        if not share:
            return [self.buf("%s%d" % (name, i)) for i in range(n)]
        box = SemBox(name)
        out = []
        for i in range(n):
            b = Buf("%s%d" % (name, i), box)
            self.all_bufs.append(b)
            out.append(b)
        return out

    def _resolve(self, tok):
        if tok[0] == "d":
            return tok[1], tok[2]
        _, e, idx = tok
        idxs, miles = self.sigpts[e]
        k = bisect.bisect_left(idxs, idx)
        if k < len(idxs):
            return self.sem[e], miles[k]
        assert not self.last_sig[e]
        self.last_ins[e].then_inc(self.sem[e], 1)
        self.mile[e] += 1
        idxs.append(self.nissued[e] - 1)
        miles.append(self.mile[e])
        self.last_sig[e] = True
        return self.sem[e], self.mile[e]

    def _wait(self, engname, toks):
        need = {}
        for tok in toks:
            if tok is None:
                continue
            if tok[0] == "c" and tok[1] == engname and engname == "pe":
                continue
            sem, val = self._resolve(tok)
            key = id(sem)
            if key not in need or need[key][1] < val:
                need[key] = (sem, val)
        for key, (sem, val) in need.items():
            wk = (engname, key)
            if self.waited.get(wk, 0) >= val:
                continue
            self.eng[engname].wait_ge(sem, val)
            self.waited[wk] = val

    def _deps(self, engname, reads, writes, waw=True):
        toks = []
        for b in reads:
            toks.append(b.last_w)
        for b in writes:
            if waw:
                if not (b.last_w is not None and b.last_w[0] == "c" and b.last_w[1] == engname):
                    toks.append(b.last_w)
            for r in b.readers:
                if r[0] == "c" and r[1] == engname and engname == "pe":
                    continue
                toks.append(r)
        return toks

    def op(self, engname, method, reads=(), writes=(), sig=None, **kw):
        assert engname in self.COMPUTE
        if sig is None:
            sig = engname != "pe"
        self._wait(engname, self._deps(engname, reads, writes))
        ins = getattr(self.eng[engname], method)(**kw)
        idx = self.nissued[engname]
        self.nissued[engname] += 1
        self.last_ins[engname] = ins
        self.last_sig[engname] = False
        if sig:
            ins.then_inc(self.sem[engname], 1)
            self.mile[engname] += 1
            self.sigpts[engname][0].append(idx)
            self.sigpts[engname][1].append(self.mile[engname])
            self.last_sig[engname] = True
        tok = ("c", engname, idx)
        for b in reads:
            b.readers.append(tok)
        for b in writes:
            b.last_w = tok
            b.readers = []
        return ins

    def dma(self, q, out, in_, reads, writes, waw=False, sem_from=None, **kw):
        assert len(writes) == 1
        wb = (sem_from if sem_from is not None else writes[0]).box
        if wb.sem is None:
            wb.sem = self.es.enter_context(self.nc.semaphore("d_" + wb.name))
            self.dma_sems.append(wb)
        self._wait(q, self._deps(q, reads, writes, waw=waw))
        ins = self.eng[q].dma_start(out=out, in_=in_, **kw)
        ins.then_inc(wb.sem, 16)
        wb.count += 16
        self.nissued[q] += 1
        if q in self.COMPUTE:
            self.last_ins[q] = ins
            self.last_sig[q] = True
        tok = ("d", wb.sem, wb.count)
        for b in reads:
            b.readers.append(tok)
        writes[0].last_w = tok
        writes[0].readers = []
        return ins

    def barrier(self):
        toks = []
        for e in self.COMPUTE:
            if self.nissued[e] > 0 and self.last_ins[e] is not None:
                if not self.last_sig[e]:
                    toks.append(("c", e, self.nissued[e] - 1))
                else:
                    idxs, miles = self.sigpts[e]
                    if idxs:
                        toks.append(("c", e, idxs[-1]))
        for b in self.dma_sems:
            toks.append(("d", b.sem, b.count))
        for e in list(self.COMPUTE) + ["sp"]:
            self._wait(e, toks)

    def final_wait(self, q, bufs):
        self._wait(q, [("d", b.box.sem, b.box.count) for b in bufs if b.box.sem is not None])


def bc(ap, shape):
    return ap.broadcast_to(list(shape))


def build(stop_after="all", dbg=(), a2_tiles=None):
    nc = bass.Bass("TRN2", target_bir_lowering=False)

    def din(name, shape, dt=F32):
        return nc.dram_tensor(name, list(shape), dt, kind="ExternalInput").ap()

    x_d = din("x", [S, D])
    mem_d = din("mem", [256, D])
    pos_d = din("pos", [128, NT], I32)
    w_in_d = din("w_in", [D, 5192])
    w_o_attn_d = din("w_o_attn", [512, D])
    convw_d = din("convw", [128, 4, 31])
    convb_d = din("convb", [128, 4])
    lng_d = din("lng", [128, 4])
    lnb_d = din("lnb", [128, 4])
    w_conv_out_d = din("w_conv_out", [512, D])
    w_out_d = din("w_out", [D, D])
    w_q_x_d = din("w_q_x", [D, D])
    w_kv_x_d = din("w_kv_x", [D, 2 * D])
    w_o_x_d = din("w_o_x", [D, D])
    g_mix_d = din("g_mix", [128, 8])
    g_x_d = din("g_x", [128, 8])
    g_mem_d = din("g_mem", [128, 8])
    g_moe_d = din("g_moe", [128, 8])
    w_router_d = din("w_router", [D, 36])
    b_router_d = din("b_router", [1, 36])
    w_eg_d = din("w_eg", [32, D, 256])
    w_eu_d = din("w_eu", [32, D, 256])
    w_ed_d = din("w_ed", [32, 256, D])
    g_final_d = din("g_final", [1, D])
    ident_d = din("ident", [128, 128])
    invf_d = din("invf", [128, 32])
    pow2_d = din("pow2", [128, BIS_ITERS + 2])

    out_d = nc.dram_tensor("out", [S, D], F32, kind="ExternalOutput").ap()

    def dscr(name, shape, dt):
        return nc.dram_tensor(name, list(shape), dt, kind="Internal").ap()

    qT_scr = dscr("qT_scr", [NT, 128, 512], BF16)
    qiT_scr = dscr("qiT_scr", [NT, 128, 512], BF16)
    z_scr = dscr("z_scr", [NSB, 128, 2048], BF16)
    ao_scr = dscr("ao_scr", [NT, 128, 512], BF16)
    x2_scr = dscr("x2_scr", [S, D], F32)

    dbg_out = {}
    for name, shape, dt in dbg:
        dbg_out[name] = nc.dram_tensor("dbg_" + name, list(shape), dt, kind="ExternalOutput").ap()

    with ExitStack() as es:
        C = Ctx(nc, es)
        out_b = C.buf("out")

        P0 = ExitStack()
        es.enter_context(P0)

        def sb(stack, name, shape, dt):
            return stack.enter_context(nc.sbuf_tensor("sb_" + name, list(shape), dt))

        def ps(stack, name, shape, dt):
            return stack.enter_context(nc.psum_tensor("ps_" + name, list(shape), dt))

        identf = sb(P0, "identf", [128, 128], F32)
        identb = sb(P0, "identb", [128, 128], BF16)
        onesf = sb(P0, "onesf", [128, 128], F32)
        onesb = sb(P0, "onesb", [128, 128], BF16)
        b_const = C.buf("const")
        C.dma("sp", identf[:], ident_d[:, :], [], [b_const])
        C.op("dve", "tensor_copy", [b_const], [b_const], out=identb[:], in_=identf[:])
        C.op("dve", "memset", [], [b_const], ap=onesf[:], constant=1.0 / 512.0)
        C.op("dve", "memset", [], [b_const], ap=onesb[:], constant=1.0)

        PA = ExitStack()
        es.enter_context(PA)
        kT = sb(PA, "kT", [128, 4, S], BF16)
        v_sb = sb(PA, "v_sb", [128, NT, 8, 65], BF16)
        kiT2 = sb(PA, "kiT2", [128, S], BF16)
        wi_sb = sb(PA, "wi_sb", [128, NT, 8], F32)
        kT_b = C.bufs("kT", NT)
        v_b = C.bufs("v", NT)
        kiT_b = C.bufs("kiT", NT)
        wi_b = C.bufs("wi", NT)
        for i in range(NT):
            C.op("pool", "memset", [], [v_b[i]], ap=v_sb[:, i, :, 64:65], constant=1.0)

        with ExitStack() as A1:
            w_sb = sb(A1, "w_inA", [128, 8, NA_COLS], BF16)
            w_b = [C.buf("w_inA")] * 8
            for kc in range(8):
                for (c0, c1) in ((0, 1024), (1024, 2048), (2048, NA_COLS)):
                    C.dma("pool", w_sb[:, kc, c0:c1], w_in_d[kc * 128:(kc + 1) * 128, c0:c1], [], [w_b[kc]])
            gfm = sb(A1, "gfm", [128, 8], F32)
            cwT = sb(A1, "cwT", [128, 4, 31], F32)
            cb4 = sb(A1, "cb4", [128, 4], F32)
            lng4 = sb(A1, "lng4", [128, 4], F32)
            lnb4 = sb(A1, "lnb4", [128, 4], F32)
            invf = sb(A1, "invf", [128, 32], F32)
            posi = sb(A1, "posi", [128, NT], I32)
            posf = sb(A1, "posf", [128, NT], F32)
            b_small = C.buf("smallA")
            C.dma("sp", gfm[:], g_mix_d[:, :], [], [b_small])
            C.dma("sp", cwT[:], convw_d[:, :, :], [], [b_small])
            C.dma("sp", cb4[:], convb_d[:, :], [], [b_small])
            C.dma("sp", lng4[:], lng_d[:, :], [], [b_small])
            C.dma("sp", lnb4[:], lnb_d[:, :], [], [b_small])
            C.dma("sp", invf[:], invf_d[:, :], [], [b_small])
            C.dma("sp", posi[:], pos_d[:, :], [], [b_small])
            cosT = sb(A1, "cosT", [128, NT, 32], F32)
            sinT = sb(A1, "sinT", [128, NT, 32], F32)
            with ExitStack() as T0:
                ua = sb(T0, "ua", [128, NT, 32], F32)
                ub = sb(T0, "ub", [128, NT, 32], F32)
                uci = sb(T0, "uci", [128, NT, 32], I32)
                b_rope = C.buf("ropetab")
                b_ua = C.buf("ua")
                b_ub = C.buf("ub")
                b_uc = C.buf("uc")
                C.op("dve", "tensor_copy", [b_small], [b_ua], out=posf[:], in_=posi[:])
                C.op("dve", "tensor_tensor", [b_ua, b_small], [b_ub], out=ua[:],
                     in0=bc(posf[:, :].unsqueeze(2), [128, NT, 32]), in1=bc(invf[:, :].unsqueeze(1), [128, NT, 32]), op=ALU.mult)
                for (tab, shift) in ((sinT, 0.0), (cosT, 0.25)):
                    C.op("dve", "tensor_scalar", [b_ub], [b_ua], out=ub[:], in0=ua[:], scalar1=shift, scalar2=None, op0=ALU.add)
                    C.op("dve", "tensor_copy", [b_ua], [b_uc], out=uci[:], in_=ub[:])
                    C.op("dve", "tensor_copy", [b_uc], [b_rope], out=tab[:], in_=uci[:])
                    C.op("dve", "tensor_tensor", [b_ua, b_rope], [b_ua], out=ub[:], in0=ub[:], in1=tab[:], op=ALU.subtract)
                    C.op("dve", "tensor_scalar", [b_ua], [b_rope], out=tab[:], in0=ub[:], scalar1=0.5, scalar2=None, op0=ALU.is_gt)
                    C.op("dve", "tensor_tensor", [b_ua, b_rope], [b_ua], out=ub[:], in0=ub[:], in1=tab[:], op=ALU.subtract)
                    C.op("dve", "tensor_scalar", [b_ua], [b_rope], out=tab[:], in0=ub[:], scalar1=-0.5, scalar2=None, op0=ALU.is_lt)
                    C.op("dve", "tensor_tensor", [b_ua, b_rope], [b_ua], out=ub[:], in0=ub[:], in1=tab[:], op=ALU.add)
                    C.op("act", "activation", [b_ua], [b_rope], out=tab[:], in_=ub[:], func=AF.Sin, scale=2.0 * np.pi)

                C.barrier()

            xt = [sb(A1, "xt%d" % j, [128, D], F32) for j in range(2)]
            xt_b = C.bufs("xt", 2)
            hn = [sb(A1, "hn%d" % j, [128, D], BF16) for j in range(2)]
            hn_b = C.bufs("hn", 2)
            st = sb(A1, "stat", [128, 8], F32)
            st_b = C.buf("stat")
            hT = sb(A1, "hT", [128, 8, 512], BF16)
            hT_b = C.bufs("hT", 4)
            rt = [sb(A1, "rt%d" % j, [128, 8, 32], F32) for j in range(4)]
            rt_b = C.bufs("rt", 4)
            rq = [sb(A1, "rq%d" % j, [128, 8, 64], BF16) for j in range(2)]
            rq_b = C.bufs("rq", 2)
            rqk = sb(A1, "rqk", [128, 2, 64], BF16)
            rqk_b = C.buf("rqk")
            qst = [sb(A1, "qst%d" % j, [128, 4, 128], BF16) for j in range(2)]
            qst_b = C.bufs("qst", 2)
            uT = [sb(A1, "uT%d" % j, [128, 4, 542], BF16) for j in range(2)]
            uT_b = [C.bufs("uT%d_" % j, 4) for j in range(2)]
            uTpad_b = C.bufs("uTpad", 2)
            sg = sb(A1, "sg", [128, 512], F32)
            sg_b = C.buf("sg")
            Dw = sb(A1, "Dw", [128, 31, 128], BF16)
            Dw_b = C.buf("Dw")
            co = sb(A1, "co", [128, 4, 512], F32)
            co_b = C.bufs("co", 4)
            sq = [sb(A1, "sq%d" % j, [128, 512], F32) for j in range(2)]
            sq_b = C.bufs("sq", 2)
            mean_sb = sb(A1, "mean_sb", [128, 512], F32)
            m2_sb = sb(A1, "m2_sb", [128, 512], F32)
            rstd_sb = sb(A1, "rstd_sb", [128, 512], F32)
            mean_b = C.buf("mean")
            m2_b = C.buf("m2")
            rstd_b = C.buf("rstdc")
            dtmp = [sb(A1, "dtmp%d" % j, [128, 512], F32) for j in range(2)]
            dtmp_b = C.bufs("dtmp", 2)
            zT = sb(A1, "zT", [128, 4, 512], BF16)
            zT_b = C.buf("zT")
            pA = ps(A1, "pA", [128, 512], F32)
            pB = ps(A1, "pB", [128, 512], F32)
            pC = ps(A1, "pC", [128, 512], F32)
            pM = ps(A1, "pM", [128, 512], F32)
            pE = ps(A1, "pE", [128, 512], F32)
            pT0 = ps(A1, "pT0", [128, 512], F32)
            pT1 = ps(A1, "pT1", [128, 512], F32)
            pTP = ps(A1, "pTP", [128, 1024], BF16)
            pA_b, pB_b, pC_b, pM_b, pE_b, pTP_b = (C.buf(n) for n in ("pA", "pB", "pC", "pM", "pE", "pTP"))
            pT = [pT0, pT1]
            pT_b = C.bufs("pT", 2)
            q_scr_b = C.bufs("qscr", NT, share=True)
            qi_scr_b = C.bufs("qiscr", NT, share=True)
            z_scr_b = C.bufs("zscr", NSB, share=True)

            C.op("pool", "memset", [], [uTpad_b[0]], ap=uT[0][:, :, 0:30], constant=0.0)
            tmc = [0]

            def rope_block(psv, nh, i, dst, dst_bufs, dup=False):
                cos_b = bc(cosT[:, i, :].unsqueeze(1), [128, nh, 32])
                sin_b = bc(sinT[:, i, :].unsqueeze(1), [128, nh, 32])
                x1 = psv[:, :, 0:32]
                x2 = psv[:, :, 32:64]
                pb = psv_buf[0]
                C.op("dve", "tensor_tensor", [pb, b_rope], [rt_b[0]], out=rt[0][:, 0:nh, :], in0=x1, in1=cos_b, op=ALU.mult)
                C.op("dve", "tensor_tensor", [pb, b_rope], [rt_b[1]], out=rt[1][:, 0:nh, :], in0=x2, in1=sin_b, op=ALU.mult)
                C.op("dve", "tensor_tensor", [pb, b_rope], [rt_b[2]], out=rt[2][:, 0:nh, :], in0=x2, in1=cos_b, op=ALU.mult)
                C.op("dve", "tensor_tensor", [pb, b_rope], [rt_b[3]], out=rt[3][:, 0:nh, :], in0=x1, in1=sin_b, op=ALU.mult)
                C.op("pool", "tensor_tensor", [rt_b[0], rt_b[1]], dst_bufs, out=dst[:, :, 0:32], in0=rt[0][:, 0:nh, :], in1=rt[1][:, 0:nh, :], op=ALU.subtract)
                C.op("pool", "tensor_tensor", [rt_b[2], rt_b[3]], dst_bufs, out=dst[:, :, 32:64], in0=rt[2][:, 0:nh, :], in1=rt[3][:, 0:nh, :], op=ALU.add)

            psv_buf = [None]

            def chain(i):
                j = i % 2
                C.dma("sp", xt[j][:], x_d[i * 128:(i + 1) * 128, :], [], [xt_b[j]])
                C.op("act", "activation", [xt_b[j]], [hn_b[j], st_b], out=hn[j][:], in_=xt[j][:], func=AF.Square, accum_out=st[:, 0:1])
                C.op("act", "activation", [st_b], [st_b], out=st[:, 1:2], in_=st[:, 0:1], func=AF.Sqrt, scale=1.0 / D, bias=EPS)
                C.op("dve", "reciprocal", [st_b], [st_b], out=st[:, 2:3], in_=st[:, 1:2])
                C.op("act", "activation", [xt_b[j], st_b], [hn_b[j]], out=hn[j][:], in_=xt[j][:], func=AF.Copy, scale=st[:, 2:3])

            def trans(i):
                j = i % 2
                t = i % 4
                for c in range(8):
                    C.op("pe", "transpose", [hn_b[j], b_const], [pTP_b], sig=(c == 7), out=pTP[:, c * 128:(c + 1) * 128],
                         in_=hn[j][:, c * 128:(c + 1) * 128], identity=identb[:])
                C.op("dve", "tensor_tensor", [pTP_b, b_small], [hT_b[t]], out=hT[:, :, t * 128:(t + 1) * 128],
                     in0=pTP[:, :].rearrange("p (c n) -> p c n", c=8), in1=bc(gfm[:, :].unsqueeze(2), [128, 8, 128]), op=ALU.mult)

            pending = []

            def flush():
                while pending:
                    pending.pop(0)()

            chain(0)
            for sbi in range(NSB):
                cur = sbi % 2
                nxt = 1 - cur
                for t in range(4):
                    i = sbi * 4 + t
                    trans(i)
                    if i + 1 < NT:
                        chain(i + 1)
                    for (name, c0, ncols) in (("q", 0, 512), ("k", 512, 512), ("v", 1024, 512), ("qi", 1536, 512), ("kw", 2048, 72)):
                        bk = tmc[0] % 2
                        tmc[0] += 1
                        for kc in range(8):
                            C.op("pe", "matmul", [hT_b[t], w_b[kc]], [pT_b[bk]], sig=(kc == 7), out=pT[bk][:, 0:512],
                                 lhsT=hT[:, kc, t * 128:(t + 1) * 128], rhs=w_sb[:, kc, c0:c0 + 512], start=(kc == 0), stop=(kc == 7))
                        flush()
                        psv_buf[0] = pT_b[bk]
                        if name == "v":
                            C.op("act", "activation", [pT_b[bk]], [v_b[i]], out=v_sb[:, i, :, 0:64],
                                 in_=pT[bk][:, :].rearrange("p (h d) -> p h d", h=8), func=AF.Copy)
                        elif name == "kw":
                            C.op("act", "activation", [pT_b[bk]], [wi_b[i]], out=wi_sb[:, i, :], in_=pT[bk][:, 64:72], func=AF.Copy)
                            psv = pT[bk][:, 0:64].rearrange("p (h d) -> p h d", h=1)
                            rope_block(psv, 1, i, rqk[:, 0:1, :], [rqk_b])
                            C.op("pool", "tensor_copy", [rqk_b], [rqk_b], out=rqk[:, 1:2, :], in_=rqk[:, 0:1, :])

                            def fin_kw(i=i):
                                C.op("pe", "transpose", [rqk_b, b_const], [pTP_b], sig=True, out=pTP[:, 0:128],
                                     in_=rqk[:, :, :].rearrange("p a d -> p (a d)"), identity=identb[:])
                                C.op("act", "activation", [pTP_b], [kiT_b[i]], out=kiT2[:, i * 128:(i + 1) * 128], in_=pTP[:, 0:128], func=AF.Copy)
                            pending.append(fin_kw)
                        else:
                            rj = tmc[0] % 2
                            psv = pT[bk][:, :].rearrange("p (h d) -> p h d", h=8)
                            rope_block(psv, 8, i, rq[rj][:, :, :], [rq_b[rj]])

                            def fin_rope(i=i, rj=rj, name=name, sj=tmc[0] % 2):
                                for c in range(4):
                                    C.op("pe", "transpose", [rq_b[rj], b_const], [pTP_b], sig=(c == 3), out=pTP[:, c * 128:(c + 1) * 128],
                                         in_=rq[rj][:, 2 * c:2 * c + 2, :].rearrange("p a d -> p (a d)"), identity=identb[:])
                                src = pTP[:, 0:512].rearrange("p (c n) -> p c n", c=4)
                                if name == "k":
                                    C.op("act", "activation", [pTP_b], [kT_b[i]], out=kT[:, :, i * 128:(i + 1) * 128], in_=src, func=AF.Copy)
                                else:
                                    C.op("act", "activation", [pTP_b], [qst_b[sj]], out=qst[sj][:, :, :], in_=src, func=AF.Copy)
                                    if name == "q":
                                        C.dma("sp", qT_scr[i].rearrange("p (c n) -> p c n", c=4), qst[sj][:, :, :], [qst_b[sj]], [q_scr_b[i]], sem_from=qst_b[sj])
                                    else:
                                        C.dma("sp", qiT_scr[i].rearrange("p (c n) -> p c n", c=4), qst[sj][:, :, :], [qst_b[sj]], [qi_scr_b[i]], sem_from=qst_b[sj])
                            pending.append(fin_rope)
                for cc in range(4):
                    for jj in range(31):
                        C.op("dve", "tensor_scalar", [b_const, b_small], [Dw_b], out=Dw[:, jj, :], in0=identb[:],
                             scalar1=cwT[:, cc, jj:jj + 1], scalar2=None, op0=ALU.mult)
                    for kc in range(8):
                        C.op("pe", "matmul", hT_b + [w_b[kc]], [pA_b], sig=(kc == 7), out=pA[:, :],
                             lhsT=w_sb[:, kc, A_OFF + cc * 128:A_OFF + (cc + 1) * 128], rhs=hT[:, kc, :], start=(kc == 0), stop=(kc == 7))
                    flush()
                    for kc in range(8):
                        C.op("pe", "matmul", hT_b + [w_b[kc]], [pB_b], sig=(kc == 7), out=pB[:, :],
                             lhsT=w_sb[:, kc, B_OFF + cc * 128:B_OFF + (cc + 1) * 128], rhs=hT[:, kc, :], start=(kc == 0), stop=(kc == 7))
                    C.op("act", "activation", [pB_b], [sg_b], out=sg[:], in_=pB[:, :], func=AF.Sigmoid)
                    C.op("dve", "tensor_tensor", [pA_b, sg_b], [uT_b[cur][cc]], out=uT[cur][:, cc, 30:542], in0=pA[:, :], in1=sg[:], op=ALU.mult)
                    for jj in range(31):
                        C.op("pe", "matmul", [Dw_b, uT_b[cur][cc], uTpad_b[cur]], [pC_b], sig=(jj == 30), out=pC[:, :],
                             lhsT=Dw[:, jj, :], rhs=uT[cur][:, cc, jj:jj + 512], start=(jj == 0), stop=(jj == 30))
                    C.op("act", "activation", [pC_b, b_small], [co_b[cc]], out=co[:, cc, :], in_=pC[:, :], func=AF.Identity, bias=cb4[:, cc:cc + 1])
                    C.op("act", "activation", [co_b[cc]], [sq_b[cc % 2]], out=sq[cc % 2][:], in_=co[:, cc, :], func=AF.Square)
                    C.op("pe", "matmul", [b_const, co_b[cc]], [pM_b], sig=(cc == 3), out=pM[:, :], lhsT=onesf[:], rhs=co[:, cc, :],
                         start=(cc == 0), stop=(cc == 3))
                    C.op("pe", "matmul", [b_const, sq_b[cc % 2]], [pE_b], sig=(cc == 3), out=pE[:, :], lhsT=onesf[:], rhs=sq[cc % 2][:],
                         start=(cc == 0), stop=(cc == 3))
                if sbi + 1 < NSB:
                    C.op("pool", "tensor_copy", uT_b[cur], [uTpad_b[nxt]], out=uT[nxt][:, :, 0:30], in_=uT[cur][:, :, 512:542])
                C.op("act", "activation", [pM_b], [mean_b], out=mean_sb[:], in_=pM[:, :], func=AF.Copy)
                C.op("pool", "tensor_tensor", [mean_b], [m2_b], out=m2_sb[:], in0=mean_sb[:], in1=mean_sb[:], op=ALU.mult)
                C.op("dve", "tensor_tensor", [pE_b, m2_b], [m2_b], out=m2_sb[:], in0=pE[:, :], in1=m2_sb[:], op=ALU.subtract)
                C.op("act", "activation", [m2_b], [m2_b], out=m2_sb[:], in_=m2_sb[:], func=AF.Sqrt, bias=EPS, scale=1.0)
                C.op("dve", "reciprocal", [m2_b], [rstd_b], out=rstd_sb[:], in_=m2_sb[:])
                for cc in range(4):
                    dj = cc % 2
                    C.op("pool", "tensor_tensor", [co_b[cc], mean_b], [dtmp_b[dj]], out=dtmp[dj][:], in0=co[:, cc, :], in1=mean_sb[:], op=ALU.subtract)
                    C.op("pool", "tensor_tensor", [dtmp_b[dj], rstd_b], [dtmp_b[dj]], out=dtmp[dj][:], in0=dtmp[dj][:], in1=rstd_sb[:], op=ALU.mult)
                    C.op("act", "activation", [dtmp_b[dj], b_small], [zT_b], out=zT[:, cc, :], in_=dtmp[dj][:], func=AF.Silu,
                         scale=lng4[:, cc:cc + 1], bias=lnb4[:, cc:cc + 1])
                C.dma("sp", z_scr[sbi].rearrange("p (c n) -> p c n", c=4), zT[:, :, :], [zT_b], [z_scr_b[sbi]], sem_from=zT_b)
            C.barrier()

        if stop_after == "A1":
            if "kT" in dbg_out:
                db = C.buf("dbg")
                C.dma("sp", dbg_out["kT"].rearrange("p (c n) -> p c n", c=4), kT[:, :, :], kT_b, [db])
                C.dma("sp", dbg_out["kiT2"][:, :], kiT2[:, :], kiT_b, [db])
                C.dma("sp", dbg_out["v"].rearrange("p (t h d) -> p t h d", t=NT, h=8), v_sb[:, :, :, :], v_b, [db])
                C.dma("sp", dbg_out["wi"].rearrange("p (t h) -> p t h", t=NT), wi_sb[:, :, :], wi_b, [db])
                C.dma("sp", dbg_out["qT"].rearrange("(t p) n -> p t n", p=128), qT_scr.rearrange("t p n -> p t n"), q_scr_b, [db])
                C.dma("sp", dbg_out["qiT"].rearrange("(t p) n -> p t n", p=128), qiT_scr.rearrange("t p n -> p t n"), qi_scr_b, [db])
                C.dma("sp", dbg_out["z"].rearrange("(t p) n -> p t n", p=128), z_scr.rearrange("t p n -> p t n"), z_scr_b, [db])
                C.final_wait("sp", [db])
            C.barrier()
            C.final_wait("sp", q_scr_b + qi_scr_b + z_scr_b)
            return nc

        with ExitStack() as A2:
            qTz = [[sb(A2, "qTz%d_%d" % (par, j), [128, 4, 128], BF16) for j in range(2)] for par in range(2)]
            qiTz = [[sb(A2, "qiTz%d_%d" % (par, j), [128, 4, 128], BF16) for j in range(2)] for par in range(2)]
            qT_tb = C.bufs("qT_t", 2)
            qiT_tb = C.bufs("qiT_t", 2)
            Dg = sb(A2, "Dg", [128, 8, 128], BF16)
            Dg_b = C.buf("Dg")
            R_sb = [sb(A2, "R_sb%d" % j, [128, 512], BF16) for j in range(2)]
            R_b = C.bufs("R_sb", 2)
            sc = [sb(A2, "sc%d" % j, [128, S], F32) for j in range(2)]
            sc_b = C.bufs("sc", 2)
            NM = [sb(A2, "NM%d" % j, [128, S], BF16) for j in range(2)]
            NM_b = C.bufs("NM", 2)
            bs = sb(A2, "bs", [128, 8], F32)
            bs_b = C.buf("bs")
            wk = sb(A2, "wk", [128, BIS_ITERS + 2], F32)
            pow2 = sb(A2, "pow2", [128, BIS_ITERS + 2], F32)
            thrc = sb(A2, "thrc", [128, 1], F32)
            b_c2 = C.buf("constA2")
            C.dma("sp", pow2[:], pow2_d[:, :], [], [b_c2])
            C.op("pool", "memset", [], [b_c2], ap=thrc[:], constant=-1e29)
            for par in range(2):
                for j in range(2):
                    C.op("dve", "memset", [], [qT_tb[j]], ap=qTz[par][j][:, :, :], constant=0.0)
                    C.op("dve", "memset", [], [qiT_tb[j]], ap=qiTz[par][j][:, :, :], constant=0.0)
            PT = [sb(A2, "PT%d" % j, [128, 512], BF16) for j in range(2)]
            PT_b = C.bufs("PT", 2)
            rden = sb(A2, "rden", [128, 8], F32)
            rden_b = C.buf("rden")
            ao = sb(A2, "ao", [128, 8, 64], BF16)
            ao_b = C.buf("ao")
            aoT_st = [sb(A2, "aoT_st%d" % j, [128, 4, 128], BF16) for j in range(2)]
            aoT_b = C.bufs("aoT_st", 2)
            pR = [ps(A2, "pR%d" % j, [128, 512], F32) for j in range(2)]
            pR_b = C.bufs("pR", 2)
            pSC = ps(A2, "pSC", [128, 512], F32)
            pSC_b = C.buf("pSC")
            pST = [ps(A2, "pST%d" % j, [128, 512], F32) for j in range(2)]
            pST_b = C.bufs("pST", 2)
            pPV = [ps(A2, "pPV%d" % j, [128, 512], F32) for j in range(2)]
            pPV_b = C.bufs("pPV", 2)
            pTP2 = ps(A2, "pTP2", [128, 1024], BF16)
            pTP2_b = C.buf("pTP2")
            ao_scr_b = C.bufs("aoscr", NT, share=True)

            C.barrier()
            tiles = list(range(NT)) if a2_tiles is None else list(a2_tiles)
            def index_phase(n_i, i):
                s2 = n_i % 2
                NK = 128 * (i + 1)
                nkc = i + 1
                nkb = (NK + 511) // 512
                for par in range(2):
                    pr = slice(par * 64, (par + 1) * 64)
                    C.dma("sp", qTz[par][s2][pr, :, :], qT_scr[i][pr, :].rearrange("p (c n) -> p c n", c=4), [q_scr_b[i]], [qT_tb[s2]])
                    C.dma("sp", qiTz[par][s2][pr, :, :], qiT_scr[i][pr, :].rearrange("p (c n) -> p c n", c=4), [qi_scr_b[i]], [qiT_tb[s2]])
                for h in range(8):
                    C.op("act", "activation", [b_const, wi_b[i]], [Dg_b], out=Dg[:, h, :], in_=identb[:], func=AF.Copy,
                         scale=wi_sb[:, i, h:h + 1])
                for kb in range(nkb):
                    k0 = kb * 512
                    W = min(512, NK - k0)
                    kbufs = kiT_b[k0 // 128:(k0 + W) // 128]

                    def r_mm(h):
                        C.op("pe", "matmul", [qiT_tb[s2]] + kbufs, [pR_b[h % 2]], sig=True, out=pR[h % 2][:, 0:W],
                             lhsT=qiTz[h % 2][s2][:, h // 2, :], rhs=kiT2[:, k0:k0 + W], start=True, stop=True)
                        C.op("act", "activation", [pR_b[h % 2]], [R_b[h % 2]], out=R_sb[h % 2][:, 0:W], in_=pR[h % 2][:, 0:W], func=AF.Relu)

                    def s_mm(h):
                        C.op("pe", "matmul", [Dg_b, R_b[h % 2]], [pSC_b], sig=(h == 7), out=pSC[:, 0:W], lhsT=Dg[:, h, :],
                             rhs=R_sb[h % 2][:, 0:W], start=(h == 0), stop=(h == 7))

                    r_mm(0)
                    for h in range(8):
                        if h + 1 < 8:
                            r_mm(h + 1)
                        s_mm(h)
                    C.op("act", "activation", [pSC_b], [sc_b[s2]], out=sc[s2][:, k0:k0 + W], in_=pSC[:, 0:W], func=AF.Copy)
                C.op("pool", "memset", [], [sc_b[s2]], ap=sc[s2][0:64, NK - 64:NK], constant=-1e30)

            def thresh_phase(n_i, i):
                s2 = n_i % 2
                NK = 128 * (i + 1)
                nkc = i + 1
                nkb = (NK + 511) // 512
                if i >= 2:
                    C.op("dve", "tensor_reduce", [sc_b[s2]], [bs_b], out=bs[:, 0:1], in_=sc[s2][:, 0:NK], axis=AX.X, op=ALU.max)
                    C.op("dve", "tensor_reduce", [sc_b[s2]], [bs_b], out=bs[:, 1:2], in_=sc[s2][:, 0:320], axis=AX.X, op=ALU.min)
                    C.op("dve", "tensor_tensor", [bs_b], [bs_b], out=bs[:, 2:3], in0=bs[:, 0:1], in1=bs[:, 1:2], op=ALU.subtract)
                    C.op("dve", "tensor_scalar", [bs_b, b_c2], [bs_b], out=wk[:], in0=pow2[:], scalar1=bs[:, 2:3], scalar2=None, op0=ALU.mult)
                    C.op("dve", "tensor_tensor", [bs_b], [bs_b], out=bs[:, 3:4], in0=bs[:, 1:2], in1=wk[:, 1:2], op=ALU.add)
                    for k in range(BIS_ITERS):
                        C.op("dve", "tensor_scalar", [sc_b[s2], bs_b], [NM_b[s2], bs_b], out=NM[s2][:, 0:NK], in0=sc[s2][:, 0:NK],
                             scalar1=bs[:, 3:4], scalar2=None, op0=ALU.is_ge, op1=ALU.add, accum_out=bs[:, 4:5])
                        C.op("dve", "tensor_scalar", [bs_b], [bs_b], out=bs[:, 5:6], in0=bs[:, 4:5], scalar1=255.5, scalar2=0.5,
                             op0=ALU.is_ge, op1=ALU.subtract)
                        if k < BIS_ITERS - 1:
                            C.op("dve", "scalar_tensor_tensor", [bs_b], [bs_b], out=bs[:, 3:4], in0=bs[:, 5:6], scalar=wk[:, k + 1:k + 2],
                                 in1=bs[:, 3:4], op0=ALU.mult, op1=ALU.add)
                    C.op("dve", "tensor_scalar", [bs_b], [bs_b], out=bs[:, 5:6], in0=bs[:, 5:6], scalar1=-0.5, scalar2=None, op0=ALU.add)
                    C.op("dve", "scalar_tensor_tensor", [bs_b], [bs_b], out=bs[:, 6:7], in0=bs[:, 5:6], scalar=wk[:, BIS_ITERS:BIS_ITERS + 1],
                         in1=bs[:, 3:4], op0=ALU.mult, op1=ALU.add)
                    thr_ap = bs[:, 6:7]
                    thr_bufs = [bs_b]
                else:
                    thr_ap = thrc[:, 0:1]
                    thr_bufs = [b_c2]
                C.op("dve", "tensor_scalar", [sc_b[s2]] + thr_bufs, [NM_b[s2]], out=NM[s2][:, 0:NK], in0=sc[s2][:, 0:NK],
                     scalar1=thr_ap, scalar2=NEG_BIG, op0=ALU.is_lt, op1=ALU.mult)

            def attn_phase(n_i, i):
                s2 = n_i % 2
                NK = 128 * (i + 1)
                nkc = i + 1
                nkb = (NK + 511) // 512
                items = [(h, kb) for h in range(8) for kb in range(nkb)]

                def st_block(n):
                    h, kb = items[n]
                    sl = n % 2
                    pb = (h % 2) * 64
                    c = h // 2
                    kcs = list(range(kb * 4, min(nkc, kb * 4 + 4)))
                    for kcl, kc in enumerate(kcs):
                        C.op("pe", "matmul", [kT_b[kc], qT_tb[s2]], [pST_b[sl]], out=pST[sl][:, kcl * 128:(kcl + 1) * 128],
                             lhsT=kT[:, c, kc * 128:(kc + 1) * 128], rhs=qTz[h % 2][s2][:, c, :], start=True, stop=False)
                        C.op("pe", "matmul", [NM_b[s2], b_const], [pST_b[sl]], sig=(kcl == len(kcs) - 1),
                             out=pST[sl][:, kcl * 128:(kcl + 1) * 128],
                             lhsT=NM[s2][:, kc * 128:(kc + 1) * 128], rhs=identb[:], start=False, stop=True)
                    C.op("act", "activation", [pST_b[sl]], [PT_b[sl]], out=PT[sl][:, 0:len(kcs) * 128], in_=pST[sl][:, 0:len(kcs) * 128],
                         func=AF.Exp, scale=0.125)

                def pv_block(n):
                    h, kb = items[n]
                    sl = n % 2
                    kcs = list(range(kb * 4, min(nkc, kb * 4 + 4)))
                    for kcl, kc in enumerate(kcs):
                        C.op("pe", "matmul", [PT_b[sl], v_b[kc]], [pPV_b[h // 4]], out=pPV[h // 4][:, (h % 4) * 65:(h % 4) * 65 + 65],
                             lhsT=PT[sl][:, kcl * 128:(kcl + 1) * 128], rhs=v_sb[:, kc, h, :], start=(kc == 0), stop=(kc == nkc - 1))

                st_block(0)
                for n in range(len(items)):
                    if n + 1 < len(items):
                        st_block(n + 1)
                    pv_block(n)
                fs = len(items) % 2
                C.op("pe", "matmul", [b_const], [pST_b[fs]], sig=True, out=pST[fs][:, 0:128], lhsT=identb[:], rhs=identb[:], start=True, stop=True)

            def attn_tail(n_i, i):
                s2 = n_i % 2
                for hh in range(2):
                    pvv = pPV[hh][:, 0:260].rearrange("p (h d) -> p h d", h=4)
                    C.op("dve", "reciprocal", [pPV_b[hh]], [rden_b], out=rden[:, hh * 4:hh * 4 + 4].unsqueeze(2), in_=pvv[:, :, 64:65])
                    C.op("dve", "tensor_tensor", [pPV_b[hh], rden_b], [ao_b], out=ao[:, hh * 4:hh * 4 + 4, :], in0=pvv[:, :, 0:64],
                         in1=bc(rden[:, hh * 4:hh * 4 + 4].unsqueeze(2), [128, 4, 64]), op=ALU.mult)
                for c in range(4):
                    C.op("pe", "transpose", [ao_b, b_const], [pTP2_b], sig=(c == 3), out=pTP2[:, c * 128:(c + 1) * 128],
                         in_=ao[:, 2 * c:2 * c + 2, :].rearrange("p a d -> p (a d)"), identity=identb[:])
                C.op("act", "activation", [pTP2_b], [aoT_b[s2]], out=aoT_st[s2][:, :, :], in_=pTP2[:, 0:512].rearrange("p (c n) -> p c n", c=4), func=AF.Copy)
                C.dma("sp", ao_scr[i].rearrange("p (c n) -> p c n", c=4), aoT_st[s2][:, :, :], [aoT_b[s2]], [ao_scr_b[i]], sem_from=aoT_b[s2])

            nt_ = len(tiles)
            index_phase(0, tiles[0])
            thresh_phase(0, tiles[0])
            if nt_ > 1:
                index_phase(1, tiles[1])
            for n_i in range(nt_):
                attn_phase(n_i, tiles[n_i])
                if n_i + 1 < nt_:
                    thresh_phase(n_i + 1, tiles[n_i + 1])
                if n_i + 2 < nt_:
                    index_phase(n_i + 2, tiles[n_i + 2])
                attn_tail(n_i, tiles[n_i])
            s2 = (nt_ - 1) % 2
            if "nm" in dbg_out:
                dbn = C.buf("dbgnm")
                C.dma("sp", dbg_out["nm"][:, :], NM[s2][:, :], [NM_b[s2]], [dbn])
                C.dma("sp", dbg_out["sc"][:, :], sc[s2][:, :], [sc_b[s2]], [dbn])
                C.final_wait("sp", [dbn])
            C.barrier()

        if stop_after == "A2":
            db = C.buf("dbg2")
            C.dma("sp", dbg_out["ao"].rearrange("(t p) n -> p t n", p=128), ao_scr.rearrange("t p n -> p t n"), ao_scr_b, [db])
            C.final_wait("sp", [db])
            return nc

        PA.close()

        xn_scr = dscr("xn_scr", [NT, 128, 1024], BF16)
        xn_scr_b = C.bufs("xnscr", NT, share=True)
        x2_scr_b = C.bufs("x2scr", NT, share=True)
        PBC = ExitStack()
        es.enter_context(PBC)
        comb = sb(PBC, "comb", [128, NT, 32], F32)
        comb_b = C.bufs("comb", NT)
        lgall = sb(PBC, "lgall", [128, NT, 36], F32)
        lgall_b = C.buf("lgall")

        def load_w(stack, name, src, rows, c0, c1, bufname, defer=False):
            nkc = rows // 128
            t = sb(stack, name, [128, nkc, c1 - c0], BF16)
            b = C.buf(bufname)

            def issue():
                for kc in range(nkc):
                    C.dma("pool", t[:, kc, :], src[kc * 128:(kc + 1) * 128, c0:c1], [], [b])
            if defer:
                return t, b, issue
            issue()
            return t, b

        def rms_rstd(x_ap, x_bufs, junk_ap, junk_bufs, st, st_b, scale_n=D):
            C.op("act", "activation", x_bufs, junk_bufs + [st_b], out=junk_ap, in_=x_ap, func=AF.Square, accum_out=st[:, 0:1])
            C.op("act", "activation", [st_b], [st_b], out=st[:, 1:2], in_=st[:, 0:1], func=AF.Sqrt, scale=1.0 / scale_n, bias=EPS)
            C.op("dve", "reciprocal", [st_b], [st_b], out=st[:, 2:3], in_=st[:, 1:2])

        with ExitStack() as B:
            wg_sb, wg_b, ld_wg = load_w(B, "w_gates", w_in_d, D, G_OFF, G_OFF + 2048, "w_gates", defer=True)
            woa_sb, woa_b, ld_woa = load_w(B, "w_oa", w_o_attn_d, 512, 0, D, "w_oa", defer=True)
            wco_sb, wco_b, ld_wco = load_w(B, "w_co", w_conv_out_d, 512, 0, D, "w_co", defer=True)
            wout_sb, wout_b, ld_wout = load_w(B, "w_outb", w_out_d, D, 0, D, "w_outb", defer=True)
            wqx_sb, wqx_b, ld_wqx = load_w(B, "w_qxb", w_q_x_d, D, 0, D, "w_qxb", defer=True)
            wox_sb, wox_b, ld_wox = load_w(B, "w_oxb", w_o_x_d, D, 0, D, "w_oxb", defer=True)
            kmT = sb(B, "kmT", [128, 8, 256], BF16)
            vm = sb(B, "vm", [128, 2, D], BF16)
            kmT_b = C.buf("kmT")
            vm_b = C.buf("vm")
            gfm = sb(B, "gfmB", [128, 8], F32)
            gxm = sb(B, "gxm", [128, 8], F32)
            gmm = sb(B, "gmm", [128, 8], F32)
            gmo = sb(B, "gmo", [128, 8], F32)
            wr_sb = sb(B, "wr_sb", [128, 8, 36], F32)
            br_sb = sb(B, "br_sb", [128, 36], F32)
            b_smB = C.buf("smallB")
            C.dma("sp", gfm[:], g_mix_d[:, :], [], [b_smB])
            C.dma("sp", gxm[:], g_x_d[:, :], [], [b_smB])
            C.dma("sp", gmm[:], g_mem_d[:, :], [], [b_smB])
            C.dma("sp", gmo[:], g_moe_d[:, :], [], [b_smB])
            C.dma("sp", wr_sb[:, :, :], w_router_d.rearrange("(c p) n -> p c n", p=128), [], [b_smB])
            C.dma("sp", br_sb[:], bc(b_router_d[0:1, :], [128, 36]), [], [b_smB])
            stB = sb(B, "statB", [128, 8], F32)
            stB_b = C.buf("statB")
            pb = [ps(B, "pb%d" % j, [128, 512], F32) for j in range(7)]
            pb_b = C.bufs("pb", 7)
            pTPb = ps(B, "pTPb", [128, 1024], BF16)
            pTPb_b = C.buf("pTPb")

            sqj_ref = []

            def norm_chain(x_ap, x_bufs, hn_ap, hn_bufs):
                if sqj_ref:
                    rms_rstd(x_ap, x_bufs, sqj_ref[0][:, :], [sqj_ref[1]], stB, stB_b)
                else:
                    rms_rstd(x_ap, x_bufs, hn_ap, hn_bufs, stB, stB_b)
                C.op("act", "activation", x_bufs + [stB_b], hn_bufs, out=hn_ap, in_=x_ap, func=AF.Copy, scale=stB[:, 2:3])

            def norm_trans(hn_ap, hn_bufs, g_sb, dstT, dst_bufs, col0):
                for c in range(8):
                    C.op("pe", "transpose", hn_bufs + [b_const], [pTPb_b], sig=(c == 7), out=pTPb[:, c * 128:(c + 1) * 128],
                         in_=hn_ap[:, c * 128:(c + 1) * 128], identity=identb[:])
                C.op("dve", "tensor_tensor", [pTPb_b, b_smB], dst_bufs, out=dstT[:, :, col0:col0 + 128],
                     in0=pTPb[:, :].rearrange("p (c n) -> p c n", c=8), in1=bc(g_sb[:, :].unsqueeze(2), [128, 8, 128]), op=ALU.mult)

            def norm_T(x_ap, x_bufs, hn_ap, hn_bufs, g_sb, dstT, dst_bufs, col0):
                norm_chain(x_ap, x_bufs, hn_ap, hn_bufs)
                norm_trans(hn_ap, hn_bufs, g_sb, dstT, dst_bufs, col0)

            with ExitStack() as BK:
                wkv_sb, wkv_b = load_w(BK, "w_kvb", w_kv_x_d, D, 0, 2 * D, "w_kvb")
                for ld in (ld_wg, ld_woa, ld_wco, ld_wout, ld_wqx, ld_wox):
                    ld()
                memt2 = [sb(BK, "memt%d" % j, [128, D], F32) for j in range(2)]
                memt2_b = C.bufs("memt", 2)
                memh = sb(BK, "memh", [128, D], BF16)
                memh_b = C.buf("memh")
                memnT = sb(BK, "memnT", [128, 8, 256], BF16)
                memnT_b = C.buf("memnT")
                for mt in range(2):
                    C.dma("sp", memt2[mt][:], mem_d[mt * 128:(mt + 1) * 128, :], [], [memt2_b[mt]])
                for mt in range(2):
                    norm_T(memt2[mt][:, :], [memt2_b[mt]], memh[:, :], [memh_b], gmm, memnT, [memnT_b], mt * 128)
                for jx in range(8):
                    bk = jx % 2
                    for kc in range(8):
                        C.op("pe", "matmul", [wkv_b, memnT_b], [pb_b[bk]], sig=(kc == 7), out=pb[bk][:, 0:256],
                             lhsT=wkv_sb[:, kc, jx * 128:(jx + 1) * 128], rhs=memnT[:, kc, :], start=(kc == 0), stop=(kc == 7))
                    C.op("act", "activation", [pb_b[bk]], [kmT_b], out=kmT[:, jx, :], in_=pb[bk][:, 0:256], func=AF.Copy)
                for mc in range(2):
                    for cb in range(2):
                        bk = 2 + (mc * 2 + cb) % 2
                        for kc in range(8):
                            C.op("pe", "matmul", [wkv_b, memnT_b], [pb_b[bk]], sig=(kc == 7), out=pb[bk][:, :],
                                 lhsT=memnT[:, kc, mc * 128:(mc + 1) * 128], rhs=wkv_sb[:, kc, D + cb * 512:D + (cb + 1) * 512],
                                 start=(kc == 0), stop=(kc == 7))
                        C.op("act", "activation", [pb_b[bk]], [vm_b], out=vm[:, mc, cb * 512:(cb + 1) * 512], in_=pb[bk][:, :], func=AF.Copy)
                C.barrier()

            xt4 = sb(B, "xt4", [128, 4, D], F32)
            xt4_b = C.bufs("xt4", 4)
            hnB = [sb(B, "hnB%d" % j, [128, D], BF16) for j in range(4)]
            hnB_b = C.bufs("hnB", 4)
            sqj = sb(B, "sqj", [128, D], BF16)
            sqj_b = C.buf("sqj")
            sqj_ref.extend([sqj, sqj_b])
            bufA = sb(B, "bufA", [128, 8, 512], BF16)
            bufA_b = C.bufs("bufA", 4)
            bufB = sb(B, "bufB", [128, 8, 512], BF16)
            bufB_b = C.bufs("bufB", 8)
            bufC = sb(B, "bufC", [128, 8, 512], BF16)
            bufC_b = C.bufs("bufC", 8)
            aoTb = sb(B, "aoTb", [128, 4, 512], BF16)
            aoTb_b = C.buf("aoTb")
            zTb = sb(B, "zTb", [128, 4, 512], BF16)
            zTb_b = C.buf("zTb")
            sgA = sb(B, "sgA", [128, 512], F32)
            sgB = sb(B, "sgB", [128, 512], F32)
            sgA_b = C.buf("sgA")
            sgB_b = C.buf("sgB")
            PT2 = sb(B, "PT2", [128, 2, 512], BF16)
            PT2_b = C.bufs("PT2", 2)
            rden2 = sb(B, "rden2", [128, 512], F32)
            rden2_b = C.buf("rden2")
            xn32s = [sb(B, "xn32_%d" % j, [128, D], F32) for j in range(2)]
            xn32s_b = C.bufs("xn32", 2)
            xnT32 = sb(B, "xnT32", [128, 8, 128], F32)
            xnT32_b = C.buf("xnT32")
            xnTb = [sb(B, "xnTb%d" % j, [128, 8, 128], BF16) for j in range(2)]
            xnTb_b = C.bufs("xnTb", 2)
            rt_ = sb(B, "rtr", [128, 128], F32)
            rt_b2 = C.buf("rtr")

            def load_x(blk, t):
                i = blk * 4 + t
                C.dma("sp", xt4[:, t, :], x_d[i * 128:(i + 1) * 128, :], [], [xt4_b[t]])

            def load_aoz(blk):
                for t in range(4):
                    i = blk * 4 + t
                    C.dma("sp", aoTb[:, :, t * 128:(t + 1) * 128], ao_scr[i].rearrange("p (c n) -> p c n", c=4), [ao_scr_b[i]], [aoTb_b])
                C.dma("sp", zTb[:, :, :], z_scr[blk].rearrange("p (c n) -> p c n", c=4), [z_scr_b[blk]], [zTb_b])

            def pipelined_norms(g_sb, ready=None):
                def ch(t):
                    norm_chain(xt4[:, t, :], [xt4_b[t]], hnB[t][:, :], [hnB_b[t]])

                def tr(t):
                    norm_trans(hnB[t][:, :], [hnB_b[t]], g_sb, bufA, [bufA_b[t]], t * 128)
                return ch, tr

            for t in range(4):
                load_x(0, t)
            load_aoz(0)
            for blk in range(NSB):
                ch, tr = pipelined_norms(gfm)
                for t in range(4):
                    ch(t)
                for t in range(4):
                    tr(t)
                for fo in range(8):
                    fs_ = slice(fo * 128, (fo + 1) * 128)
                    b0, b1, b2 = (0, 1, 2) if fo % 2 == 0 else (4, 5, 6)
                    for c in range(4):
                        C.op("pe", "matmul", [woa_b, aoTb_b], [pb_b[b0]], sig=(c == 3), out=pb[b0][:, :], lhsT=woa_sb[:, c, fs_], rhs=aoTb[:, c, :],
                             start=(c == 0), stop=(c == 3))
                    for c in range(4):
                        C.op("pe", "matmul", [wco_b, zTb_b], [pb_b[b1]], sig=(c == 3), out=pb[b1][:, :], lhsT=wco_sb[:, c, fs_], rhs=zTb[:, c, :],
                             start=(c == 0), stop=(c == 3))
                    for kc in range(8):
                        C.op("pe", "matmul", [wg_b] + bufA_b, [pb_b[b2]], sig=(kc == 7), out=pb[b2][:, :], lhsT=wg_sb[:, kc, fs_], rhs=bufA[:, kc, :],
                             start=(kc == 0), stop=(kc == 7))
                    for kc in range(8):
                        C.op("pe", "matmul", [wg_b] + bufA_b, [pb_b[3]], sig=(kc == 7), out=pb[3][:, :],
                             lhsT=wg_sb[:, kc, D + fo * 128:D + (fo + 1) * 128], rhs=bufA[:, kc, :], start=(kc == 0), stop=(kc == 7))
                    C.op("act", "activation", [pb_b[b2]], [sgA_b], out=sgA[:], in_=pb[b2][:, :], func=AF.Sigmoid)
                    C.op("act", "activation", [pb_b[3]], [sgB_b], out=sgB[:], in_=pb[3][:, :], func=AF.Sigmoid)
                    C.op("dve", "tensor_tensor", [pb_b[b0], sgA_b], [sgA_b], out=sgA[:], in0=pb[b0][:, :], in1=sgA[:], op=ALU.mult)
                    C.op("dve", "tensor_tensor", [pb_b[b1], sgB_b], [sgB_b], out=sgB[:], in0=pb[b1][:, :], in1=sgB[:], op=ALU.mult)
                    C.op("pool", "tensor_tensor", [sgA_b, sgB_b], [bufB_b[fo]], out=bufB[:, fo, :], in0=sgA[:], in1=sgB[:], op=ALU.add)
                if blk + 1 < NSB:
                    load_aoz(blk + 1)
                ch, tr = pipelined_norms(gxm)
                for t in range(4):
                    if t >= 2:
                        tr(t - 2)
                    for cb in range(2):
                        bk = 4 + (t * 2 + cb) % 2
                        for kc in range(8):
                            C.op("pe", "matmul", [wout_b, bufB_b[kc]], [pb_b[bk]], sig=(kc == 7), out=pb[bk][:, :],
                                 lhsT=bufB[:, kc, t * 128:(t + 1) * 128], rhs=wout_sb[:, kc, cb * 512:(cb + 1) * 512], start=(kc == 0), stop=(kc == 7))
                        C.op("dve", "tensor_tensor", [pb_b[bk], xt4_b[t]], [xt4_b[t]], out=xt4[:, t, cb * 512:(cb + 1) * 512], in0=pb[bk][:, :],
                             in1=xt4[:, t, cb * 512:(cb + 1) * 512], op=ALU.add)
                    ch(t)
                tr(2)
                tr(3)
                for fo in range(8):
                    bk = 4 + fo % 2
                    for kc in range(8):
                        C.op("pe", "matmul", [wqx_b] + bufA_b, [pb_b[bk]], sig=(kc == 7), out=pb[bk][:, :],
                             lhsT=wqx_sb[:, kc, fo * 128:(fo + 1) * 128], rhs=bufA[:, kc, :], start=(kc == 0), stop=(kc == 7))
                    C.op("act", "activation", [pb_b[bk]], [bufB_b[fo]], out=bufB[:, fo, :], in_=pb[bk][:, :], func=AF.Copy)
                for h in range(4):
                    for mc in range(2):
                        for dc in range(2):
                            C.op("pe", "matmul", [kmT_b, bufB_b[h * 2 + dc]], [pb_b[mc]], sig=(dc == 1), out=pb[mc][:, :],
                                 lhsT=kmT[:, h * 2 + dc, mc * 128:(mc + 1) * 128], rhs=bufB[:, h * 2 + dc, :], start=(dc == 0), stop=(dc == 1))
                        C.op("act", "activation", [pb_b[mc]], [PT2_b[mc]], out=PT2[:, mc, :], in_=pb[mc][:, :], func=AF.Exp, scale=1.0 / 16.0)
                    for mc in range(2):
                        C.op("pe", "matmul", [b_const, PT2_b[mc]], [pb_b[2]], sig=(mc == 1), out=pb[2][:, :], lhsT=onesb[:], rhs=PT2[:, mc, :],
                             start=(mc == 0), stop=(mc == 1))
                    C.op("dve", "reciprocal", [pb_b[2]], [rden2_b], out=rden2[:], in_=pb[2][:, :])
                    for dvc in range(2):
                        bk = 3 if dvc == 0 else 6
                        for mc in range(2):
                            C.op("pe", "matmul", [vm_b, PT2_b[mc]], [pb_b[bk]], sig=(mc == 1), out=pb[bk][:, :],
                                 lhsT=vm[:, mc, h * 256 + dvc * 128:h * 256 + (dvc + 1) * 128], rhs=PT2[:, mc, :], start=(mc == 0), stop=(mc == 1))
                        C.op("dve", "tensor_tensor", [pb_b[bk], rden2_b], [bufC_b[h * 2 + dvc]], out=bufC[:, h * 2 + dvc, :], in0=pb[bk][:, :],
                             in1=rden2[:], op=ALU.mult)
                def wox(t):
                    i = blk * 4 + t
                    for cb in range(2):
                        bk = 4 + (t * 2 + cb) % 2
                        for kc in range(8):
                            C.op("pe", "matmul", [wox_b, bufC_b[kc]], [pb_b[bk]], sig=(kc == 7), out=pb[bk][:, :],
                                 lhsT=bufC[:, kc, t * 128:(t + 1) * 128], rhs=wox_sb[:, kc, cb * 512:(cb + 1) * 512], start=(kc == 0), stop=(kc == 7))
                        C.op("dve", "tensor_tensor", [pb_b[bk], xt4_b[t]], [xt4_b[t]], out=xt4[:, t, cb * 512:(cb + 1) * 512], in0=pb[bk][:, :],
                             in1=xt4[:, t, cb * 512:(cb + 1) * 512], op=ALU.add)
                    C.dma("sp", x2_scr[i * 128:(i + 1) * 128, :], xt4[:, t, :], [xt4_b[t]], [x2_scr_b[i]], sem_from=xt4_b[t])
                    xn32, xn32_b = xn32s[t % 2], xn32s_b[t % 2]
                    rms_rstd(xt4[:, t, :], [xt4_b[t]], sqj[:, :], [sqj_b], stB, stB_b)
                    C.op("act", "activation", [xt4_b[t], stB_b], [xn32_b], out=xn32[:, :], in_=xt4[:, t, :], func=AF.Copy, scale=stB[:, 2:3])

                def b6(t):
                    i = blk * 4 + t
                    xn32, xn32_b = xn32s[t % 2], xn32s_b[t % 2]
                    for c in range(8):
                        C.op("pe", "transpose", [xn32_b, b_const], [pb_b[c // 4]], sig=(c % 4 == 3), out=pb[c // 4][:, (c % 4) * 128:(c % 4 + 1) * 128],
                             in_=xn32[:, c * 128:(c + 1) * 128], identity=identf[:])
                    for hh in range(2):
                        C.op("dve", "tensor_tensor", [pb_b[hh], b_smB], [xnT32_b], out=xnT32[:, hh * 4:hh * 4 + 4, :],
                             in0=pb[hh][:, :].rearrange("p (c n) -> p c n", c=4), in1=bc(gmo[:, hh * 4:hh * 4 + 4].unsqueeze(2), [128, 4, 128]), op=ALU.mult)
                    xj = i % 2
                    C.op("pool", "tensor_copy", [xnT32_b], [xnTb_b[xj]], out=xnTb[xj][:, :, :], in_=xnT32[:, :, :])
                    C.dma("sp", xn_scr[i].rearrange("p (c n) -> p c n", c=8), xnTb[xj][:, :, :], [xnTb_b[xj]], [xn_scr_b[i]], sem_from=xnTb_b[xj])
                    for kc in range(8):
                        C.op("pe", "matmul", [xnT32_b, b_smB], [pb_b[2]], out=pb[2][:, 0:36], lhsT=xnT32[:, kc, :], rhs=wr_sb[:, kc, :],
                             start=(kc == 0), stop=(kc == 7))
                    C.op("pe", "matmul", [b_const], [pb_b[3]], sig=True, out=pb[3][:, 0:128], lhsT=identb[:], rhs=identb[:], start=True, stop=True)
                    C.op("dve", "tensor_tensor", [pb_b[2], b_smB], [lgall_b], out=lgall[:, i, :], in0=pb[2][:, 0:36], in1=br_sb[:, :], op=ALU.add)
                wox(0)
                wox(1)
                b6(0)
                wox(2)
                b6(1)
                wox(3)
                b6(2)
                if blk + 1 < NSB:
                    for t in range(3):
                        load_x(blk + 1, t)
                b6(3)
                if blk + 1 < NSB:
                    load_x(blk + 1, 3)
            C.barrier()

        if stop_after == "B":
            db = C.buf("dbgB")
            C.dma("sp", dbg_out["x2"][:, :], x2_scr[:, :], x2_scr_b, [db])
            C.dma("sp", dbg_out["comb"].rearrange("p (t e) -> p t e", t=NT), comb[:, :, :], comb_b, [db])
            C.dma("sp", dbg_out["xn"].rearrange("(t p) n -> p t n", p=128), xn_scr.rearrange("t p n -> p t n"), xn_scr_b, [db])
            C.final_wait("sp", [db])
            return nc

        with ExitStack() as CC:
            xnT = sb(CC, "xnT", [128, 8, S], BF16)
            xnT_blk = C.bufs("xnT", NSB)
            xnT_b = [xnT_blk[i // 4] for i in range(NT)]
            for i in range(NT):
                C.dma("sp", xnT[:, :, i * 128:(i + 1) * 128], xn_scr[i].rearrange("p (c n) -> p c n", c=8), [xn_scr_b[i]], [xnT_b[i]])
            ysb = sb(CC, "ysb", [128, 16, D], F32)
            ysb_b = C.bufs("ysb", 16)
            NSLOT = 3
            wgs = [sb(CC, "wgs%d" % j, [128, 8, 256], BF16) for j in range(NSLOT)]
            wus = [sb(CC, "wus%d" % j, [128, 8, 256], BF16) for j in range(NSLOT)]
            wds = [sb(CC, "wds%d" % j, [128, 2, D], BF16) for j in range(NSLOT)]
            wslot_b = C.bufs("wslot", NSLOT)
            actT = [sb(CC, "actT%d" % j, [128, 2, 512], BF16) for j in range(2)]
            actT_b = [C.bufs("actT%d_" % j, 2) for j in range(2)]
            ssb = [sb(CC, "ssb%d" % j, [128, 512], BF16) for j in range(2)]
            ssb_b = C.bufs("ssb", 2)
            x2t = [sb(CC, "x2t%d" % j, [128, D], F32) for j in range(2)]
            x2t_b = C.bufs("x2t", 2)
            ot = [sb(CC, "ot%d" % j, [128, D], F32) for j in range(2)]
            ot_b = C.bufs("ot", 2)
            gfin = sb(CC, "gfin", [128, D], F32)
            gfin_b = C.buf("gfin")
            C.dma("sp", gfin[:], bc(g_final_d[0:1, :], [128, D]), [], [gfin_b])
            stC = sb(CC, "statC", [128, 8], F32)
            stC_b = C.buf("statC")
            pg = [ps(CC, "pg%d" % j, [128, 512], F32) for j in range(2)]
            pu = [ps(CC, "pu%d" % j, [128, 512], F32) for j in range(2)]
            py = [ps(CC, "py%d" % j, [128, 1024], F32) for j in range(2)]
            pg_b = C.bufs("pg", 2)
            pu_b = C.bufs("pu", 2)
            py_b = C.bufs("py", 2)

            with ExitStack() as BR:
                T = NT
                o1 = ot[1]
                gmax = o1[:, 0:32]
                oneh = o1[:, 32:160].rearrange("p (t g) -> p t g", g=4)
                dgl = o1[:, 160:288].rearrange("p (t g) -> p t g", g=4)
                sume = o1[:, 288:320]
                ggate = o1[:, 320:352]
                elsel = o1[:, 352:608].rearrange("p (t e) -> p t e", e=8)
                m8 = o1[:, 608:864].rearrange("p (t e) -> p t e", e=8)
                sc1 = o1[:, 864:992].rearrange("p (k t) -> p k t", k=4)
                tmpe = ot[0][:, :].rearrange("p (t f) -> p t f", f=32)
                eqa = x2t[0][:, 0:256].rearrange("p (t e) -> p t e", e=8)
                eqb = x2t[0][:, 256:512].rearrange("p (t e) -> p t e", e=8)
                rb_ = C.buf("routing")
                rr, ww = [lgall_b, rb_], [rb_, ot_b[0], ot_b[1], x2t_b[0]]
                gl = lgall[:, :, 0:4]
                el4 = lgall[:, :, 4:36].rearrange("p t (g e) -> p t g e", g=4)
                C.op("dve", "tensor_reduce", rr, ww, out=gmax[:, :], in_=gl, axis=AX.X, op=ALU.max)
                C.op("dve", "tensor_tensor", rr, ww, out=oneh[:, :, :], in0=gl, in1=bc(gmax[:, :].unsqueeze(2), [128, T, 4]), op=ALU.is_equal)
                C.op("dve", "tensor_tensor", rr, ww, out=dgl[:, :, :], in0=gl, in1=bc(gmax[:, :].unsqueeze(2), [128, T, 4]), op=ALU.subtract)
                C.op("act", "activation", rr, ww, out=dgl[:, :, :], in_=dgl[:, :, :], func=AF.Exp)
                C.op("dve", "tensor_reduce", rr, ww, out=sume[:, :], in_=dgl[:, :, :], axis=AX.X, op=ALU.add)
                C.op("dve", "reciprocal", rr, ww, out=ggate[:, :], in_=sume[:, :])
                C.op("dve", "tensor_tensor", rr, ww, out=tmpe[:, :, :].rearrange("p t (g e) -> p t g e", g=4), in0=el4,
                     in1=bc(oneh[:, :, :].unsqueeze(3), [128, T, 4, 8]), op=ALU.mult)
                C.op("dve", "tensor_reduce", rr, ww, out=elsel[:, :, :], in_=tmpe[:, :, :].rearrange("p t (g e) -> p t e g", g=4), axis=AX.X, op=ALU.add)
                for t in range(T):
                    C.op("dve", "max", rr, ww, out=m8[:, t, :], in_=elsel[:, t, :])
                m1 = m8[:, :, 0]
                m2 = m8[:, :, 1]
                e2, s1, w1, w2 = sc1[:, 0, :], sc1[:, 1, :], sc1[:, 2, :], sc1[:, 3, :]
                C.op("dve", "tensor_tensor", rr, ww, out=e2, in0=m2, in1=m1, op=ALU.subtract)
                C.op("act", "activation", rr, ww, out=e2, in_=e2, func=AF.Exp)
                C.op("dve", "tensor_scalar", rr, ww, out=s1, in0=e2, scalar1=1.0, scalar2=None, op0=ALU.add)
                C.op("dve", "reciprocal", rr, ww, out=s1, in_=s1)
                C.op("dve", "tensor_tensor", rr, ww, out=w1, in0=s1, in1=ggate[:, :], op=ALU.mult)
                C.op("dve", "tensor_tensor", rr, ww, out=w2, in0=w1, in1=e2, op=ALU.mult)
                C.op("dve", "tensor_tensor", rr, ww, out=eqa[:, :, :], in0=elsel[:, :, :], in1=bc(m1.unsqueeze(2), [128, T, 8]), op=ALU.is_equal)
                C.op("dve", "tensor_tensor", rr, ww, out=eqa[:, :, :], in0=eqa[:, :, :], in1=bc(w1.unsqueeze(2), [128, T, 8]), op=ALU.mult)
                C.op("dve", "tensor_tensor", rr, ww, out=eqb[:, :, :], in0=elsel[:, :, :], in1=bc(m2.unsqueeze(2), [128, T, 8]), op=ALU.is_equal)
                C.op("dve", "tensor_tensor", rr, ww, out=eqb[:, :, :], in0=eqb[:, :, :], in1=bc(w2.unsqueeze(2), [128, T, 8]), op=ALU.mult)
                C.op("dve", "tensor_tensor", rr, ww, out=eqa[:, :, :], in0=eqa[:, :, :], in1=eqb[:, :, :], op=ALU.add)
                C.op("dve", "tensor_tensor", rr, comb_b, out=comb[:, :, :].rearrange("p t (g e) -> p t g e", g=4),
                     in0=bc(oneh[:, :, :].unsqueeze(3), [128, T, 4, 8]), in1=bc(eqa[:, :, :].unsqueeze(2), [128, T, 4, 8]), op=ALU.mult)


            for hf in range(2):
                units = [(e, tb) for e in range(32) for tb in range(4)]

                def load_expert(e):
                    sl = e % NSLOT
                    C.dma("pool", wgs[sl][:, :, :], w_eg_d[e].rearrange("(c p) f -> p c f", p=128), [], [wslot_b[sl]])
                    C.dma("pool", wus[sl][:, :, :], w_eu_d[e].rearrange("(c p) f -> p c f", p=128), [], [wslot_b[sl]])
                    C.dma("pool", wds[sl][:, :, :], w_ed_d[e].rearrange("(c p) n -> p c n", p=128), [], [wslot_b[sl]])

                gu_cnt = [0]

                def gu(n):
                    e, tb = units[n]
                    sl = e % NSLOT
                    a = n % 2
                    tok0 = (hf * 16 + tb * 4) * 128
                    xb = xnT_b[hf * 16 + tb * 4:hf * 16 + tb * 4 + 4]
                    for fc in range(2):
                        k2 = gu_cnt[0] % 2
                        gu_cnt[0] += 1
                        for kc in range(8):
                            C.op("pe", "matmul", [wslot_b[sl]] + xb, [pg_b[k2]], sig=(kc == 7), out=pg[k2][:, :],
                                 lhsT=wgs[sl][:, kc, fc * 128:(fc + 1) * 128], rhs=xnT[:, kc, tok0:tok0 + 512], start=(kc == 0), stop=(kc == 7))
                        for kc in range(8):
                            C.op("pe", "matmul", [wslot_b[sl]] + xb, [pu_b[k2]], sig=(kc == 7), out=pu[k2][:, :],
                                 lhsT=wus[sl][:, kc, fc * 128:(fc + 1) * 128], rhs=xnT[:, kc, tok0:tok0 + 512], start=(kc == 0), stop=(kc == 7))
                        C.op("act", "activation", [pg_b[k2]], [ssb_b[k2]], out=ssb[k2][:], in_=pg[k2][:, :], func=AF.Silu)
                        C.op("dve", "tensor_tensor", [pu_b[k2], ssb_b[k2]], [actT_b[a][fc]], out=actT[a][:, fc, :], in0=pu[k2][:, :], in1=ssb[k2][:], op=ALU.mult)

                def down(n):
                    e, tb = units[n]
                    sl = e % NSLOT
                    a = n % 2
                    for t in range(4):
                        yt = tb * 4 + t
                        i = hf * 16 + yt
                        k2 = (n * 4 + t) % 2
                        for cb in range(2):
                            for fc in range(2):
                                C.op("pe", "matmul", [wslot_b[sl], actT_b[a][fc]], [py_b[k2]], sig=(cb == 1 and fc == 1),
                                     out=py[k2][:, cb * 512:(cb + 1) * 512], lhsT=actT[a][:, fc, t * 128:(t + 1) * 128],
                                     rhs=wds[sl][:, fc, cb * 512:(cb + 1) * 512], start=(fc == 0), stop=(fc == 1))
                        if e == 0:
                            j = yt % 2
                            C.dma("sp", x2t[j][:], x2_scr[i * 128:(i + 1) * 128, :], [x2_scr_b[i]], [x2t_b[j]])
                            C.op("dve", "scalar_tensor_tensor", [py_b[k2], comb_b[i], x2t_b[j]], [ysb_b[yt]], out=ysb[:, yt, :], in0=py[k2][:, :],
                                 scalar=comb[:, i, e:e + 1], in1=x2t[j][:], op0=ALU.mult, op1=ALU.add)
                        else:
                            C.op("dve", "scalar_tensor_tensor", [py_b[k2], comb_b[i], ysb_b[yt]], [ysb_b[yt]], out=ysb[:, yt, :], in0=py[k2][:, :],
                                 scalar=comb[:, i, e:e + 1], in1=ysb[:, yt, :], op0=ALU.mult, op1=ALU.add)

                def tail(yt):
                    i = hf * 16 + yt
                    j = yt % 2
                    rms_rstd(ysb[:, yt, :], [ysb_b[yt]], ot[j][:, :], [ot_b[j]], stC, stC_b)
                    C.op("dve", "scalar_tensor_tensor", [ysb_b[yt], stC_b, gfin_b], [ot_b[j]], out=ot[j][:], in0=ysb[:, yt, :], scalar=stC[:, 2:3],
                         in1=gfin[:], op0=ALU.mult, op1=ALU.mult)
                    C.dma("sp", out_d[i * 128:(i + 1) * 128, :], ot[j][:], [ot_b[j]], [out_b], sem_from=ot_b[j])

                load_expert(0)
                load_expert(1)
                gu(0)
                for n in range(len(units)):
                    e, tb = units[n]
                    if tb == 0 and e + 2 < 32:
                        load_expert(e + 2)
                    if n + 1 < len(units):
                        gu(n + 1)
                    down(n)
                    if e == 31:
                        for t in range(4):
                            tail(tb * 4 + t)
            C.barrier()
            C.final_wait("sp", ot_b)
    return nc


def make_in_maps(inputs):
    f32 = np.float32
    x = np.asarray(inputs["x"], f32)
    mem = np.asarray(inputs["mem"], f32)
    pos = np.asarray(inputs["positions"]).astype(np.int32)
    B = x.shape[0]

    def fm(v, n):
        return np.ascontiguousarray(np.asarray(v, f32).reshape(n, 128).T)

    w_re = np.asarray(inputs["w_router_expert"], f32)[0]
    w_router = np.concatenate([np.asarray(inputs["w_router_group"], f32)[0],
                               np.ascontiguousarray(w_re.transpose(1, 0, 2)).reshape(D, 32)], axis=1)
    b_router = np.concatenate([np.asarray(inputs["b_router_group"], f32)[0].reshape(-1),
                               np.asarray(inputs["b_router_expert"], f32)[0].reshape(-1)])[None, :]
    convw = np.asarray(inputs["conv_w"], f32)[0]
    convwT = np.ascontiguousarray(convw.T.reshape(4, 128, 31).transpose(1, 0, 2))
    inv_freq = (10000.0 ** (-np.arange(0, 64, 2, dtype=np.float64) / 64)).astype(np.float32)
    invf = np.tile((inv_freq.astype(np.float64) / (2 * np.pi)).astype(f32)[None, :], (128, 1))
    shared = {
        "w_in": np.ascontiguousarray(np.asarray(inputs["w_in"], f32)[0]),
        "w_o_attn": np.ascontiguousarray(np.asarray(inputs["w_o_attn"], f32)[0]),
        "convw": convwT,
        "convb": fm(np.asarray(inputs["conv_b"])[0], 4),
        "lng": fm(np.asarray(inputs["conv_ln_g"])[0], 4),
        "lnb": fm(np.asarray(inputs["conv_ln_b"])[0], 4),
        "w_conv_out": np.ascontiguousarray(np.asarray(inputs["w_conv_out"], f32)[0]),
        "w_out": np.ascontiguousarray(np.asarray(inputs["w_out"], f32)[0]),
        "w_q_x": np.ascontiguousarray(np.asarray(inputs["w_q_x"], f32)[0]),
        "w_kv_x": np.ascontiguousarray(np.asarray(inputs["w_kv_x"], f32)[0]),
        "w_o_x": np.ascontiguousarray(np.asarray(inputs["w_o_x"], f32)[0]),
        "g_mix": fm(np.asarray(inputs["norm_mix_g"])[0], 8),
        "g_x": fm(np.asarray(inputs["norm_x_g"])[0], 8),
        "g_mem": fm(np.asarray(inputs["norm_mem_g"])[0], 8),
        "g_moe": fm(np.asarray(inputs["norm_moe_g"])[0], 8),
        "w_router": np.ascontiguousarray(w_router),
        "b_router": np.ascontiguousarray(b_router.astype(f32)),
        "w_eg": np.ascontiguousarray(np.asarray(inputs["w_exp_gate"], f32)[0].reshape(32, D, 256)),
        "w_eu": np.ascontiguousarray(np.asarray(inputs["w_exp_up"], f32)[0].reshape(32, D, 256)),
        "w_ed": np.ascontiguousarray(np.asarray(inputs["w_exp_down"], f32)[0].reshape(32, 256, D)),
        "g_final": np.ascontiguousarray(np.asarray(inputs["norm_final_g"], f32).reshape(1, D)),
        "ident": np.eye(128, dtype=f32),
        "invf": invf,
        "pow2": np.tile((2.0 ** -np.arange(BIS_ITERS + 2)).astype(f32)[None, :], (128, 1)),
    }
    maps = []
    for b in range(B):
        m = dict(shared)
        m["x"] = np.ascontiguousarray(x[b])
        m["mem"] = np.ascontiguousarray(mem[b])
        m["pos"] = np.ascontiguousarray(pos[b].reshape(NT, 128).T)
        maps.append(m)
    return maps


def kernel(**inputs):
    maps = make_in_maps(inputs)
    nc = build()
    res = run_bass_kernel_spmd(nc, maps, core_ids=list(range(len(maps))))
    return np.stack([np.asarray(r["out"], np.float32) for r in res.results], axis=0)
```
